# Optimizing a Trainium2 kernel written in Bass

```python
import jax, jax.numpy as jnp
from jax import lax
import numpy as np

D_MODEL = 2048
BATCH = 4
SEQ = 4096
DEPTH = 2

GRID_W = 64
CTX_LEN = 256
EPS = 1e-6

HEAD_DIM = 128
RET_HEADS = (D_MODEL // 2) // HEAD_DIM
RET_WIDTH = RET_HEADS * HEAD_DIM
RET_CHUNK = 128
QK_SCALE = HEAD_DIM ** -0.5
ROPE_BASE = 10000.0

SGU_CHUNK = 128
SGU_GROUPS = 8
SGU_GROUP_DIM = (D_MODEL // 2) // SGU_GROUPS
SGU_WIDTH = SGU_GROUPS * SGU_GROUP_DIM

AB_IN_WIDTH = 4 * RET_WIDTH + 2 * SGU_WIDTH
AB_OUT_WIDTH = RET_WIDTH + SGU_WIDTH

CONV_WIDTH = D_MODEL
CONV_K = 3

N_GROUPS = 4
EXPERTS_PER_GROUP = 4
N_EXPERTS = N_GROUPS * EXPERTS_PER_GROUP
TOP_K_INNER = 2
D_EXPERT = D_MODEL // 2

N_EVEN = (DEPTH + 1) // 2
N_ODD = DEPTH // 2

kernel_name = "hybrid_retention_sgu_shortconv_hmoe_dit"


def rmsnorm(x, g):
    x32 = x.astype(jnp.float32)
    y = x32 * lax.rsqrt(jnp.mean(x32 * x32, axis=-1, keepdims=True) + EPS)
    return (y * g.astype(jnp.float32)).astype(x.dtype)


def modulate(x, g, shift, scale):
    return rmsnorm(x, g) * (1 + scale) + shift


def head_groupnorm(y):
    y32 = y.astype(jnp.float32)
    mu = jnp.mean(y32, axis=-1, keepdims=True)
    var = jnp.mean(jnp.square(y32 - mu), axis=-1, keepdims=True)
    return (y32 - mu) * lax.rsqrt(var + EPS)


def axial_rope_tables(n_tokens):
    t = jnp.arange(n_tokens)
    row = (t // GRID_W).astype(jnp.float32)
    col = (t % GRID_W).astype(jnp.float32)
    n_freq = HEAD_DIM // 4
    inv = ROPE_BASE ** (-jnp.arange(n_freq, dtype=jnp.float32) / n_freq)
    ang = jnp.concatenate([row[:, None] * inv, col[:, None] * inv], axis=-1)
    return jnp.cos(ang), jnp.sin(ang)


def apply_rope(x, cos, sin):
    x1, x2 = jnp.split(x, 2, axis=-1)
    cos = cos.astype(x.dtype)
    sin = sin.astype(x.dtype)
    return jnp.concatenate([x1 * cos - x2 * sin, x1 * sin + x2 * cos], axis=-1)


def retention_chunkwise(q, k, v, log_gamma, s0, include_diag):
    B, H, T, d = q.shape
    C = RET_CHUNK
    N = T // C
    qc = q.reshape(B, H, N, C, d)
    kc = k.reshape(B, H, N, C, d)
    vc = v.reshape(B, H, N, C, d)
    lg = log_gamma.astype(jnp.float32)
    i = jnp.arange(C, dtype=jnp.float32)
    diff = i[:, None] - i[None, :]
    mask = (diff >= 0) if include_diag else (diff > 0)
    decay = jnp.exp(jnp.where(mask[None], diff[None] * lg[:, None, None], -jnp.inf))
    scores = jnp.einsum('bhnid,bhnjd->bhnij', qc, kc) * decay[None, :, None].astype(q.dtype)
    y_intra = jnp.einsum('bhnij,bhnjd->bhnid', scores, vc)
    q_decay = jnp.exp((i + 1.0)[None, :] * lg[:, None])
    k_decay = jnp.exp((C - 1.0 - i)[None, :] * lg[:, None])
    chunk_decay = jnp.exp(C * lg)
    u = jnp.einsum('bhnjd,bhnje->nbhde', kc * k_decay[None, :, None, :, None].astype(k.dtype), vc)
    u = u.astype(jnp.float32)

    def step(s, u_n):
        return chunk_decay[None, :, None, None] * s + u_n, s

    s_final, s_prev = lax.scan(step, s0, u)
    y_cross = jnp.einsum('bhnid,nbhde->bhnie', qc, s_prev) * q_decay[None, :, None, :, None]
    y = (y_intra + y_cross).reshape(B, H, T, d)
    return y, s_final


def bidir_retention(q, k, v, lg_fwd, lg_bwd, s0_fwd, s0_bwd):
    flip = lambda t: jnp.flip(t, axis=2)
    y_f, s_f = retention_chunkwise(q, k, v, lg_fwd, s0_fwd, True)
    y_b, s_b = retention_chunkwise(flip(q), flip(k), flip(v), lg_bwd, s0_bwd, False)
    return y_f + flip(y_b), s_f, s_b


def spatial_gating(u, v, w_s, b_s):
    B, T, _ = u.shape
    n = T // SGU_CHUNK
    v = v.reshape(B, n, SGU_CHUNK, SGU_GROUPS, SGU_GROUP_DIM)
    v32 = v.astype(jnp.float32)
    mu = jnp.mean(v32, axis=-1, keepdims=True)
    var = jnp.mean(jnp.square(v32 - mu), axis=-1, keepdims=True)
    vn = ((v32 - mu) * lax.rsqrt(var + EPS)).astype(v.dtype)
    s = jnp.einsum('gpq,bnqgc->bnpgc', w_s, vn) + b_s.T[None, None, :, :, None]
    return u * s.reshape(B, T, SGU_WIDTH)


def mixer_ab(a_lat, a_ctx, w_in, w_out, decay_logit, w_s, b_s, cos, sin, with_ctx):
    lg_f = jax.nn.log_sigmoid(decay_logit[0].astype(jnp.float32))
    lg_b = jax.nn.log_sigmoid(decay_logit[1].astype(jnp.float32))
    split_at = [RET_WIDTH, 2 * RET_WIDTH, 3 * RET_WIDTH, 4 * RET_WIDTH, 4 * RET_WIDTH + SGU_WIDTH]

    def heads(t):
        B, T, _ = t.shape
        return t.reshape(B, T, RET_HEADS, HEAD_DIM).transpose(0, 2, 1, 3)

    def unheads(t):
        B, H, T, d = t.shape
        return t.transpose(0, 2, 1, 3).reshape(B, T, H * d)

    def finish(z_g, y_ret, z_u, z_v):
        r = jax.nn.silu(z_g) * unheads(head_groupnorm(y_ret)).astype(z_g.dtype)
        s = spatial_gating(jax.nn.gelu(z_u), jax.nn.gelu(z_v), w_s, b_s)
        return jnp.concatenate([r, s], axis=-1) @ w_out

    z_c = a_ctx @ w_in
    q_c, k_c, v_c, g_c, u_c, vs_c = jnp.split(z_c, split_at, axis=-1)
    zeros = jnp.zeros((a_ctx.shape[0], RET_HEADS, HEAD_DIM, HEAD_DIM), jnp.float32)
    y_c, s_f, s_b = bidir_retention(heads(q_c) * QK_SCALE, heads(k_c), heads(v_c), lg_f, lg_b, zeros, zeros)
    out_c = finish(g_c, y_c, u_c, vs_c) if with_ctx else None

    z_l = a_lat @ w_in
    q_l, k_l, v_l, g_l, u_l, vs_l = jnp.split(z_l, split_at, axis=-1)
    q_l = apply_rope(heads(q_l), cos, sin) * QK_SCALE
    k_l = apply_rope(heads(k_l), cos, sin)
    y_l, _, _ = bidir_retention(q_l, k_l, heads(v_l), lg_f, lg_b, s_f, s_b)
    out_l = finish(g_l, y_l, u_l, vs_l)
    return out_l, out_c


def conv3_centred(h, w, b, axis):
    n = h.shape[axis]
    half = CONV_K // 2
    pad = [(0, 0)] * h.ndim
    pad[axis] = (half, half)
    hp = jnp.pad(h, pad)
    y = b
    for j in range(CONV_K):
        y = y + lax.slice_in_dim(hp, j, j + n, axis=axis) * w[j]
    return y


def mixer_c(h, w_in, conv_w, conv_b, w_out, rows):
    z = h @ w_in
    gate_b, gate_c, hv = jnp.split(z, 3, axis=-1)
    xg = gate_c * hv
    if rows is None:
        y = conv3_centred(xg, conv_w, conv_b, axis=1)
    else:
        B, T, W = xg.shape
        y = conv3_centred(xg.reshape(B, rows, GRID_W, W), conv_w, conv_b, axis=2).reshape(B, T, W)
    return (gate_b * y) @ w_out


def hier_moe(h, w_r1, b_r1, w_r2, b_r2, w1, w3, w2):
    logit1 = (h @ w_r1 + b_r1).astype(jnp.float32)
    p1 = jax.nn.softmax(logit1, axis=-1)
    grp = jnp.argmax(logit1, axis=-1)
    p_grp = jnp.take_along_axis(p1, grp[:, None], axis=-1)
    logit2 = (jnp.einsum('nd,gde->nge', h, w_r2) + b_r2).astype(jnp.float32)
    logit2 = jnp.take_along_axis(logit2, grp[:, None, None], axis=1)[:, 0]
    top_v, top_i = lax.top_k(logit2, TOP_K_INNER)
    w_sel = jax.nn.softmax(top_v, axis=-1) * p_grp
    expert_idx = grp[:, None] * EXPERTS_PER_GROUP + top_i
    comb = jnp.sum(jax.nn.one_hot(expert_idx, N_EXPERTS, dtype=jnp.float32) * w_sel[..., None], axis=1)
    comb = comb.astype(h.dtype)
    y = jnp.zeros_like(h)
    for e in range(N_EXPERTS):
        a = jax.nn.silu(h @ w1[e]) * (h @ w3[e])
        y = y + comb[:, e:e + 1] * (a @ w2[e])
    return y


def setup_inputs(seed: int = 0) -> dict:
    key = jax.random.key(seed)
    ks = jax.random.split(key, 26)
    f32 = jnp.float32
    D = D_MODEL

    def nrm(k, shape, scale):
        return jax.random.normal(k, shape, f32) * scale

    decay_base = jnp.asarray(np.log(2.0 ** (5 + np.arange(RET_HEADS)) - 1.0), f32)
    return {
        "x": nrm(ks[0], (BATCH, SEQ, D), 1.0),
        "c": nrm(ks[1], (BATCH, D), 1.0),
        "ctx": nrm(ks[2], (BATCH, CTX_LEN, D), 1.0),
        "c_ctx": nrm(ks[3], (D,), 1.0),
        "w_mod": nrm(ks[4], (DEPTH, D, 6 * D), 0.5 * D ** -0.5),
        "b_mod": nrm(ks[5], (DEPTH, 6 * D), 0.02),
        "g_mix": 1.0 + nrm(ks[6], (DEPTH, D), 0.02),
        "g_ffn": 1.0 + nrm(ks[7], (DEPTH, D), 0.02),
        "ab_w_in": nrm(ks[8], (N_EVEN, D, AB_IN_WIDTH), D ** -0.5),
        "ab_w_out": nrm(ks[9], (N_EVEN, AB_OUT_WIDTH, D), AB_OUT_WIDTH ** -0.5),
        "ret_decay_logit": decay_base[None, None, :] + nrm(ks[10], (N_EVEN, 2, RET_HEADS), 0.1),
        "sgu_w_s": nrm(ks[11], (N_EVEN, SGU_GROUPS, SGU_CHUNK, SGU_CHUNK), 0.5 * SGU_CHUNK ** -0.5),
        "sgu_b_s": 1.0 + nrm(ks[12], (N_EVEN, SGU_GROUPS, SGU_CHUNK), 0.02),
        "cv_w_in": nrm(ks[13], (N_ODD, D, 3 * CONV_WIDTH), D ** -0.5),
        "cv_conv_w": nrm(ks[14], (N_ODD, CONV_K, CONV_WIDTH), CONV_K ** -0.5),
        "cv_conv_b": nrm(ks[15], (N_ODD, CONV_WIDTH), 0.02),
        "cv_w_out": nrm(ks[16], (N_ODD, CONV_WIDTH, D), CONV_WIDTH ** -0.5),
        "moe_w_r1": nrm(ks[17], (DEPTH, D, N_GROUPS), D ** -0.5),
        "moe_b_r1": nrm(ks[18], (DEPTH, N_GROUPS), 0.01),
        "moe_w_r2": nrm(ks[19], (DEPTH, N_GROUPS, D, EXPERTS_PER_GROUP), D ** -0.5),
        "moe_b_r2": nrm(ks[20], (DEPTH, N_GROUPS, EXPERTS_PER_GROUP), 0.01),
        "moe_w1": nrm(ks[21], (DEPTH, N_EXPERTS, D, D_EXPERT), D ** -0.5),
        "moe_w3": nrm(ks[22], (DEPTH, N_EXPERTS, D, D_EXPERT), D ** -0.5),
        "moe_w2": nrm(ks[23], (DEPTH, N_EXPERTS, D_EXPERT, D), D_EXPERT ** -0.5),
        "g_final": 1.0 + nrm(ks[24], (D,), 0.02),
    }


def reference(x, c, ctx, c_ctx, w_mod, b_mod, g_mix, g_ffn,
              ab_w_in, ab_w_out, ret_decay_logit, sgu_w_s, sgu_b_s,
              cv_w_in, cv_conv_w, cv_conv_b, cv_w_out,
              moe_w_r1, moe_b_r1, moe_w_r2, moe_b_r2, moe_w1, moe_w3, moe_w2,
              g_final):
    B, T, D = x.shape
    rows = T // GRID_W
    cos, sin = axial_rope_tables(T)
    for i in range(DEPTH):
        last = i == DEPTH - 1
        even = i % 2 == 0
        use_ctx = even or not last
        mod = jax.nn.silu(c) @ w_mod[i] + b_mod[i]
        sh1, sc1, gt1, sh2, sc2, gt2 = jnp.split(mod[:, None, :], 6, axis=-1)
        a_lat = modulate(x, g_mix[i], sh1, sc1)
        if use_ctx:
            cmod = jax.nn.silu(c_ctx) @ w_mod[i] + b_mod[i]
            csh1, csc1, cgt1, csh2, csc2, cgt2 = jnp.split(cmod, 6)
            a_ctx = modulate(ctx, g_mix[i], csh1, csc1)
        j = i // 2
        if even:
            m_lat, m_ctx = mixer_ab(a_lat, a_ctx, ab_w_in[j], ab_w_out[j], ret_decay_logit[j],
                                    sgu_w_s[j], sgu_b_s[j], cos, sin, not last)
        else:
            m_lat = mixer_c(a_lat, cv_w_in[j], cv_conv_w[j], cv_conv_b[j], cv_w_out[j], rows)
            m_ctx = None if last else mixer_c(a_ctx, cv_w_in[j], cv_conv_w[j], cv_conv_b[j], cv_w_out[j], None)
        x = x + gt1 * m_lat
        f_lat = modulate(x, g_ffn[i], sh2, sc2).reshape(-1, D)
        if last:
            y = hier_moe(f_lat, moe_w_r1[i], moe_b_r1[i], moe_w_r2[i], moe_b_r2[i],
                         moe_w1[i], moe_w3[i], moe_w2[i])
            x = x + gt2 * y.reshape(B, T, D)
        else:
            ctx = ctx + cgt1 * m_ctx
            f_ctx = modulate(ctx, g_ffn[i], csh2, csc2).reshape(-1, D)
            y = hier_moe(jnp.concatenate([f_lat, f_ctx], axis=0), moe_w_r1[i], moe_b_r1[i],
                         moe_w_r2[i], moe_b_r2[i], moe_w1[i], moe_w3[i], moe_w2[i])
            x = x + gt2 * y[:B * T].reshape(B, T, D)
            ctx = ctx + cgt2 * y[B * T:].reshape(ctx.shape)
    return rmsnorm(x, g_final)
```

```python
from contextlib import ExitStack
import numpy as np
import concourse.bass as bass
import concourse.mybir as mybir
from concourse.bass_utils import run_bass_kernel_spmd

F32 = mybir.dt.float32
BF16 = mybir.dt.bfloat16
AF = mybir.ActivationFunctionType
ALU = mybir.AluOpType

D = 2048
EPS = 1e-6
HD = 128
NH = 8
CTX = 256
GRID_W = 64
NEXP = 16
DE = 1024


class Buf:
    def __init__(self, ap, name):
        self.a = ap
        self.name = name
        self.last_write = None
        self.reads = []
        self.dsem = None
        self.dcount = 0


class FW:
    def __init__(self, nc):
        self.nc = nc
        self.engs = {"pe": nc.tensor, "act": nc.scalar, "dve": nc.vector, "pool": nc.gpsimd, "sp": nc.sync}
        self.sems = {}
        self.cnt = {}
        self.dcounts = {}
        self.waited = {k: {} for k in self.engs}
        for k in self.engs:
            self.sems[k] = nc.alloc_semaphore("s_" + k)
            self.cnt[k] = 0
        self.nbuf = 0
        self.free_dsems = []
        self.dbufs = []

    def _deps(self, reads, writes):
        deps = []
        for b in reads:
            if b.last_write is not None:
                deps.append(b.last_write)
        for b in writes:
            if b.last_write is not None:
                deps.append(b.last_write)
            deps.extend(b.reads)
        return deps

    def _emit_waits(self, ek, deps):
        eng = self.engs[ek]
        need = {}
        for (sk, v) in deps:
            if v > need.get(sk, 0):
                need[sk] = v
        for sk, v in need.items():
            if self.waited[ek].get(sk, 0) >= v:
                continue
            self.waited[ek][sk] = v
            eng.wait_ge(self.sems[sk], v)

    @staticmethod
    def _compact(reads):
        m = {}
        for sk, v in reads:
            if v > m.get(sk, 0):
                m[sk] = v
        return list(m.items())

    def op(self, ek, fn, reads=(), writes=()):
        self._emit_waits(ek, self._deps(reads, writes))
        ins = fn()
        self.cnt[ek] += 1
        ins.then_inc(self.sems[ek], 1)
        tok = (ek, self.cnt[ek])
        for b in writes:
            b.last_write = tok
            b.reads = []
        for b in reads:
            if b not in writes:
                b.reads.append(tok)
                if len(b.reads) > 8:
                    b.reads = self._compact(b.reads)
        return ins

    def dma(self, qk, fn, dst, srcs=()):
        reads = list(srcs)
        writes = [dst]
        self._emit_waits(qk, self._deps(reads, writes))
        ins = fn()
        if dst.dsem is None:
            if self.free_dsems:
                key = self.free_dsems.pop()
                dst.dcount = self.dcounts[key]
            else:
                key = "d%d" % self.nbuf
                self.nbuf += 1
                self.sems[key] = self.nc.alloc_semaphore(key)
            dst.dsem = key
            self.dbufs.append(dst)
        key = dst.dsem
        dst.dcount += 16
        self.dcounts[key] = dst.dcount
        ins.then_inc(self.sems[key], 16)
        tok = (key, dst.dcount)
        dst.last_write = tok
        dst.reads = []
        for b in reads:
            b.reads.append(tok)
            if len(b.reads) > 8:
                b.reads = self._compact(b.reads)
        return ins

    def barrier(self):
        deps = [(k, self.cnt[k]) for k in self.engs if self.cnt[k] > 0]
        deps += list(self.dcounts.items())
        for ek in self.engs:
            self._emit_waits(ek, deps)
        for b in self.dbufs:
            self.free_dsems.append(b.dsem)
            b.dsem = None
        self.dbufs = []


class _Stop(Exception):
    pass


def build(NT, stop=99):
    try:
        return _build(NT, stop)
    except _Stop as e:
        return e.args[0]


def _build(NT, stop):
    NC = NT // 128
    stage = [0]

    import os
    substop = int(os.environ.get("KSUB", "0"))

    def sub(k):
        if stage[0] + 1 == stop and substop == k:
            fw.barrier()
            raise _Stop(nc)

    def chk():
        stage[0] += 1
        if stage[0] >= stop:
            raise _Stop(nc)
    KC = D // 128
    nc = bass.Bass("TRN2", target_bir_lowering=False)
    fw = FW(nc)
    V, A, G, T, S = nc.vector, nc.scalar, nc.gpsimd, nc.tensor, nc.sync

    def ein(name, shape):
        return Buf(nc.dram_tensor(name, shape, F32, kind="ExternalInput").ap(), name)

    def dint(name, shape, dt=F32):
        return Buf(nc.dram_tensor(name, shape, dt, kind="Internal").ap(), name)

    x_own = ein("x_own", [NT, D]); x_for = ein("x_for", [NT, D]); ctx_l = ein("ctx_l", [CTX, D])
    cT = ein("cT", [128, KC, 2])
    w_mod = ein("w_mod", [2, D, 6 * D]); b_mod = ein("b_mod", [2, 6 * D])
    g_mix = ein("g_mix", [2, D]); g_ffn = ein("g_ffn", [2, D]); g_final = ein("g_final", [1, D])
    ab_w_in = ein("ab_w_in", [D, 6144]); ab_w_out = ein("ab_w_out", [D, D])
    dl = ein("dl", [1, 16])
    wsT = ein("wsT", [8, 128, 128]); bsT = ein("bsT", [128, 8])
    rope = ein("rope", [6, NT, 64])
    cv_w_in = ein("cv_w_in", [D, 6144]); cv_w_out = ein("cv_w_out", [D, D])
    conv_w = ein("conv_w", [3, D]); conv_b = ein("conv_b", [1, D])
    wr = ein("wr", [2, D, 20]); br = ein("br", [2, 20])
    moe_w1 = ein("moe_w1", [2, NEXP, D, DE]); moe_w3 = ein("moe_w3", [2, NEXP, D, DE]); moe_w2 = ein("moe_w2", [2, NEXP, DE, D])
    cst = ein("cst", [128, 128 * 3 + 8 + NC + 4])
    out = Buf(nc.dram_tensor("out", [NT, D], F32, kind="ExternalOutput").ap(), "out")

    Xd = nc.dram_tensor("Xd", [NT, D], F32, kind="Internal").ap()
    Xt = [Buf(Xd[n * 128:(n + 1) * 128, :], "X%d" % n) for n in range(NC)]
    Zd = nc.dram_tensor("Zd", [NT + 2, 6144], F32, kind="Internal").ap()
    Zt = [Buf(Zd[1 + n * 128:1 + (n + 1) * 128, :], "Z%d" % n) for n in range(NC)]
    Zpad = Buf(Zd[0:1, :], "Zpad")
    Zfd = nc.dram_tensor("Zfd", [NT, 2048], F32, kind="Internal").ap()
    Zft = [Buf(Zfd[n * 128:(n + 1) * 128, :], "Zf%d" % n) for n in range(NC)]
    Zcd = nc.dram_tensor("Zcd", [CTX, 2048], F32, kind="Internal").ap()
    Zct = [Buf(Zcd[n * 128:(n + 1) * 128, :], "Zc%d" % n) for n in range(2)]
    Rd = nc.dram_tensor("Rd", [NT, D], F32, kind="Internal").ap()
    Rt = [Buf(Rd[n * 128:(n + 1) * 128, :], "R%d" % n) for n in range(NC)]
    MODd = nc.dram_tensor("MODd", [2, 2, 6 * D], F32, kind="Internal").ap()
    MOD = Buf(MODd, "MOD")

    dq = ["sp", "act"]
    dqi = [0]

    def q():
        dqi[0] ^= 1
        return dq[dqi[0]]

    def qeng(k):
        return {"sp": S, "act": A, "pool": G}[k]

    def ld(dst, dst_ap, src_ap, srcs, k=None):
        k = k or q()
        fw.dma(k, lambda: qeng(k).dma_start(out=dst_ap, in_=src_ap), dst, srcs)

    with ExitStack() as glob:
        uid = [0]

        def sb(stack, name, shape, dt=F32):
            uid[0] += 1
            t = stack.enter_context(nc.sbuf_tensor("%s_%d" % (name, uid[0]), shape, dt))
            return Buf(t[:], name)

        def ps(stack, name, shape, dt=F32):
            uid[0] += 1
            t = stack.enter_context(nc.psum_tensor("%s_%d" % (name, uid[0]), shape, dt))
            return Buf(t[:], name)

        cs = sb(glob, "cs", [128, 128 * 3 + 8 + NC + 4])
        ld(cs, cs.a, cst.a, [cst])
        identf = cs.a[:, 0:128]
        R1 = cs.a[:, 128:256]
        R2 = cs.a[:, 256:384]
        cc = 384
        ET = 392
        identb = sb(glob, "identb", [128, 128], BF16)
        fw.op("dve", lambda: V.tensor_copy(out=identb.a, in_=identf), [cs], [identb])
        identF = sb(glob, "identF", [128, 128])
        fw.op("dve", lambda: V.tensor_copy(out=identF.a, in_=identf), [cs], [identF])
        Zpad2 = Buf(Zd[NT + 1:NT + 2, :], "Zpad2")
        with ExitStack() as st:
            zero = sb(st, "zero", [1, 6144])
            fw.op("dve", lambda: V.memset(zero.a, 0.0), [], [zero])
            ld(Zpad, Zd[0:1, :], zero.a, [zero])
            ld(Zpad2, Zd[NT + 1:NT + 2, :], zero.a, [zero])
            fw.barrier()
            chk()

        with ExitStack() as st:
            scT = sb(st, "scT", [128, KC, 2])
            ld(scT, scT.a, cT.a, [cT])
            fw.op("act", lambda: A.activation(out=scT.a, in_=scT.a, func=AF.Silu), [scT], [scT])
            wm = [sb(st, "wm%d" % i, [128, KC, 512]) for i in range(2)]
            bm = [sb(st, "bm%d" % i, [2, 512]) for i in range(2)]
            mo = [sb(st, "mo%d" % i, [2, 512]) for i in range(2)]
            pm = [ps(st, "pm%d" % i, [2, 512]) for i in range(2)]
            it = 0
            for i in range(2):
                for cb in range(24):
                    j = it % 2
                    it += 1
                    ld(wm[j], wm[j].a, w_mod.a[i, :, cb * 512:(cb + 1) * 512].rearrange("(k p) n -> p k n", p=128), [w_mod])
                    ld(bm[j], bm[j].a, b_mod.a[i:i + 1, cb * 512:(cb + 1) * 512].to_broadcast((2, 512)), [b_mod])
                    for k in range(KC):
                        fw.op("pe", lambda: T.matmul(pm[j].a, lhsT=scT.a[:, k, :], rhs=wm[j].a[:, k, :], start=(k == 0), stop=(k == KC - 1)), [scT, wm[j]], [pm[j]])
                    fw.op("dve", lambda: V.tensor_tensor(out=mo[j].a, in0=pm[j].a, in1=bm[j].a, op=ALU.add), [pm[j], bm[j]], [mo[j]])
                    ld(MOD, MODd[i, :, cb * 512:(cb + 1) * 512], mo[j].a, [mo[j]], k="sp")
            fw.barrier()
            chk()

        def modrow(i, row, j):
            return MODd[i, row:row + 1, j * D:(j + 1) * D].to_broadcast((128, D))

        def norm_mod_tile(st, bufs, src_buf, src_ap, Gt, SHt, out_bf=None, out_f=None):
            xt = bufs["xt"][bufs["i"] % 2]
            bufs["i"] += 1
            ld(xt, xt.a, src_ap, [src_buf])
            sub(8)
            junk, ssq, rstd = bufs["junk"], bufs["ssq"], bufs["rstd"]
            fw.op("act", lambda: A.activation(out=junk.a, in_=xt.a, func=AF.Square), [xt], [junk])
            fw.op("dve", lambda: V.tensor_reduce(out=ssq.a, in_=junk.a, axis=mybir.AxisListType.X, op=ALU.add), [junk], [ssq])
            fw.op("dve", lambda: V.tensor_scalar(out=ssq.a, in0=ssq.a, scalar1=1.0 / D, scalar2=EPS, op0=ALU.mult, op1=ALU.add), [ssq], [ssq])
            fw.op("act", lambda: A.activation(out=ssq.a, in_=ssq.a, func=AF.Sqrt), [ssq], [ssq])
            fw.op("dve", lambda: V.reciprocal(out=rstd.a, in_=ssq.a), [ssq], [rstd])
            fw.op("dve", lambda: V.scalar_tensor_tensor(out=junk.a, in0=xt.a, scalar=rstd.a[:, 0:1], in1=Gt.a, op0=ALU.mult, op1=ALU.mult), [xt, rstd, Gt], [junk])
            sub(9)
            if SHt is None:
                return junk
            if out_f is not None:
                fw.op("dve", lambda: V.scalar_tensor_tensor(out=out_f.a, in0=junk.a, scalar=1.0, in1=SHt.a, op0=ALU.mult, op1=ALU.add), [junk, SHt], [out_f])
                if out_bf is not None:
                    fw.op("act", lambda: A.copy(out=out_bf.a, in_=out_f.a), [out_f], [out_bf])
            else:
                fw.op("dve", lambda: V.tensor_tensor(out=out_bf.a, in0=junk.a, in1=SHt.a, op=ALU.add), [junk, SHt], [out_bf])
            return None

        gs_tmp = {}

        def make_GS(st, i, row, jsh, jsc, gvec_ap, gbuf):
            Gt = sb(st, "Gt%d%d%d" % (i, row, jsh), [128, D])
            SHt = sb(st, "SHt%d%d%d" % (i, row, jsh), [128, D])
            if "gtmp" not in gs_tmp or gs_tmp["st"] is not st:
                gs_tmp["gtmp"] = sb(st, "gtmp", [128, D]); gs_tmp["st"] = st
            tmp = gs_tmp["gtmp"]
            ld(Gt, Gt.a, modrow(i, row, jsc), [MOD])
            ld(tmp, tmp.a, gvec_ap.to_broadcast((128, D)), [gbuf])
            ld(SHt, SHt.a, modrow(i, row, jsh), [MOD])
            fw.op("dve", lambda: V.scalar_tensor_tensor(out=Gt.a, in0=Gt.a, scalar=1.0, in1=tmp.a, op0=ALU.add, op1=ALU.mult), [Gt, tmp], [Gt])
            return Gt, SHt

        def transpose_tile(src_bf, dstT, col0, pT, nk=KC):
            for k0 in range(0, nk, 8):
                p = pT[(k0 // 8) % len(pT)]
                for k in range(k0, min(nk, k0 + 8)):
                    fw.op("pe", lambda: T.transpose(out=p.a[:, k - k0, :], in_=src_bf.a[:, k * 128:(k + 1) * 128], identity=identb.a), [src_bf, identb], [p])
                n = min(nk, k0 + 8) - k0
                if (k0 // 8) % 2 == 0:
                    fw.op("act", lambda: A.copy(out=dstT.a[:, k0:k0 + n, col0:col0 + 128], in_=p.a[:, 0:n, :]), [p], [dstT])
                else:
                    fw.op("dve", lambda: V.tensor_copy(out=dstT.a[:, k0:k0 + n, col0:col0 + 128], in_=p.a[:, 0:n, :]), [p], [dstT])

        def load_w(wbuf, w_ap, wsrc, kc):
            fw.dma("pool", lambda: G.dma_start(out=wbuf.a, in_=w_ap.rearrange("(k p) n -> p k n", p=128)), wbuf, [wsrc])

        with ExitStack() as st:
            Gl, SHl = make_GS(st, 0, 0, 0, 1, g_mix.a[0:1, :], g_mix)
            Gc, SHc = make_GS(st, 0, 1, 0, 1, g_mix.a[0:1, :], g_mix)
            bufs = {"xt": [sb(st, "xt%d" % i, [128, D]) for i in range(1)] * 2, "i": 0, "junk": sb(st, "junk", [128, D]),
                    "ssq": sb(st, "ssq", [128, 1]), "rstd": sb(st, "rstd", [128, 1])}
            abf = [sb(st, "abf%d" % i, [128, D], BF16) for i in range(2)]
            aT = sb(st, "aT", [128, KC, NT], BF16)
            wb = [sb(st, "wb%d" % i, [128, KC, 512], BF16) for i in range(2)]
            osb = [sb(st, "osb%d" % i, [128, 512]) for i in range(3)]
            pT = [ps(st, "pT%d" % i, [128, 8, 128], BF16) for i in range(2)]
            pg = [ps(st, "pg%d" % i, [128, 512]) for i in range(4)]
            cnt = {"w": 0, "o": 0, "p": 0}

            def inproj(src_buf, src_ap_fn, ntiles, Gt, SHt, w_all, wsrc, cbs, zts, zcol0):
                for n in range(ntiles):
                    ab = abf[n % 2]
                    norm_mod_tile(st, bufs, src_buf, src_ap_fn(n), Gt, SHt, out_bf=ab)
                    transpose_tile(ab, aT, n * 128, pT)
                for cb in cbs:
                    w = wb[cnt["w"] % 2]
                    cnt["w"] += 1
                    load_w(w, w_all.a[:, cb * 512:(cb + 1) * 512], wsrc, KC)
                    for n in range(ntiles):
                        p = pg[cnt["p"] % 4]
                        cnt["p"] += 1
                        for k in range(KC):
                            fw.op("pe", lambda: T.matmul(p.a, lhsT=aT.a[:, k, n * 128:(n + 1) * 128], rhs=w.a[:, k, :], start=(k == 0), stop=(k == KC - 1)), [aT, w], [p])
                        o = osb[cnt["o"] % 3]
                        cnt["o"] += 1
                        if cnt["o"] % 2:
                            fw.op("act", lambda: A.copy(out=o.a, in_=p.a), [p], [o])
                        else:
                            fw.op("dve", lambda: V.tensor_copy(out=o.a, in_=p.a), [p], [o])
                        c0 = cb * 512 - zcol0
                        ld(zts[n], zts[n].a[:, c0:c0 + 512], o.a, [o])

            kvb = [2, 3, 4, 5]
            inproj(x_for, lambda n: x_for.a[n * 128:(n + 1) * 128, :], NC, Gl, SHl, ab_w_in, ab_w_in, kvb, Zft, 1024)
            inproj(ctx_l, lambda n: ctx_l.a[n * 128:(n + 1) * 128, :], 2, Gc, SHc, ab_w_in, ab_w_in, kvb, Zct, 1024)
            inproj(x_own, lambda n: x_own.a[n * 128:(n + 1) * 128, :], NC, Gl, SHl, ab_w_in, ab_w_in, list(range(12)), Zt, 0)
            fw.barrier()
            chk()

        with ExitStack() as st:
            lg = sb(st, "lg", [128, 16])
            t1 = sb(st, "t1", [128, 16]); t2 = sb(st, "t2", [128, 16]); t3 = sb(st, "t3", [128, 16]); t4 = sb(st, "t4", [128, 16])
            ld(lg, lg.a, dl.a.to_broadcast((128, 16)), [dl])
            fw.op("act", lambda: A.activation(out=t1.a, in_=lg.a, func=AF.Exp, scale=-1.0), [lg], [t1])
            fw.op("dve", lambda: V.tensor_scalar(out=t2.a, in0=t1.a, scalar1=-0.25, scalar2=1.0 / 3.0, op0=ALU.mult, op1=ALU.add), [t1], [t2])
            fw.op("dve", lambda: V.tensor_tensor(out=t2.a, in0=t2.a, in1=t1.a, op=ALU.mult), [t2, t1], [t2])
            fw.op("dve", lambda: V.tensor_scalar(out=t2.a, in0=t2.a, scalar1=-0.5, scalar2=None, op0=ALU.add), [t2], [t2])
            fw.op("dve", lambda: V.tensor_tensor(out=t2.a, in0=t2.a, in1=t1.a, op=ALU.mult), [t2, t1], [t2])
            fw.op("dve", lambda: V.tensor_scalar(out=t2.a, in0=t2.a, scalar1=1.0, scalar2=None, op0=ALU.add), [t2], [t2])
            fw.op("dve", lambda: V.tensor_tensor(out=t2.a, in0=t2.a, in1=t1.a, op=ALU.mult), [t2, t1], [t2])
            fw.op("dve", lambda: V.tensor_scalar(out=t3.a, in0=t1.a, scalar1=1.0, scalar2=None, op0=ALU.add), [t1], [t3])
            fw.op("act", lambda: A.activation(out=t3.a, in_=t3.a, func=AF.Ln), [t3], [t3])
            fw.op("dve", lambda: V.tensor_scalar(out=t4.a, in0=t1.a, scalar1=0.1, scalar2=None, op0=ALU.is_lt), [t1], [t4])
            fw.op("dve", lambda: V.tensor_tensor(out=t2.a, in0=t2.a, in1=t3.a, op=ALU.subtract), [t2, t3], [t2])
            fw.op("dve", lambda: V.tensor_tensor(out=t2.a, in0=t2.a, in1=t4.a, op=ALU.mult), [t2, t4], [t2])
            fw.op("dve", lambda: V.tensor_tensor(out=t2.a, in0=t2.a, in1=t3.a, op=ALU.add), [t2, t3], [t2])
            fw.op("dve", lambda: V.tensor_scalar(out=lg.a, in0=t2.a, scalar1=-1.0, scalar2=None, op0=ALU.mult), [t2], [lg])

            ropeT = sb(st, "ropeT", [128, 6, NC, 64])
            for r in range(6):
                ld(ropeT, ropeT.a[:, r, :, :], rope.a[r].rearrange("(n p) c -> p n c", p=128), [rope])
            NE = NC + 4
            ew = sb(st, "ew", [128, NE]); ewB = sb(st, "ewB", [128, NE])
            dcol = sb(st, "dcol", [128, 8])
            Dh = sb(st, "Dh", [128, 128]); dtmp = sb(st, "dtmp", [128, 128])
            qs = [sb(st, "qs%d" % i, [128, NC, 128]) for i in range(1)] * 2
            ks = [sb(st, "ks%d" % i, [128, NC, 128]) for i in range(1)] * 2
            vs = [sb(st, "vs%d" % i, [128, NC, 128]) for i in range(1)] * 2
            gs = [sb(st, "gs%d" % i, [128, NC, 128]) for i in range(1)] * 2
            kf = sb(st, "kf", [128, NC + 2, 128]); vf = sb(st, "vf", [128, NC + 2, 128])
            rq = sb(st, "rq", [128, NC, 128]); rk = sb(st, "rk", [128, NC, 128]); rtmp = sb(st, "rtmp", [128, NC, 64])
            qb = sb(st, "qb", [128, NC, 128], BF16); kb = sb(st, "kb", [128, NC, 128], BF16)
            vb = sb(st, "vb", [128, NC, 128], BF16); vA = sb(st, "vA", [128, NC, 128], BF16); vB = sb(st, "vB", [128, NC, 128], BF16)
            kfb = sb(st, "kfb", [128, NC + 2, 128], BF16); vfB = sb(st, "vfB", [128, NC + 2, 128], BF16); vcA = sb(st, "vcA", [128, 2, 128], BF16)
            SA = sb(st, "SA", [128, NC + 1, 128]); SB = sb(st, "SB", [128, NC + 1, 128])
            SAb = sb(st, "SAb", [128, NC + 1, 128], BF16); SBb = sb(st, "SBb", [128, NC + 1, 128], BF16)
            qT = [sb(st, "qT%d" % i, [128, 2, 128], BF16) for i in range(2)]
            ATb = [sb(st, "ATb%d" % i, [128, 128], BF16) for i in range(2)]
            ysb = [sb(st, "ysb%d" % i, [128, 128]) for i in range(2)]
            stt_ = sb(st, "bnst", [128, 6]); mv = sb(st, "mv", [128, 2]); rs = sb(st, "rs", [128, 1])
            sg = sb(st, "sg", [128, 128])
            rout = [sb(st, "rout%d" % i, [128, NC, 128]) for i in range(2)]
            pS = [ps(st, "pS%d" % i, [128, 128]) for i in range(2)]
            pU = [ps(st, "pU%d" % i, [128, 2, 128]) for i in range(2)]
            pTq = ps(st, "pTq", [128, 2, 128], BF16)
            pSc = ps(st, "pSc", [128, 128])
            pY = ps(st, "pY", [128, 3, 128])

            def rope_apply(dst, src, ci, si, nchunk):
                x1 = src.a[:, 0:nchunk, 0:64]; x2 = src.a[:, 0:nchunk, 64:128]
                cth = ropeT.a[:, ci, 0:nchunk, :]; sth = ropeT.a[:, si, 0:nchunk, :]
                tm = rtmp.a[:, 0:nchunk, :]
                fw.op("dve", lambda: V.tensor_tensor(out=dst.a[:, 0:nchunk, 0:64], in0=x1, in1=cth, op=ALU.mult), [src, ropeT], [dst])
                fw.op("dve", lambda: V.tensor_tensor(out=tm, in0=x2, in1=sth, op=ALU.mult), [src, ropeT], [rtmp])
                fw.op("dve", lambda: V.tensor_tensor(out=dst.a[:, 0:nchunk, 0:64], in0=dst.a[:, 0:nchunk, 0:64], in1=tm, op=ALU.subtract), [dst, rtmp], [dst])
                fw.op("dve", lambda: V.tensor_tensor(out=dst.a[:, 0:nchunk, 64:128], in0=x1, in1=sth, op=ALU.mult), [src, ropeT], [dst])
                fw.op("dve", lambda: V.tensor_tensor(out=tm, in0=x2, in1=cth, op=ALU.mult), [src, ropeT], [rtmp])
                fw.op("dve", lambda: V.tensor_tensor(out=dst.a[:, 0:nchunk, 64:128], in0=dst.a[:, 0:nchunk, 64:128], in1=tm, op=ALU.add), [dst, rtmp], [dst])

            for h in range(NH):
                j = h % 2
                c0 = h * 128
                ld(qs[j], qs[j].a, Zd[1:NT + 1, c0:c0 + 128].rearrange("(n p) c -> p n c", p=128), Zt)
                ld(ks[j], ks[j].a, Zd[1:NT + 1, 1024 + c0:1024 + c0 + 128].rearrange("(n p) c -> p n c", p=128), Zt)
                ld(vs[j], vs[j].a, Zd[1:NT + 1, 2048 + c0:2048 + c0 + 128].rearrange("(n p) c -> p n c", p=128), Zt)
                ld(gs[j], gs[j].a, Zd[1:NT + 1, 3072 + c0:3072 + c0 + 128].rearrange("(n p) c -> p n c", p=128), Zt)
                ld(kf, kf.a[:, 0:NC, :], Zfd[:, c0:c0 + 128].rearrange("(n p) c -> p n c", p=128), Zft)
                ld(kf, kf.a[:, NC:NC + 2, :], Zcd[:, c0:c0 + 128].rearrange("(n p) c -> p n c", p=128), Zct)
                ld(vf, vf.a[:, 0:NC, :], Zfd[:, 1024 + c0:1024 + c0 + 128].rearrange("(n p) c -> p n c", p=128), Zft)
                ld(vf, vf.a[:, NC:NC + 2, :], Zcd[:, 1024 + c0:1024 + c0 + 128].rearrange("(n p) c -> p n c", p=128), Zct)
                lgA = lg.a[:, h:h + 1]; lgB = lg.a[:, 8 + h:9 + h]
                fw.op("dve", lambda: V.tensor_scalar(out=dcol.a[:, 0:1], in0=cs.a[:, cc + 0:cc + 1], scalar1=lgA, scalar2=None, op0=ALU.mult), [cs, lg], [dcol])
                fw.op("dve", lambda: V.tensor_scalar(out=dcol.a[:, 1:2], in0=cs.a[:, cc + 1:cc + 2], scalar1=lgB, scalar2=None, op0=ALU.mult), [cs, lg], [dcol])
                fw.op("dve", lambda: V.tensor_scalar(out=dcol.a[:, 2:3], in0=cs.a[:, cc + 2:cc + 3], scalar1=lgA, scalar2=None, op0=ALU.mult), [cs, lg], [dcol])
                fw.op("dve", lambda: V.tensor_scalar(out=dcol.a[:, 3:4], in0=cs.a[:, cc + 3:cc + 4], scalar1=lgB, scalar2=None, op0=ALU.mult), [cs, lg], [dcol])
                fw.op("dve", lambda: V.tensor_scalar(out=dcol.a[:, 4:5], in0=cs.a[:, cc + 4:cc + 5], scalar1=lgA, scalar2=None, op0=ALU.mult), [cs, lg], [dcol])
                fw.op("dve", lambda: V.tensor_scalar(out=dcol.a[:, 5:6], in0=cs.a[:, cc + 4:cc + 5], scalar1=lgB, scalar2=None, op0=ALU.mult), [cs, lg], [dcol])
                fw.op("act", lambda: A.activation(out=dcol.a[:, 0:6], in_=dcol.a[:, 0:6], func=AF.Exp), [dcol], [dcol])
                fw.op("dve", lambda: V.tensor_scalar(out=ewB.a[:, 0:NC + 2], in0=cs.a[:, ET:ET + NC + 2], scalar1=lgB, scalar2=None, op0=ALU.mult), [cs, lg], [ewB])
                fw.op("dve", lambda: V.tensor_scalar(out=ewB.a[:, NC + 2:NC + 4], in0=cs.a[:, ET + NC + 2:ET + NC + 4], scalar1=lgA, scalar2=None, op0=ALU.mult), [cs, lg], [ewB])
                fw.op("act", lambda: A.activation(out=ew.a, in_=ewB.a, func=AF.Exp), [ewB], [ew])
                fw.op("dve", lambda: V.tensor_scalar(out=dtmp.a, in0=R1, scalar1=lgA, scalar2=None, op0=ALU.mult), [cs, lg], [dtmp])
                fw.op("dve", lambda: V.scalar_tensor_tensor(out=dtmp.a, in0=R2, scalar=lgB, in1=dtmp.a, op0=ALU.mult, op1=ALU.add), [cs, lg, dtmp], [dtmp])
                fw.op("act", lambda: A.activation(out=Dh.a, in_=dtmp.a, func=AF.Exp), [dtmp], [Dh])
                rope_apply(rq, qs[j], 0, 1, NC)
                rope_apply(rk, ks[j], 2, 3, NC)
                fw.op("act", lambda: A.copy(out=qb.a, in_=rq.a), [rq], [qb])
                fw.op("act", lambda: A.copy(out=kb.a, in_=rk.a), [rk], [kb])
                fw.op("act", lambda: A.copy(out=vb.a, in_=vs[j].a), [vs[j]], [vb])
                fw.op("dve", lambda: V.tensor_scalar(out=vA.a, in0=vs[j].a, scalar1=dcol.a[:, 0:1], scalar2=None, op0=ALU.mult), [vs[j], dcol], [vA])
                fw.op("dve", lambda: V.tensor_scalar(out=vB.a, in0=vs[j].a, scalar1=dcol.a[:, 1:2], scalar2=None, op0=ALU.mult), [vs[j], dcol], [vB])
                rope_apply(rk, kf, 4, 5, NC)
                fw.op("act", lambda: A.copy(out=kfb.a[:, 0:NC, :], in_=rk.a), [rk], [kfb])
                fw.op("act", lambda: A.copy(out=kfb.a[:, NC:NC + 2, :], in_=kf.a[:, NC:NC + 2, :]), [kf], [kfb])
                for c in range(NC + 2):
                    fw.op("dve", lambda: V.tensor_scalar(out=vfB.a[:, c, :], in0=vf.a[:, c, :], scalar1=ew.a[:, c:c + 1], scalar2=None, op0=ALU.mult), [vf, ew], [vfB])
                for c in range(2):
                    fw.op("dve", lambda: V.tensor_scalar(out=vcA.a[:, c, :], in0=vf.a[:, NC + c, :], scalar1=ew.a[:, NC + 2 + c:NC + 3 + c], scalar2=None, op0=ALU.mult), [vf, ew], [vcA])
                for c in range(2):
                    fw.op("pe", lambda: T.matmul(pS[0].a, lhsT=kfb.a[:, NC + c, :], rhs=vcA.a[:, c, :], start=(c == 0), stop=(c == 1)), [kfb, vcA], [pS[0]])
                fw.op("act", lambda: A.copy(out=SA.a[:, 0, :], in_=pS[0].a), [pS[0]], [SA])
                for c in range(NC + 2):
                    fw.op("pe", lambda: T.matmul(pS[1].a, lhsT=kfb.a[:, c, :], rhs=vfB.a[:, c, :], start=(c == 0), stop=(c == NC + 1)), [kfb, vfB], [pS[1]])
                fw.op("act", lambda: A.copy(out=SB.a[:, NC, :], in_=pS[1].a), [pS[1]], [SB])
                for n in range(NC):
                    p = pU[n % 2]
                    fw.op("pe", lambda: T.matmul(p.a[:, 0, :], lhsT=kb.a[:, n, :], rhs=vA.a[:, n, :], start=True, stop=True), [kb, vA], [p])
                    fw.op("dve", lambda: V.scalar_tensor_tensor(out=SA.a[:, n + 1, :], in0=SA.a[:, n, :], scalar=dcol.a[:, 4:5], in1=p.a[:, 0, :], op0=ALU.mult, op1=ALU.add), [SA, dcol, p], [SA])
                for n in range(NC - 1, -1, -1):
                    p = pU[n % 2]
                    fw.op("pe", lambda: T.matmul(p.a[:, 1, :], lhsT=kb.a[:, n, :], rhs=vB.a[:, n, :], start=True, stop=True), [kb, vB], [p])
                    fw.op("dve", lambda: V.scalar_tensor_tensor(out=SB.a[:, n, :], in0=SB.a[:, n + 1, :], scalar=dcol.a[:, 5:6], in1=p.a[:, 1, :], op0=ALU.mult, op1=ALU.add), [SB, dcol, p], [SB])
                fw.op("act", lambda: A.copy(out=SAb.a, in_=SA.a), [SA], [SAb])
                fw.op("act", lambda: A.copy(out=SBb.a, in_=SB.a), [SB], [SBb])
                ro = rout[j]
                for n in range(NC):
                    qt = qT[n % 2]; at = ATb[n % 2]; y = ysb[n % 2]
                    fw.op("pe", lambda: T.transpose(out=pTq.a[:, 0, :], in_=qb.a[:, n, :], identity=identb.a), [qb, identb], [pTq])
                    fw.op("pe", lambda: T.transpose(out=pTq.a[:, 1, :], in_=kb.a[:, n, :], identity=identb.a), [kb, identb], [pTq])
                    fw.op("act", lambda: A.copy(out=qt.a, in_=pTq.a), [pTq], [qt])
                    fw.op("pe", lambda: T.matmul(pSc.a, lhsT=qt.a[:, 1, :], rhs=qt.a[:, 0, :], start=True, stop=True), [qt], [pSc])
                    fw.op("dve", lambda: V.tensor_tensor(out=at.a, in0=pSc.a, in1=Dh.a, op=ALU.mult), [pSc, Dh], [at])
                    fw.op("pe", lambda: T.matmul(pY.a[:, 0, :], lhsT=at.a, rhs=vb.a[:, n, :], start=True, stop=True), [at, vb], [pY])
                    fw.op("pe", lambda: T.matmul(pY.a[:, 1, :], lhsT=qt.a[:, 0, :], rhs=SAb.a[:, n, :], start=True, stop=True), [qt, SAb], [pY])
                    fw.op("pe", lambda: T.matmul(pY.a[:, 2, :], lhsT=qt.a[:, 0, :], rhs=SBb.a[:, n + 1, :], start=True, stop=True), [qt, SBb], [pY])
                    fw.op("act", lambda: A.copy(out=y.a, in_=pY.a[:, 0, :]), [pY], [y])
                    fw.op("dve", lambda: V.scalar_tensor_tensor(out=y.a, in0=pY.a[:, 1, :], scalar=dcol.a[:, 2:3], in1=y.a, op0=ALU.mult, op1=ALU.add), [pY, dcol, y], [y])
                    fw.op("dve", lambda: V.scalar_tensor_tensor(out=y.a, in0=pY.a[:, 2, :], scalar=dcol.a[:, 3:4], in1=y.a, op0=ALU.mult, op1=ALU.add), [pY, dcol, y], [y])
                    fw.op("dve", lambda: V.bn_stats(out=stt_.a, in_=y.a), [y], [stt_])
                    fw.op("dve", lambda: V.bn_aggr(out=mv.a, in_=stt_.a), [stt_], [mv])
                    fw.op("dve", lambda: V.tensor_scalar(out=rs.a, in0=mv.a[:, 1:2], scalar1=EPS, scalar2=None, op0=ALU.add), [mv], [rs])
                    fw.op("act", lambda: A.activation(out=rs.a, in_=rs.a, func=AF.Sqrt), [rs], [rs])
                    fw.op("dve", lambda: V.reciprocal(out=rs.a, in_=rs.a), [rs], [rs])
                    fw.op("dve", lambda: V.tensor_scalar(out=y.a, in0=y.a, scalar1=mv.a[:, 0:1], scalar2=rs.a[:, 0:1], op0=ALU.subtract, op1=ALU.mult), [y, mv, rs], [y])
                    fw.op("act", lambda: A.activation(out=sg.a, in_=gs[j].a[:, n, :], func=AF.Silu), [gs[j]], [sg])
                    fw.op("dve", lambda: V.tensor_tensor(out=ro.a[:, n, :], in0=y.a, in1=sg.a, op=ALU.mult), [y, sg], [ro])
                for n in range(NC):
                    ld(Rt[n], Rt[n].a[:, c0:c0 + 128], ro.a[:, n, :], [ro])
            fw.barrier()
            chk()

        def gelu(dst, src, tmp, reads):
            fw.op("dve", lambda: V.tensor_tensor(out=tmp.a, in0=src.a, in1=src.a, op=ALU.mult), reads, [tmp])
            fw.op("dve", lambda: V.tensor_scalar(out=tmp.a, in0=tmp.a, scalar1=0.044715, scalar2=1.0, op0=ALU.mult, op1=ALU.add), [tmp], [tmp])
            fw.op("dve", lambda: V.tensor_tensor(out=tmp.a, in0=tmp.a, in1=src.a, op=ALU.mult), [tmp] + reads, [tmp])
            fw.op("act", lambda: A.activation(out=tmp.a, in_=tmp.a, func=AF.Sigmoid, scale=1.5957691216057308), [tmp], [tmp])
            fw.op("dve", lambda: V.tensor_tensor(out=dst.a, in0=tmp.a, in1=src.a, op=ALU.mult), [tmp] + reads, [dst])

        with ExitStack() as st:
            us = [sb(st, "us%d" % i, [128, NC, 128]) for i in range(2)]
            vs2 = [sb(st, "vs2%d" % i, [128, NC, 128]) for i in range(2)]
            gu = sb(st, "gu", [128, NC, 128]); gv = sb(st, "gv", [128, NC, 128]); tmpg = sb(st, "tmpg", [128, NC, 128])
            vn = sb(st, "vn", [128, NC, 128], BF16)
            wsf = sb(st, "wsf", [128, 8, 128]); wsb = sb(st, "wsb", [128, 8, 128], BF16)
            bsb = sb(st, "bsb", [128, 8])
            stt2 = sb(st, "bnst2", [128, 6]); mv2 = sb(st, "mv2", [128, 2]); rs2 = sb(st, "rs2", [128, 1])
            ro2 = [sb(st, "ro2%d" % i, [128, NC, 128]) for i in range(2)]
            pG = [ps(st, "pG%d" % i, [128, 128]) for i in range(2)]
            ld(wsf, wsf.a, wsT.a.rearrange("g q p -> q g p"), [wsT])
            ld(bsb, bsb.a, bsT.a, [bsT])
            fw.op("act", lambda: A.copy(out=wsb.a, in_=wsf.a), [wsf], [wsb])
            for g in range(8):
                j = g % 2
                c0 = g * 128
                ld(us[j], us[j].a, Zd[1:NT + 1, 4096 + c0:4096 + c0 + 128].rearrange("(n p) c -> p n c", p=128), Zt)
                ld(vs2[j], vs2[j].a, Zd[1:NT + 1, 5120 + c0:5120 + c0 + 128].rearrange("(n p) c -> p n c", p=128), Zt)
                gelu(gu, us[j], tmpg, [us[j]])
                gelu(gv, vs2[j], tmpg, [vs2[j]])
                for n in range(NC):
                    fw.op("dve", lambda: V.bn_stats(out=stt2.a, in_=gv.a[:, n, :]), [gv], [stt2])
                    fw.op("dve", lambda: V.bn_aggr(out=mv2.a, in_=stt2.a), [stt2], [mv2])
                    fw.op("dve", lambda: V.tensor_scalar(out=rs2.a, in0=mv2.a[:, 1:2], scalar1=EPS, scalar2=None, op0=ALU.add), [mv2], [rs2])
                    fw.op("act", lambda: A.activation(out=rs2.a, in_=rs2.a, func=AF.Sqrt), [rs2], [rs2])
                    fw.op("dve", lambda: V.reciprocal(out=rs2.a, in_=rs2.a), [rs2], [rs2])
                    fw.op("dve", lambda: V.tensor_scalar(out=vn.a[:, n, :], in0=gv.a[:, n, :], scalar1=mv2.a[:, 0:1], scalar2=rs2.a[:, 0:1], op0=ALU.subtract, op1=ALU.mult), [gv, mv2, rs2], [vn])
                for n in range(NC):
                    p = pG[n % 2]
                    fw.op("pe", lambda: T.matmul(p.a, lhsT=wsb.a[:, g, :], rhs=vn.a[:, n, :], start=True, stop=True), [wsb, vn], [p])
                    fw.op("dve", lambda: V.scalar_tensor_tensor(out=ro2[j].a[:, n, :], in0=p.a, scalar=bsb.a[:, g:g + 1], in1=gu.a[:, n, :], op0=ALU.add, op1=ALU.mult), [p, bsb, gu], [ro2[j]])
                for n in range(NC):
                    ld(Rt[n], Rt[n].a[:, 1024 + c0:1024 + c0 + 128], ro2[j].a[:, n, :], [ro2[j]])
            fw.barrier()
            chk()

        def outproj_and_moe(i, w_out, x_src_tiles, last):
            with ExitStack() as st:
                fT = sb(st, "fT", [128, KC, NT], BF16)
                comb = sb(st, "comb", [128, NC, 16])
                GT2 = sb(st, "GT2", [128, D])
                ld(GT2, GT2.a, modrow(i, 0, 5), [MOD])
                with ExitStack() as s2:
                    GT1 = sb(s2, "GT1", [128, D])
                    ld(GT1, GT1.a, modrow(i, 0, 2), [MOD])
                    rf = [sb(s2, "rf%d" % k, [128, D]) for k in range(2)]
                    rb = [sb(s2, "rb%d" % k, [128, D], BF16) for k in range(2)]
                    wb = [sb(s2, "wo%d" % k, [128, KC, 512], BF16) for k in range(2)]
                    xin = [sb(s2, "xin%d" % k, [128, 512]) for k in range(3)]
                    pT = [ps(s2, "pT%d" % k, [128, 8, 128], BF16) for k in range(2)]
                    pg = [ps(s2, "pg%d" % k, [128, 512]) for k in range(4)]
                    rT = fT
                    for n in range(NC):
                        ld(rf[n % 2], rf[n % 2].a, Rt[n].a, [Rt[n]])
                        fw.op("act", lambda: A.copy(out=rb[n % 2].a, in_=rf[n % 2].a), [rf[n % 2]], [rb[n % 2]])
                        transpose_tile(rb[n % 2], rT, n * 128, pT)
                    ci = 0
                    for cb in range(4):
                        w = wb[cb % 2]
                        load_w(w, w_out.a[:, cb * 512:(cb + 1) * 512], w_out, KC)
                        for n in range(NC):
                            p = pg[ci % 4]; xi = xin[ci % 3]; ci += 1
                            xb_, xap = x_src_tiles[n]
                            ld(xi, xi.a, xap[:, cb * 512:(cb + 1) * 512], [xb_])
                            for k in range(KC):
                                fw.op("pe", lambda: T.matmul(p.a, lhsT=rT.a[:, k, n * 128:(n + 1) * 128], rhs=w.a[:, k, :], start=(k == 0), stop=(k == KC - 1)), [rT, w], [p])
                            fw.op("dve", lambda: V.tensor_tensor(out=p.a, in0=p.a, in1=GT1.a[:, cb * 512:(cb + 1) * 512], op=ALU.mult), [p, GT1], [p])
                            fw.op("dve", lambda: V.tensor_tensor(out=xi.a, in0=p.a, in1=xi.a, op=ALU.add), [p, xi], [xi])
                            ld(Xt[n], Xt[n].a[:, cb * 512:(cb + 1) * 512], xi.a, [xi])
                    fw.barrier()
                    chk()
                with ExitStack() as s2:
                    Gf, SHf = make_GS(s2, i, 0, 3, 4, g_ffn.a[i:i + 1, :], g_ffn)
                    bufs = {"xt": [sb(s2, "xt%d" % k, [128, D]) for k in range(2)], "i": 0, "junk": sb(s2, "junk", [128, D]),
                            "ssq": sb(s2, "ssq", [128, 1]), "rstd": sb(s2, "rstd", [128, 1])}
                    ff = [sb(s2, "ff%d" % k, [128, D]) for k in range(2)]
                    fTf = sb(s2, "fTf", [128, KC, 128])
                    wrs = sb(s2, "wrs", [128, KC, 20]); brs = sb(s2, "brs", [128, 20])
                    L = sb(s2, "L", [128, 20]); m1 = sb(s2, "m1", [128, 4]); oh1 = sb(s2, "oh1", [128, 4]); e1 = sb(s2, "e1", [128, 4])
                    l2 = sb(s2, "l2", [128, 4]); l2b = sb(s2, "l2b", [128, 4]); ohA = sb(s2, "ohA", [128, 4]); ohB = sb(s2, "ohB", [128, 4])
                    inner = sb(s2, "inner", [128, 4])
                    pF = [ps(s2, "pF%d" % k, [128, 4, 128]) for k in range(2)]
                    pL = ps(s2, "pL", [128, 20])
                    sub(5)
                    ld(wrs, wrs.a, wr.a[i].rearrange("(k p) n -> p k n", p=128), [wr])
                    sub(6)
                    ld(brs, brs.a, br.a[i:i + 1, :].to_broadcast((128, 20)), [br])
                    sub(7)
                    for n in range(NC):
                        f = ff[n % 2]
                        norm_mod_tile(s2, bufs, Xt[n], Xt[n].a, Gf, SHf, out_f=f)
                        sub(1)
                        for k0 in range(0, KC, 4):
                            p = pF[(k0 // 4) % 2]
                            for k in range(k0, k0 + 4):
                                fw.op("pe", lambda: T.transpose(out=p.a[:, k - k0, :], in_=f.a[:, k * 128:(k + 1) * 128], identity=identF.a), [f, identF], [p])
                            sub(10)
                            fw.op("act", lambda: A.copy(out=fTf.a[:, k0:k0 + 4, :], in_=p.a), [p], [fTf])
                            sub(11)
                            fw.op("act", lambda: A.copy(out=fT.a[:, k0:k0 + 4, n * 128:(n + 1) * 128], in_=p.a), [p], [fT])
                            sub(12)
                        sub(2)
                        for k in range(KC):
                            fw.op("pe", lambda: T.matmul(pL.a, lhsT=fTf.a[:, k, :], rhs=wrs.a[:, k, :], start=(k == 0), stop=(k == KC - 1)), [fTf, wrs], [pL])
                        fw.op("dve", lambda: V.tensor_tensor(out=L.a, in0=pL.a, in1=brs.a, op=ALU.add), [pL, brs], [L])
                        sub(3)
                        fw.op("dve", lambda: V.tensor_reduce(out=m1.a[:, 0:1], in_=L.a[:, 0:4], axis=mybir.AxisListType.X, op=ALU.max), [L], [m1])
                        fw.op("dve", lambda: V.tensor_scalar(out=oh1.a, in0=L.a[:, 0:4], scalar1=m1.a[:, 0:1], scalar2=None, op0=ALU.is_equal), [L, m1], [oh1])
                        fw.op("dve", lambda: V.tensor_scalar(out=e1.a, in0=L.a[:, 0:4], scalar1=m1.a[:, 0:1], scalar2=None, op0=ALU.subtract), [L, m1], [e1])
                        fw.op("act", lambda: A.activation(out=e1.a, in_=e1.a, func=AF.Exp), [e1], [e1])
                        fw.op("dve", lambda: V.tensor_reduce(out=m1.a[:, 1:2], in_=e1.a, axis=mybir.AxisListType.X, op=ALU.add), [e1], [m1])
                        fw.op("dve", lambda: V.reciprocal(out=m1.a[:, 1:2], in_=m1.a[:, 1:2]), [m1], [m1])
                        fw.op("dve", lambda: V.tensor_scalar(out=l2.a, in0=L.a[:, 4:8], scalar1=oh1.a[:, 0:1], scalar2=None, op0=ALU.mult), [L, oh1], [l2])
                        for g in range(1, 4):
                            fw.op("dve", lambda: V.scalar_tensor_tensor(out=l2.a, in0=L.a[:, 4 + 4 * g:8 + 4 * g], scalar=oh1.a[:, g:g + 1], in1=l2.a, op0=ALU.mult, op1=ALU.add), [L, oh1, l2], [l2])
                        fw.op("dve", lambda: V.tensor_reduce(out=m1.a[:, 2:3], in_=l2.a, axis=mybir.AxisListType.X, op=ALU.max), [l2], [m1])
                        fw.op("dve", lambda: V.tensor_scalar(out=ohA.a, in0=l2.a, scalar1=m1.a[:, 2:3], scalar2=None, op0=ALU.is_equal), [l2, m1], [ohA])
                        fw.op("dve", lambda: V.scalar_tensor_tensor(out=l2b.a, in0=ohA.a, scalar=-1e30, in1=l2.a, op0=ALU.mult, op1=ALU.add), [ohA, l2], [l2b])
                        fw.op("dve", lambda: V.tensor_reduce(out=m1.a[:, 3:4], in_=l2b.a, axis=mybir.AxisListType.X, op=ALU.max), [l2b], [m1])
                        fw.op("dve", lambda: V.tensor_scalar(out=ohB.a, in0=l2b.a, scalar1=m1.a[:, 3:4], scalar2=None, op0=ALU.is_equal), [l2b, m1], [ohB])
                        fw.op("dve", lambda: V.tensor_tensor(out=e1.a[:, 0:1], in0=m1.a[:, 3:4], in1=m1.a[:, 2:3], op=ALU.subtract), [m1], [e1])
                        fw.op("act", lambda: A.activation(out=e1.a[:, 0:1], in_=e1.a[:, 0:1], func=AF.Exp), [e1], [e1])
                        fw.op("dve", lambda: V.tensor_scalar(out=e1.a[:, 1:2], in0=e1.a[:, 0:1], scalar1=1.0, scalar2=None, op0=ALU.add), [e1], [e1])
                        fw.op("dve", lambda: V.reciprocal(out=e1.a[:, 1:2], in_=e1.a[:, 1:2]), [e1], [e1])
                        fw.op("dve", lambda: V.tensor_tensor(out=e1.a[:, 2:3], in0=e1.a[:, 0:1], in1=e1.a[:, 1:2], op=ALU.mult), [e1], [e1])
                        fw.op("dve", lambda: V.tensor_scalar(out=e1.a[:, 1:3], in0=e1.a[:, 1:3], scalar1=m1.a[:, 1:2], scalar2=None, op0=ALU.mult), [e1, m1], [e1])
                        fw.op("dve", lambda: V.tensor_scalar(out=inner.a, in0=ohA.a, scalar1=e1.a[:, 1:2], scalar2=None, op0=ALU.mult), [ohA, e1], [inner])
                        fw.op("dve", lambda: V.scalar_tensor_tensor(out=inner.a, in0=ohB.a, scalar=e1.a[:, 2:3], in1=inner.a, op0=ALU.mult, op1=ALU.add), [ohB, e1, inner], [inner])
                        for g in range(4):
                            fw.op("dve", lambda: V.tensor_scalar(out=comb.a[:, n, 4 * g:4 * g + 4], in0=inner.a, scalar1=oh1.a[:, g:g + 1], scalar2=None, op0=ALU.mult), [inner, oh1], [comb])
                    fw.barrier()
                    chk()
                with ExitStack() as s2:
                    w1b = [sb(s2, "w1b%d" % k, [128, KC, 512], BF16) for k in range(2)]
                    w3b = [sb(s2, "w3b%d" % k, [128, KC, 512], BF16) for k in range(2)]
                    w2b = [sb(s2, "w2b%d" % k, [128, 4, D], BF16) for k in range(2)]
                    sl = [sb(s2, "sl%d" % k, [128, 512]) for k in range(2)]
                    ab = [sb(s2, "ab%d" % k, [128, DE], BF16) for k in range(2)]
                    aTt = [sb(s2, "aTt%d" % k, [128, 8, 128], BF16) for k in range(2)]
                    yo = [sb(s2, "yo%d" % k, [128, 512]) for k in range(3)]
                    ph = [ps(s2, "ph%d" % k, [128, 512]) for k in range(4)]
                    pTt = [ps(s2, "pTt", [128, 8, 128], BF16)]
                    py = [ps(s2, "py%d" % k, [128, 512]) for k in range(3)]
                    yi = 0
                    for e in range(NEXP):
                        for hf in range(2):
                            load_w(w1b[hf], moe_w1.a[i, e, :, hf * 512:(hf + 1) * 512], moe_w1, KC)
                            load_w(w3b[hf], moe_w3.a[i, e, :, hf * 512:(hf + 1) * 512], moe_w3, KC)
                            load_w(w2b[hf], moe_w2.a[i, e, hf * 512:(hf + 1) * 512, :], moe_w2, 4)
                        for n in range(NC):
                            a_ = ab[n % 2]; at = aTt[n % 2]
                            for hf in range(2):
                                p1 = ph[hf]; p3 = ph[2 + hf]
                                for k in range(KC):
                                    fw.op("pe", lambda: T.matmul(p1.a, lhsT=fT.a[:, k, n * 128:(n + 1) * 128], rhs=w1b[hf].a[:, k, :], start=(k == 0), stop=(k == KC - 1)), [fT, w1b[hf]], [p1])
                                for k in range(KC):
                                    fw.op("pe", lambda: T.matmul(p3.a, lhsT=fT.a[:, k, n * 128:(n + 1) * 128], rhs=w3b[hf].a[:, k, :], start=(k == 0), stop=(k == KC - 1)), [fT, w3b[hf]], [p3])
                                fw.op("act", lambda: A.activation(out=sl[hf].a, in_=p1.a, func=AF.Silu), [p1], [sl[hf]])
                                fw.op("dve", lambda: V.tensor_tensor(out=a_.a[:, hf * 512:(hf + 1) * 512], in0=p3.a, in1=sl[hf].a, op=ALU.mult), [p3, sl[hf]], [a_])
                            transpose_tile(a_, at, 0, pTt, nk=8)
                            for cb in range(4):
                                p = py[yi % 3]; y = yo[yi % 3]; yi += 1
                                for k in range(8):
                                    fw.op("pe", lambda: T.matmul(p.a, lhsT=at.a[:, k, :], rhs=w2b[k // 4].a[:, k % 4, cb * 512:(cb + 1) * 512], start=(k == 0), stop=(k == 7)), [at, w2b[k // 4]], [p])
                                fw.op("dve", lambda: V.scalar_tensor_tensor(out=y.a, in0=p.a, scalar=comb.a[:, n, e:e + 1], in1=GT2.a[:, cb * 512:(cb + 1) * 512], op0=ALU.mult, op1=ALU.mult), [p, comb, GT2], [y])
                                fw.dma("pool", lambda: G.dma_start(out=Xt[n].a[:, cb * 512:(cb + 1) * 512], in_=y.a, accum_op=ALU.add), Xt[n], [y])
                    fw.barrier()
                    chk()

        outproj_and_moe(0, ab_w_out, [(x_own, x_own.a[n * 128:(n + 1) * 128, :]) for n in range(NC)], False)

        with ExitStack() as st:
            Gl, SHl = make_GS(st, 1, 0, 0, 1, g_mix.a[1:2, :], g_mix)
            bufs = {"xt": [sb(st, "xt%d" % i, [128, D]) for i in range(2)], "i": 0, "junk": sb(st, "junk", [128, D]),
                    "ssq": sb(st, "ssq", [128, 1]), "rstd": sb(st, "rstd", [128, 1])}
            abf = [sb(st, "abf%d" % i, [128, D], BF16) for i in range(2)]
            aT = sb(st, "aT", [128, KC, NT], BF16)
            wb = [sb(st, "wb%d" % i, [128, KC, 512], BF16) for i in range(2)]
            osb = [sb(st, "osb%d" % i, [128, 512]) for i in range(3)]
            pT = [ps(st, "pT%d" % i, [128, 8, 128], BF16) for i in range(2)]
            pg = [ps(st, "pg%d" % i, [128, 512]) for i in range(4)]
            for n in range(NC):
                ab = abf[n % 2]
                norm_mod_tile(st, bufs, Xt[n], Xt[n].a, Gl, SHl, out_bf=ab)
                transpose_tile(ab, aT, n * 128, pT)
            ci = 0
            for cb in range(12):
                w = wb[cb % 2]
                load_w(w, cv_w_in.a[:, cb * 512:(cb + 1) * 512], cv_w_in, KC)
                for n in range(NC):
                    p = pg[ci % 4]; o = osb[ci % 3]; ci += 1
                    for k in range(KC):
                        fw.op("pe", lambda: T.matmul(p.a, lhsT=aT.a[:, k, n * 128:(n + 1) * 128], rhs=w.a[:, k, :], start=(k == 0), stop=(k == KC - 1)), [aT, w], [p])
                    if ci % 2:
                        fw.op("act", lambda: A.copy(out=o.a, in_=p.a), [p], [o])
                    else:
                        fw.op("dve", lambda: V.tensor_copy(out=o.a, in_=p.a), [p], [o])
                    ld(Zt[n], Zt[n].a[:, cb * 512:(cb + 1) * 512], o.a, [o])
            fw.barrier()
            chk()
        with ExitStack() as st:
            CW = sb(st, "CW", [128, 3, D]); CB = sb(st, "CB", [128, D])
            for j in range(3):
                ld(CW, CW.a[:, j, :], conv_w.a[j:j + 1, :].to_broadcast((128, D)), [conv_w])
            ld(CB, CB.a, conv_b.a.to_broadcast((128, D)), [conv_b])
            gc = [sb(st, "gc%d" % j, [128, D]) for j in range(3)]
            hv = [sb(st, "hv%d" % j, [128, D]) for j in range(3)]
            gbt = sb(st, "gbt", [128, D]); acc = sb(st, "acc", [128, D])
            zall = Zt + [Zpad, Zpad2]
            for n in range(NC):
                r0 = 1 + n * 128
                for j in range(3):
                    ld(gc[j], gc[j].a, Zd[r0 + j - 1:r0 + j - 1 + 128, 2048:4096], zall)
                    ld(hv[j], hv[j].a, Zd[r0 + j - 1:r0 + j - 1 + 128, 4096:6144], zall)
                ld(gbt, gbt.a, Zt[n].a[:, 0:2048], [Zt[n]])
                for j in range(3):
                    fw.op("dve", lambda: V.scalar_tensor_tensor(out=gc[j].a, in0=gc[j].a, scalar=1.0, in1=hv[j].a, op0=ALU.mult, op1=ALU.mult), [gc[j], hv[j]], [gc[j]])
                fw.op("dve", lambda: V.scalar_tensor_tensor(out=acc.a, in0=gc[0].a, scalar=cs.a[:, cc + 5:cc + 6], in1=CW.a[:, 0, :], op0=ALU.mult, op1=ALU.mult), [gc[0], cs, CW], [acc])
                fw.op("dve", lambda: V.scalar_tensor_tensor(out=acc.a, in0=acc.a, scalar=1.0, in1=CB.a, op0=ALU.mult, op1=ALU.add), [acc, CB], [acc])
                fw.op("dve", lambda: V.scalar_tensor_tensor(out=gc[1].a, in0=gc[1].a, scalar=1.0, in1=CW.a[:, 1, :], op0=ALU.mult, op1=ALU.mult), [gc[1], CW], [gc[1]])
                fw.op("dve", lambda: V.scalar_tensor_tensor(out=acc.a, in0=acc.a, scalar=1.0, in1=gc[1].a, op0=ALU.mult, op1=ALU.add), [acc, gc[1]], [acc])
                fw.op("dve", lambda: V.scalar_tensor_tensor(out=gc[2].a, in0=gc[2].a, scalar=cs.a[:, cc + 6:cc + 7], in1=CW.a[:, 2, :], op0=ALU.mult, op1=ALU.mult), [gc[2], cs, CW], [gc[2]])
                fw.op("dve", lambda: V.scalar_tensor_tensor(out=acc.a, in0=acc.a, scalar=1.0, in1=gc[2].a, op0=ALU.mult, op1=ALU.add), [acc, gc[2]], [acc])
                fw.op("dve", lambda: V.scalar_tensor_tensor(out=acc.a, in0=acc.a, scalar=1.0, in1=gbt.a, op0=ALU.mult, op1=ALU.mult), [acc, gbt], [acc])
                ld(Rt[n], Rt[n].a, acc.a, [acc])
            fw.barrier()
            chk()

        outproj_and_moe(1, cv_w_out, [(Xt[n], Xt[n].a) for n in range(NC)], True)

        with ExitStack() as st:
            Gfin = sb(st, "Gfin", [128, D])
            ld(Gfin, Gfin.a, g_final.a.to_broadcast((128, D)), [g_final])
            bufs = {"xt": [sb(st, "xt%d" % i, [128, D]) for i in range(2)], "i": 0, "junk": sb(st, "junk", [128, D]),
                    "ssq": sb(st, "ssq", [128, 1]), "rstd": sb(st, "rstd", [128, 1])}
            ot = [sb(st, "ot%d" % i, [128, D]) for i in range(2)]
            for n in range(NC):
                jk = norm_mod_tile(st, bufs, Xt[n], Xt[n].a, Gfin, None)
                fw.op("act", lambda: A.copy(out=ot[n % 2].a, in_=jk.a), [jk], [ot[n % 2]])
                ld(out, out.a[n * 128:(n + 1) * 128, :], ot[n % 2].a, [ot[n % 2]], k="sp")
            fw.barrier()
            chk()
    return nc


def rope_tables(pos):
    row = (pos // GRID_W).astype(np.float32)
    col = (pos % GRID_W).astype(np.float32)
    nf = HD // 4
    inv = (10000.0 ** (-np.arange(nf, dtype=np.float32) / nf)).astype(np.float32)
    ang = np.concatenate([row[:, None] * inv, col[:, None] * inv], axis=-1).astype(np.float32)
    return np.cos(ang).astype(np.float32), np.sin(ang).astype(np.float32)


def make_consts(NT):
    NC = NT // 128
    m = np.arange(128, dtype=np.float32)
    ident = np.eye(128, dtype=np.float32)
    R1 = np.maximum(m[None, :] - m[:, None], 0)
    R2 = np.maximum(m[:, None] - m[None, :], 0)
    cols = np.stack([127 - m, m, m + 1, 128 - m, np.full(128, 128.0, np.float32),
                     (np.arange(128) % GRID_W != 0).astype(np.float32),
                     (np.arange(128) % GRID_W != GRID_W - 1).astype(np.float32), np.zeros(128, np.float32)], axis=1)
    et = [128 * c + m for c in range(NC)] + [NT + 128 * c + m for c in range(2)] + [255 - 128 * c - m for c in range(2)]
    et = np.stack(et, axis=1)
    return np.concatenate([ident, R1, R2, cols, et], axis=1).astype(np.float32)


def prepare_inputs(inp, T):
    B = inp["x"].shape[0]
    NT = T // 2
    qs = np.float32(HD ** -0.5)
    cst = make_consts(NT)
    maps = []
    shared = {
        "w_mod": inp["w_mod"], "b_mod": inp["b_mod"], "g_mix": inp["g_mix"], "g_ffn": inp["g_ffn"],
        "g_final": inp["g_final"][None, :], "ab_w_in": inp["ab_w_in"][0], "ab_w_out": inp["ab_w_out"][0],
        "cv_w_in": inp["cv_w_in"][0], "cv_w_out": inp["cv_w_out"][0], "conv_b": inp["cv_conv_b"],
        "moe_w1": inp["moe_w1"], "moe_w3": inp["moe_w3"], "moe_w2": inp["moe_w2"], "cst": cst,
        "wr": np.concatenate([inp["moe_w_r1"], inp["moe_w_r2"].transpose(0, 2, 1, 3).reshape(2, D, 16)], axis=2),
        "br": np.concatenate([inp["moe_b_r1"], inp["moe_b_r2"].reshape(2, 16)], axis=1),
    }
    for b in range(B):
        for h in range(2):
            pos_all = np.arange(T)
            if h == 0:
                own = pos_all[:NT]; forg = pos_all[NT:]
                ctxl = inp["ctx"][b]
                dlv = inp["ret_decay_logit"][0].reshape(1, 16)
                ws = inp["sgu_w_s"][0]; bs = inp["sgu_b_s"][0]
                cw = inp["cv_conv_w"][0]
            else:
                own = pos_all[::-1][:NT]; forg = pos_all[:NT][::-1]
                ctxl = inp["ctx"][b][::-1]
                dlv = inp["ret_decay_logit"][0][::-1].reshape(1, 16)
                ws = inp["sgu_w_s"][0][:, ::-1, ::-1]; bs = inp["sgu_b_s"][0][:, ::-1]
                cw = inp["cv_conv_w"][0][::-1]
            co, so = rope_tables(own)
            cf, sf = rope_tables(forg)
            cT = np.stack([inp["c"][b].reshape(16, 128).T, inp["c_ctx"].reshape(16, 128).T], axis=2)
            m = dict(shared)
            m.update({
                "x_own": inp["x"][b][own], "x_for": inp["x"][b][forg], "ctx_l": ctxl, "cT": cT, "dl": dlv,
                "wsT": ws.transpose(0, 2, 1), "bsT": bs.T,
                "rope": np.stack([co * qs, so * qs, co, so, cf, sf]), "conv_w": cw,
            })
            maps.append({k: np.ascontiguousarray(v, dtype=np.float32) for k, v in m.items()})
    return maps


def kernel(**inputs):
    inp = {k: np.asarray(v) for k, v in inputs.items()}
    B, T, _ = inp["x"].shape
    NT = T // 2
    maps = prepare_inputs(inp, T)
    nc = build(NT)
    res = run_bass_kernel_spmd(nc, maps, core_ids=list(range(len(maps))))
    out = np.empty((B, T, D), np.float32)
    for b in range(B):
        out[b, :NT] = res.results[2 * b]["out"]
        out[b, NT:] = res.results[2 * b + 1]["out"][::-1]
    return out
```

```python
from contextlib import ExitStack
import numpy as np
import concourse.bass as bass
import concourse.mybir as mybir
from concourse.bass_utils import run_bass_kernel_spmd

F32 = mybir.dt.float32
BF16 = mybir.dt.bfloat16
I32 = mybir.dt.int32
AF = mybir.ActivationFunctionType
ALU = mybir.AluOpType

D = 2048
EPS = 1e-6
HD = 128
NH = 8
CTX = 256
GRID_W = 64
NEXP = 16
DE = 1024


class Buf:
    def __init__(self, ap, name):
        self.a = ap
        self.name = name
        self.last_write = None
        self.reads = []
        self.dsem = None
        self.dcount = 0


class FW:
    def __init__(self, nc):
        self.nc = nc
        self.engs = {"pe": nc.tensor, "act": nc.scalar, "dve": nc.vector, "pool": nc.gpsimd, "sp": nc.sync}
        self.sems = {}
        self.cnt = {}
        self.dcounts = {}
        self.waited = {k: {} for k in self.engs}
        for k in self.engs:
            self.sems[k] = nc.alloc_semaphore("s_" + k)
            self.cnt[k] = 0
        self.nbuf = 0
        self.free_dsems = []
        self.dbufs = []

    def _deps(self, reads, writes):
        deps = []
        for b in reads:
            if b.last_write is not None:
                deps.append(b.last_write)
        for b in writes:
            if b.last_write is not None:
                deps.append(b.last_write)
            deps.extend(b.reads)
        return deps

    def _emit_waits(self, ek, deps):
        eng = self.engs[ek]
        need = {}
        for (sk, v) in deps:
            if v > need.get(sk, 0):
                need[sk] = v
        for sk, v in need.items():
            if self.waited[ek].get(sk, 0) >= v:
                continue
            self.waited[ek][sk] = v
            eng.wait_ge(self.sems[sk], v)

    @staticmethod
    def _compact(reads):
        m = {}
        for sk, v in reads:
            if v > m.get(sk, 0):
                m[sk] = v
        return list(m.items())

    def op(self, ek, fn, reads=(), writes=()):
        self._emit_waits(ek, self._deps(reads, writes))
        ins = fn()
        self.cnt[ek] += 1
        ins.then_inc(self.sems[ek], 1)
        tok = (ek, self.cnt[ek])
        for b in writes:
            b.last_write = tok
            b.reads = []
        for b in reads:
            if b not in writes:
                b.reads.append(tok)
                if len(b.reads) > 8:
                    b.reads = self._compact(b.reads)
        return ins

    def dma(self, qk, fn, dst, srcs=()):
        reads = list(srcs)
        writes = [dst]
        self._emit_waits(qk, self._deps(reads, writes))
        ins = fn()
        if dst.dsem is None:
            if self.free_dsems:
                key = self.free_dsems.pop()
                dst.dcount = self.dcounts[key]
            else:
                key = "d%d" % self.nbuf
                self.nbuf += 1
                self.sems[key] = self.nc.alloc_semaphore(key)
            dst.dsem = key
            self.dbufs.append(dst)
        key = dst.dsem
        dst.dcount += 16
        self.dcounts[key] = dst.dcount
        ins.then_inc(self.sems[key], 16)
        tok = (key, dst.dcount)
        dst.last_write = tok
        dst.reads = []
        for b in reads:
            b.reads.append(tok)
            if len(b.reads) > 8:
                b.reads = self._compact(b.reads)
        return ins

    def barrier(self):
        deps = [(k, self.cnt[k]) for k in self.engs if self.cnt[k] > 0]
        deps += list(self.dcounts.items())
        for ek in self.engs:
            self._emit_waits(ek, deps)
        for b in self.dbufs:
            self.free_dsems.append(b.dsem)
            b.dsem = None
        self.dbufs = []


class _Stop(Exception):
    pass


def build(NT, stop=99):
    try:
        return _build(NT, stop)
    except _Stop as e:
        return e.args[0]


def _build(NT, stop):
    NC = NT // 128
    stage = [0]

    import os
    substop = int(os.environ.get("KSUB", "0"))

    def sub(k):
        if stage[0] + 1 == stop and substop == k:
            fw.barrier()
            raise _Stop(nc)

    def chk():
        stage[0] += 1
        if stage[0] >= stop:
            raise _Stop(nc)
    KC = D // 128
    nc = bass.Bass("TRN2", target_bir_lowering=False)
    fw = FW(nc)
    V, A, G, T, S = nc.vector, nc.scalar, nc.gpsimd, nc.tensor, nc.sync

    def ein(name, shape):
        return Buf(nc.dram_tensor(name, shape, F32, kind="ExternalInput").ap(), name)

    def dint(name, shape, dt=F32):
        return Buf(nc.dram_tensor(name, shape, dt, kind="Internal").ap(), name)

    x_own = ein("x_own", [NT, D]); x_for = ein("x_for", [NT, D]); ctx_l = ein("ctx_l", [CTX, D])
    cT = ein("cT", [128, KC, 2])
    w_mod = ein("w_mod", [2, D, 6 * D]); b_mod = ein("b_mod", [2, 6 * D])
    g_mix = ein("g_mix", [2, D]); g_ffn = ein("g_ffn", [2, D]); g_final = ein("g_final", [1, D])
    ab_w_in = ein("ab_w_in", [D, 6144]); ab_w_out = ein("ab_w_out", [D, D])
    dl = ein("dl", [1, 16])
    wsT = ein("wsT", [8, 128, 128]); bsT = ein("bsT", [128, 8])
    rope = ein("rope", [6, NT, 64])
    cv_w_in = ein("cv_w_in", [D, 6144]); cv_w_out = ein("cv_w_out", [D, D])
    conv_w = ein("conv_w", [3, D]); conv_b = ein("conv_b", [1, D])
    wr = ein("wr", [2, D, 20]); br = ein("br", [2, 20])
    moe_w1 = [ein("moe_w1_%d" % l, [16384, D]) for l in range(2)]
    moe_w3 = [ein("moe_w3_%d" % l, [16384, D]) for l in range(2)]
    moe_w2 = [ein("moe_w2_%d" % l, [16384, D]) for l in range(2)]
    bcW = G.alloc_register("bcW"); G.reg_mov(bcW, 16383)
    bcS = G.alloc_register("bcS"); G.reg_mov(bcS, (2 * NC + 16) * 128 - 1)
    CW_ = 128 * 3 + 8 + NC + 4
    TRI = CW_; BASE = CW_ + 256; KV = BASE + 16
    CWT = KV + 2 * NC + 16
    cst = ein("cst", [128, CWT])
    out = Buf(nc.dram_tensor("out", [NT, D], F32, kind="ExternalOutput").ap(), "out")

    Xd = nc.dram_tensor("Xd", [NT, D], F32, kind="Internal").ap()
    Xt = [Buf(Xd[n * 128:(n + 1) * 128, :], "X%d" % n) for n in range(NC)]
    Zd = nc.dram_tensor("Zd", [NT + 2, 6144], F32, kind="Internal").ap()
    Zt = [Buf(Zd[1 + n * 128:1 + (n + 1) * 128, :], "Z%d" % n) for n in range(NC)]
    Zpad = Buf(Zd[0:1, :], "Zpad")
    Zfd = nc.dram_tensor("Zfd", [NT, 2048], F32, kind="Internal").ap()
    Zft = [Buf(Zfd[n * 128:(n + 1) * 128, :], "Zf%d" % n) for n in range(NC)]
    Zcd = nc.dram_tensor("Zcd", [CTX, 2048], F32, kind="Internal").ap()
    Zct = [Buf(Zcd[n * 128:(n + 1) * 128, :], "Zc%d" % n) for n in range(2)]
    Rd = nc.dram_tensor("Rd", [NT, D], F32, kind="Internal").ap()
    Rt = [Buf(Rd[n * 128:(n + 1) * 128, :], "R%d" % n) for n in range(NC)]
    NS_ = 2 * NC + 16
    FS = Buf(nc.dram_tensor("FSd", [NS_ * 128, D], BF16, kind="Internal").ap(), "FS")
    YS = Buf(nc.dram_tensor("YSd", [NS_ * 128, D], F32, kind="Internal").ap(), "YS")
    MODd = nc.dram_tensor("MODd", [2, 2, 6 * D], F32, kind="Internal").ap()
    MOD = Buf(MODd, "MOD")

    dq = ["sp", "act"]
    dqi = [0]

    def q():
        dqi[0] ^= 1
        return dq[dqi[0]]

    def qeng(k):
        return {"sp": S, "act": A, "pool": G}[k]

    def ld(dst, dst_ap, src_ap, srcs, k=None):
        k = k or q()
        fw.dma(k, lambda: qeng(k).dma_start(out=dst_ap, in_=src_ap), dst, srcs)

    with ExitStack() as glob:
        uid = [0]

        def sb(stack, name, shape, dt=F32):
            uid[0] += 1
            t = stack.enter_context(nc.sbuf_tensor("%s_%d" % (name, uid[0]), shape, dt))
            return Buf(t[:], name)

        def ps(stack, name, shape, dt=F32):
            uid[0] += 1
            t = stack.enter_context(nc.psum_tensor("%s_%d" % (name, uid[0]), shape, dt))
            return Buf(t[:], name)

        cs = sb(glob, "cs", [128, CWT])
        ld(cs, cs.a, cst.a, [cst])
        identf = cs.a[:, 0:128]
        R1 = cs.a[:, 128:256]
        R2 = cs.a[:, 256:384]
        cc = 384
        ET = 392
        identb = sb(glob, "identb", [128, 128], BF16)
        fw.op("dve", lambda: V.tensor_copy(out=identb.a, in_=identf), [cs], [identb])
        identF = sb(glob, "identF", [128, 128])
        fw.op("dve", lambda: V.tensor_copy(out=identF.a, in_=identf), [cs], [identF])
        Zpad2 = Buf(Zd[NT + 1:NT + 2, :], "Zpad2")
        with ExitStack() as st:
            zero = sb(st, "zero", [1, 6144])
            fw.op("dve", lambda: V.memset(zero.a, 0.0), [], [zero])
            ld(Zpad, Zd[0:1, :], zero.a, [zero])
            ld(Zpad2, Zd[NT + 1:NT + 2, :], zero.a, [zero])
            fw.barrier()
            chk()

        with ExitStack() as st:
            scT = sb(st, "scT", [128, KC, 2])
            ld(scT, scT.a, cT.a, [cT])
            fw.op("act", lambda: A.activation(out=scT.a, in_=scT.a, func=AF.Silu), [scT], [scT])
            wm = [sb(st, "wm%d" % i, [128, KC, 512]) for i in range(2)]
            bm = [sb(st, "bm%d" % i, [2, 512]) for i in range(2)]
            mo = [sb(st, "mo%d" % i, [2, 512]) for i in range(2)]
            pm = [ps(st, "pm%d" % i, [2, 512]) for i in range(2)]
            it = 0
            for i in range(2):
                for cb in range(24):
                    j = it % 2
                    it += 1
                    ld(wm[j], wm[j].a, w_mod.a[i, :, cb * 512:(cb + 1) * 512].rearrange("(k p) n -> p k n", p=128), [w_mod])
                    ld(bm[j], bm[j].a, b_mod.a[i:i + 1, cb * 512:(cb + 1) * 512].to_broadcast((2, 512)), [b_mod])
                    for k in range(KC):
                        fw.op("pe", lambda: T.matmul(pm[j].a, lhsT=scT.a[:, k, :], rhs=wm[j].a[:, k, :], start=(k == 0), stop=(k == KC - 1)), [scT, wm[j]], [pm[j]])
                    fw.op("dve", lambda: V.tensor_tensor(out=mo[j].a, in0=pm[j].a, in1=bm[j].a, op=ALU.add), [pm[j], bm[j]], [mo[j]])
                    ld(MOD, MODd[i, :, cb * 512:(cb + 1) * 512], mo[j].a, [mo[j]], k="sp")
            fw.barrier()
            chk()

        def modrow(i, row, j):
            return MODd[i, row:row + 1, j * D:(j + 1) * D].to_broadcast((128, D))

        def norm_mod_tile(st, bufs, src_buf, src_ap, Gt, SHt, out_bf=None, out_f=None):
            xt = bufs["xt"][bufs["i"] % 2]
            bufs["i"] += 1
            ld(xt, xt.a, src_ap, [src_buf])
            sub(8)
            junk, ssq, rstd = bufs["junk"], bufs["ssq"], bufs["rstd"]
            fw.op("act", lambda: A.activation(out=junk.a, in_=xt.a, func=AF.Square), [xt], [junk])
            fw.op("dve", lambda: V.tensor_reduce(out=ssq.a, in_=junk.a, axis=mybir.AxisListType.X, op=ALU.add), [junk], [ssq])
            fw.op("dve", lambda: V.tensor_scalar(out=ssq.a, in0=ssq.a, scalar1=1.0 / D, scalar2=EPS, op0=ALU.mult, op1=ALU.add), [ssq], [ssq])
            fw.op("act", lambda: A.activation(out=ssq.a, in_=ssq.a, func=AF.Sqrt), [ssq], [ssq])
            fw.op("dve", lambda: V.reciprocal(out=rstd.a, in_=ssq.a), [ssq], [rstd])
            fw.op("dve", lambda: V.scalar_tensor_tensor(out=junk.a, in0=xt.a, scalar=rstd.a[:, 0:1], in1=Gt.a, op0=ALU.mult, op1=ALU.mult), [xt, rstd, Gt], [junk])
            sub(9)
            if SHt is None:
                return junk
            if out_f is not None:
                fw.op("dve", lambda: V.scalar_tensor_tensor(out=out_f.a, in0=junk.a, scalar=1.0, in1=SHt.a, op0=ALU.mult, op1=ALU.add), [junk, SHt], [out_f])
                if out_bf is not None:
                    fw.op("act", lambda: A.copy(out=out_bf.a, in_=out_f.a), [out_f], [out_bf])
            else:
                fw.op("dve", lambda: V.tensor_tensor(out=out_bf.a, in0=junk.a, in1=SHt.a, op=ALU.add), [junk, SHt], [out_bf])
            return None

        gs_tmp = {}

        def make_GS(st, i, row, jsh, jsc, gvec_ap, gbuf):
            Gt = sb(st, "Gt%d%d%d" % (i, row, jsh), [128, D])
            SHt = sb(st, "SHt%d%d%d" % (i, row, jsh), [128, D])
            if "gtmp" not in gs_tmp or gs_tmp["st"] is not st:
                gs_tmp["gtmp"] = sb(st, "gtmp", [128, D]); gs_tmp["st"] = st
            tmp = gs_tmp["gtmp"]
            ld(Gt, Gt.a, modrow(i, row, jsc), [MOD])
            ld(tmp, tmp.a, gvec_ap.to_broadcast((128, D)), [gbuf])
            ld(SHt, SHt.a, modrow(i, row, jsh), [MOD])
            fw.op("dve", lambda: V.scalar_tensor_tensor(out=Gt.a, in0=Gt.a, scalar=1.0, in1=tmp.a, op0=ALU.add, op1=ALU.mult), [Gt, tmp], [Gt])
            return Gt, SHt

        def transpose_tile(src_bf, dstT, col0, pT, nk=KC):
            for k0 in range(0, nk, 8):
                p = pT[(k0 // 8) % len(pT)]
                for k in range(k0, min(nk, k0 + 8)):
                    fw.op("pe", lambda: T.transpose(out=p.a[:, k - k0, :], in_=src_bf.a[:, k * 128:(k + 1) * 128], identity=identb.a), [src_bf, identb], [p])
                n = min(nk, k0 + 8) - k0
                if (k0 // 8) % 2 == 0:
                    fw.op("act", lambda: A.copy(out=dstT.a[:, k0:k0 + n, col0:col0 + 128], in_=p.a[:, 0:n, :]), [p], [dstT])
                else:
                    fw.op("dve", lambda: V.tensor_copy(out=dstT.a[:, k0:k0 + n, col0:col0 + 128], in_=p.a[:, 0:n, :]), [p], [dstT])

        def load_w(wbuf, w_ap, wsrc, kc):
            fw.dma("pool", lambda: G.dma_start(out=wbuf.a, in_=w_ap.rearrange("(k p) n -> p k n", p=128)), wbuf, [wsrc])

        with ExitStack() as st:
            Gl, SHl = make_GS(st, 0, 0, 0, 1, g_mix.a[0:1, :], g_mix)
            Gc, SHc = make_GS(st, 0, 1, 0, 1, g_mix.a[0:1, :], g_mix)
            bufs = {"xt": [sb(st, "xt%d" % i, [128, D]) for i in range(1)] * 2, "i": 0, "junk": sb(st, "junk", [128, D]),
                    "ssq": sb(st, "ssq", [128, 1]), "rstd": sb(st, "rstd", [128, 1])}
            abf = [sb(st, "abf%d" % i, [128, D], BF16) for i in range(2)]
            aT = sb(st, "aT", [128, KC, NT], BF16)
            wb = [sb(st, "wb%d" % i, [128, KC, 512], BF16) for i in range(2)]
            osb = [sb(st, "osb%d" % i, [128, 512]) for i in range(3)]
            pT = [ps(st, "pT%d" % i, [128, 8, 128], BF16) for i in range(2)]
            pg = [ps(st, "pg%d" % i, [128, 512]) for i in range(4)]
            cnt = {"w": 0, "o": 0, "p": 0}

            def inproj(src_buf, src_ap_fn, ntiles, Gt, SHt, w_all, wsrc, cbs, zts, zcol0):
                for n in range(ntiles):
                    ab = abf[n % 2]
                    norm_mod_tile(st, bufs, src_buf, src_ap_fn(n), Gt, SHt, out_bf=ab)
                    transpose_tile(ab, aT, n * 128, pT)
                for cb in cbs:
                    w = wb[cnt["w"] % 2]
                    cnt["w"] += 1
                    load_w(w, w_all.a[:, cb * 512:(cb + 1) * 512], wsrc, KC)
                    for n in range(ntiles):
                        p = pg[cnt["p"] % 4]
                        cnt["p"] += 1
                        for k in range(KC):
                            fw.op("pe", lambda: T.matmul(p.a, lhsT=aT.a[:, k, n * 128:(n + 1) * 128], rhs=w.a[:, k, :], start=(k == 0), stop=(k == KC - 1)), [aT, w], [p])
                        o = osb[cnt["o"] % 3]
                        cnt["o"] += 1
                        if cnt["o"] % 2:
                            fw.op("act", lambda: A.copy(out=o.a, in_=p.a), [p], [o])
                        else:
                            fw.op("dve", lambda: V.tensor_copy(out=o.a, in_=p.a), [p], [o])
                        c0 = cb * 512 - zcol0
                        ld(zts[n], zts[n].a[:, c0:c0 + 512], o.a, [o])

            kvb = [2, 3, 4, 5]
            inproj(x_for, lambda n: x_for.a[n * 128:(n + 1) * 128, :], NC, Gl, SHl, ab_w_in, ab_w_in, kvb, Zft, 1024)
            inproj(ctx_l, lambda n: ctx_l.a[n * 128:(n + 1) * 128, :], 2, Gc, SHc, ab_w_in, ab_w_in, kvb, Zct, 1024)
            inproj(x_own, lambda n: x_own.a[n * 128:(n + 1) * 128, :], NC, Gl, SHl, ab_w_in, ab_w_in, list(range(12)), Zt, 0)
            fw.barrier()
            chk()

        with ExitStack() as st:
            lg = sb(st, "lg", [128, 16])
            t1 = sb(st, "t1", [128, 16]); t2 = sb(st, "t2", [128, 16]); t3 = sb(st, "t3", [128, 16]); t4 = sb(st, "t4", [128, 16])
            ld(lg, lg.a, dl.a.to_broadcast((128, 16)), [dl])
            fw.op("act", lambda: A.activation(out=t1.a, in_=lg.a, func=AF.Exp, scale=-1.0), [lg], [t1])
            fw.op("dve", lambda: V.tensor_scalar(out=t2.a, in0=t1.a, scalar1=-0.25, scalar2=1.0 / 3.0, op0=ALU.mult, op1=ALU.add), [t1], [t2])
            fw.op("dve", lambda: V.tensor_tensor(out=t2.a, in0=t2.a, in1=t1.a, op=ALU.mult), [t2, t1], [t2])
            fw.op("dve", lambda: V.tensor_scalar(out=t2.a, in0=t2.a, scalar1=-0.5, scalar2=None, op0=ALU.add), [t2], [t2])
            fw.op("dve", lambda: V.tensor_tensor(out=t2.a, in0=t2.a, in1=t1.a, op=ALU.mult), [t2, t1], [t2])
            fw.op("dve", lambda: V.tensor_scalar(out=t2.a, in0=t2.a, scalar1=1.0, scalar2=None, op0=ALU.add), [t2], [t2])
            fw.op("dve", lambda: V.tensor_tensor(out=t2.a, in0=t2.a, in1=t1.a, op=ALU.mult), [t2, t1], [t2])
            fw.op("dve", lambda: V.tensor_scalar(out=t3.a, in0=t1.a, scalar1=1.0, scalar2=None, op0=ALU.add), [t1], [t3])
            fw.op("act", lambda: A.activation(out=t3.a, in_=t3.a, func=AF.Ln), [t3], [t3])
            fw.op("dve", lambda: V.tensor_scalar(out=t4.a, in0=t1.a, scalar1=0.1, scalar2=None, op0=ALU.is_lt), [t1], [t4])
            fw.op("dve", lambda: V.tensor_tensor(out=t2.a, in0=t2.a, in1=t3.a, op=ALU.subtract), [t2, t3], [t2])
            fw.op("dve", lambda: V.tensor_tensor(out=t2.a, in0=t2.a, in1=t4.a, op=ALU.mult), [t2, t4], [t2])
            fw.op("dve", lambda: V.tensor_tensor(out=t2.a, in0=t2.a, in1=t3.a, op=ALU.add), [t2, t3], [t2])
            fw.op("dve", lambda: V.tensor_scalar(out=lg.a, in0=t2.a, scalar1=-1.0, scalar2=None, op0=ALU.mult), [t2], [lg])

            ropeT = sb(st, "ropeT", [128, 6, NC, 64])
            for r in range(6):
                ld(ropeT, ropeT.a[:, r, :, :], rope.a[r].rearrange("(n p) c -> p n c", p=128), [rope])
            NE = NC + 4
            ew = sb(st, "ew", [128, NE]); ewB = sb(st, "ewB", [128, NE])
            dcol = sb(st, "dcol", [128, 8])
            Dh = sb(st, "Dh", [128, 128]); dtmp = sb(st, "dtmp", [128, 128])
            qs = [sb(st, "qs%d" % i, [128, NC, 128]) for i in range(1)] * 2
            ks = [sb(st, "ks%d" % i, [128, NC, 128]) for i in range(1)] * 2
            vs = [sb(st, "vs%d" % i, [128, NC, 128]) for i in range(1)] * 2
            gs = [sb(st, "gs%d" % i, [128, NC, 128]) for i in range(1)] * 2
            kf = sb(st, "kf", [128, NC + 2, 128]); vf = sb(st, "vf", [128, NC + 2, 128])
            rq = sb(st, "rq", [128, NC, 128]); rk = sb(st, "rk", [128, NC, 128]); rtmp = sb(st, "rtmp", [128, NC, 64])
            qb = sb(st, "qb", [128, NC, 128], BF16); kb = sb(st, "kb", [128, NC, 128], BF16)
            vb = sb(st, "vb", [128, NC, 128], BF16); vA = sb(st, "vA", [128, NC, 128], BF16); vB = sb(st, "vB", [128, NC, 128], BF16)
            kfb = sb(st, "kfb", [128, NC + 2, 128], BF16); vfB = sb(st, "vfB", [128, NC + 2, 128], BF16); vcA = sb(st, "vcA", [128, 2, 128], BF16)
            SA = sb(st, "SA", [128, NC + 1, 128]); SB = sb(st, "SB", [128, NC + 1, 128])
            SAb = sb(st, "SAb", [128, NC + 1, 128], BF16); SBb = sb(st, "SBb", [128, NC + 1, 128], BF16)
            qT = [sb(st, "qT%d" % i, [128, 2, 128], BF16) for i in range(2)]
            ATb = [sb(st, "ATb%d" % i, [128, 128], BF16) for i in range(2)]
            ysb = [sb(st, "ysb%d" % i, [128, 128]) for i in range(2)]
            stt_ = sb(st, "bnst", [128, 6]); mv = sb(st, "mv", [128, 2]); rs = sb(st, "rs", [128, 1])
            sg = sb(st, "sg", [128, 128])
            rout = [sb(st, "rout%d" % i, [128, NC, 128]) for i in range(2)]
            pS = [ps(st, "pS%d" % i, [128, 128]) for i in range(2)]
            pU = [ps(st, "pU%d" % i, [128, 2, 128]) for i in range(2)]
            pTq = ps(st, "pTq", [128, 2, 128], BF16)
            pSc = ps(st, "pSc", [128, 128])
            pY = ps(st, "pY", [128, 3, 128])

            def rope_apply(dst, src, ci, si, nchunk):
                x1 = src.a[:, 0:nchunk, 0:64]; x2 = src.a[:, 0:nchunk, 64:128]
                cth = ropeT.a[:, ci, 0:nchunk, :]; sth = ropeT.a[:, si, 0:nchunk, :]
                tm = rtmp.a[:, 0:nchunk, :]
                fw.op("dve", lambda: V.tensor_tensor(out=dst.a[:, 0:nchunk, 0:64], in0=x1, in1=cth, op=ALU.mult), [src, ropeT], [dst])
                fw.op("dve", lambda: V.tensor_tensor(out=tm, in0=x2, in1=sth, op=ALU.mult), [src, ropeT], [rtmp])
                fw.op("dve", lambda: V.tensor_tensor(out=dst.a[:, 0:nchunk, 0:64], in0=dst.a[:, 0:nchunk, 0:64], in1=tm, op=ALU.subtract), [dst, rtmp], [dst])
                fw.op("dve", lambda: V.tensor_tensor(out=dst.a[:, 0:nchunk, 64:128], in0=x1, in1=sth, op=ALU.mult), [src, ropeT], [dst])
                fw.op("dve", lambda: V.tensor_tensor(out=tm, in0=x2, in1=cth, op=ALU.mult), [src, ropeT], [rtmp])
                fw.op("dve", lambda: V.tensor_tensor(out=dst.a[:, 0:nchunk, 64:128], in0=dst.a[:, 0:nchunk, 64:128], in1=tm, op=ALU.add), [dst, rtmp], [dst])

            for h in range(NH):
                j = h % 2
                c0 = h * 128
                ld(qs[j], qs[j].a, Zd[1:NT + 1, c0:c0 + 128].rearrange("(n p) c -> p n c", p=128), Zt)
                ld(ks[j], ks[j].a, Zd[1:NT + 1, 1024 + c0:1024 + c0 + 128].rearrange("(n p) c -> p n c", p=128), Zt)
                ld(vs[j], vs[j].a, Zd[1:NT + 1, 2048 + c0:2048 + c0 + 128].rearrange("(n p) c -> p n c", p=128), Zt)
                ld(gs[j], gs[j].a, Zd[1:NT + 1, 3072 + c0:3072 + c0 + 128].rearrange("(n p) c -> p n c", p=128), Zt)
                ld(kf, kf.a[:, 0:NC, :], Zfd[:, c0:c0 + 128].rearrange("(n p) c -> p n c", p=128), Zft)
                ld(kf, kf.a[:, NC:NC + 2, :], Zcd[:, c0:c0 + 128].rearrange("(n p) c -> p n c", p=128), Zct)
                ld(vf, vf.a[:, 0:NC, :], Zfd[:, 1024 + c0:1024 + c0 + 128].rearrange("(n p) c -> p n c", p=128), Zft)
                ld(vf, vf.a[:, NC:NC + 2, :], Zcd[:, 1024 + c0:1024 + c0 + 128].rearrange("(n p) c -> p n c", p=128), Zct)
                lgA = lg.a[:, h:h + 1]; lgB = lg.a[:, 8 + h:9 + h]
                fw.op("dve", lambda: V.tensor_scalar(out=dcol.a[:, 0:1], in0=cs.a[:, cc + 0:cc + 1], scalar1=lgA, scalar2=None, op0=ALU.mult), [cs, lg], [dcol])
                fw.op("dve", lambda: V.tensor_scalar(out=dcol.a[:, 1:2], in0=cs.a[:, cc + 1:cc + 2], scalar1=lgB, scalar2=None, op0=ALU.mult), [cs, lg], [dcol])
                fw.op("dve", lambda: V.tensor_scalar(out=dcol.a[:, 2:3], in0=cs.a[:, cc + 2:cc + 3], scalar1=lgA, scalar2=None, op0=ALU.mult), [cs, lg], [dcol])
                fw.op("dve", lambda: V.tensor_scalar(out=dcol.a[:, 3:4], in0=cs.a[:, cc + 3:cc + 4], scalar1=lgB, scalar2=None, op0=ALU.mult), [cs, lg], [dcol])
                fw.op("dve", lambda: V.tensor_scalar(out=dcol.a[:, 4:5], in0=cs.a[:, cc + 4:cc + 5], scalar1=lgA, scalar2=None, op0=ALU.mult), [cs, lg], [dcol])
                fw.op("dve", lambda: V.tensor_scalar(out=dcol.a[:, 5:6], in0=cs.a[:, cc + 4:cc + 5], scalar1=lgB, scalar2=None, op0=ALU.mult), [cs, lg], [dcol])
                fw.op("act", lambda: A.activation(out=dcol.a[:, 0:6], in_=dcol.a[:, 0:6], func=AF.Exp), [dcol], [dcol])
                fw.op("dve", lambda: V.tensor_scalar(out=ewB.a[:, 0:NC + 2], in0=cs.a[:, ET:ET + NC + 2], scalar1=lgB, scalar2=None, op0=ALU.mult), [cs, lg], [ewB])
                fw.op("dve", lambda: V.tensor_scalar(out=ewB.a[:, NC + 2:NC + 4], in0=cs.a[:, ET + NC + 2:ET + NC + 4], scalar1=lgA, scalar2=None, op0=ALU.mult), [cs, lg], [ewB])
                fw.op("act", lambda: A.activation(out=ew.a, in_=ewB.a, func=AF.Exp), [ewB], [ew])
                fw.op("dve", lambda: V.tensor_scalar(out=dtmp.a, in0=R1, scalar1=lgA, scalar2=None, op0=ALU.mult), [cs, lg], [dtmp])
                fw.op("dve", lambda: V.scalar_tensor_tensor(out=dtmp.a, in0=R2, scalar=lgB, in1=dtmp.a, op0=ALU.mult, op1=ALU.add), [cs, lg, dtmp], [dtmp])
                fw.op("act", lambda: A.activation(out=Dh.a, in_=dtmp.a, func=AF.Exp), [dtmp], [Dh])
                rope_apply(rq, qs[j], 0, 1, NC)
                rope_apply(rk, ks[j], 2, 3, NC)
                fw.op("act", lambda: A.copy(out=qb.a, in_=rq.a), [rq], [qb])
                fw.op("act", lambda: A.copy(out=kb.a, in_=rk.a), [rk], [kb])
                fw.op("act", lambda: A.copy(out=vb.a, in_=vs[j].a), [vs[j]], [vb])
                fw.op("dve", lambda: V.tensor_scalar(out=vA.a, in0=vs[j].a, scalar1=dcol.a[:, 0:1], scalar2=None, op0=ALU.mult), [vs[j], dcol], [vA])
                fw.op("dve", lambda: V.tensor_scalar(out=vB.a, in0=vs[j].a, scalar1=dcol.a[:, 1:2], scalar2=None, op0=ALU.mult), [vs[j], dcol], [vB])
                rope_apply(rk, kf, 4, 5, NC)
                fw.op("act", lambda: A.copy(out=kfb.a[:, 0:NC, :], in_=rk.a), [rk], [kfb])
                fw.op("act", lambda: A.copy(out=kfb.a[:, NC:NC + 2, :], in_=kf.a[:, NC:NC + 2, :]), [kf], [kfb])
                for c in range(NC + 2):
                    fw.op("dve", lambda: V.tensor_scalar(out=vfB.a[:, c, :], in0=vf.a[:, c, :], scalar1=ew.a[:, c:c + 1], scalar2=None, op0=ALU.mult), [vf, ew], [vfB])
                for c in range(2):
                    fw.op("dve", lambda: V.tensor_scalar(out=vcA.a[:, c, :], in0=vf.a[:, NC + c, :], scalar1=ew.a[:, NC + 2 + c:NC + 3 + c], scalar2=None, op0=ALU.mult), [vf, ew], [vcA])
                for c in range(2):
                    fw.op("pe", lambda: T.matmul(pS[0].a, lhsT=kfb.a[:, NC + c, :], rhs=vcA.a[:, c, :], start=(c == 0), stop=(c == 1)), [kfb, vcA], [pS[0]])
                fw.op("act", lambda: A.copy(out=SA.a[:, 0, :], in_=pS[0].a), [pS[0]], [SA])
                for c in range(NC + 2):
                    fw.op("pe", lambda: T.matmul(pS[1].a, lhsT=kfb.a[:, c, :], rhs=vfB.a[:, c, :], start=(c == 0), stop=(c == NC + 1)), [kfb, vfB], [pS[1]])
                fw.op("act", lambda: A.copy(out=SB.a[:, NC, :], in_=pS[1].a), [pS[1]], [SB])
                for n in range(NC):
                    p = pU[n % 2]
                    fw.op("pe", lambda: T.matmul(p.a[:, 0, :], lhsT=kb.a[:, n, :], rhs=vA.a[:, n, :], start=True, stop=True), [kb, vA], [p])
                    fw.op("dve", lambda: V.scalar_tensor_tensor(out=SA.a[:, n + 1, :], in0=SA.a[:, n, :], scalar=dcol.a[:, 4:5], in1=p.a[:, 0, :], op0=ALU.mult, op1=ALU.add), [SA, dcol, p], [SA])
                for n in range(NC - 1, -1, -1):
                    p = pU[n % 2]
                    fw.op("pe", lambda: T.matmul(p.a[:, 1, :], lhsT=kb.a[:, n, :], rhs=vB.a[:, n, :], start=True, stop=True), [kb, vB], [p])
                    fw.op("dve", lambda: V.scalar_tensor_tensor(out=SB.a[:, n, :], in0=SB.a[:, n + 1, :], scalar=dcol.a[:, 5:6], in1=p.a[:, 1, :], op0=ALU.mult, op1=ALU.add), [SB, dcol, p], [SB])
                fw.op("act", lambda: A.copy(out=SAb.a, in_=SA.a), [SA], [SAb])
                fw.op("act", lambda: A.copy(out=SBb.a, in_=SB.a), [SB], [SBb])
                ro = rout[j]
                for n in range(NC):
                    qt = qT[n % 2]; at = ATb[n % 2]; y = ysb[n % 2]
                    fw.op("pe", lambda: T.transpose(out=pTq.a[:, 0, :], in_=qb.a[:, n, :], identity=identb.a), [qb, identb], [pTq])
                    fw.op("pe", lambda: T.transpose(out=pTq.a[:, 1, :], in_=kb.a[:, n, :], identity=identb.a), [kb, identb], [pTq])
                    fw.op("act", lambda: A.copy(out=qt.a, in_=pTq.a), [pTq], [qt])
                    fw.op("pe", lambda: T.matmul(pSc.a, lhsT=qt.a[:, 1, :], rhs=qt.a[:, 0, :], start=True, stop=True), [qt], [pSc])
                    fw.op("dve", lambda: V.tensor_tensor(out=at.a, in0=pSc.a, in1=Dh.a, op=ALU.mult), [pSc, Dh], [at])
                    fw.op("pe", lambda: T.matmul(pY.a[:, 0, :], lhsT=at.a, rhs=vb.a[:, n, :], start=True, stop=True), [at, vb], [pY])
                    fw.op("pe", lambda: T.matmul(pY.a[:, 1, :], lhsT=qt.a[:, 0, :], rhs=SAb.a[:, n, :], start=True, stop=True), [qt, SAb], [pY])
                    fw.op("pe", lambda: T.matmul(pY.a[:, 2, :], lhsT=qt.a[:, 0, :], rhs=SBb.a[:, n + 1, :], start=True, stop=True), [qt, SBb], [pY])
                    fw.op("act", lambda: A.copy(out=y.a, in_=pY.a[:, 0, :]), [pY], [y])
                    fw.op("dve", lambda: V.scalar_tensor_tensor(out=y.a, in0=pY.a[:, 1, :], scalar=dcol.a[:, 2:3], in1=y.a, op0=ALU.mult, op1=ALU.add), [pY, dcol, y], [y])
                    fw.op("dve", lambda: V.scalar_tensor_tensor(out=y.a, in0=pY.a[:, 2, :], scalar=dcol.a[:, 3:4], in1=y.a, op0=ALU.mult, op1=ALU.add), [pY, dcol, y], [y])
                    fw.op("dve", lambda: V.bn_stats(out=stt_.a, in_=y.a), [y], [stt_])
                    fw.op("dve", lambda: V.bn_aggr(out=mv.a, in_=stt_.a), [stt_], [mv])
                    fw.op("dve", lambda: V.tensor_scalar(out=rs.a, in0=mv.a[:, 1:2], scalar1=EPS, scalar2=None, op0=ALU.add), [mv], [rs])
                    fw.op("act", lambda: A.activation(out=rs.a, in_=rs.a, func=AF.Sqrt), [rs], [rs])
                    fw.op("dve", lambda: V.reciprocal(out=rs.a, in_=rs.a), [rs], [rs])
                    fw.op("dve", lambda: V.tensor_scalar(out=y.a, in0=y.a, scalar1=mv.a[:, 0:1], scalar2=rs.a[:, 0:1], op0=ALU.subtract, op1=ALU.mult), [y, mv, rs], [y])
                    fw.op("act", lambda: A.activation(out=sg.a, in_=gs[j].a[:, n, :], func=AF.Silu), [gs[j]], [sg])
                    fw.op("dve", lambda: V.tensor_tensor(out=ro.a[:, n, :], in0=y.a, in1=sg.a, op=ALU.mult), [y, sg], [ro])
                for n in range(NC):
                    ld(Rt[n], Rt[n].a[:, c0:c0 + 128], ro.a[:, n, :], [ro])
            fw.barrier()
            chk()

        def gelu(dst, src, tmp, reads):
            fw.op("dve", lambda: V.tensor_tensor(out=tmp.a, in0=src.a, in1=src.a, op=ALU.mult), reads, [tmp])
            fw.op("dve", lambda: V.tensor_scalar(out=tmp.a, in0=tmp.a, scalar1=0.044715, scalar2=1.0, op0=ALU.mult, op1=ALU.add), [tmp], [tmp])
            fw.op("dve", lambda: V.tensor_tensor(out=tmp.a, in0=tmp.a, in1=src.a, op=ALU.mult), [tmp] + reads, [tmp])
            fw.op("act", lambda: A.activation(out=tmp.a, in_=tmp.a, func=AF.Sigmoid, scale=1.5957691216057308), [tmp], [tmp])
            fw.op("dve", lambda: V.tensor_tensor(out=dst.a, in0=tmp.a, in1=src.a, op=ALU.mult), [tmp] + reads, [dst])

        with ExitStack() as st:
            us = [sb(st, "us%d" % i, [128, NC, 128]) for i in range(2)]
            vs2 = [sb(st, "vs2%d" % i, [128, NC, 128]) for i in range(2)]
            gu = sb(st, "gu", [128, NC, 128]); gv = sb(st, "gv", [128, NC, 128]); tmpg = sb(st, "tmpg", [128, NC, 128])
            vn = sb(st, "vn", [128, NC, 128], BF16)
            wsf = sb(st, "wsf", [128, 8, 128]); wsb = sb(st, "wsb", [128, 8, 128], BF16)
            bsb = sb(st, "bsb", [128, 8])
            stt2 = sb(st, "bnst2", [128, 6]); mv2 = sb(st, "mv2", [128, 2]); rs2 = sb(st, "rs2", [128, 1])
            ro2 = [sb(st, "ro2%d" % i, [128, NC, 128]) for i in range(2)]
            pG = [ps(st, "pG%d" % i, [128, 128]) for i in range(2)]
            ld(wsf, wsf.a, wsT.a.rearrange("g q p -> q g p"), [wsT])
            ld(bsb, bsb.a, bsT.a, [bsT])
            fw.op("act", lambda: A.copy(out=wsb.a, in_=wsf.a), [wsf], [wsb])
            for g in range(8):
                j = g % 2
                c0 = g * 128
                ld(us[j], us[j].a, Zd[1:NT + 1, 4096 + c0:4096 + c0 + 128].rearrange("(n p) c -> p n c", p=128), Zt)
                ld(vs2[j], vs2[j].a, Zd[1:NT + 1, 5120 + c0:5120 + c0 + 128].rearrange("(n p) c -> p n c", p=128), Zt)
                gelu(gu, us[j], tmpg, [us[j]])
                gelu(gv, vs2[j], tmpg, [vs2[j]])
                for n in range(NC):
                    fw.op("dve", lambda: V.bn_stats(out=stt2.a, in_=gv.a[:, n, :]), [gv], [stt2])
                    fw.op("dve", lambda: V.bn_aggr(out=mv2.a, in_=stt2.a), [stt2], [mv2])
                    fw.op("dve", lambda: V.tensor_scalar(out=rs2.a, in0=mv2.a[:, 1:2], scalar1=EPS, scalar2=None, op0=ALU.add), [mv2], [rs2])
                    fw.op("act", lambda: A.activation(out=rs2.a, in_=rs2.a, func=AF.Sqrt), [rs2], [rs2])
                    fw.op("dve", lambda: V.reciprocal(out=rs2.a, in_=rs2.a), [rs2], [rs2])
                    fw.op("dve", lambda: V.tensor_scalar(out=vn.a[:, n, :], in0=gv.a[:, n, :], scalar1=mv2.a[:, 0:1], scalar2=rs2.a[:, 0:1], op0=ALU.subtract, op1=ALU.mult), [gv, mv2, rs2], [vn])
                for n in range(NC):
                    p = pG[n % 2]
                    fw.op("pe", lambda: T.matmul(p.a, lhsT=wsb.a[:, g, :], rhs=vn.a[:, n, :], start=True, stop=True), [wsb, vn], [p])
                    fw.op("dve", lambda: V.scalar_tensor_tensor(out=ro2[j].a[:, n, :], in0=p.a, scalar=bsb.a[:, g:g + 1], in1=gu.a[:, n, :], op0=ALU.add, op1=ALU.mult), [p, bsb, gu], [ro2[j]])
                for n in range(NC):
                    ld(Rt[n], Rt[n].a[:, 1024 + c0:1024 + c0 + 128], ro2[j].a[:, n, :], [ro2[j]])
            fw.barrier()
            chk()

        def outproj_and_moe(i, w_out, x_src_tiles, last):
            with ExitStack() as st:
                with ExitStack() as s2:
                    GT1 = sb(s2, "GT1", [128, D])
                    ld(GT1, GT1.a, modrow(i, 0, 2), [MOD])
                    rf = [sb(s2, "rf%d" % k, [128, D]) for k in range(2)]
                    rb = [sb(s2, "rb%d" % k, [128, D], BF16) for k in range(2)]
                    wb = [sb(s2, "wo%d" % k, [128, KC, 512], BF16) for k in range(2)]
                    xin = [sb(s2, "xin%d" % k, [128, 512]) for k in range(3)]
                    pT = [ps(s2, "pT%d" % k, [128, 8, 128], BF16) for k in range(2)]
                    pg = [ps(s2, "pg%d" % k, [128, 512]) for k in range(4)]
                    rT = sb(s2, "rT", [128, KC, NT], BF16)
                    for n in range(NC):
                        ld(rf[n % 2], rf[n % 2].a, Rt[n].a, [Rt[n]])
                        fw.op("act", lambda: A.copy(out=rb[n % 2].a, in_=rf[n % 2].a), [rf[n % 2]], [rb[n % 2]])
                        transpose_tile(rb[n % 2], rT, n * 128, pT)
                    ci = 0
                    for cb in range(4):
                        w = wb[cb % 2]
                        load_w(w, w_out.a[:, cb * 512:(cb + 1) * 512], w_out, KC)
                        for n in range(NC):
                            p = pg[ci % 4]; xi = xin[ci % 3]; ci += 1
                            xb_, xap = x_src_tiles[n]
                            ld(xi, xi.a, xap[:, cb * 512:(cb + 1) * 512], [xb_])
                            for k in range(KC):
                                fw.op("pe", lambda: T.matmul(p.a, lhsT=rT.a[:, k, n * 128:(n + 1) * 128], rhs=w.a[:, k, :], start=(k == 0), stop=(k == KC - 1)), [rT, w], [p])
                            fw.op("dve", lambda: V.tensor_tensor(out=p.a, in0=p.a, in1=GT1.a[:, cb * 512:(cb + 1) * 512], op=ALU.mult), [p, GT1], [p])
                            fw.op("dve", lambda: V.tensor_tensor(out=xi.a, in0=p.a, in1=xi.a, op=ALU.add), [p, xi], [xi])
                            ld(Xt[n], Xt[n].a[:, cb * 512:(cb + 1) * 512], xi.a, [xi])
                    fw.barrier()
                    chk()
                NS = 2 * NC + 16
                MA16 = sb(st, "MA16", [128, NC, 16]); MB16 = sb(st, "MB16", [128, NC, 16]); wAB = sb(st, "wAB", [128, NC, 2])
                slotAi = sb(st, "slotAi", [128, NC], I32); slotBi = sb(st, "slotBi", [128, NC], I32)
                WIi = sb(st, "WIi", [128, NS, 16], I32)
                with ExitStack() as s2:
                    Ftok = sb(s2, "Ftok", [128, NC, D], BF16)
                    Gf, SHf = make_GS(s2, i, 0, 3, 4, g_ffn.a[i:i + 1, :], g_ffn)
                    bufs = {"xt": [sb(s2, "xt%d" % k, [128, D]) for k in range(2)], "i": 0, "junk": sb(s2, "junk", [128, D]),
                            "ssq": sb(s2, "ssq", [128, 1]), "rstd": sb(s2, "rstd", [128, 1])}
                    ff = [sb(s2, "ff%d" % k, [128, D]) for k in range(2)]
                    fTf = sb(s2, "fTf", [128, KC, 128])
                    wrs = sb(s2, "wrs", [128, KC, 20]); brs = sb(s2, "brs", [128, 20])
                    L = sb(s2, "L", [128, 20]); m1 = sb(s2, "m1", [128, 4]); oh1 = sb(s2, "oh1", [128, 4]); e1 = sb(s2, "e1", [128, 4])
                    l2 = sb(s2, "l2", [128, 4]); l2b = sb(s2, "l2b", [128, 4]); ohA = sb(s2, "ohA", [128, 4]); ohB = sb(s2, "ohB", [128, 4])
                    pF = [ps(s2, "pF%d" % k, [128, 4, 128]) for k in range(2)]
                    pL = ps(s2, "pL", [128, 20])
                    ld(wrs, wrs.a, wr.a[i].rearrange("(k p) n -> p k n", p=128), [wr])
                    ld(brs, brs.a, br.a[i:i + 1, :].to_broadcast((128, 20)), [br])
                    for n in range(NC):
                        f = ff[n % 2]
                        norm_mod_tile(s2, bufs, Xt[n], Xt[n].a, Gf, SHf, out_f=f)
                        fw.op("act", lambda: A.copy(out=Ftok.a[:, n, :], in_=f.a), [f], [Ftok])
                        for k0 in range(0, KC, 4):
                            p = pF[(k0 // 4) % 2]
                            for k in range(k0, k0 + 4):
                                fw.op("pe", lambda: T.transpose(out=p.a[:, k - k0, :], in_=f.a[:, k * 128:(k + 1) * 128], identity=identF.a), [f, identF], [p])
                            fw.op("act", lambda: A.copy(out=fTf.a[:, k0:k0 + 4, :], in_=p.a), [p], [fTf])
                        for k in range(KC):
                            fw.op("pe", lambda: T.matmul(pL.a, lhsT=fTf.a[:, k, :], rhs=wrs.a[:, k, :], start=(k == 0), stop=(k == KC - 1)), [fTf, wrs], [pL])
                        fw.op("dve", lambda: V.tensor_tensor(out=L.a, in0=pL.a, in1=brs.a, op=ALU.add), [pL, brs], [L])
                        fw.op("dve", lambda: V.tensor_reduce(out=m1.a[:, 0:1], in_=L.a[:, 0:4], axis=mybir.AxisListType.X, op=ALU.max), [L], [m1])
                        fw.op("dve", lambda: V.tensor_scalar(out=oh1.a, in0=L.a[:, 0:4], scalar1=m1.a[:, 0:1], scalar2=None, op0=ALU.is_equal), [L, m1], [oh1])
                        fw.op("dve", lambda: V.tensor_scalar(out=e1.a, in0=L.a[:, 0:4], scalar1=m1.a[:, 0:1], scalar2=None, op0=ALU.subtract), [L, m1], [e1])
                        fw.op("act", lambda: A.activation(out=e1.a, in_=e1.a, func=AF.Exp), [e1], [e1])
                        fw.op("dve", lambda: V.tensor_reduce(out=m1.a[:, 1:2], in_=e1.a, axis=mybir.AxisListType.X, op=ALU.add), [e1], [m1])
                        fw.op("dve", lambda: V.reciprocal(out=m1.a[:, 1:2], in_=m1.a[:, 1:2]), [m1], [m1])
                        fw.op("dve", lambda: V.tensor_scalar(out=l2.a, in0=L.a[:, 4:8], scalar1=oh1.a[:, 0:1], scalar2=None, op0=ALU.mult), [L, oh1], [l2])
                        for g in range(1, 4):
                            fw.op("dve", lambda: V.scalar_tensor_tensor(out=l2.a, in0=L.a[:, 4 + 4 * g:8 + 4 * g], scalar=oh1.a[:, g:g + 1], in1=l2.a, op0=ALU.mult, op1=ALU.add), [L, oh1, l2], [l2])
                        fw.op("dve", lambda: V.tensor_reduce(out=m1.a[:, 2:3], in_=l2.a, axis=mybir.AxisListType.X, op=ALU.max), [l2], [m1])
                        fw.op("dve", lambda: V.tensor_scalar(out=ohA.a, in0=l2.a, scalar1=m1.a[:, 2:3], scalar2=None, op0=ALU.is_equal), [l2, m1], [ohA])
                        fw.op("dve", lambda: V.scalar_tensor_tensor(out=l2b.a, in0=ohA.a, scalar=-1e30, in1=l2.a, op0=ALU.mult, op1=ALU.add), [ohA, l2], [l2b])
                        fw.op("dve", lambda: V.tensor_reduce(out=m1.a[:, 3:4], in_=l2b.a, axis=mybir.AxisListType.X, op=ALU.max), [l2b], [m1])
                        fw.op("dve", lambda: V.tensor_scalar(out=ohB.a, in0=l2b.a, scalar1=m1.a[:, 3:4], scalar2=None, op0=ALU.is_equal), [l2b, m1], [ohB])
                        fw.op("dve", lambda: V.tensor_tensor(out=e1.a[:, 0:1], in0=m1.a[:, 3:4], in1=m1.a[:, 2:3], op=ALU.subtract), [m1], [e1])
                        fw.op("act", lambda: A.activation(out=e1.a[:, 0:1], in_=e1.a[:, 0:1], func=AF.Exp), [e1], [e1])
                        fw.op("dve", lambda: V.tensor_scalar(out=e1.a[:, 1:2], in0=e1.a[:, 0:1], scalar1=1.0, scalar2=None, op0=ALU.add), [e1], [e1])
                        fw.op("dve", lambda: V.reciprocal(out=e1.a[:, 1:2], in_=e1.a[:, 1:2]), [e1], [e1])
                        fw.op("dve", lambda: V.tensor_tensor(out=e1.a[:, 2:3], in0=e1.a[:, 0:1], in1=e1.a[:, 1:2], op=ALU.mult), [e1], [e1])
                        fw.op("dve", lambda: V.tensor_scalar(out=wAB.a[:, n, :], in0=e1.a[:, 1:3], scalar1=m1.a[:, 1:2], scalar2=None, op0=ALU.mult), [e1, m1], [wAB])
                        for g in range(4):
                            fw.op("dve", lambda: V.tensor_scalar(out=MA16.a[:, n, 4 * g:4 * g + 4], in0=ohA.a, scalar1=oh1.a[:, g:g + 1], scalar2=None, op0=ALU.mult), [ohA, oh1], [MA16])
                            fw.op("dve", lambda: V.tensor_scalar(out=MB16.a[:, n, 4 * g:4 * g + 4], in0=ohB.a, scalar1=oh1.a[:, g:g + 1], scalar2=None, op0=ALU.mult), [ohB, oh1], [MB16])
                    Mf = sb(s2, "Mf", [128, NC, 16]); Mb = sb(s2, "Mb", [128, NC * 16], BF16)
                    cntS = sb(s2, "cntS", [128, NC, 16]); ptS = sb(s2, "ptS", [128, NC, 16]); rk = sb(s2, "rk", [128, NC, 16]); rk2 = sb(s2, "rk2", [128, NC, 16])
                    ne = sb(s2, "ne", [128, 16]); tl = sb(s2, "tl", [128, 16]); se = sb(s2, "se", [128, 16]); st128 = sb(s2, "st128", [128, 16])
                    slf = sb(s2, "slf", [128, 2, NC]); ek = sb(s2, "ek", [128, NS]); chg = sb(s2, "chg", [128, NS]); off = sb(s2, "off", [128, NS])
                    WIf = sb(s2, "WIf", [128, NS, 16])
                    trib = sb(s2, "trib", [128, 2, 128], BF16)
                    pP = [ps(s2, "pP%d" % k, [128, NC * 16]) for k in range(2)]
                    fw.op("dve", lambda: V.tensor_copy(out=trib.a, in_=cs.a[:, TRI:TRI + 256]), [cs], [trib])
                    fw.op("dve", lambda: V.tensor_tensor(out=Mf.a, in0=MA16.a, in1=MB16.a, op=ALU.add), [MA16, MB16], [Mf])
                    fw.op("dve", lambda: V.tensor_copy(out=Mb.a, in_=Mf.a), [Mf], [Mb])
                    fw.op("pe", lambda: T.matmul(pP[0].a, lhsT=trib.a[:, 0, :], rhs=Mb.a, start=True, stop=True), [trib, Mb], [pP[0]])
                    fw.op("pe", lambda: T.matmul(pP[1].a, lhsT=trib.a[:, 1, :], rhs=Mb.a, start=True, stop=True), [trib, Mb], [pP[1]])
                    fw.op("act", lambda: A.copy(out=cntS.a, in_=pP[1].a), [pP[1]], [cntS])
                    fw.op("dve", lambda: V.memset(ptS.a[:, 0, :], 0.0), [], [ptS])
                    for n in range(1, NC):
                        fw.op("dve", lambda: V.tensor_tensor(out=ptS.a[:, n, :], in0=ptS.a[:, n - 1, :], in1=cntS.a[:, n - 1, :], op=ALU.add), [ptS, cntS], [ptS])
                    fw.op("dve", lambda: V.tensor_tensor(out=ne.a, in0=ptS.a[:, NC - 1, :], in1=cntS.a[:, NC - 1, :], op=ALU.add), [ptS, cntS], [ne])
                    fw.op("dve", lambda: V.memset(tl.a, 0.0), [], [tl])
                    for j in range(NC):
                        fw.op("dve", lambda: V.scalar_tensor_tensor(out=tl.a, in0=ne.a, scalar=128.0 * j, in1=tl.a, op0=ALU.is_gt, op1=ALU.add), [ne, tl], [tl])
                    fw.op("dve", lambda: V.memset(se.a[:, 0:1], 0.0), [], [se])
                    for e in range(1, 16):
                        fw.op("dve", lambda: V.tensor_tensor(out=se.a[:, e:e + 1], in0=se.a[:, e - 1:e], in1=tl.a[:, e - 1:e], op=ALU.add), [se, tl], [se])
                    fw.op("dve", lambda: V.tensor_scalar(out=st128.a, in0=se.a, scalar1=128.0, scalar2=None, op0=ALU.mult), [se], [st128])
                    fw.op("dve", lambda: V.tensor_tensor(out=rk.a, in0=pP[0].a, in1=ptS.a, op=ALU.add), [pP[0], ptS], [rk])
                    for n in range(NC):
                        fw.op("dve", lambda: V.tensor_tensor(out=rk.a[:, n, :], in0=rk.a[:, n, :], in1=st128.a, op=ALU.add), [rk, st128], [rk])
                    fw.op("dve", lambda: V.tensor_tensor(out=rk2.a, in0=rk.a, in1=MA16.a, op=ALU.mult), [rk, MA16], [rk2])
                    fw.op("dve", lambda: V.tensor_reduce(out=slf.a[:, 0, :], in_=rk2.a, axis=mybir.AxisListType.X, op=ALU.add), [rk2], [slf])
                    fw.op("dve", lambda: V.tensor_tensor(out=rk2.a, in0=rk.a, in1=MB16.a, op=ALU.mult), [rk, MB16], [rk2])
                    fw.op("dve", lambda: V.tensor_reduce(out=slf.a[:, 1, :], in_=rk2.a, axis=mybir.AxisListType.X, op=ALU.add), [rk2], [slf])
                    fw.op("dve", lambda: V.tensor_copy(out=slotAi.a, in_=slf.a[:, 0, :]), [slf], [slotAi])
                    fw.op("dve", lambda: V.tensor_copy(out=slotBi.a, in_=slf.a[:, 1, :]), [slf], [slotBi])
                    fw.op("dve", lambda: V.memset(ek.a, -1.0), [], [ek])
                    for e in range(16):
                        fw.op("dve", lambda: V.scalar_tensor_tensor(out=ek.a, in0=cs.a[:, KV:KV + NS], scalar=se.a[:, e:e + 1], in1=ek.a, op0=ALU.is_ge, op1=ALU.add), [cs, se, ek], [ek])
                    fw.op("dve", lambda: V.memset(chg.a[:, 0:1], 1.0), [], [chg])
                    fw.op("dve", lambda: V.tensor_tensor(out=chg.a[:, 1:NS], in0=ek.a[:, 1:NS], in1=ek.a[:, 0:NS - 1], op=ALU.not_equal), [ek], [chg])
                    fw.op("dve", lambda: V.tensor_scalar(out=off.a, in0=chg.a, scalar1=-1.0e6, scalar2=1.0e6, op0=ALU.mult, op1=ALU.add), [chg], [off])
                    fw.op("dve", lambda: V.scalar_tensor_tensor(out=off.a, in0=ek.a, scalar=1024.0, in1=off.a, op0=ALU.mult, op1=ALU.add), [ek, off], [off])
                    for k in range(NS):
                        fw.op("dve", lambda: V.tensor_scalar(out=WIf.a[:, k, :], in0=cs.a[:, BASE:BASE + 16], scalar1=off.a[:, k:k + 1], scalar2=None, op0=ALU.add), [cs, off], [WIf])
                    fw.op("dve", lambda: V.tensor_copy(out=WIi.a, in_=WIf.a), [WIf], [WIi])
                    for n in range(NC):
                        for sl_ in (slotAi, slotBi):
                            fw.dma("pool", lambda: G.indirect_dma_start(out=FS.a, out_offset=bass.IndirectOffsetOnAxis(ap=sl_.a[:, n:n + 1], axis=0), in_=Ftok.a[:, n, :], in_offset=None, bounds_check=bcS, oob_is_err=False), FS, [Ftok, sl_])
                    fw.barrier()
                    chk()
                with ExitStack() as s2:
                    w1c = [sb(s2, "w1c%d" % k, [128, D], BF16) for k in range(8)]
                    w3c = [sb(s2, "w3c%d" % k, [128, D], BF16) for k in range(8)]
                    w2c = [sb(s2, "w2c%d" % k, [128, D], BF16) for k in range(8)]
                    ftl = [sb(s2, "ftl%d" % k, [128, D], BF16) for k in range(2)]
                    fTk = [sb(s2, "fTk%d" % k, [128, KC, 128], BF16) for k in range(2)]
                    sl = [sb(s2, "sl%d" % k, [128, 512]) for k in range(2)]
                    ab = [sb(s2, "ab%d" % k, [128, DE], BF16) for k in range(2)]
                    aTt = [sb(s2, "aTt%d" % k, [128, 8, 128], BF16) for k in range(2)]
                    yo = [sb(s2, "yo%d" % k, [128, D]) for k in range(2)]
                    ph = [ps(s2, "ph%d" % k, [128, 512]) for k in range(4)]
                    pTf = [ps(s2, "pTf%d" % k, [128, 8, 128], BF16) for k in range(2)]
                    py = [ps(s2, "py%d" % k, [128, 512]) for k in range(2)]
                    yi = 0
                    for k in range(NS):
                        for j in range(8):
                            fw.dma("pool", lambda: G.indirect_dma_start(out=w1c[j].a, out_offset=None, in_=moe_w1[i].a, in_offset=bass.IndirectOffsetOnAxis(ap=WIi.a[:, k, j:j + 1], axis=0), bounds_check=bcW, oob_is_err=False), w1c[j], [moe_w1[i], WIi])
                            fw.dma("pool", lambda: G.indirect_dma_start(out=w3c[j].a, out_offset=None, in_=moe_w3[i].a, in_offset=bass.IndirectOffsetOnAxis(ap=WIi.a[:, k, j:j + 1], axis=0), bounds_check=bcW, oob_is_err=False), w3c[j], [moe_w3[i], WIi])
                        for j in range(8):
                            fw.dma("pool", lambda: G.indirect_dma_start(out=w2c[j].a, out_offset=None, in_=moe_w2[i].a, in_offset=bass.IndirectOffsetOnAxis(ap=WIi.a[:, k, 8 + j:9 + j], axis=0), bounds_check=bcW, oob_is_err=False), w2c[j], [moe_w2[i], WIi])
                        ft = ftl[k % 2]; fT_ = fTk[k % 2]; a_ = ab[k % 2]; at = aTt[k % 2]; y = yo[k % 2]
                        ld(ft, ft.a, FS.a[k * 128:(k + 1) * 128, :], [FS])
                        transpose_tile(ft, fT_, 0, pTf)
                        for hf in range(2):
                            p1 = ph[hf]; p3 = ph[2 + hf]
                            for kk in range(KC):
                                c0 = (kk % 2) * 1024 + hf * 512
                                fw.op("pe", lambda: T.matmul(p1.a, lhsT=fT_.a[:, kk, :], rhs=w1c[kk // 2].a[:, c0:c0 + 512], start=(kk == 0), stop=(kk == KC - 1)), [fT_, w1c[kk // 2]], [p1])
                            for kk in range(KC):
                                c0 = (kk % 2) * 1024 + hf * 512
                                fw.op("pe", lambda: T.matmul(p3.a, lhsT=fT_.a[:, kk, :], rhs=w3c[kk // 2].a[:, c0:c0 + 512], start=(kk == 0), stop=(kk == KC - 1)), [fT_, w3c[kk // 2]], [p3])
                            fw.op("act", lambda: A.activation(out=sl[hf].a, in_=p1.a, func=AF.Silu), [p1], [sl[hf]])
                            fw.op("dve", lambda: V.tensor_tensor(out=a_.a[:, hf * 512:(hf + 1) * 512], in0=p3.a, in1=sl[hf].a, op=ALU.mult), [p3, sl[hf]], [a_])
                        transpose_tile(a_, at, 0, pTf[1:2], nk=8)
                        for cb in range(4):
                            p = py[yi % 2]; yi += 1
                            for k8 in range(8):
                                fw.op("pe", lambda: T.matmul(p.a, lhsT=at.a[:, k8, :], rhs=w2c[k8].a[:, cb * 512:(cb + 1) * 512], start=(k8 == 0), stop=(k8 == 7)), [at, w2c[k8]], [p])
                            if cb % 2:
                                fw.op("act", lambda: A.copy(out=y.a[:, cb * 512:(cb + 1) * 512], in_=p.a), [p], [y])
                            else:
                                fw.op("dve", lambda: V.tensor_copy(out=y.a[:, cb * 512:(cb + 1) * 512], in_=p.a), [p], [y])
                        ld(YS, YS.a[k * 128:(k + 1) * 128, :], y.a, [y])
                    fw.barrier()
                    chk()
                with ExitStack() as s2:
                    GT2 = sb(s2, "GT2", [128, D])
                    ld(GT2, GT2.a, modrow(i, 0, 5), [MOD])
                    YA = [sb(s2, "YA%d" % k, [128, D]) for k in range(2)]
                    YB = [sb(s2, "YB%d" % k, [128, D]) for k in range(2)]
                    xc = [sb(s2, "xc%d" % k, [128, D]) for k in range(2)]
                    for n in range(NC):
                        ya = YA[n % 2]; yb = YB[n % 2]; x_ = xc[n % 2]
                        fw.dma("pool", lambda: G.indirect_dma_start(out=ya.a, out_offset=None, in_=YS.a, in_offset=bass.IndirectOffsetOnAxis(ap=slotAi.a[:, n:n + 1], axis=0), bounds_check=bcS, oob_is_err=False), ya, [YS, slotAi])
                        fw.dma("pool", lambda: G.indirect_dma_start(out=yb.a, out_offset=None, in_=YS.a, in_offset=bass.IndirectOffsetOnAxis(ap=slotBi.a[:, n:n + 1], axis=0), bounds_check=bcS, oob_is_err=False), yb, [YS, slotBi])
                        ld(x_, x_.a, Xt[n].a, [Xt[n]])
                        fw.op("dve", lambda: V.tensor_scalar(out=ya.a, in0=ya.a, scalar1=wAB.a[:, n, 0:1], scalar2=None, op0=ALU.mult), [ya, wAB], [ya])
                        fw.op("dve", lambda: V.scalar_tensor_tensor(out=ya.a, in0=yb.a, scalar=wAB.a[:, n, 1:2], in1=ya.a, op0=ALU.mult, op1=ALU.add), [yb, wAB, ya], [ya])
                        fw.op("dve", lambda: V.scalar_tensor_tensor(out=ya.a, in0=ya.a, scalar=1.0, in1=GT2.a, op0=ALU.mult, op1=ALU.mult), [ya, GT2], [ya])
                        fw.op("dve", lambda: V.scalar_tensor_tensor(out=x_.a, in0=ya.a, scalar=1.0, in1=x_.a, op0=ALU.mult, op1=ALU.add), [ya, x_], [x_])
                        ld(Xt[n], Xt[n].a, x_.a, [x_])
                    fw.barrier()
                    chk()

        outproj_and_moe(0, ab_w_out, [(x_own, x_own.a[n * 128:(n + 1) * 128, :]) for n in range(NC)], False)

        with ExitStack() as st:
            Gl, SHl = make_GS(st, 1, 0, 0, 1, g_mix.a[1:2, :], g_mix)
            bufs = {"xt": [sb(st, "xt%d" % i, [128, D]) for i in range(2)], "i": 0, "junk": sb(st, "junk", [128, D]),
                    "ssq": sb(st, "ssq", [128, 1]), "rstd": sb(st, "rstd", [128, 1])}
            abf = [sb(st, "abf%d" % i, [128, D], BF16) for i in range(2)]
            aT = sb(st, "aT", [128, KC, NT], BF16)
            wb = [sb(st, "wb%d" % i, [128, KC, 512], BF16) for i in range(2)]
            osb = [sb(st, "osb%d" % i, [128, 512]) for i in range(3)]
            pT = [ps(st, "pT%d" % i, [128, 8, 128], BF16) for i in range(2)]
            pg = [ps(st, "pg%d" % i, [128, 512]) for i in range(4)]
            for n in range(NC):
                ab = abf[n % 2]
                norm_mod_tile(st, bufs, Xt[n], Xt[n].a, Gl, SHl, out_bf=ab)
                transpose_tile(ab, aT, n * 128, pT)
            ci = 0
            for cb in range(12):
                w = wb[cb % 2]
                load_w(w, cv_w_in.a[:, cb * 512:(cb + 1) * 512], cv_w_in, KC)
                for n in range(NC):
                    p = pg[ci % 4]; o = osb[ci % 3]; ci += 1
                    for k in range(KC):
                        fw.op("pe", lambda: T.matmul(p.a, lhsT=aT.a[:, k, n * 128:(n + 1) * 128], rhs=w.a[:, k, :], start=(k == 0), stop=(k == KC - 1)), [aT, w], [p])
                    if ci % 2:
                        fw.op("act", lambda: A.copy(out=o.a, in_=p.a), [p], [o])
                    else:
                        fw.op("dve", lambda: V.tensor_copy(out=o.a, in_=p.a), [p], [o])
                    ld(Zt[n], Zt[n].a[:, cb * 512:(cb + 1) * 512], o.a, [o])
            fw.barrier()
            chk()
        with ExitStack() as st:
            CW = sb(st, "CW", [128, 3, D]); CB = sb(st, "CB", [128, D])
            for j in range(3):
                ld(CW, CW.a[:, j, :], conv_w.a[j:j + 1, :].to_broadcast((128, D)), [conv_w])
            ld(CB, CB.a, conv_b.a.to_broadcast((128, D)), [conv_b])
            gc = [sb(st, "gc%d" % j, [128, D]) for j in range(3)]
            hv = [sb(st, "hv%d" % j, [128, D]) for j in range(3)]
            gbt = sb(st, "gbt", [128, D]); acc = sb(st, "acc", [128, D])
            zall = Zt + [Zpad, Zpad2]
            for n in range(NC):
                r0 = 1 + n * 128
                for j in range(3):
                    ld(gc[j], gc[j].a, Zd[r0 + j - 1:r0 + j - 1 + 128, 2048:4096], zall)
                    ld(hv[j], hv[j].a, Zd[r0 + j - 1:r0 + j - 1 + 128, 4096:6144], zall)
                ld(gbt, gbt.a, Zt[n].a[:, 0:2048], [Zt[n]])
                for j in range(3):
                    fw.op("dve", lambda: V.scalar_tensor_tensor(out=gc[j].a, in0=gc[j].a, scalar=1.0, in1=hv[j].a, op0=ALU.mult, op1=ALU.mult), [gc[j], hv[j]], [gc[j]])
                fw.op("dve", lambda: V.scalar_tensor_tensor(out=acc.a, in0=gc[0].a, scalar=cs.a[:, cc + 5:cc + 6], in1=CW.a[:, 0, :], op0=ALU.mult, op1=ALU.mult), [gc[0], cs, CW], [acc])
                fw.op("dve", lambda: V.scalar_tensor_tensor(out=acc.a, in0=acc.a, scalar=1.0, in1=CB.a, op0=ALU.mult, op1=ALU.add), [acc, CB], [acc])
                fw.op("dve", lambda: V.scalar_tensor_tensor(out=gc[1].a, in0=gc[1].a, scalar=1.0, in1=CW.a[:, 1, :], op0=ALU.mult, op1=ALU.mult), [gc[1], CW], [gc[1]])
                fw.op("dve", lambda: V.scalar_tensor_tensor(out=acc.a, in0=acc.a, scalar=1.0, in1=gc[1].a, op0=ALU.mult, op1=ALU.add), [acc, gc[1]], [acc])
                fw.op("dve", lambda: V.scalar_tensor_tensor(out=gc[2].a, in0=gc[2].a, scalar=cs.a[:, cc + 6:cc + 7], in1=CW.a[:, 2, :], op0=ALU.mult, op1=ALU.mult), [gc[2], cs, CW], [gc[2]])
                fw.op("dve", lambda: V.scalar_tensor_tensor(out=acc.a, in0=acc.a, scalar=1.0, in1=gc[2].a, op0=ALU.mult, op1=ALU.add), [acc, gc[2]], [acc])
                fw.op("dve", lambda: V.scalar_tensor_tensor(out=acc.a, in0=acc.a, scalar=1.0, in1=gbt.a, op0=ALU.mult, op1=ALU.mult), [acc, gbt], [acc])
                ld(Rt[n], Rt[n].a, acc.a, [acc])
            fw.barrier()
            chk()

        outproj_and_moe(1, cv_w_out, [(Xt[n], Xt[n].a) for n in range(NC)], True)

        with ExitStack() as st:
            Gfin = sb(st, "Gfin", [128, D])
            ld(Gfin, Gfin.a, g_final.a.to_broadcast((128, D)), [g_final])
            bufs = {"xt": [sb(st, "xt%d" % i, [128, D]) for i in range(2)], "i": 0, "junk": sb(st, "junk", [128, D]),
                    "ssq": sb(st, "ssq", [128, 1]), "rstd": sb(st, "rstd", [128, 1])}
            ot = [sb(st, "ot%d" % i, [128, D]) for i in range(2)]
            for n in range(NC):
                jk = norm_mod_tile(st, bufs, Xt[n], Xt[n].a, Gfin, None)
                fw.op("act", lambda: A.copy(out=ot[n % 2].a, in_=jk.a), [jk], [ot[n % 2]])
                ld(out, out.a[n * 128:(n + 1) * 128, :], ot[n % 2].a, [ot[n % 2]], k="sp")
            fw.barrier()
            chk()
    return nc


def rope_tables(pos):
    row = (pos // GRID_W).astype(np.float32)
    col = (pos % GRID_W).astype(np.float32)
    nf = HD // 4
    inv = (10000.0 ** (-np.arange(nf, dtype=np.float32) / nf)).astype(np.float32)
    ang = np.concatenate([row[:, None] * inv, col[:, None] * inv], axis=-1).astype(np.float32)
    return np.cos(ang).astype(np.float32), np.sin(ang).astype(np.float32)


def make_consts(NT):
    NC = NT // 128
    m = np.arange(128, dtype=np.float32)
    ident = np.eye(128, dtype=np.float32)
    R1 = np.maximum(m[None, :] - m[:, None], 0)
    R2 = np.maximum(m[:, None] - m[None, :], 0)
    cols = np.stack([127 - m, m, m + 1, 128 - m, np.full(128, 128.0, np.float32),
                     (np.arange(128) % GRID_W != 0).astype(np.float32),
                     (np.arange(128) % GRID_W != GRID_W - 1).astype(np.float32), np.zeros(128, np.float32)], axis=1)
    et = [128 * c + m for c in range(NC)] + [NT + 128 * c + m for c in range(2)] + [255 - 128 * c - m for c in range(2)]
    et = np.stack(et, axis=1)
    tri = (m[:, None] < m[None, :]).astype(np.float32)
    ones = np.ones((128, 128), np.float32)
    base1 = m[:, None] * 8 + np.arange(8, dtype=np.float32)[None, :]
    base2 = np.arange(8, dtype=np.float32)[None, :] * 128 + m[:, None]
    kv = np.broadcast_to(np.arange(2 * NC + 16, dtype=np.float32)[None, :], (128, 2 * NC + 16))
    return np.concatenate([ident, R1, R2, cols, et, tri, ones, base1, base2, kv], axis=1).astype(np.float32)


def prepare_inputs(inp, T):
    B = inp["x"].shape[0]
    NT = T // 2
    qs = np.float32(HD ** -0.5)
    cst = make_consts(NT)
    maps = []
    shared = {
        "w_mod": inp["w_mod"], "b_mod": inp["b_mod"], "g_mix": inp["g_mix"], "g_ffn": inp["g_ffn"],
        "g_final": inp["g_final"][None, :], "ab_w_in": inp["ab_w_in"][0], "ab_w_out": inp["ab_w_out"][0],
        "cv_w_in": inp["cv_w_in"][0], "cv_w_out": inp["cv_w_out"][0], "conv_b": inp["cv_conv_b"],
        "cst": cst,
        "wr": np.concatenate([inp["moe_w_r1"], inp["moe_w_r2"].transpose(0, 2, 1, 3).reshape(2, D, 16)], axis=2),
        "br": np.concatenate([inp["moe_b_r1"], inp["moe_b_r2"].reshape(2, 16)], axis=1),
    }
    for l in range(2):
        shared["moe_w1_%d" % l] = np.ascontiguousarray(inp["moe_w1"][l].reshape(16, 16, 128, DE).transpose(0, 2, 1, 3).reshape(16384, D))
        shared["moe_w3_%d" % l] = np.ascontiguousarray(inp["moe_w3"][l].reshape(16, 16, 128, DE).transpose(0, 2, 1, 3).reshape(16384, D))
        shared["moe_w2_%d" % l] = inp["moe_w2"][l].reshape(16384, D)
    shared = {k: np.ascontiguousarray(v, dtype=np.float32) for k, v in shared.items()}
    for b in range(B):
        for h in range(2):
            pos_all = np.arange(T)
            if h == 0:
                own = pos_all[:NT]; forg = pos_all[NT:]
                ctxl = inp["ctx"][b]
                dlv = inp["ret_decay_logit"][0].reshape(1, 16)
                ws = inp["sgu_w_s"][0]; bs = inp["sgu_b_s"][0]
                cw = inp["cv_conv_w"][0]
            else:
                own = pos_all[::-1][:NT]; forg = pos_all[:NT][::-1]
                ctxl = inp["ctx"][b][::-1]
                dlv = inp["ret_decay_logit"][0][::-1].reshape(1, 16)
                ws = inp["sgu_w_s"][0][:, ::-1, ::-1]; bs = inp["sgu_b_s"][0][:, ::-1]
                cw = inp["cv_conv_w"][0][::-1]
            co, so = rope_tables(own)
            cf, sf = rope_tables(forg)
            cT = np.stack([inp["c"][b].reshape(16, 128).T, inp["c_ctx"].reshape(16, 128).T], axis=2)
            m = dict(shared)
            m.update({
                "x_own": inp["x"][b][own], "x_for": inp["x"][b][forg], "ctx_l": ctxl, "cT": cT, "dl": dlv,
                "wsT": ws.transpose(0, 2, 1), "bsT": bs.T,
                "rope": np.stack([co * qs, so * qs, co, so, cf, sf]), "conv_w": cw,
            })
            maps.append({k: np.ascontiguousarray(v, dtype=np.float32) for k, v in m.items()})
    return maps


def kernel(**inputs):
    inp = {k: np.asarray(v) for k, v in inputs.items()}
    B, T, _ = inp["x"].shape
    NT = T // 2
    maps = prepare_inputs(inp, T)
    nc = build(NT)
    res = run_bass_kernel_spmd(nc, maps, core_ids=list(range(len(maps))))
    out = np.empty((B, T, D), np.float32)
    for b in range(B):
        out[b, :NT] = res.results[2 * b]["out"]
        out[b, NT:] = res.results[2 * b + 1]["out"][::-1]
    return out
```

```python
from contextlib import ExitStack
import numpy as np
import concourse.bass as bass
import concourse.mybir as mybir
from concourse.bass_utils import run_bass_kernel_spmd

F32 = mybir.dt.float32
BF16 = mybir.dt.bfloat16
I32 = mybir.dt.int32
AF = mybir.ActivationFunctionType
ALU = mybir.AluOpType

D = 2048
EPS = 1e-6
HD = 128
NH = 8
CTX = 256
GRID_W = 64
NEXP = 16
DE = 1024


class Buf:
    def __init__(self, ap, name):
        self.a = ap
        self.name = name
        self.last_write = None
        self.reads = []
        self.dsem = None
        self.dcount = 0


class FW:
    def __init__(self, nc):
        self.nc = nc
        self.engs = {"pe": nc.tensor, "act": nc.scalar, "dve": nc.vector, "pool": nc.gpsimd, "sp": nc.sync}
        self.sems = {}
        self.cnt = {}
        self.dcounts = {}
        self.waited = {k: {} for k in self.engs}
        for k in self.engs:
            self.sems[k] = nc.alloc_semaphore("s_" + k)
            self.cnt[k] = 0
        self.nbuf = 0
        self.free_dsems = []
        self.dbufs = []

    def _deps(self, reads, writes):
        deps = []
        for b in reads:
            if b.last_write is not None:
                deps.append(b.last_write)
        for b in writes:
            if b.last_write is not None:
                deps.append(b.last_write)
            deps.extend(b.reads)
        return deps

    def _emit_waits(self, ek, deps):
        eng = self.engs[ek]
        need = {}
        for (sk, v) in deps:
            if v > need.get(sk, 0):
                need[sk] = v
        for sk, v in need.items():
            if self.waited[ek].get(sk, 0) >= v:
                continue
            self.waited[ek][sk] = v
            eng.wait_ge(self.sems[sk], v)

    @staticmethod
    def _compact(reads):
        m = {}
        for sk, v in reads:
            if v > m.get(sk, 0):
                m[sk] = v
        return list(m.items())

    def op(self, ek, fn, reads=(), writes=()):
        self._emit_waits(ek, self._deps(reads, writes))
        ins = fn()
        self.cnt[ek] += 1
        ins.then_inc(self.sems[ek], 1)
        tok = (ek, self.cnt[ek])
        for b in writes:
            b.last_write = tok
            b.reads = []
        for b in reads:
            if b not in writes:
                b.reads.append(tok)
                if len(b.reads) > 8:
                    b.reads = self._compact(b.reads)
        return ins

    def dma(self, qk, fn, dst, srcs=()):
        reads = list(srcs)
        writes = [dst]
        self._emit_waits(qk, self._deps(reads, writes))
        ins = fn()
        if dst.dsem is None:
            if self.free_dsems:
                key = self.free_dsems.pop()
                dst.dcount = self.dcounts[key]
            else:
                key = "d%d" % self.nbuf
                self.nbuf += 1
                self.sems[key] = self.nc.alloc_semaphore(key)
            dst.dsem = key
            self.dbufs.append(dst)
        key = dst.dsem
        dst.dcount += 16
        self.dcounts[key] = dst.dcount
        ins.then_inc(self.sems[key], 16)
        tok = (key, dst.dcount)
        dst.last_write = tok
        dst.reads = []
        for b in reads:
            b.reads.append(tok)
            if len(b.reads) > 8:
                b.reads = self._compact(b.reads)
        return ins

    def barrier(self):
        deps = [(k, self.cnt[k]) for k in self.engs if self.cnt[k] > 0]
        deps += list(self.dcounts.items())
        for ek in self.engs:
            self._emit_waits(ek, deps)
        for b in self.dbufs:
            self.free_dsems.append(b.dsem)
            b.dsem = None
        self.dbufs = []


class _Stop(Exception):
    pass


def build(NT, stop=99):
    try:
        return _build(NT, stop)
    except _Stop as e:
        return e.args[0]


def _build(NT, stop):
    NC = NT // 128
    stage = [0]

    import os
    substop = int(os.environ.get("KSUB", "0"))

    def sub(k):
        if stage[0] + 1 == stop and substop == k:
            fw.barrier()
            raise _Stop(nc)

    def chk():
        stage[0] += 1
        if stage[0] >= stop:
            raise _Stop(nc)
    KC = D // 128
    nc = bass.Bass("TRN2", target_bir_lowering=False)
    fw = FW(nc)
    V, A, G, T, S = nc.vector, nc.scalar, nc.gpsimd, nc.tensor, nc.sync

    def ein(name, shape):
        return Buf(nc.dram_tensor(name, shape, F32, kind="ExternalInput").ap(), name)

    def dint(name, shape, dt=F32):
        return Buf(nc.dram_tensor(name, shape, dt, kind="Internal").ap(), name)

    x_own = ein("x_own", [NT, D]); x_for = ein("x_for", [NT, D]); ctx_l = ein("ctx_l", [CTX, D])
    cT = ein("cT", [128, KC, 2])
    w_mod = ein("w_mod", [2, D, 6 * D]); b_mod = ein("b_mod", [2, 6 * D])
    g_mix = ein("g_mix", [2, D]); g_ffn = ein("g_ffn", [2, D]); g_final = ein("g_final", [1, D])
    ab_w_in = ein("ab_w_in", [D, 6144]); ab_w_out = ein("ab_w_out", [D, D])
    dl = ein("dl", [1, 16])
    wsT = ein("wsT", [8, 128, 128]); bsT = ein("bsT", [128, 8])
    rope = ein("rope", [6, NT, 64])
    cv_w_in = ein("cv_w_in", [D, 6144]); cv_w_out = ein("cv_w_out", [D, D])
    conv_w = ein("conv_w", [3, D]); conv_b = ein("conv_b", [1, D])
    wr = ein("wr", [2, D, 20]); br = ein("br", [2, 20])
    moe_w1 = [ein("moe_w1_%d" % l, [16384, D]) for l in range(2)]
    moe_w3 = [ein("moe_w3_%d" % l, [16384, D]) for l in range(2)]
    moe_w2 = [ein("moe_w2_%d" % l, [16384, D]) for l in range(2)]
    bcW = G.alloc_register("bcW"); G.reg_mov(bcW, 16383)
    bcS = G.alloc_register("bcS"); G.reg_mov(bcS, (2 * NC + 16) * 128 - 1)
    CW_ = 128 * 3 + 8 + NC + 4
    TRI = CW_; BASE = CW_ + 256; KV = BASE + 16
    CWT = KV + 2 * NC + 16
    cst = ein("cst", [128, CWT])
    out = Buf(nc.dram_tensor("out", [NT, D], F32, kind="ExternalOutput").ap(), "out")

    Xd = nc.dram_tensor("Xd", [NT, D], F32, kind="Internal").ap()
    Xt = [Buf(Xd[n * 128:(n + 1) * 128, :], "X%d" % n) for n in range(NC)]
    Zd = nc.dram_tensor("Zd", [NT + 2, 6144], F32, kind="Internal").ap()
    Zt = [Buf(Zd[1 + n * 128:1 + (n + 1) * 128, :], "Z%d" % n) for n in range(NC)]
    Zpad = Buf(Zd[0:1, :], "Zpad")
    Zfd = nc.dram_tensor("Zfd", [NT, 2048], F32, kind="Internal").ap()
    Zft = [Buf(Zfd[n * 128:(n + 1) * 128, :], "Zf%d" % n) for n in range(NC)]
    Zcd = nc.dram_tensor("Zcd", [CTX, 2048], F32, kind="Internal").ap()
    Zct = [Buf(Zcd[n * 128:(n + 1) * 128, :], "Zc%d" % n) for n in range(2)]
    Rd = nc.dram_tensor("Rd", [NT, D], F32, kind="Internal").ap()
    Rt = [Buf(Rd[n * 128:(n + 1) * 128, :], "R%d" % n) for n in range(NC)]
    NS_ = 2 * NC + 16
    FS = Buf(nc.dram_tensor("FSd", [NS_ * 128, D], BF16, kind="Internal").ap(), "FS")
    YS = Buf(nc.dram_tensor("YSd", [NS_ * 128, D], F32, kind="Internal").ap(), "YS")
    MODd = nc.dram_tensor("MODd", [2, 2, 6 * D], F32, kind="Internal").ap()
    MOD = Buf(MODd, "MOD")

    dq = ["sp", "act"]
    dqi = [0]

    def q():
        dqi[0] ^= 1
        return dq[dqi[0]]

    def qeng(k):
        return {"sp": S, "act": A, "pool": G}[k]

    def ld(dst, dst_ap, src_ap, srcs, k=None):
        k = k or q()
        fw.dma(k, lambda: qeng(k).dma_start(out=dst_ap, in_=src_ap), dst, srcs)

    with ExitStack() as glob:
        uid = [0]

        def sb(stack, name, shape, dt=F32):
            uid[0] += 1
            t = stack.enter_context(nc.sbuf_tensor("%s_%d" % (name, uid[0]), shape, dt))
            return Buf(t[:], name)

        def ps(stack, name, shape, dt=F32):
            uid[0] += 1
            t = stack.enter_context(nc.psum_tensor("%s_%d" % (name, uid[0]), shape, dt))
            return Buf(t[:], name)

        cs = sb(glob, "cs", [128, CWT])
        ld(cs, cs.a, cst.a, [cst])
        identf = cs.a[:, 0:128]
        R1 = cs.a[:, 128:256]
        R2 = cs.a[:, 256:384]
        cc = 384
        ET = 392
        identb = sb(glob, "identb", [128, 128], BF16)
        fw.op("dve", lambda: V.tensor_copy(out=identb.a, in_=identf), [cs], [identb])
        identF = sb(glob, "identF", [128, 128])
        fw.op("dve", lambda: V.tensor_copy(out=identF.a, in_=identf), [cs], [identF])
        Zpad2 = Buf(Zd[NT + 1:NT + 2, :], "Zpad2")
        with ExitStack() as st:
            zero = sb(st, "zero", [1, 6144])
            fw.op("dve", lambda: V.memset(zero.a, 0.0), [], [zero])
            ld(Zpad, Zd[0:1, :], zero.a, [zero])
            ld(Zpad2, Zd[NT + 1:NT + 2, :], zero.a, [zero])
            fw.barrier()
            chk()

        with ExitStack() as st:
            scT = sb(st, "scT", [128, KC, 2])
            ld(scT, scT.a, cT.a, [cT])
            fw.op("act", lambda: A.activation(out=scT.a, in_=scT.a, func=AF.Silu), [scT], [scT])
            wm = [sb(st, "wm%d" % i, [128, KC, 512]) for i in range(2)]
            bm = [sb(st, "bm%d" % i, [2, 512]) for i in range(2)]
            mo = [sb(st, "mo%d" % i, [2, 512]) for i in range(2)]
            pm = [ps(st, "pm%d" % i, [2, 512]) for i in range(2)]
            it = 0
            for i in range(2):
                for cb in range(24):
                    j = it % 2
                    it += 1
                    ld(wm[j], wm[j].a, w_mod.a[i, :, cb * 512:(cb + 1) * 512].rearrange("(k p) n -> p k n", p=128), [w_mod])
                    ld(bm[j], bm[j].a, b_mod.a[i:i + 1, cb * 512:(cb + 1) * 512].to_broadcast((2, 512)), [b_mod])
                    for k in range(KC):
                        fw.op("pe", lambda: T.matmul(pm[j].a, lhsT=scT.a[:, k, :], rhs=wm[j].a[:, k, :], start=(k == 0), stop=(k == KC - 1)), [scT, wm[j]], [pm[j]])
                    fw.op("dve", lambda: V.tensor_tensor(out=mo[j].a, in0=pm[j].a, in1=bm[j].a, op=ALU.add), [pm[j], bm[j]], [mo[j]])
                    ld(MOD, MODd[i, :, cb * 512:(cb + 1) * 512], mo[j].a, [mo[j]], k="sp")
            fw.barrier()
            chk()

        def modrow(i, row, j):
            return MODd[i, row:row + 1, j * D:(j + 1) * D].to_broadcast((128, D))

        def norm_mod_tile(st, bufs, src_buf, src_ap, Gt, SHt, out_bf=None, out_f=None):
            xt = bufs["xt"][bufs["i"] % 2]
            bufs["i"] += 1
            ld(xt, xt.a, src_ap, [src_buf])
            sub(8)
            junk, ssq, rstd = bufs["junk"], bufs["ssq"], bufs["rstd"]
            fw.op("act", lambda: A.activation(out=junk.a, in_=xt.a, func=AF.Square), [xt], [junk])
            fw.op("dve", lambda: V.tensor_reduce(out=ssq.a, in_=junk.a, axis=mybir.AxisListType.X, op=ALU.add), [junk], [ssq])
            fw.op("dve", lambda: V.tensor_scalar(out=ssq.a, in0=ssq.a, scalar1=1.0 / D, scalar2=EPS, op0=ALU.mult, op1=ALU.add), [ssq], [ssq])
            fw.op("act", lambda: A.activation(out=ssq.a, in_=ssq.a, func=AF.Sqrt), [ssq], [ssq])
            fw.op("dve", lambda: V.reciprocal(out=rstd.a, in_=ssq.a), [ssq], [rstd])
            fw.op("dve", lambda: V.scalar_tensor_tensor(out=junk.a, in0=xt.a, scalar=rstd.a[:, 0:1], in1=Gt.a, op0=ALU.mult, op1=ALU.mult), [xt, rstd, Gt], [junk])
            sub(9)
            if SHt is None:
                return junk
            if out_f is not None:
                fw.op("dve", lambda: V.scalar_tensor_tensor(out=out_f.a, in0=junk.a, scalar=1.0, in1=SHt.a, op0=ALU.mult, op1=ALU.add), [junk, SHt], [out_f])
                if out_bf is not None:
                    fw.op("act", lambda: A.copy(out=out_bf.a, in_=out_f.a), [out_f], [out_bf])
            else:
                fw.op("dve", lambda: V.tensor_tensor(out=out_bf.a, in0=junk.a, in1=SHt.a, op=ALU.add), [junk, SHt], [out_bf])
            return None

        gs_tmp = {}

        def make_GS(st, i, row, jsh, jsc, gvec_ap, gbuf):
            Gt = sb(st, "Gt%d%d%d" % (i, row, jsh), [128, D])
            SHt = sb(st, "SHt%d%d%d" % (i, row, jsh), [128, D])
            if "gtmp" not in gs_tmp or gs_tmp["st"] is not st:
                gs_tmp["gtmp"] = sb(st, "gtmp", [128, D]); gs_tmp["st"] = st
            tmp = gs_tmp["gtmp"]
            ld(Gt, Gt.a, modrow(i, row, jsc), [MOD])
            ld(tmp, tmp.a, gvec_ap.to_broadcast((128, D)), [gbuf])
            ld(SHt, SHt.a, modrow(i, row, jsh), [MOD])
            fw.op("dve", lambda: V.scalar_tensor_tensor(out=Gt.a, in0=Gt.a, scalar=1.0, in1=tmp.a, op0=ALU.add, op1=ALU.mult), [Gt, tmp], [Gt])
            return Gt, SHt

        def transpose_tile(src_bf, dstT, col0, pT, nk=KC):
            for k0 in range(0, nk, 8):
                p = pT[(k0 // 8) % len(pT)]
                for k in range(k0, min(nk, k0 + 8)):
                    fw.op("pe", lambda: T.transpose(out=p.a[:, k - k0, :], in_=src_bf.a[:, k * 128:(k + 1) * 128], identity=identb.a), [src_bf, identb], [p])
                n = min(nk, k0 + 8) - k0
                if (k0 // 8) % 2 == 0:
                    fw.op("act", lambda: A.copy(out=dstT.a[:, k0:k0 + n, col0:col0 + 128], in_=p.a[:, 0:n, :]), [p], [dstT])
                else:
                    fw.op("dve", lambda: V.tensor_copy(out=dstT.a[:, k0:k0 + n, col0:col0 + 128], in_=p.a[:, 0:n, :]), [p], [dstT])

        def load_w(wbuf, w_ap, wsrc, kc):
            fw.dma("pool", lambda: G.dma_start(out=wbuf.a, in_=w_ap.rearrange("(k p) n -> p k n", p=128)), wbuf, [wsrc])

        with ExitStack() as st:
            Gl, SHl = make_GS(st, 0, 0, 0, 1, g_mix.a[0:1, :], g_mix)
            Gc, SHc = make_GS(st, 0, 1, 0, 1, g_mix.a[0:1, :], g_mix)
            bufs = {"xt": [sb(st, "xt%d" % i, [128, D]) for i in range(1)] * 2, "i": 0, "junk": sb(st, "junk", [128, D]),
                    "ssq": sb(st, "ssq", [128, 1]), "rstd": sb(st, "rstd", [128, 1])}
            abf = [sb(st, "abf%d" % i, [128, D], BF16) for i in range(2)]
            aT = sb(st, "aT", [128, KC, NT], BF16)
            wb = [sb(st, "wb%d" % i, [128, KC, 512], BF16) for i in range(2)]
            osb = [sb(st, "osb%d" % i, [128, 512]) for i in range(3)]
            pT = [ps(st, "pT%d" % i, [128, 8, 128], BF16) for i in range(2)]
            pg = [ps(st, "pg%d" % i, [128, 512]) for i in range(4)]
            cnt = {"w": 0, "o": 0, "p": 0}

            def inproj(src_buf, src_ap_fn, ntiles, Gt, SHt, w_all, wsrc, cbs, zts, zcol0):
                for n in range(ntiles):
                    ab = abf[n % 2]
                    norm_mod_tile(st, bufs, src_buf, src_ap_fn(n), Gt, SHt, out_bf=ab)
                    transpose_tile(ab, aT, n * 128, pT)
                for cb in cbs:
                    w = wb[cnt["w"] % 2]
                    cnt["w"] += 1
                    load_w(w, w_all.a[:, cb * 512:(cb + 1) * 512], wsrc, KC)
                    for n in range(ntiles):
                        p = pg[cnt["p"] % 4]
                        cnt["p"] += 1
                        for k in range(KC):
                            fw.op("pe", lambda: T.matmul(p.a, lhsT=aT.a[:, k, n * 128:(n + 1) * 128], rhs=w.a[:, k, :], start=(k == 0), stop=(k == KC - 1)), [aT, w], [p])
                        o = osb[cnt["o"] % 3]
                        cnt["o"] += 1
                        if cnt["o"] % 2:
                            fw.op("act", lambda: A.copy(out=o.a, in_=p.a), [p], [o])
                        else:
                            fw.op("dve", lambda: V.tensor_copy(out=o.a, in_=p.a), [p], [o])
                        c0 = cb * 512 - zcol0
                        ld(zts[n], zts[n].a[:, c0:c0 + 512], o.a, [o])

            kvb = [2, 3, 4, 5]
            inproj(x_for, lambda n: x_for.a[n * 128:(n + 1) * 128, :], NC, Gl, SHl, ab_w_in, ab_w_in, kvb, Zft, 1024)
            inproj(ctx_l, lambda n: ctx_l.a[n * 128:(n + 1) * 128, :], 2, Gc, SHc, ab_w_in, ab_w_in, kvb, Zct, 1024)
            inproj(x_own, lambda n: x_own.a[n * 128:(n + 1) * 128, :], NC, Gl, SHl, ab_w_in, ab_w_in, list(range(12)), Zt, 0)
            fw.barrier()
            chk()

        with ExitStack() as st:
            lg = sb(st, "lg", [128, 16])
            t1 = sb(st, "t1", [128, 16]); t2 = sb(st, "t2", [128, 16]); t3 = sb(st, "t3", [128, 16]); t4 = sb(st, "t4", [128, 16])
            ld(lg, lg.a, dl.a.to_broadcast((128, 16)), [dl])
            fw.op("act", lambda: A.activation(out=t1.a, in_=lg.a, func=AF.Exp, scale=-1.0), [lg], [t1])
            fw.op("dve", lambda: V.tensor_scalar(out=t2.a, in0=t1.a, scalar1=-0.25, scalar2=1.0 / 3.0, op0=ALU.mult, op1=ALU.add), [t1], [t2])
            fw.op("dve", lambda: V.tensor_tensor(out=t2.a, in0=t2.a, in1=t1.a, op=ALU.mult), [t2, t1], [t2])
            fw.op("dve", lambda: V.tensor_scalar(out=t2.a, in0=t2.a, scalar1=-0.5, scalar2=None, op0=ALU.add), [t2], [t2])
            fw.op("dve", lambda: V.tensor_tensor(out=t2.a, in0=t2.a, in1=t1.a, op=ALU.mult), [t2, t1], [t2])
            fw.op("dve", lambda: V.tensor_scalar(out=t2.a, in0=t2.a, scalar1=1.0, scalar2=None, op0=ALU.add), [t2], [t2])
            fw.op("dve", lambda: V.tensor_tensor(out=t2.a, in0=t2.a, in1=t1.a, op=ALU.mult), [t2, t1], [t2])
            fw.op("dve", lambda: V.tensor_scalar(out=t3.a, in0=t1.a, scalar1=1.0, scalar2=None, op0=ALU.add), [t1], [t3])
            fw.op("act", lambda: A.activation(out=t3.a, in_=t3.a, func=AF.Ln), [t3], [t3])
            fw.op("dve", lambda: V.tensor_scalar(out=t4.a, in0=t1.a, scalar1=0.1, scalar2=None, op0=ALU.is_lt), [t1], [t4])
            fw.op("dve", lambda: V.tensor_tensor(out=t2.a, in0=t2.a, in1=t3.a, op=ALU.subtract), [t2, t3], [t2])
            fw.op("dve", lambda: V.tensor_tensor(out=t2.a, in0=t2.a, in1=t4.a, op=ALU.mult), [t2, t4], [t2])
            fw.op("dve", lambda: V.tensor_tensor(out=t2.a, in0=t2.a, in1=t3.a, op=ALU.add), [t2, t3], [t2])
            fw.op("dve", lambda: V.tensor_scalar(out=lg.a, in0=t2.a, scalar1=-1.0, scalar2=None, op0=ALU.mult), [t2], [lg])

            ropeT = sb(st, "ropeT", [128, 6, NC, 64])
            for r in range(6):
                ld(ropeT, ropeT.a[:, r, :, :], rope.a[r].rearrange("(n p) c -> p n c", p=128), [rope])
            NE = NC + 4
            ew = sb(st, "ew", [128, NE]); ewB = sb(st, "ewB", [128, NE])
            dcol = sb(st, "dcol", [128, 8])
            Dh = sb(st, "Dh", [128, 128]); dtmp = sb(st, "dtmp", [128, 128])
            qs = [sb(st, "qs%d" % i, [128, NC, 128]) for i in range(1)] * 2
            ks = [sb(st, "ks%d" % i, [128, NC, 128]) for i in range(1)] * 2
            vs = [sb(st, "vs%d" % i, [128, NC, 128]) for i in range(1)] * 2
            gs = [sb(st, "gs%d" % i, [128, NC, 128]) for i in range(1)] * 2
            kf = sb(st, "kf", [128, NC + 2, 128]); vf = sb(st, "vf", [128, NC + 2, 128])
            rq = sb(st, "rq", [128, NC, 128]); rk = sb(st, "rk", [128, NC, 128]); rtmp = sb(st, "rtmp", [128, NC, 64])
            qb = sb(st, "qb", [128, NC, 128], BF16); kb = sb(st, "kb", [128, NC, 128], BF16)
            vb = sb(st, "vb", [128, NC, 128], BF16); vA = sb(st, "vA", [128, NC, 128], BF16); vB = sb(st, "vB", [128, NC, 128], BF16)
            kfb = sb(st, "kfb", [128, NC + 2, 128], BF16); vfB = sb(st, "vfB", [128, NC + 2, 128], BF16); vcA = sb(st, "vcA", [128, 2, 128], BF16)
            SA = sb(st, "SA", [128, NC + 1, 128]); SB = sb(st, "SB", [128, NC + 1, 128])
            SAb = sb(st, "SAb", [128, NC + 1, 128], BF16); SBb = sb(st, "SBb", [128, NC + 1, 128], BF16)
            qT = [sb(st, "qT%d" % i, [128, 2, 128], BF16) for i in range(2)]
            ATb = [sb(st, "ATb%d" % i, [128, 128], BF16) for i in range(2)]
            ysb = [sb(st, "ysb%d" % i, [128, 128]) for i in range(2)]
            stt_ = sb(st, "bnst", [128, 6]); mv = sb(st, "mv", [128, 2]); rs = sb(st, "rs", [128, 1])
            sg = sb(st, "sg", [128, 128])
            rout = [sb(st, "rout%d" % i, [128, NC, 128]) for i in range(2)]
            pS = [ps(st, "pS%d" % i, [128, 128]) for i in range(2)]
            pU = [ps(st, "pU%d" % i, [128, 2, 128]) for i in range(2)]
            pTq = ps(st, "pTq", [128, 2, 128], BF16)
            pSc = ps(st, "pSc", [128, 128])
            pY = ps(st, "pY", [128, 3, 128])

            def rope_apply(dst, src, ci, si, nchunk):
                x1 = src.a[:, 0:nchunk, 0:64]; x2 = src.a[:, 0:nchunk, 64:128]
                cth = ropeT.a[:, ci, 0:nchunk, :]; sth = ropeT.a[:, si, 0:nchunk, :]
                tm = rtmp.a[:, 0:nchunk, :]
                fw.op("dve", lambda: V.tensor_tensor(out=dst.a[:, 0:nchunk, 0:64], in0=x1, in1=cth, op=ALU.mult), [src, ropeT], [dst])
                fw.op("dve", lambda: V.tensor_tensor(out=tm, in0=x2, in1=sth, op=ALU.mult), [src, ropeT], [rtmp])
                fw.op("dve", lambda: V.tensor_tensor(out=dst.a[:, 0:nchunk, 0:64], in0=dst.a[:, 0:nchunk, 0:64], in1=tm, op=ALU.subtract), [dst, rtmp], [dst])
                fw.op("dve", lambda: V.tensor_tensor(out=dst.a[:, 0:nchunk, 64:128], in0=x1, in1=sth, op=ALU.mult), [src, ropeT], [dst])
                fw.op("dve", lambda: V.tensor_tensor(out=tm, in0=x2, in1=cth, op=ALU.mult), [src, ropeT], [rtmp])
                fw.op("dve", lambda: V.tensor_tensor(out=dst.a[:, 0:nchunk, 64:128], in0=dst.a[:, 0:nchunk, 64:128], in1=tm, op=ALU.add), [dst, rtmp], [dst])

            for h in range(NH):
                j = h % 2
                c0 = h * 128
                ld(qs[j], qs[j].a, Zd[1:NT + 1, c0:c0 + 128].rearrange("(n p) c -> p n c", p=128), Zt)
                ld(ks[j], ks[j].a, Zd[1:NT + 1, 1024 + c0:1024 + c0 + 128].rearrange("(n p) c -> p n c", p=128), Zt)
                ld(vs[j], vs[j].a, Zd[1:NT + 1, 2048 + c0:2048 + c0 + 128].rearrange("(n p) c -> p n c", p=128), Zt)
                ld(gs[j], gs[j].a, Zd[1:NT + 1, 3072 + c0:3072 + c0 + 128].rearrange("(n p) c -> p n c", p=128), Zt)
                ld(kf, kf.a[:, 0:NC, :], Zfd[:, c0:c0 + 128].rearrange("(n p) c -> p n c", p=128), Zft)
                ld(kf, kf.a[:, NC:NC + 2, :], Zcd[:, c0:c0 + 128].rearrange("(n p) c -> p n c", p=128), Zct)
                ld(vf, vf.a[:, 0:NC, :], Zfd[:, 1024 + c0:1024 + c0 + 128].rearrange("(n p) c -> p n c", p=128), Zft)
                ld(vf, vf.a[:, NC:NC + 2, :], Zcd[:, 1024 + c0:1024 + c0 + 128].rearrange("(n p) c -> p n c", p=128), Zct)
                lgA = lg.a[:, h:h + 1]; lgB = lg.a[:, 8 + h:9 + h]
                fw.op("dve", lambda: V.tensor_scalar(out=dcol.a[:, 0:1], in0=cs.a[:, cc + 0:cc + 1], scalar1=lgA, scalar2=None, op0=ALU.mult), [cs, lg], [dcol])
                fw.op("dve", lambda: V.tensor_scalar(out=dcol.a[:, 1:2], in0=cs.a[:, cc + 1:cc + 2], scalar1=lgB, scalar2=None, op0=ALU.mult), [cs, lg], [dcol])
                fw.op("dve", lambda: V.tensor_scalar(out=dcol.a[:, 2:3], in0=cs.a[:, cc + 2:cc + 3], scalar1=lgA, scalar2=None, op0=ALU.mult), [cs, lg], [dcol])
                fw.op("dve", lambda: V.tensor_scalar(out=dcol.a[:, 3:4], in0=cs.a[:, cc + 3:cc + 4], scalar1=lgB, scalar2=None, op0=ALU.mult), [cs, lg], [dcol])
                fw.op("dve", lambda: V.tensor_scalar(out=dcol.a[:, 4:5], in0=cs.a[:, cc + 4:cc + 5], scalar1=lgA, scalar2=None, op0=ALU.mult), [cs, lg], [dcol])
                fw.op("dve", lambda: V.tensor_scalar(out=dcol.a[:, 5:6], in0=cs.a[:, cc + 4:cc + 5], scalar1=lgB, scalar2=None, op0=ALU.mult), [cs, lg], [dcol])
                fw.op("act", lambda: A.activation(out=dcol.a[:, 0:6], in_=dcol.a[:, 0:6], func=AF.Exp), [dcol], [dcol])
                fw.op("dve", lambda: V.tensor_scalar(out=ewB.a[:, 0:NC + 2], in0=cs.a[:, ET:ET + NC + 2], scalar1=lgB, scalar2=None, op0=ALU.mult), [cs, lg], [ewB])
                fw.op("dve", lambda: V.tensor_scalar(out=ewB.a[:, NC + 2:NC + 4], in0=cs.a[:, ET + NC + 2:ET + NC + 4], scalar1=lgA, scalar2=None, op0=ALU.mult), [cs, lg], [ewB])
                fw.op("act", lambda: A.activation(out=ew.a, in_=ewB.a, func=AF.Exp), [ewB], [ew])
                fw.op("dve", lambda: V.tensor_scalar(out=dtmp.a, in0=R1, scalar1=lgA, scalar2=None, op0=ALU.mult), [cs, lg], [dtmp])
                fw.op("dve", lambda: V.scalar_tensor_tensor(out=dtmp.a, in0=R2, scalar=lgB, in1=dtmp.a, op0=ALU.mult, op1=ALU.add), [cs, lg, dtmp], [dtmp])
                fw.op("act", lambda: A.activation(out=Dh.a, in_=dtmp.a, func=AF.Exp), [dtmp], [Dh])
                rope_apply(rq, qs[j], 0, 1, NC)
                rope_apply(rk, ks[j], 2, 3, NC)
                fw.op("act", lambda: A.copy(out=qb.a, in_=rq.a), [rq], [qb])
                fw.op("act", lambda: A.copy(out=kb.a, in_=rk.a), [rk], [kb])
                fw.op("act", lambda: A.copy(out=vb.a, in_=vs[j].a), [vs[j]], [vb])
                fw.op("dve", lambda: V.tensor_scalar(out=vA.a, in0=vs[j].a, scalar1=dcol.a[:, 0:1], scalar2=None, op0=ALU.mult), [vs[j], dcol], [vA])
                fw.op("dve", lambda: V.tensor_scalar(out=vB.a, in0=vs[j].a, scalar1=dcol.a[:, 1:2], scalar2=None, op0=ALU.mult), [vs[j], dcol], [vB])
                rope_apply(rk, kf, 4, 5, NC)
                fw.op("act", lambda: A.copy(out=kfb.a[:, 0:NC, :], in_=rk.a), [rk], [kfb])
                fw.op("act", lambda: A.copy(out=kfb.a[:, NC:NC + 2, :], in_=kf.a[:, NC:NC + 2, :]), [kf], [kfb])
                for c in range(NC + 2):
                    fw.op("dve", lambda: V.tensor_scalar(out=vfB.a[:, c, :], in0=vf.a[:, c, :], scalar1=ew.a[:, c:c + 1], scalar2=None, op0=ALU.mult), [vf, ew], [vfB])
                for c in range(2):
                    fw.op("dve", lambda: V.tensor_scalar(out=vcA.a[:, c, :], in0=vf.a[:, NC + c, :], scalar1=ew.a[:, NC + 2 + c:NC + 3 + c], scalar2=None, op0=ALU.mult), [vf, ew], [vcA])
                for c in range(2):
                    fw.op("pe", lambda: T.matmul(pS[0].a, lhsT=kfb.a[:, NC + c, :], rhs=vcA.a[:, c, :], start=(c == 0), stop=(c == 1)), [kfb, vcA], [pS[0]])
                fw.op("act", lambda: A.copy(out=SA.a[:, 0, :], in_=pS[0].a), [pS[0]], [SA])
                for c in range(NC + 2):
                    fw.op("pe", lambda: T.matmul(pS[1].a, lhsT=kfb.a[:, c, :], rhs=vfB.a[:, c, :], start=(c == 0), stop=(c == NC + 1)), [kfb, vfB], [pS[1]])
                fw.op("act", lambda: A.copy(out=SB.a[:, NC, :], in_=pS[1].a), [pS[1]], [SB])
                for n in range(NC):
                    p = pU[n % 2]
                    fw.op("pe", lambda: T.matmul(p.a[:, 0, :], lhsT=kb.a[:, n, :], rhs=vA.a[:, n, :], start=True, stop=True), [kb, vA], [p])
                    fw.op("dve", lambda: V.scalar_tensor_tensor(out=SA.a[:, n + 1, :], in0=SA.a[:, n, :], scalar=dcol.a[:, 4:5], in1=p.a[:, 0, :], op0=ALU.mult, op1=ALU.add), [SA, dcol, p], [SA])
                for n in range(NC - 1, -1, -1):
                    p = pU[n % 2]
                    fw.op("pe", lambda: T.matmul(p.a[:, 1, :], lhsT=kb.a[:, n, :], rhs=vB.a[:, n, :], start=True, stop=True), [kb, vB], [p])
                    fw.op("dve", lambda: V.scalar_tensor_tensor(out=SB.a[:, n, :], in0=SB.a[:, n + 1, :], scalar=dcol.a[:, 5:6], in1=p.a[:, 1, :], op0=ALU.mult, op1=ALU.add), [SB, dcol, p], [SB])
                fw.op("act", lambda: A.copy(out=SAb.a, in_=SA.a), [SA], [SAb])
                fw.op("act", lambda: A.copy(out=SBb.a, in_=SB.a), [SB], [SBb])
                ro = rout[j]
                for n in range(NC):
                    qt = qT[n % 2]; at = ATb[n % 2]; y = ysb[n % 2]
                    fw.op("pe", lambda: T.transpose(out=pTq.a[:, 0, :], in_=qb.a[:, n, :], identity=identb.a), [qb, identb], [pTq])
                    fw.op("pe", lambda: T.transpose(out=pTq.a[:, 1, :], in_=kb.a[:, n, :], identity=identb.a), [kb, identb], [pTq])
                    fw.op("act", lambda: A.copy(out=qt.a, in_=pTq.a), [pTq], [qt])
                    fw.op("pe", lambda: T.matmul(pSc.a, lhsT=qt.a[:, 1, :], rhs=qt.a[:, 0, :], start=True, stop=True), [qt], [pSc])
                    fw.op("dve", lambda: V.tensor_tensor(out=at.a, in0=pSc.a, in1=Dh.a, op=ALU.mult), [pSc, Dh], [at])
                    fw.op("pe", lambda: T.matmul(pY.a[:, 0, :], lhsT=at.a, rhs=vb.a[:, n, :], start=True, stop=True), [at, vb], [pY])
                    fw.op("pe", lambda: T.matmul(pY.a[:, 1, :], lhsT=qt.a[:, 0, :], rhs=SAb.a[:, n, :], start=True, stop=True), [qt, SAb], [pY])
                    fw.op("pe", lambda: T.matmul(pY.a[:, 2, :], lhsT=qt.a[:, 0, :], rhs=SBb.a[:, n + 1, :], start=True, stop=True), [qt, SBb], [pY])
                    fw.op("act", lambda: A.copy(out=y.a, in_=pY.a[:, 0, :]), [pY], [y])
                    fw.op("dve", lambda: V.scalar_tensor_tensor(out=y.a, in0=pY.a[:, 1, :], scalar=dcol.a[:, 2:3], in1=y.a, op0=ALU.mult, op1=ALU.add), [pY, dcol, y], [y])
                    fw.op("dve", lambda: V.scalar_tensor_tensor(out=y.a, in0=pY.a[:, 2, :], scalar=dcol.a[:, 3:4], in1=y.a, op0=ALU.mult, op1=ALU.add), [pY, dcol, y], [y])
                    fw.op("dve", lambda: V.bn_stats(out=stt_.a, in_=y.a), [y], [stt_])
                    fw.op("dve", lambda: V.bn_aggr(out=mv.a, in_=stt_.a), [stt_], [mv])
                    fw.op("dve", lambda: V.tensor_scalar(out=rs.a, in0=mv.a[:, 1:2], scalar1=EPS, scalar2=None, op0=ALU.add), [mv], [rs])
                    fw.op("act", lambda: A.activation(out=rs.a, in_=rs.a, func=AF.Sqrt), [rs], [rs])
                    fw.op("dve", lambda: V.reciprocal(out=rs.a, in_=rs.a), [rs], [rs])
                    fw.op("dve", lambda: V.tensor_scalar(out=y.a, in0=y.a, scalar1=mv.a[:, 0:1], scalar2=rs.a[:, 0:1], op0=ALU.subtract, op1=ALU.mult), [y, mv, rs], [y])
                    fw.op("act", lambda: A.activation(out=sg.a, in_=gs[j].a[:, n, :], func=AF.Silu), [gs[j]], [sg])
                    fw.op("dve", lambda: V.tensor_tensor(out=ro.a[:, n, :], in0=y.a, in1=sg.a, op=ALU.mult), [y, sg], [ro])
                for n in range(NC):
                    ld(Rt[n], Rt[n].a[:, c0:c0 + 128], ro.a[:, n, :], [ro])
            fw.barrier()
            chk()

        def gelu(dst, src, tmp, reads):
            fw.op("dve", lambda: V.tensor_tensor(out=tmp.a, in0=src.a, in1=src.a, op=ALU.mult), reads, [tmp])
            fw.op("dve", lambda: V.tensor_scalar(out=tmp.a, in0=tmp.a, scalar1=0.044715, scalar2=1.0, op0=ALU.mult, op1=ALU.add), [tmp], [tmp])
            fw.op("dve", lambda: V.tensor_tensor(out=tmp.a, in0=tmp.a, in1=src.a, op=ALU.mult), [tmp] + reads, [tmp])
            fw.op("act", lambda: A.activation(out=tmp.a, in_=tmp.a, func=AF.Sigmoid, scale=1.5957691216057308), [tmp], [tmp])
            fw.op("dve", lambda: V.tensor_tensor(out=dst.a, in0=tmp.a, in1=src.a, op=ALU.mult), [tmp] + reads, [dst])

        with ExitStack() as st:
            us = [sb(st, "us%d" % i, [128, NC, 128]) for i in range(2)]
            vs2 = [sb(st, "vs2%d" % i, [128, NC, 128]) for i in range(2)]
            gu = sb(st, "gu", [128, NC, 128]); gv = sb(st, "gv", [128, NC, 128]); tmpg = sb(st, "tmpg", [128, NC, 128])
            vn = sb(st, "vn", [128, NC, 128], BF16)
            wsf = sb(st, "wsf", [128, 8, 128]); wsb = sb(st, "wsb", [128, 8, 128], BF16)
            bsb = sb(st, "bsb", [128, 8])
            stt2 = sb(st, "bnst2", [128, 6]); mv2 = sb(st, "mv2", [128, 2]); rs2 = sb(st, "rs2", [128, 1])
            ro2 = [sb(st, "ro2%d" % i, [128, NC, 128]) for i in range(2)]
            pG = [ps(st, "pG%d" % i, [128, 128]) for i in range(2)]
            ld(wsf, wsf.a, wsT.a.rearrange("g q p -> q g p"), [wsT])
            ld(bsb, bsb.a, bsT.a, [bsT])
            fw.op("act", lambda: A.copy(out=wsb.a, in_=wsf.a), [wsf], [wsb])
            for g in range(8):
                j = g % 2
                c0 = g * 128
                ld(us[j], us[j].a, Zd[1:NT + 1, 4096 + c0:4096 + c0 + 128].rearrange("(n p) c -> p n c", p=128), Zt)
                ld(vs2[j], vs2[j].a, Zd[1:NT + 1, 5120 + c0:5120 + c0 + 128].rearrange("(n p) c -> p n c", p=128), Zt)
                gelu(gu, us[j], tmpg, [us[j]])
                gelu(gv, vs2[j], tmpg, [vs2[j]])
                for n in range(NC):
                    fw.op("dve", lambda: V.bn_stats(out=stt2.a, in_=gv.a[:, n, :]), [gv], [stt2])
                    fw.op("dve", lambda: V.bn_aggr(out=mv2.a, in_=stt2.a), [stt2], [mv2])
                    fw.op("dve", lambda: V.tensor_scalar(out=rs2.a, in0=mv2.a[:, 1:2], scalar1=EPS, scalar2=None, op0=ALU.add), [mv2], [rs2])
                    fw.op("act", lambda: A.activation(out=rs2.a, in_=rs2.a, func=AF.Sqrt), [rs2], [rs2])
                    fw.op("dve", lambda: V.reciprocal(out=rs2.a, in_=rs2.a), [rs2], [rs2])
                    fw.op("dve", lambda: V.tensor_scalar(out=vn.a[:, n, :], in0=gv.a[:, n, :], scalar1=mv2.a[:, 0:1], scalar2=rs2.a[:, 0:1], op0=ALU.subtract, op1=ALU.mult), [gv, mv2, rs2], [vn])
                for n in range(NC):
                    p = pG[n % 2]
                    fw.op("pe", lambda: T.matmul(p.a, lhsT=wsb.a[:, g, :], rhs=vn.a[:, n, :], start=True, stop=True), [wsb, vn], [p])
                    fw.op("dve", lambda: V.scalar_tensor_tensor(out=ro2[j].a[:, n, :], in0=p.a, scalar=bsb.a[:, g:g + 1], in1=gu.a[:, n, :], op0=ALU.add, op1=ALU.mult), [p, bsb, gu], [ro2[j]])
                for n in range(NC):
                    ld(Rt[n], Rt[n].a[:, 1024 + c0:1024 + c0 + 128], ro2[j].a[:, n, :], [ro2[j]])
            fw.barrier()
            chk()

        def outproj_and_moe(i, w_out, x_src_tiles, last):
            with ExitStack() as st:
                with ExitStack() as s2:
                    GT1 = sb(s2, "GT1", [128, D])
                    ld(GT1, GT1.a, modrow(i, 0, 2), [MOD])
                    rf = [sb(s2, "rf%d" % k, [128, D]) for k in range(2)]
                    rb = [sb(s2, "rb%d" % k, [128, D], BF16) for k in range(2)]
                    wb = [sb(s2, "wo%d" % k, [128, KC, 512], BF16) for k in range(2)]
                    xin = [sb(s2, "xin%d" % k, [128, 512]) for k in range(3)]
                    pT = [ps(s2, "pT%d" % k, [128, 8, 128], BF16) for k in range(2)]
                    pg = [ps(s2, "pg%d" % k, [128, 512]) for k in range(4)]
                    rT = sb(s2, "rT", [128, KC, NT], BF16)
                    for n in range(NC):
                        ld(rf[n % 2], rf[n % 2].a, Rt[n].a, [Rt[n]])
                        fw.op("act", lambda: A.copy(out=rb[n % 2].a, in_=rf[n % 2].a), [rf[n % 2]], [rb[n % 2]])
                        transpose_tile(rb[n % 2], rT, n * 128, pT)
                    ci = 0
                    for cb in range(4):
                        w = wb[cb % 2]
                        load_w(w, w_out.a[:, cb * 512:(cb + 1) * 512], w_out, KC)
                        for n in range(NC):
                            p = pg[ci % 4]; xi = xin[ci % 3]; ci += 1
                            xb_, xap = x_src_tiles[n]
                            ld(xi, xi.a, xap[:, cb * 512:(cb + 1) * 512], [xb_])
                            for k in range(KC):
                                fw.op("pe", lambda: T.matmul(p.a, lhsT=rT.a[:, k, n * 128:(n + 1) * 128], rhs=w.a[:, k, :], start=(k == 0), stop=(k == KC - 1)), [rT, w], [p])
                            fw.op("dve", lambda: V.tensor_tensor(out=p.a, in0=p.a, in1=GT1.a[:, cb * 512:(cb + 1) * 512], op=ALU.mult), [p, GT1], [p])
                            fw.op("dve", lambda: V.tensor_tensor(out=xi.a, in0=p.a, in1=xi.a, op=ALU.add), [p, xi], [xi])
                            ld(Xt[n], Xt[n].a[:, cb * 512:(cb + 1) * 512], xi.a, [xi])
                    fw.barrier()
                    chk()
                NS = 2 * NC + 16
                MA16 = sb(st, "MA16", [128, NC, 16]); MB16 = sb(st, "MB16", [128, NC, 16]); wAB = sb(st, "wAB", [128, NC, 2])
                slotAi = sb(st, "slotAi", [128, NC], I32); slotBi = sb(st, "slotBi", [128, NC], I32)
                WIi = sb(st, "WIi", [128, NS, 16], I32)
                with ExitStack() as s2:
                    Ftok = sb(s2, "Ftok", [128, NC, D], BF16)
                    Gf, SHf = make_GS(s2, i, 0, 3, 4, g_ffn.a[i:i + 1, :], g_ffn)
                    bufs = {"xt": [sb(s2, "xt%d" % k, [128, D]) for k in range(2)], "i": 0, "junk": sb(s2, "junk", [128, D]),
                            "ssq": sb(s2, "ssq", [128, 1]), "rstd": sb(s2, "rstd", [128, 1])}
                    ff = [sb(s2, "ff%d" % k, [128, D]) for k in range(2)]
                    fTf = sb(s2, "fTf", [128, KC, 128])
                    wrs = sb(s2, "wrs", [128, KC, 20]); brs = sb(s2, "brs", [128, 20])
                    L = sb(s2, "L", [128, 20]); m1 = sb(s2, "m1", [128, 4]); oh1 = sb(s2, "oh1", [128, 4]); e1 = sb(s2, "e1", [128, 4])
                    l2 = sb(s2, "l2", [128, 4]); l2b = sb(s2, "l2b", [128, 4]); ohA = sb(s2, "ohA", [128, 4]); ohB = sb(s2, "ohB", [128, 4])
                    pF = [ps(s2, "pF%d" % k, [128, 4, 128]) for k in range(2)]
                    pL = ps(s2, "pL", [128, 20])
                    ld(wrs, wrs.a, wr.a[i].rearrange("(k p) n -> p k n", p=128), [wr])
                    ld(brs, brs.a, br.a[i:i + 1, :].to_broadcast((128, 20)), [br])
                    for n in range(NC):
                        f = ff[n % 2]
                        norm_mod_tile(s2, bufs, Xt[n], Xt[n].a, Gf, SHf, out_f=f)
                        fw.op("act", lambda: A.copy(out=Ftok.a[:, n, :], in_=f.a), [f], [Ftok])
                        for k0 in range(0, KC, 4):
                            p = pF[(k0 // 4) % 2]
                            for k in range(k0, k0 + 4):
                                fw.op("pe", lambda: T.transpose(out=p.a[:, k - k0, :], in_=f.a[:, k * 128:(k + 1) * 128], identity=identF.a), [f, identF], [p])
                            fw.op("act", lambda: A.copy(out=fTf.a[:, k0:k0 + 4, :], in_=p.a), [p], [fTf])
                        for k in range(KC):
                            fw.op("pe", lambda: T.matmul(pL.a, lhsT=fTf.a[:, k, :], rhs=wrs.a[:, k, :], start=(k == 0), stop=(k == KC - 1)), [fTf, wrs], [pL])
                        fw.op("dve", lambda: V.tensor_tensor(out=L.a, in0=pL.a, in1=brs.a, op=ALU.add), [pL, brs], [L])
                        fw.op("dve", lambda: V.tensor_reduce(out=m1.a[:, 0:1], in_=L.a[:, 0:4], axis=mybir.AxisListType.X, op=ALU.max), [L], [m1])
                        fw.op("dve", lambda: V.tensor_scalar(out=oh1.a, in0=L.a[:, 0:4], scalar1=m1.a[:, 0:1], scalar2=None, op0=ALU.is_equal), [L, m1], [oh1])
                        fw.op("dve", lambda: V.tensor_scalar(out=e1.a, in0=L.a[:, 0:4], scalar1=m1.a[:, 0:1], scalar2=None, op0=ALU.subtract), [L, m1], [e1])
                        fw.op("act", lambda: A.activation(out=e1.a, in_=e1.a, func=AF.Exp), [e1], [e1])
                        fw.op("dve", lambda: V.tensor_reduce(out=m1.a[:, 1:2], in_=e1.a, axis=mybir.AxisListType.X, op=ALU.add), [e1], [m1])
                        fw.op("dve", lambda: V.reciprocal(out=m1.a[:, 1:2], in_=m1.a[:, 1:2]), [m1], [m1])
                        fw.op("dve", lambda: V.tensor_scalar(out=l2.a, in0=L.a[:, 4:8], scalar1=oh1.a[:, 0:1], scalar2=None, op0=ALU.mult), [L, oh1], [l2])
                        for g in range(1, 4):
                            fw.op("dve", lambda: V.scalar_tensor_tensor(out=l2.a, in0=L.a[:, 4 + 4 * g:8 + 4 * g], scalar=oh1.a[:, g:g + 1], in1=l2.a, op0=ALU.mult, op1=ALU.add), [L, oh1, l2], [l2])
                        fw.op("dve", lambda: V.tensor_reduce(out=m1.a[:, 2:3], in_=l2.a, axis=mybir.AxisListType.X, op=ALU.max), [l2], [m1])
                        fw.op("dve", lambda: V.tensor_scalar(out=ohA.a, in0=l2.a, scalar1=m1.a[:, 2:3], scalar2=None, op0=ALU.is_equal), [l2, m1], [ohA])
                        fw.op("dve", lambda: V.scalar_tensor_tensor(out=l2b.a, in0=ohA.a, scalar=-1e30, in1=l2.a, op0=ALU.mult, op1=ALU.add), [ohA, l2], [l2b])
                        fw.op("dve", lambda: V.tensor_reduce(out=m1.a[:, 3:4], in_=l2b.a, axis=mybir.AxisListType.X, op=ALU.max), [l2b], [m1])
                        fw.op("dve", lambda: V.tensor_scalar(out=ohB.a, in0=l2b.a, scalar1=m1.a[:, 3:4], scalar2=None, op0=ALU.is_equal), [l2b, m1], [ohB])
                        fw.op("dve", lambda: V.tensor_tensor(out=e1.a[:, 0:1], in0=m1.a[:, 3:4], in1=m1.a[:, 2:3], op=ALU.subtract), [m1], [e1])
                        fw.op("act", lambda: A.activation(out=e1.a[:, 0:1], in_=e1.a[:, 0:1], func=AF.Exp), [e1], [e1])
                        fw.op("dve", lambda: V.tensor_scalar(out=e1.a[:, 1:2], in0=e1.a[:, 0:1], scalar1=1.0, scalar2=None, op0=ALU.add), [e1], [e1])
                        fw.op("dve", lambda: V.reciprocal(out=e1.a[:, 1:2], in_=e1.a[:, 1:2]), [e1], [e1])
                        fw.op("dve", lambda: V.tensor_tensor(out=e1.a[:, 2:3], in0=e1.a[:, 0:1], in1=e1.a[:, 1:2], op=ALU.mult), [e1], [e1])
                        fw.op("dve", lambda: V.tensor_scalar(out=wAB.a[:, n, :], in0=e1.a[:, 1:3], scalar1=m1.a[:, 1:2], scalar2=None, op0=ALU.mult), [e1, m1], [wAB])
                        for g in range(4):
                            fw.op("dve", lambda: V.tensor_scalar(out=MA16.a[:, n, 4 * g:4 * g + 4], in0=ohA.a, scalar1=oh1.a[:, g:g + 1], scalar2=None, op0=ALU.mult), [ohA, oh1], [MA16])
                            fw.op("dve", lambda: V.tensor_scalar(out=MB16.a[:, n, 4 * g:4 * g + 4], in0=ohB.a, scalar1=oh1.a[:, g:g + 1], scalar2=None, op0=ALU.mult), [ohB, oh1], [MB16])
                    Mf = sb(s2, "Mf", [128, NC, 16]); Mb = sb(s2, "Mb", [128, NC * 16], BF16)
                    cntS = sb(s2, "cntS", [128, NC, 16]); ptS = sb(s2, "ptS", [128, NC, 16]); rk = sb(s2, "rk", [128, NC, 16]); rk2 = sb(s2, "rk2", [128, NC, 16])
                    ne = sb(s2, "ne", [128, 16]); tl = sb(s2, "tl", [128, 16]); se = sb(s2, "se", [128, 16]); st128 = sb(s2, "st128", [128, 16])
                    slf = sb(s2, "slf", [128, 2, NC]); ek = sb(s2, "ek", [128, NS]); chg = sb(s2, "chg", [128, NS]); off = sb(s2, "off", [128, NS])
                    WIf = sb(s2, "WIf", [128, NS, 16])
                    trib = sb(s2, "trib", [128, 2, 128], BF16)
                    pP = [ps(s2, "pP%d" % k, [128, NC * 16]) for k in range(2)]
                    fw.op("dve", lambda: V.tensor_copy(out=trib.a, in_=cs.a[:, TRI:TRI + 256]), [cs], [trib])
                    fw.op("dve", lambda: V.tensor_tensor(out=Mf.a, in0=MA16.a, in1=MB16.a, op=ALU.add), [MA16, MB16], [Mf])
                    fw.op("dve", lambda: V.tensor_copy(out=Mb.a, in_=Mf.a), [Mf], [Mb])
                    fw.op("pe", lambda: T.matmul(pP[0].a, lhsT=trib.a[:, 0, :], rhs=Mb.a, start=True, stop=True), [trib, Mb], [pP[0]])
                    fw.op("pe", lambda: T.matmul(pP[1].a, lhsT=trib.a[:, 1, :], rhs=Mb.a, start=True, stop=True), [trib, Mb], [pP[1]])
                    fw.op("act", lambda: A.copy(out=cntS.a, in_=pP[1].a), [pP[1]], [cntS])
                    fw.op("dve", lambda: V.memset(ptS.a[:, 0, :], 0.0), [], [ptS])
                    for n in range(1, NC):
                        fw.op("dve", lambda: V.tensor_tensor(out=ptS.a[:, n, :], in0=ptS.a[:, n - 1, :], in1=cntS.a[:, n - 1, :], op=ALU.add), [ptS, cntS], [ptS])
                    fw.op("dve", lambda: V.tensor_tensor(out=ne.a, in0=ptS.a[:, NC - 1, :], in1=cntS.a[:, NC - 1, :], op=ALU.add), [ptS, cntS], [ne])
                    fw.op("dve", lambda: V.memset(tl.a, 0.0), [], [tl])
                    for j in range(NC):
                        fw.op("dve", lambda: V.scalar_tensor_tensor(out=tl.a, in0=ne.a, scalar=128.0 * j, in1=tl.a, op0=ALU.is_gt, op1=ALU.add), [ne, tl], [tl])
                    fw.op("dve", lambda: V.memset(se.a[:, 0:1], 0.0), [], [se])
                    for e in range(1, 16):
                        fw.op("dve", lambda: V.tensor_tensor(out=se.a[:, e:e + 1], in0=se.a[:, e - 1:e], in1=tl.a[:, e - 1:e], op=ALU.add), [se, tl], [se])
                    fw.op("dve", lambda: V.tensor_scalar(out=st128.a, in0=se.a, scalar1=128.0, scalar2=None, op0=ALU.mult), [se], [st128])
                    fw.op("dve", lambda: V.tensor_tensor(out=rk.a, in0=pP[0].a, in1=ptS.a, op=ALU.add), [pP[0], ptS], [rk])
                    for n in range(NC):
                        fw.op("dve", lambda: V.tensor_tensor(out=rk.a[:, n, :], in0=rk.a[:, n, :], in1=st128.a, op=ALU.add), [rk, st128], [rk])
                    fw.op("dve", lambda: V.tensor_tensor(out=rk2.a, in0=rk.a, in1=MA16.a, op=ALU.mult), [rk, MA16], [rk2])
                    fw.op("dve", lambda: V.tensor_reduce(out=slf.a[:, 0, :], in_=rk2.a, axis=mybir.AxisListType.X, op=ALU.add), [rk2], [slf])
                    fw.op("dve", lambda: V.tensor_tensor(out=rk2.a, in0=rk.a, in1=MB16.a, op=ALU.mult), [rk, MB16], [rk2])
                    fw.op("dve", lambda: V.tensor_reduce(out=slf.a[:, 1, :], in_=rk2.a, axis=mybir.AxisListType.X, op=ALU.add), [rk2], [slf])
                    fw.op("dve", lambda: V.tensor_copy(out=slotAi.a, in_=slf.a[:, 0, :]), [slf], [slotAi])
                    fw.op("dve", lambda: V.tensor_copy(out=slotBi.a, in_=slf.a[:, 1, :]), [slf], [slotBi])
                    fw.op("dve", lambda: V.memset(ek.a, -1.0), [], [ek])
                    for e in range(16):
                        fw.op("dve", lambda: V.scalar_tensor_tensor(out=ek.a, in0=cs.a[:, KV:KV + NS], scalar=se.a[:, e:e + 1], in1=ek.a, op0=ALU.is_ge, op1=ALU.add), [cs, se, ek], [ek])
                    fw.op("dve", lambda: V.memset(chg.a[:, 0:1], 1.0), [], [chg])
                    fw.op("dve", lambda: V.tensor_tensor(out=chg.a[:, 1:NS], in0=ek.a[:, 1:NS], in1=ek.a[:, 0:NS - 1], op=ALU.not_equal), [ek], [chg])
                    fw.op("dve", lambda: V.tensor_scalar(out=off.a, in0=chg.a, scalar1=-1.0e6, scalar2=1.0e6, op0=ALU.mult, op1=ALU.add), [chg], [off])
                    fw.op("dve", lambda: V.scalar_tensor_tensor(out=off.a, in0=ek.a, scalar=1024.0, in1=off.a, op0=ALU.mult, op1=ALU.add), [ek, off], [off])
                    for k in range(NS):
                        fw.op("dve", lambda: V.tensor_scalar(out=WIf.a[:, k, :], in0=cs.a[:, BASE:BASE + 16], scalar1=off.a[:, k:k + 1], scalar2=None, op0=ALU.add), [cs, off], [WIf])
                    fw.op("dve", lambda: V.tensor_copy(out=WIi.a, in_=WIf.a), [WIf], [WIi])
                    for n in range(NC):
                        for sl_ in (slotAi, slotBi):
                            fw.dma("pool", lambda: G.indirect_dma_start(out=FS.a, out_offset=bass.IndirectOffsetOnAxis(ap=sl_.a[:, n:n + 1], axis=0), in_=Ftok.a[:, n, :], in_offset=None, bounds_check=bcS, oob_is_err=False), FS, [Ftok, sl_])
                    fw.barrier()
                    chk()
                with ExitStack() as s2:
                    w1c = [sb(s2, "w1c%d" % k, [128, D], BF16) for k in range(8)]
                    w3c = [sb(s2, "w3c%d" % k, [128, D], BF16) for k in range(8)]
                    w2c = [sb(s2, "w2c%d" % k, [128, D], BF16) for k in range(8)]
                    ftl = [sb(s2, "ftl%d" % k, [128, D], BF16) for k in range(2)]
                    fTk = [sb(s2, "fTk%d" % k, [128, KC, 128], BF16) for k in range(2)]
                    sl = [sb(s2, "sl%d" % k, [128, 512]) for k in range(2)]
                    ab = [sb(s2, "ab%d" % k, [128, DE], BF16) for k in range(2)]
                    aTt = [sb(s2, "aTt%d" % k, [128, 8, 128], BF16) for k in range(2)]
                    yo = [sb(s2, "yo%d" % k, [128, D]) for k in range(2)]
                    ph = [ps(s2, "ph%d" % k, [128, 512]) for k in range(4)]
                    pTf = [ps(s2, "pTf%d" % k, [128, 8, 128], BF16) for k in range(2)]
                    py = [ps(s2, "py%d" % k, [128, 512]) for k in range(2)]
                    yi = 0
                    for k in range(NS):
                        for j in range(8):
                            fw.dma("pool", lambda: G.indirect_dma_start(out=w1c[j].a, out_offset=None, in_=moe_w1[i].a, in_offset=bass.IndirectOffsetOnAxis(ap=WIi.a[:, k, j:j + 1], axis=0), bounds_check=bcW, oob_is_err=False), w1c[j], [moe_w1[i], WIi])
                        for j in range(8):
                            fw.dma("pool", lambda: G.indirect_dma_start(out=w3c[j].a, out_offset=None, in_=moe_w3[i].a, in_offset=bass.IndirectOffsetOnAxis(ap=WIi.a[:, k, j:j + 1], axis=0), bounds_check=bcW, oob_is_err=False), w3c[j], [moe_w3[i], WIi])
                        for j in range(8):
                            fw.dma("pool", lambda: G.indirect_dma_start(out=w2c[j].a, out_offset=None, in_=moe_w2[i].a, in_offset=bass.IndirectOffsetOnAxis(ap=WIi.a[:, k, 8 + j:9 + j], axis=0), bounds_check=bcW, oob_is_err=False), w2c[j], [moe_w2[i], WIi])
                        ft = ftl[k % 2]; fT_ = fTk[k % 2]; a_ = ab[k % 2]; at = aTt[k % 2]; y = yo[k % 2]
                        ld(ft, ft.a, FS.a[k * 128:(k + 1) * 128, :], [FS])
                        transpose_tile(ft, fT_, 0, pTf)
                        for hf in range(2):
                            p1 = ph[hf]
                            for kk in range(KC):
                                c0 = (kk % 2) * 1024 + hf * 512
                                fw.op("pe", lambda: T.matmul(p1.a, lhsT=fT_.a[:, kk, :], rhs=w1c[kk // 2].a[:, c0:c0 + 512], start=(kk == 0), stop=(kk == KC - 1)), [fT_, w1c[kk // 2]], [p1])
                            fw.op("act", lambda: A.activation(out=sl[hf].a, in_=p1.a, func=AF.Silu), [p1], [sl[hf]])
                        for hf in range(2):
                            p3 = ph[2 + hf]
                            for kk in range(KC):
                                c0 = (kk % 2) * 1024 + hf * 512
                                fw.op("pe", lambda: T.matmul(p3.a, lhsT=fT_.a[:, kk, :], rhs=w3c[kk // 2].a[:, c0:c0 + 512], start=(kk == 0), stop=(kk == KC - 1)), [fT_, w3c[kk // 2]], [p3])
                            fw.op("dve", lambda: V.tensor_tensor(out=a_.a[:, hf * 512:(hf + 1) * 512], in0=p3.a, in1=sl[hf].a, op=ALU.mult), [p3, sl[hf]], [a_])
                        transpose_tile(a_, at, 0, pTf[1:2], nk=8)
                        for cb in range(4):
                            p = py[yi % 2]; yi += 1
                            for k8 in range(8):
                                fw.op("pe", lambda: T.matmul(p.a, lhsT=at.a[:, k8, :], rhs=w2c[k8].a[:, cb * 512:(cb + 1) * 512], start=(k8 == 0), stop=(k8 == 7)), [at, w2c[k8]], [p])
                            if cb % 2:
                                fw.op("act", lambda: A.copy(out=y.a[:, cb * 512:(cb + 1) * 512], in_=p.a), [p], [y])
                            else:
                                fw.op("dve", lambda: V.tensor_copy(out=y.a[:, cb * 512:(cb + 1) * 512], in_=p.a), [p], [y])
                        ld(YS, YS.a[k * 128:(k + 1) * 128, :], y.a, [y])
                    fw.barrier()
                    chk()
                with ExitStack() as s2:
                    GT2 = sb(s2, "GT2", [128, D])
                    ld(GT2, GT2.a, modrow(i, 0, 5), [MOD])
                    YA = [sb(s2, "YA%d" % k, [128, D]) for k in range(2)]
                    YB = [sb(s2, "YB%d" % k, [128, D]) for k in range(2)]
                    xc = [sb(s2, "xc%d" % k, [128, D]) for k in range(2)]
                    for n in range(NC):
                        ya = YA[n % 2]; yb = YB[n % 2]; x_ = xc[n % 2]
                        fw.dma("pool", lambda: G.indirect_dma_start(out=ya.a, out_offset=None, in_=YS.a, in_offset=bass.IndirectOffsetOnAxis(ap=slotAi.a[:, n:n + 1], axis=0), bounds_check=bcS, oob_is_err=False), ya, [YS, slotAi])
                        fw.dma("pool", lambda: G.indirect_dma_start(out=yb.a, out_offset=None, in_=YS.a, in_offset=bass.IndirectOffsetOnAxis(ap=slotBi.a[:, n:n + 1], axis=0), bounds_check=bcS, oob_is_err=False), yb, [YS, slotBi])
                        ld(x_, x_.a, Xt[n].a, [Xt[n]])
                        fw.op("dve", lambda: V.tensor_scalar(out=ya.a, in0=ya.a, scalar1=wAB.a[:, n, 0:1], scalar2=None, op0=ALU.mult), [ya, wAB], [ya])
                        fw.op("dve", lambda: V.scalar_tensor_tensor(out=ya.a, in0=yb.a, scalar=wAB.a[:, n, 1:2], in1=ya.a, op0=ALU.mult, op1=ALU.add), [yb, wAB, ya], [ya])
                        fw.op("dve", lambda: V.scalar_tensor_tensor(out=ya.a, in0=ya.a, scalar=1.0, in1=GT2.a, op0=ALU.mult, op1=ALU.mult), [ya, GT2], [ya])
                        fw.op("dve", lambda: V.scalar_tensor_tensor(out=x_.a, in0=ya.a, scalar=1.0, in1=x_.a, op0=ALU.mult, op1=ALU.add), [ya, x_], [x_])
                        ld(Xt[n], Xt[n].a, x_.a, [x_])
                    fw.barrier()
                    chk()

        outproj_and_moe(0, ab_w_out, [(x_own, x_own.a[n * 128:(n + 1) * 128, :]) for n in range(NC)], False)

        with ExitStack() as st:
            Gl, SHl = make_GS(st, 1, 0, 0, 1, g_mix.a[1:2, :], g_mix)
            bufs = {"xt": [sb(st, "xt%d" % i, [128, D]) for i in range(2)], "i": 0, "junk": sb(st, "junk", [128, D]),
                    "ssq": sb(st, "ssq", [128, 1]), "rstd": sb(st, "rstd", [128, 1])}
            abf = [sb(st, "abf%d" % i, [128, D], BF16) for i in range(2)]
            aT = sb(st, "aT", [128, KC, NT], BF16)
            wb = [sb(st, "wb%d" % i, [128, KC, 512], BF16) for i in range(2)]
            osb = [sb(st, "osb%d" % i, [128, 512]) for i in range(3)]
            pT = [ps(st, "pT%d" % i, [128, 8, 128], BF16) for i in range(2)]
            pg = [ps(st, "pg%d" % i, [128, 512]) for i in range(4)]
            for n in range(NC):
                ab = abf[n % 2]
                norm_mod_tile(st, bufs, Xt[n], Xt[n].a, Gl, SHl, out_bf=ab)
                transpose_tile(ab, aT, n * 128, pT)
            ci = 0
            for cb in range(12):
                w = wb[cb % 2]
                load_w(w, cv_w_in.a[:, cb * 512:(cb + 1) * 512], cv_w_in, KC)
                for n in range(NC):
                    p = pg[ci % 4]; o = osb[ci % 3]; ci += 1
                    for k in range(KC):
                        fw.op("pe", lambda: T.matmul(p.a, lhsT=aT.a[:, k, n * 128:(n + 1) * 128], rhs=w.a[:, k, :], start=(k == 0), stop=(k == KC - 1)), [aT, w], [p])
                    if ci % 2:
                        fw.op("act", lambda: A.copy(out=o.a, in_=p.a), [p], [o])
                    else:
                        fw.op("dve", lambda: V.tensor_copy(out=o.a, in_=p.a), [p], [o])
                    ld(Zt[n], Zt[n].a[:, cb * 512:(cb + 1) * 512], o.a, [o])
            fw.barrier()
            chk()
        with ExitStack() as st:
            CW = sb(st, "CW", [128, 3, D]); CB = sb(st, "CB", [128, D])
            for j in range(3):
                ld(CW, CW.a[:, j, :], conv_w.a[j:j + 1, :].to_broadcast((128, D)), [conv_w])
            ld(CB, CB.a, conv_b.a.to_broadcast((128, D)), [conv_b])
            gc = [sb(st, "gc%d" % j, [128, D]) for j in range(3)]
            hv = [sb(st, "hv%d" % j, [128, D]) for j in range(3)]
            gbt = sb(st, "gbt", [128, D]); acc = sb(st, "acc", [128, D])
            zall = Zt + [Zpad, Zpad2]
            for n in range(NC):
                r0 = 1 + n * 128
                for j in range(3):
                    ld(gc[j], gc[j].a, Zd[r0 + j - 1:r0 + j - 1 + 128, 2048:4096], zall)
                    ld(hv[j], hv[j].a, Zd[r0 + j - 1:r0 + j - 1 + 128, 4096:6144], zall)
                ld(gbt, gbt.a, Zt[n].a[:, 0:2048], [Zt[n]])
                for j in range(3):
                    fw.op("dve", lambda: V.scalar_tensor_tensor(out=gc[j].a, in0=gc[j].a, scalar=1.0, in1=hv[j].a, op0=ALU.mult, op1=ALU.mult), [gc[j], hv[j]], [gc[j]])
                fw.op("dve", lambda: V.scalar_tensor_tensor(out=acc.a, in0=gc[0].a, scalar=cs.a[:, cc + 5:cc + 6], in1=CW.a[:, 0, :], op0=ALU.mult, op1=ALU.mult), [gc[0], cs, CW], [acc])
                fw.op("dve", lambda: V.scalar_tensor_tensor(out=acc.a, in0=acc.a, scalar=1.0, in1=CB.a, op0=ALU.mult, op1=ALU.add), [acc, CB], [acc])
                fw.op("dve", lambda: V.scalar_tensor_tensor(out=gc[1].a, in0=gc[1].a, scalar=1.0, in1=CW.a[:, 1, :], op0=ALU.mult, op1=ALU.mult), [gc[1], CW], [gc[1]])
                fw.op("dve", lambda: V.scalar_tensor_tensor(out=acc.a, in0=acc.a, scalar=1.0, in1=gc[1].a, op0=ALU.mult, op1=ALU.add), [acc, gc[1]], [acc])
                fw.op("dve", lambda: V.scalar_tensor_tensor(out=gc[2].a, in0=gc[2].a, scalar=cs.a[:, cc + 6:cc + 7], in1=CW.a[:, 2, :], op0=ALU.mult, op1=ALU.mult), [gc[2], cs, CW], [gc[2]])
                fw.op("dve", lambda: V.scalar_tensor_tensor(out=acc.a, in0=acc.a, scalar=1.0, in1=gc[2].a, op0=ALU.mult, op1=ALU.add), [acc, gc[2]], [acc])
                fw.op("dve", lambda: V.scalar_tensor_tensor(out=acc.a, in0=acc.a, scalar=1.0, in1=gbt.a, op0=ALU.mult, op1=ALU.mult), [acc, gbt], [acc])
                ld(Rt[n], Rt[n].a, acc.a, [acc])
            fw.barrier()
            chk()

        outproj_and_moe(1, cv_w_out, [(Xt[n], Xt[n].a) for n in range(NC)], True)

        with ExitStack() as st:
            Gfin = sb(st, "Gfin", [128, D])
            ld(Gfin, Gfin.a, g_final.a.to_broadcast((128, D)), [g_final])
            bufs = {"xt": [sb(st, "xt%d" % i, [128, D]) for i in range(2)], "i": 0, "junk": sb(st, "junk", [128, D]),
                    "ssq": sb(st, "ssq", [128, 1]), "rstd": sb(st, "rstd", [128, 1])}
            ot = [sb(st, "ot%d" % i, [128, D]) for i in range(2)]
            for n in range(NC):
                jk = norm_mod_tile(st, bufs, Xt[n], Xt[n].a, Gfin, None)
                fw.op("act", lambda: A.copy(out=ot[n % 2].a, in_=jk.a), [jk], [ot[n % 2]])
                ld(out, out.a[n * 128:(n + 1) * 128, :], ot[n % 2].a, [ot[n % 2]], k="sp")
            fw.barrier()
            chk()
    return nc


def rope_tables(pos):
    row = (pos // GRID_W).astype(np.float32)
    col = (pos % GRID_W).astype(np.float32)
    nf = HD // 4
    inv = (10000.0 ** (-np.arange(nf, dtype=np.float32) / nf)).astype(np.float32)
    ang = np.concatenate([row[:, None] * inv, col[:, None] * inv], axis=-1).astype(np.float32)
    return np.cos(ang).astype(np.float32), np.sin(ang).astype(np.float32)


def make_consts(NT):
    NC = NT // 128
    m = np.arange(128, dtype=np.float32)
    ident = np.eye(128, dtype=np.float32)
    R1 = np.maximum(m[None, :] - m[:, None], 0)
    R2 = np.maximum(m[:, None] - m[None, :], 0)
    cols = np.stack([127 - m, m, m + 1, 128 - m, np.full(128, 128.0, np.float32),
                     (np.arange(128) % GRID_W != 0).astype(np.float32),
                     (np.arange(128) % GRID_W != GRID_W - 1).astype(np.float32), np.zeros(128, np.float32)], axis=1)
    et = [128 * c + m for c in range(NC)] + [NT + 128 * c + m for c in range(2)] + [255 - 128 * c - m for c in range(2)]
    et = np.stack(et, axis=1)
    tri = (m[:, None] < m[None, :]).astype(np.float32)
    ones = np.ones((128, 128), np.float32)
    base1 = m[:, None] * 8 + np.arange(8, dtype=np.float32)[None, :]
    base2 = np.arange(8, dtype=np.float32)[None, :] * 128 + m[:, None]
    kv = np.broadcast_to(np.arange(2 * NC + 16, dtype=np.float32)[None, :], (128, 2 * NC + 16))
    return np.concatenate([ident, R1, R2, cols, et, tri, ones, base1, base2, kv], axis=1).astype(np.float32)


def prepare_inputs(inp, T):
    B = inp["x"].shape[0]
    NT = T // 2
    qs = np.float32(HD ** -0.5)
    cst = make_consts(NT)
    maps = []
    shared = {
        "w_mod": inp["w_mod"], "b_mod": inp["b_mod"], "g_mix": inp["g_mix"], "g_ffn": inp["g_ffn"],
        "g_final": inp["g_final"][None, :], "ab_w_in": inp["ab_w_in"][0], "ab_w_out": inp["ab_w_out"][0],
        "cv_w_in": inp["cv_w_in"][0], "cv_w_out": inp["cv_w_out"][0], "conv_b": inp["cv_conv_b"],
        "cst": cst,
        "wr": np.concatenate([inp["moe_w_r1"], inp["moe_w_r2"].transpose(0, 2, 1, 3).reshape(2, D, 16)], axis=2),
        "br": np.concatenate([inp["moe_b_r1"], inp["moe_b_r2"].reshape(2, 16)], axis=1),
    }
    for l in range(2):
        shared["moe_w1_%d" % l] = np.ascontiguousarray(inp["moe_w1"][l].reshape(16, 16, 128, DE).transpose(0, 2, 1, 3).reshape(16384, D))
        shared["moe_w3_%d" % l] = np.ascontiguousarray(inp["moe_w3"][l].reshape(16, 16, 128, DE).transpose(0, 2, 1, 3).reshape(16384, D))
        shared["moe_w2_%d" % l] = inp["moe_w2"][l].reshape(16384, D)
    shared = {k: np.ascontiguousarray(v, dtype=np.float32) for k, v in shared.items()}
    for b in range(B):
        for h in range(2):
            pos_all = np.arange(T)
            if h == 0:
                own = pos_all[:NT]; forg = pos_all[NT:]
                ctxl = inp["ctx"][b]
                dlv = inp["ret_decay_logit"][0].reshape(1, 16)
                ws = inp["sgu_w_s"][0]; bs = inp["sgu_b_s"][0]
                cw = inp["cv_conv_w"][0]
            else:
                own = pos_all[::-1][:NT]; forg = pos_all[:NT][::-1]
                ctxl = inp["ctx"][b][::-1]
                dlv = inp["ret_decay_logit"][0][::-1].reshape(1, 16)
                ws = inp["sgu_w_s"][0][:, ::-1, ::-1]; bs = inp["sgu_b_s"][0][:, ::-1]
                cw = inp["cv_conv_w"][0][::-1]
            co, so = rope_tables(own)
            cf, sf = rope_tables(forg)
            cT = np.stack([inp["c"][b].reshape(16, 128).T, inp["c_ctx"].reshape(16, 128).T], axis=2)
            m = dict(shared)
            m.update({
                "x_own": inp["x"][b][own], "x_for": inp["x"][b][forg], "ctx_l": ctxl, "cT": cT, "dl": dlv,
                "wsT": ws.transpose(0, 2, 1), "bsT": bs.T,
                "rope": np.stack([co * qs, so * qs, co, so, cf, sf]), "conv_w": cw,
            })
            maps.append({k: np.ascontiguousarray(v, dtype=np.float32) for k, v in m.items()})
    return maps


def kernel(**inputs):
    inp = {k: np.asarray(v) for k, v in inputs.items()}
    B, T, _ = inp["x"].shape
    NT = T // 2
    maps = prepare_inputs(inp, T)
    nc = build(NT)
    res = run_bass_kernel_spmd(nc, maps, core_ids=list(range(len(maps))))
    out = np.empty((B, T, D), np.float32)
    for b in range(B):
        out[b, :NT] = res.results[2 * b]["out"]
        out[b, NT:] = res.results[2 * b + 1]["out"][::-1]
    return out
```

```python
from contextlib import ExitStack
import numpy as np
import concourse.bass as bass
import concourse.mybir as mybir
from concourse.bass_utils import run_bass_kernel_spmd

F32 = mybir.dt.float32
BF16 = mybir.dt.bfloat16
I32 = mybir.dt.int32
AF = mybir.ActivationFunctionType
ALU = mybir.AluOpType

D = 2048
EPS = 1e-6
HD = 128
NH = 8
CTX = 256
GRID_W = 64
NEXP = 16
DE = 1024


class Buf:
    def __init__(self, ap, name):
        self.a = ap
        self.name = name
        self.last_write = None
        self.reads = []
        self.dsem = None
        self.dcount = 0


class FW:
    def __init__(self, nc):
        self.nc = nc
        self.engs = {"pe": nc.tensor, "act": nc.scalar, "dve": nc.vector, "pool": nc.gpsimd, "sp": nc.sync}
        self.sems = {}
        self.cnt = {}
        self.dcounts = {}
        self.waited = {k: {} for k in self.engs}
        for k in self.engs:
            self.sems[k] = nc.alloc_semaphore("s_" + k)
            self.cnt[k] = 0
        self.nbuf = 0
        self.free_dsems = []
        self.dbufs = []

    def _deps(self, reads, writes):
        deps = []
        for b in reads:
            if b.last_write is not None:
                deps.append(b.last_write)
        for b in writes:
            if b.last_write is not None:
                deps.append(b.last_write)
            deps.extend(b.reads)
        return deps

    def _emit_waits(self, ek, deps):
        eng = self.engs[ek]
        need = {}
        for (sk, v) in deps:
            if v > need.get(sk, 0):
                need[sk] = v
        for sk, v in need.items():
            if ek == "pe" and sk == "pe":
                continue
            if self.waited[ek].get(sk, 0) >= v:
                continue
            self.waited[ek][sk] = v
            eng.wait_ge(self.sems[sk], v)

    @staticmethod
    def _compact(reads):
        m = {}
        for sk, v in reads:
            if v > m.get(sk, 0):
                m[sk] = v
        return list(m.items())

    def op(self, ek, fn, reads=(), writes=()):
        self._emit_waits(ek, self._deps(reads, writes))
        ins = fn()
        self.cnt[ek] += 1
        ins.then_inc(self.sems[ek], 1)
        tok = (ek, self.cnt[ek])
        for b in writes:
            b.last_write = tok
            b.reads = []
        for b in reads:
            if b not in writes:
                b.reads.append(tok)
                if len(b.reads) > 8:
                    b.reads = self._compact(b.reads)
        return ins

    def dma(self, qk, fn, dst, srcs=()):
        reads = list(srcs)
        writes = [dst]
        self._emit_waits(qk, self._deps(reads, writes))
        ins = fn()
        if dst.dsem is None:
            if self.free_dsems:
                key = self.free_dsems.pop()
                dst.dcount = self.dcounts[key]
            else:
                key = "d%d" % self.nbuf
                self.nbuf += 1
                self.sems[key] = self.nc.alloc_semaphore(key)
            dst.dsem = key
            self.dbufs.append(dst)
        key = dst.dsem
        dst.dcount += 16
        self.dcounts[key] = dst.dcount
        ins.then_inc(self.sems[key], 16)
        tok = (key, dst.dcount)
        dst.last_write = tok
        dst.reads = []
        for b in reads:
            b.reads.append(tok)
            if len(b.reads) > 8:
                b.reads = self._compact(b.reads)
        return ins

    def barrier(self):
        deps = [(k, self.cnt[k]) for k in self.engs if self.cnt[k] > 0]
        deps += list(self.dcounts.items())
        for ek in self.engs:
            self._emit_waits(ek, deps)
        for b in self.dbufs:
            self.free_dsems.append(b.dsem)
            b.dsem = None
        self.dbufs = []


class _Stop(Exception):
    pass


def build(NT, stop=99):
    try:
        return _build(NT, stop)
    except _Stop as e:
        return e.args[0]


def _build(NT, stop):
    NC = NT // 128
    stage = [0]

    import os
    substop = int(os.environ.get("KSUB", "0"))

    def sub(k):
        if stage[0] + 1 == stop and substop == k:
            fw.barrier()
            raise _Stop(nc)

    def chk():
        stage[0] += 1
        if stage[0] >= stop:
            raise _Stop(nc)
    KC = D // 128
    nc = bass.Bass("TRN2", target_bir_lowering=False)
    fw = FW(nc)
    V, A, G, T, S = nc.vector, nc.scalar, nc.gpsimd, nc.tensor, nc.sync

    def ein(name, shape):
        return Buf(nc.dram_tensor(name, shape, F32, kind="ExternalInput").ap(), name)

    def dint(name, shape, dt=F32):
        return Buf(nc.dram_tensor(name, shape, dt, kind="Internal").ap(), name)

    x_own = ein("x_own", [NT, D]); x_for = ein("x_for", [NT, D]); ctx_l = ein("ctx_l", [CTX, D])
    cT = ein("cT", [128, KC, 2])
    w_mod = ein("w_mod", [2, D, 6 * D]); b_mod = ein("b_mod", [2, 6 * D])
    g_mix = ein("g_mix", [2, D]); g_ffn = ein("g_ffn", [2, D]); g_final = ein("g_final", [1, D])
    ab_w_in = ein("ab_w_in", [D, 6144]); ab_w_out = ein("ab_w_out", [D, D])
    dl = ein("dl", [1, 16])
    wsT = ein("wsT", [8, 128, 128]); bsT = ein("bsT", [128, 8])
    rope = ein("rope", [6, NT, 64])
    cv_w_in = ein("cv_w_in", [D, 6144]); cv_w_out = ein("cv_w_out", [D, D])
    conv_w = ein("conv_w", [3, D]); conv_b = ein("conv_b", [1, D])
    wr = ein("wr", [2, D, 20]); br = ein("br", [2, 20])
    moe_w1 = [ein("moe_w1_%d" % l, [16384, D]) for l in range(2)]
    moe_w3 = [ein("moe_w3_%d" % l, [16384, D]) for l in range(2)]
    moe_w2 = [ein("moe_w2_%d" % l, [16384, D]) for l in range(2)]
    bcW = G.alloc_register("bcW"); G.reg_mov(bcW, 16383)
    bcS = G.alloc_register("bcS"); G.reg_mov(bcS, (2 * NC + 16) * 128 - 1)
    CW_ = 128 * 3 + 8 + NC + 4
    TRI = CW_; BASE = CW_ + 256; KV = BASE + 16
    CWT = KV + 2 * NC + 16
    cst = ein("cst", [128, CWT])
    out = Buf(nc.dram_tensor("out", [NT, D], F32, kind="ExternalOutput").ap(), "out")

    Xd = nc.dram_tensor("Xd", [NT, D], F32, kind="Internal").ap()
    Xt = [Buf(Xd[n * 128:(n + 1) * 128, :], "X%d" % n) for n in range(NC)]
    Zd = nc.dram_tensor("Zd", [NT + 2, 6144], F32, kind="Internal").ap()
    Zt = [Buf(Zd[1 + n * 128:1 + (n + 1) * 128, :], "Z%d" % n) for n in range(NC)]
    Zpad = Buf(Zd[0:1, :], "Zpad")
    Zfd = nc.dram_tensor("Zfd", [NT, 2048], F32, kind="Internal").ap()
    Zft = [Buf(Zfd[n * 128:(n + 1) * 128, :], "Zf%d" % n) for n in range(NC)]
    Zcd = nc.dram_tensor("Zcd", [CTX, 2048], F32, kind="Internal").ap()
    Zct = [Buf(Zcd[n * 128:(n + 1) * 128, :], "Zc%d" % n) for n in range(2)]
    Rd = nc.dram_tensor("Rd", [NT, D], F32, kind="Internal").ap()
    Rt = [Buf(Rd[n * 128:(n + 1) * 128, :], "R%d" % n) for n in range(NC)]
    NS_ = 2 * NC + 16
    FS = Buf(nc.dram_tensor("FSd", [NS_ * 128, D], BF16, kind="Internal").ap(), "FS")
    YS = Buf(nc.dram_tensor("YSd", [NS_ * 128, D], F32, kind="Internal").ap(), "YS")
    MODd = nc.dram_tensor("MODd", [2, 2, 6 * D], F32, kind="Internal").ap()
    MOD = Buf(MODd, "MOD")

    dq = ["sp", "act"]
    dqi = [0]

    def q():
        dqi[0] ^= 1
        return dq[dqi[0]]

    def qeng(k):
        return {"sp": S, "act": A, "pool": G}[k]

    def ld(dst, dst_ap, src_ap, srcs, k=None):
        k = k or q()
        fw.dma(k, lambda: qeng(k).dma_start(out=dst_ap, in_=src_ap), dst, srcs)

    with ExitStack() as glob:
        uid = [0]

        def sb(stack, name, shape, dt=F32):
            uid[0] += 1
            t = stack.enter_context(nc.sbuf_tensor("%s_%d" % (name, uid[0]), shape, dt))
            return Buf(t[:], name)

        def ps(stack, name, shape, dt=F32):
            uid[0] += 1
            t = stack.enter_context(nc.psum_tensor("%s_%d" % (name, uid[0]), shape, dt))
            return Buf(t[:], name)

        cs = sb(glob, "cs", [128, CWT])
        ld(cs, cs.a, cst.a, [cst])
        identf = cs.a[:, 0:128]
        R1 = cs.a[:, 128:256]
        R2 = cs.a[:, 256:384]
        cc = 384
        ET = 392
        identb = sb(glob, "identb", [128, 128], BF16)
        fw.op("dve", lambda: V.tensor_copy(out=identb.a, in_=identf), [cs], [identb])
        identF = sb(glob, "identF", [128, 128])
        fw.op("dve", lambda: V.tensor_copy(out=identF.a, in_=identf), [cs], [identF])
        Zpad2 = Buf(Zd[NT + 1:NT + 2, :], "Zpad2")
        with ExitStack() as st:
            zero = sb(st, "zero", [1, 6144])
            fw.op("dve", lambda: V.memset(zero.a, 0.0), [], [zero])
            ld(Zpad, Zd[0:1, :], zero.a, [zero])
            ld(Zpad2, Zd[NT + 1:NT + 2, :], zero.a, [zero])
            fw.barrier()
            chk()

        with ExitStack() as st:
            scT = sb(st, "scT", [128, KC, 2])
            ld(scT, scT.a, cT.a, [cT])
            fw.op("act", lambda: A.activation(out=scT.a, in_=scT.a, func=AF.Silu), [scT], [scT])
            wm = [sb(st, "wm%d" % i, [128, KC, 512]) for i in range(2)]
            bm = [sb(st, "bm%d" % i, [2, 512]) for i in range(2)]
            mo = [sb(st, "mo%d" % i, [2, 512]) for i in range(2)]
            pm = [ps(st, "pm%d" % i, [2, 512]) for i in range(2)]
            it = 0
            for i in range(2):
                for cb in range(24):
                    j = it % 2
                    it += 1
                    ld(wm[j], wm[j].a, w_mod.a[i, :, cb * 512:(cb + 1) * 512].rearrange("(k p) n -> p k n", p=128), [w_mod])
                    ld(bm[j], bm[j].a, b_mod.a[i:i + 1, cb * 512:(cb + 1) * 512].to_broadcast((2, 512)), [b_mod])
                    for k in range(KC):
                        fw.op("pe", lambda: T.matmul(pm[j].a, lhsT=scT.a[:, k, :], rhs=wm[j].a[:, k, :], start=(k == 0), stop=(k == KC - 1)), [scT, wm[j]], [pm[j]])
                    fw.op("dve", lambda: V.tensor_tensor(out=mo[j].a, in0=pm[j].a, in1=bm[j].a, op=ALU.add), [pm[j], bm[j]], [mo[j]])
                    ld(MOD, MODd[i, :, cb * 512:(cb + 1) * 512], mo[j].a, [mo[j]], k="sp")
            fw.barrier()
            chk()

        def modrow(i, row, j):
            return MODd[i, row:row + 1, j * D:(j + 1) * D].to_broadcast((128, D))

        def norm_mod_tile(st, bufs, src_buf, src_ap, Gt, SHt, out_bf=None, out_f=None):
            xt = bufs["xt"][bufs["i"] % 2]
            bufs["i"] += 1
            ld(xt, xt.a, src_ap, [src_buf])
            sub(8)
            junk, ssq, rstd = bufs["junk"], bufs["ssq"], bufs["rstd"]
            fw.op("act", lambda: A.activation(out=junk.a, in_=xt.a, func=AF.Square), [xt], [junk])
            fw.op("dve", lambda: V.tensor_reduce(out=ssq.a, in_=junk.a, axis=mybir.AxisListType.X, op=ALU.add), [junk], [ssq])
            fw.op("dve", lambda: V.tensor_scalar(out=ssq.a, in0=ssq.a, scalar1=1.0 / D, scalar2=EPS, op0=ALU.mult, op1=ALU.add), [ssq], [ssq])
            fw.op("act", lambda: A.activation(out=ssq.a, in_=ssq.a, func=AF.Sqrt), [ssq], [ssq])
            fw.op("dve", lambda: V.reciprocal(out=rstd.a, in_=ssq.a), [ssq], [rstd])
            fw.op("dve", lambda: V.scalar_tensor_tensor(out=junk.a, in0=xt.a, scalar=rstd.a[:, 0:1], in1=Gt.a, op0=ALU.mult, op1=ALU.mult), [xt, rstd, Gt], [junk])
            sub(9)
            if SHt is None:
                return junk
            if out_f is not None:
                fw.op("dve", lambda: V.scalar_tensor_tensor(out=out_f.a, in0=junk.a, scalar=1.0, in1=SHt.a, op0=ALU.mult, op1=ALU.add), [junk, SHt], [out_f])
                if out_bf is not None:
                    fw.op("act", lambda: A.copy(out=out_bf.a, in_=out_f.a), [out_f], [out_bf])
            else:
                fw.op("dve", lambda: V.tensor_tensor(out=out_bf.a, in0=junk.a, in1=SHt.a, op=ALU.add), [junk, SHt], [out_bf])
            return None

        gs_tmp = {}

        def make_GS(st, i, row, jsh, jsc, gvec_ap, gbuf):
            Gt = sb(st, "Gt%d%d%d" % (i, row, jsh), [128, D])
            SHt = sb(st, "SHt%d%d%d" % (i, row, jsh), [128, D])
            if "gtmp" not in gs_tmp or gs_tmp["st"] is not st:
                gs_tmp["gtmp"] = sb(st, "gtmp", [128, D]); gs_tmp["st"] = st
            tmp = gs_tmp["gtmp"]
            ld(Gt, Gt.a, modrow(i, row, jsc), [MOD])
            ld(tmp, tmp.a, gvec_ap.to_broadcast((128, D)), [gbuf])
            ld(SHt, SHt.a, modrow(i, row, jsh), [MOD])
            fw.op("dve", lambda: V.scalar_tensor_tensor(out=Gt.a, in0=Gt.a, scalar=1.0, in1=tmp.a, op0=ALU.add, op1=ALU.mult), [Gt, tmp], [Gt])
            return Gt, SHt

        def transpose_tile(src_bf, dstT, col0, pT, nk=KC):
            for k0 in range(0, nk, 8):
                p = pT[(k0 // 8) % len(pT)]
                for k in range(k0, min(nk, k0 + 8)):
                    fw.op("pe", lambda: T.transpose(out=p.a[:, k - k0, :], in_=src_bf.a[:, k * 128:(k + 1) * 128], identity=identb.a), [src_bf, identb], [p])
                n = min(nk, k0 + 8) - k0
                if (k0 // 8) % 2 == 0:
                    fw.op("act", lambda: A.copy(out=dstT.a[:, k0:k0 + n, col0:col0 + 128], in_=p.a[:, 0:n, :]), [p], [dstT])
                else:
                    fw.op("dve", lambda: V.tensor_copy(out=dstT.a[:, k0:k0 + n, col0:col0 + 128], in_=p.a[:, 0:n, :]), [p], [dstT])

        def load_w(wbuf, w_ap, wsrc, kc):
            fw.dma("pool", lambda: G.dma_start(out=wbuf.a, in_=w_ap.rearrange("(k p) n -> p k n", p=128)), wbuf, [wsrc])

        with ExitStack() as st:
            Gl, SHl = make_GS(st, 0, 0, 0, 1, g_mix.a[0:1, :], g_mix)
            Gc, SHc = make_GS(st, 0, 1, 0, 1, g_mix.a[0:1, :], g_mix)
            bufs = {"xt": [sb(st, "xt%d" % i, [128, D]) for i in range(1)] * 2, "i": 0, "junk": sb(st, "junk", [128, D]),
                    "ssq": sb(st, "ssq", [128, 1]), "rstd": sb(st, "rstd", [128, 1])}
            abf = [sb(st, "abf%d" % i, [128, D], BF16) for i in range(2)]
            aT = sb(st, "aT", [128, KC, NT], BF16)
            wb = [sb(st, "wb%d" % i, [128, KC, 512], BF16) for i in range(2)]
            osb = [sb(st, "osb%d" % i, [128, 512]) for i in range(3)]
            pT = [ps(st, "pT%d" % i, [128, 8, 128], BF16) for i in range(2)]
            pg = [ps(st, "pg%d" % i, [128, 512]) for i in range(4)]
            cnt = {"w": 0, "o": 0, "p": 0}

            def inproj(src_buf, src_ap_fn, ntiles, Gt, SHt, w_all, wsrc, cbs, zts, zcol0):
                for n in range(ntiles):
                    ab = abf[n % 2]
                    norm_mod_tile(st, bufs, src_buf, src_ap_fn(n), Gt, SHt, out_bf=ab)
                    transpose_tile(ab, aT, n * 128, pT)
                for cb in cbs:
                    w = wb[cnt["w"] % 2]
                    cnt["w"] += 1
                    load_w(w, w_all.a[:, cb * 512:(cb + 1) * 512], wsrc, KC)
                    for n in range(ntiles):
                        p = pg[cnt["p"] % 4]
                        cnt["p"] += 1
                        for k in range(KC):
                            fw.op("pe", lambda: T.matmul(p.a, lhsT=aT.a[:, k, n * 128:(n + 1) * 128], rhs=w.a[:, k, :], start=(k == 0), stop=(k == KC - 1)), [aT, w], [p])
                        o = osb[cnt["o"] % 3]
                        cnt["o"] += 1
                        if cnt["o"] % 2:
                            fw.op("act", lambda: A.copy(out=o.a, in_=p.a), [p], [o])
                        else:
                            fw.op("dve", lambda: V.tensor_copy(out=o.a, in_=p.a), [p], [o])
                        c0 = cb * 512 - zcol0
                        ld(zts[n], zts[n].a[:, c0:c0 + 512], o.a, [o])

            kvb = [2, 3, 4, 5]
            inproj(x_for, lambda n: x_for.a[n * 128:(n + 1) * 128, :], NC, Gl, SHl, ab_w_in, ab_w_in, kvb, Zft, 1024)
            inproj(ctx_l, lambda n: ctx_l.a[n * 128:(n + 1) * 128, :], 2, Gc, SHc, ab_w_in, ab_w_in, kvb, Zct, 1024)
            inproj(x_own, lambda n: x_own.a[n * 128:(n + 1) * 128, :], NC, Gl, SHl, ab_w_in, ab_w_in, list(range(12)), Zt, 0)
            fw.barrier()
            chk()

        with ExitStack() as st:
            lg = sb(st, "lg", [128, 16])
            t1 = sb(st, "t1", [128, 16]); t2 = sb(st, "t2", [128, 16]); t3 = sb(st, "t3", [128, 16]); t4 = sb(st, "t4", [128, 16])
            ld(lg, lg.a, dl.a.to_broadcast((128, 16)), [dl])
            fw.op("act", lambda: A.activation(out=t1.a, in_=lg.a, func=AF.Exp, scale=-1.0), [lg], [t1])
            fw.op("dve", lambda: V.tensor_scalar(out=t2.a, in0=t1.a, scalar1=-0.25, scalar2=1.0 / 3.0, op0=ALU.mult, op1=ALU.add), [t1], [t2])
            fw.op("dve", lambda: V.tensor_tensor(out=t2.a, in0=t2.a, in1=t1.a, op=ALU.mult), [t2, t1], [t2])
            fw.op("dve", lambda: V.tensor_scalar(out=t2.a, in0=t2.a, scalar1=-0.5, scalar2=None, op0=ALU.add), [t2], [t2])
            fw.op("dve", lambda: V.tensor_tensor(out=t2.a, in0=t2.a, in1=t1.a, op=ALU.mult), [t2, t1], [t2])
            fw.op("dve", lambda: V.tensor_scalar(out=t2.a, in0=t2.a, scalar1=1.0, scalar2=None, op0=ALU.add), [t2], [t2])
            fw.op("dve", lambda: V.tensor_tensor(out=t2.a, in0=t2.a, in1=t1.a, op=ALU.mult), [t2, t1], [t2])
            fw.op("dve", lambda: V.tensor_scalar(out=t3.a, in0=t1.a, scalar1=1.0, scalar2=None, op0=ALU.add), [t1], [t3])
            fw.op("act", lambda: A.activation(out=t3.a, in_=t3.a, func=AF.Ln), [t3], [t3])
            fw.op("dve", lambda: V.tensor_scalar(out=t4.a, in0=t1.a, scalar1=0.1, scalar2=None, op0=ALU.is_lt), [t1], [t4])
            fw.op("dve", lambda: V.tensor_tensor(out=t2.a, in0=t2.a, in1=t3.a, op=ALU.subtract), [t2, t3], [t2])
            fw.op("dve", lambda: V.tensor_tensor(out=t2.a, in0=t2.a, in1=t4.a, op=ALU.mult), [t2, t4], [t2])
            fw.op("dve", lambda: V.tensor_tensor(out=t2.a, in0=t2.a, in1=t3.a, op=ALU.add), [t2, t3], [t2])
            fw.op("dve", lambda: V.tensor_scalar(out=lg.a, in0=t2.a, scalar1=-1.0, scalar2=None, op0=ALU.mult), [t2], [lg])

            ropeT = sb(st, "ropeT", [128, 6, NC, 64])
            for r in range(6):
                ld(ropeT, ropeT.a[:, r, :, :], rope.a[r].rearrange("(n p) c -> p n c", p=128), [rope])
            NE = NC + 4
            ew = sb(st, "ew", [128, NE]); ewB = sb(st, "ewB", [128, NE])
            dcol = sb(st, "dcol", [128, 8])
            Dh = sb(st, "Dh", [128, 128]); dtmp = sb(st, "dtmp", [128, 128])
            qs = [sb(st, "qs%d" % i, [128, NC, 128]) for i in range(1)] * 2
            ks = [sb(st, "ks%d" % i, [128, NC, 128]) for i in range(1)] * 2
            vs = [sb(st, "vs%d" % i, [128, NC, 128]) for i in range(1)] * 2
            gs = [sb(st, "gs%d" % i, [128, NC, 128]) for i in range(1)] * 2
            kf = sb(st, "kf", [128, NC + 2, 128]); vf = sb(st, "vf", [128, NC + 2, 128])
            rq = sb(st, "rq", [128, NC, 128]); rk = sb(st, "rk", [128, NC, 128]); rtmp = sb(st, "rtmp", [128, NC, 64])
            qb = sb(st, "qb", [128, NC, 128], BF16); kb = sb(st, "kb", [128, NC, 128], BF16)
            vb = sb(st, "vb", [128, NC, 128], BF16); vA = sb(st, "vA", [128, NC, 128], BF16); vB = sb(st, "vB", [128, NC, 128], BF16)
            kfb = sb(st, "kfb", [128, NC + 2, 128], BF16); vfB = sb(st, "vfB", [128, NC + 2, 128], BF16); vcA = sb(st, "vcA", [128, 2, 128], BF16)
            SA = sb(st, "SA", [128, NC + 1, 128]); SB = sb(st, "SB", [128, NC + 1, 128])
            SAb = sb(st, "SAb", [128, NC + 1, 128], BF16); SBb = sb(st, "SBb", [128, NC + 1, 128], BF16)
            qT = [sb(st, "qT%d" % i, [128, 2, 128], BF16) for i in range(2)]
            ATb = [sb(st, "ATb%d" % i, [128, 128], BF16) for i in range(2)]
            ysb = [sb(st, "ysb%d" % i, [128, 128]) for i in range(2)]
            stt_ = sb(st, "bnst", [128, 6]); mv = sb(st, "mv", [128, 2]); rs = sb(st, "rs", [128, 1])
            sg = sb(st, "sg", [128, 128])
            rout = [sb(st, "rout%d" % i, [128, NC, 128]) for i in range(2)]
            pS = [ps(st, "pS%d" % i, [128, 128]) for i in range(2)]
            pU = [ps(st, "pU%d" % i, [128, 2, 128]) for i in range(2)]
            pTq = ps(st, "pTq", [128, 2, 128], BF16)
            pSc = ps(st, "pSc", [128, 128])
            pY = ps(st, "pY", [128, 3, 128])

            def rope_apply(dst, src, ci, si, nchunk):
                x1 = src.a[:, 0:nchunk, 0:64]; x2 = src.a[:, 0:nchunk, 64:128]
                cth = ropeT.a[:, ci, 0:nchunk, :]; sth = ropeT.a[:, si, 0:nchunk, :]
                tm = rtmp.a[:, 0:nchunk, :]
                fw.op("dve", lambda: V.tensor_tensor(out=dst.a[:, 0:nchunk, 0:64], in0=x1, in1=cth, op=ALU.mult), [src, ropeT], [dst])
                fw.op("dve", lambda: V.tensor_tensor(out=tm, in0=x2, in1=sth, op=ALU.mult), [src, ropeT], [rtmp])
                fw.op("dve", lambda: V.tensor_tensor(out=dst.a[:, 0:nchunk, 0:64], in0=dst.a[:, 0:nchunk, 0:64], in1=tm, op=ALU.subtract), [dst, rtmp], [dst])
                fw.op("dve", lambda: V.tensor_tensor(out=dst.a[:, 0:nchunk, 64:128], in0=x1, in1=sth, op=ALU.mult), [src, ropeT], [dst])
                fw.op("dve", lambda: V.tensor_tensor(out=tm, in0=x2, in1=cth, op=ALU.mult), [src, ropeT], [rtmp])
                fw.op("dve", lambda: V.tensor_tensor(out=dst.a[:, 0:nchunk, 64:128], in0=dst.a[:, 0:nchunk, 64:128], in1=tm, op=ALU.add), [dst, rtmp], [dst])

            for h in range(NH):
                j = h % 2
                c0 = h * 128
                ld(qs[j], qs[j].a, Zd[1:NT + 1, c0:c0 + 128].rearrange("(n p) c -> p n c", p=128), Zt)
                ld(ks[j], ks[j].a, Zd[1:NT + 1, 1024 + c0:1024 + c0 + 128].rearrange("(n p) c -> p n c", p=128), Zt)
                ld(vs[j], vs[j].a, Zd[1:NT + 1, 2048 + c0:2048 + c0 + 128].rearrange("(n p) c -> p n c", p=128), Zt)
                ld(gs[j], gs[j].a, Zd[1:NT + 1, 3072 + c0:3072 + c0 + 128].rearrange("(n p) c -> p n c", p=128), Zt)
                ld(kf, kf.a[:, 0:NC, :], Zfd[:, c0:c0 + 128].rearrange("(n p) c -> p n c", p=128), Zft)
                ld(kf, kf.a[:, NC:NC + 2, :], Zcd[:, c0:c0 + 128].rearrange("(n p) c -> p n c", p=128), Zct)
                ld(vf, vf.a[:, 0:NC, :], Zfd[:, 1024 + c0:1024 + c0 + 128].rearrange("(n p) c -> p n c", p=128), Zft)
                ld(vf, vf.a[:, NC:NC + 2, :], Zcd[:, 1024 + c0:1024 + c0 + 128].rearrange("(n p) c -> p n c", p=128), Zct)
                lgA = lg.a[:, h:h + 1]; lgB = lg.a[:, 8 + h:9 + h]
                fw.op("dve", lambda: V.tensor_scalar(out=dcol.a[:, 0:1], in0=cs.a[:, cc + 0:cc + 1], scalar1=lgA, scalar2=None, op0=ALU.mult), [cs, lg], [dcol])
                fw.op("dve", lambda: V.tensor_scalar(out=dcol.a[:, 1:2], in0=cs.a[:, cc + 1:cc + 2], scalar1=lgB, scalar2=None, op0=ALU.mult), [cs, lg], [dcol])
                fw.op("dve", lambda: V.tensor_scalar(out=dcol.a[:, 2:3], in0=cs.a[:, cc + 2:cc + 3], scalar1=lgA, scalar2=None, op0=ALU.mult), [cs, lg], [dcol])
                fw.op("dve", lambda: V.tensor_scalar(out=dcol.a[:, 3:4], in0=cs.a[:, cc + 3:cc + 4], scalar1=lgB, scalar2=None, op0=ALU.mult), [cs, lg], [dcol])
                fw.op("dve", lambda: V.tensor_scalar(out=dcol.a[:, 4:5], in0=cs.a[:, cc + 4:cc + 5], scalar1=lgA, scalar2=None, op0=ALU.mult), [cs, lg], [dcol])
                fw.op("dve", lambda: V.tensor_scalar(out=dcol.a[:, 5:6], in0=cs.a[:, cc + 4:cc + 5], scalar1=lgB, scalar2=None, op0=ALU.mult), [cs, lg], [dcol])
                fw.op("act", lambda: A.activation(out=dcol.a[:, 0:6], in_=dcol.a[:, 0:6], func=AF.Exp), [dcol], [dcol])
                fw.op("dve", lambda: V.tensor_scalar(out=ewB.a[:, 0:NC + 2], in0=cs.a[:, ET:ET + NC + 2], scalar1=lgB, scalar2=None, op0=ALU.mult), [cs, lg], [ewB])
                fw.op("dve", lambda: V.tensor_scalar(out=ewB.a[:, NC + 2:NC + 4], in0=cs.a[:, ET + NC + 2:ET + NC + 4], scalar1=lgA, scalar2=None, op0=ALU.mult), [cs, lg], [ewB])
                fw.op("act", lambda: A.activation(out=ew.a, in_=ewB.a, func=AF.Exp), [ewB], [ew])
                fw.op("dve", lambda: V.tensor_scalar(out=dtmp.a, in0=R1, scalar1=lgA, scalar2=None, op0=ALU.mult), [cs, lg], [dtmp])
                fw.op("dve", lambda: V.scalar_tensor_tensor(out=dtmp.a, in0=R2, scalar=lgB, in1=dtmp.a, op0=ALU.mult, op1=ALU.add), [cs, lg, dtmp], [dtmp])
                fw.op("act", lambda: A.activation(out=Dh.a, in_=dtmp.a, func=AF.Exp), [dtmp], [Dh])
                rope_apply(rq, qs[j], 0, 1, NC)
                rope_apply(rk, ks[j], 2, 3, NC)
                fw.op("act", lambda: A.copy(out=qb.a, in_=rq.a), [rq], [qb])
                fw.op("act", lambda: A.copy(out=kb.a, in_=rk.a), [rk], [kb])
                fw.op("act", lambda: A.copy(out=vb.a, in_=vs[j].a), [vs[j]], [vb])
                fw.op("dve", lambda: V.tensor_scalar(out=vA.a, in0=vs[j].a, scalar1=dcol.a[:, 0:1], scalar2=None, op0=ALU.mult), [vs[j], dcol], [vA])
                fw.op("dve", lambda: V.tensor_scalar(out=vB.a, in0=vs[j].a, scalar1=dcol.a[:, 1:2], scalar2=None, op0=ALU.mult), [vs[j], dcol], [vB])
                rope_apply(rk, kf, 4, 5, NC)
                fw.op("act", lambda: A.copy(out=kfb.a[:, 0:NC, :], in_=rk.a), [rk], [kfb])
                fw.op("act", lambda: A.copy(out=kfb.a[:, NC:NC + 2, :], in_=kf.a[:, NC:NC + 2, :]), [kf], [kfb])
                for c in range(NC + 2):
                    fw.op("dve", lambda: V.tensor_scalar(out=vfB.a[:, c, :], in0=vf.a[:, c, :], scalar1=ew.a[:, c:c + 1], scalar2=None, op0=ALU.mult), [vf, ew], [vfB])
                for c in range(2):
                    fw.op("dve", lambda: V.tensor_scalar(out=vcA.a[:, c, :], in0=vf.a[:, NC + c, :], scalar1=ew.a[:, NC + 2 + c:NC + 3 + c], scalar2=None, op0=ALU.mult), [vf, ew], [vcA])
                for c in range(2):
                    fw.op("pe", lambda: T.matmul(pS[0].a, lhsT=kfb.a[:, NC + c, :], rhs=vcA.a[:, c, :], start=(c == 0), stop=(c == 1)), [kfb, vcA], [pS[0]])
                fw.op("act", lambda: A.copy(out=SA.a[:, 0, :], in_=pS[0].a), [pS[0]], [SA])
                for c in range(NC + 2):
                    fw.op("pe", lambda: T.matmul(pS[1].a, lhsT=kfb.a[:, c, :], rhs=vfB.a[:, c, :], start=(c == 0), stop=(c == NC + 1)), [kfb, vfB], [pS[1]])
                fw.op("act", lambda: A.copy(out=SB.a[:, NC, :], in_=pS[1].a), [pS[1]], [SB])
                for n in range(NC):
                    p = pU[n % 2]
                    fw.op("pe", lambda: T.matmul(p.a[:, 0, :], lhsT=kb.a[:, n, :], rhs=vA.a[:, n, :], start=True, stop=True), [kb, vA], [p])
                    fw.op("dve", lambda: V.scalar_tensor_tensor(out=SA.a[:, n + 1, :], in0=SA.a[:, n, :], scalar=dcol.a[:, 4:5], in1=p.a[:, 0, :], op0=ALU.mult, op1=ALU.add), [SA, dcol, p], [SA])
                for n in range(NC - 1, -1, -1):
                    p = pU[n % 2]
                    fw.op("pe", lambda: T.matmul(p.a[:, 1, :], lhsT=kb.a[:, n, :], rhs=vB.a[:, n, :], start=True, stop=True), [kb, vB], [p])
                    fw.op("dve", lambda: V.scalar_tensor_tensor(out=SB.a[:, n, :], in0=SB.a[:, n + 1, :], scalar=dcol.a[:, 5:6], in1=p.a[:, 1, :], op0=ALU.mult, op1=ALU.add), [SB, dcol, p], [SB])
                fw.op("act", lambda: A.copy(out=SAb.a, in_=SA.a), [SA], [SAb])
                fw.op("act", lambda: A.copy(out=SBb.a, in_=SB.a), [SB], [SBb])
                ro = rout[j]
                for n in range(NC):
                    qt = qT[n % 2]; at = ATb[n % 2]; y = ysb[n % 2]
                    fw.op("pe", lambda: T.transpose(out=pTq.a[:, 0, :], in_=qb.a[:, n, :], identity=identb.a), [qb, identb], [pTq])
                    fw.op("pe", lambda: T.transpose(out=pTq.a[:, 1, :], in_=kb.a[:, n, :], identity=identb.a), [kb, identb], [pTq])
                    fw.op("act", lambda: A.copy(out=qt.a, in_=pTq.a), [pTq], [qt])
                    fw.op("pe", lambda: T.matmul(pSc.a, lhsT=qt.a[:, 1, :], rhs=qt.a[:, 0, :], start=True, stop=True), [qt], [pSc])
                    fw.op("dve", lambda: V.tensor_tensor(out=at.a, in0=pSc.a, in1=Dh.a, op=ALU.mult), [pSc, Dh], [at])
                    fw.op("pe", lambda: T.matmul(pY.a[:, 0, :], lhsT=at.a, rhs=vb.a[:, n, :], start=True, stop=True), [at, vb], [pY])
                    fw.op("pe", lambda: T.matmul(pY.a[:, 1, :], lhsT=qt.a[:, 0, :], rhs=SAb.a[:, n, :], start=True, stop=True), [qt, SAb], [pY])
                    fw.op("pe", lambda: T.matmul(pY.a[:, 2, :], lhsT=qt.a[:, 0, :], rhs=SBb.a[:, n + 1, :], start=True, stop=True), [qt, SBb], [pY])
                    fw.op("act", lambda: A.copy(out=y.a, in_=pY.a[:, 0, :]), [pY], [y])
                    fw.op("dve", lambda: V.scalar_tensor_tensor(out=y.a, in0=pY.a[:, 1, :], scalar=dcol.a[:, 2:3], in1=y.a, op0=ALU.mult, op1=ALU.add), [pY, dcol, y], [y])
                    fw.op("dve", lambda: V.scalar_tensor_tensor(out=y.a, in0=pY.a[:, 2, :], scalar=dcol.a[:, 3:4], in1=y.a, op0=ALU.mult, op1=ALU.add), [pY, dcol, y], [y])
                    fw.op("dve", lambda: V.bn_stats(out=stt_.a, in_=y.a), [y], [stt_])
                    fw.op("dve", lambda: V.bn_aggr(out=mv.a, in_=stt_.a), [stt_], [mv])
                    fw.op("dve", lambda: V.tensor_scalar(out=rs.a, in0=mv.a[:, 1:2], scalar1=EPS, scalar2=None, op0=ALU.add), [mv], [rs])
                    fw.op("act", lambda: A.activation(out=rs.a, in_=rs.a, func=AF.Sqrt), [rs], [rs])
                    fw.op("dve", lambda: V.reciprocal(out=rs.a, in_=rs.a), [rs], [rs])
                    fw.op("dve", lambda: V.tensor_scalar(out=y.a, in0=y.a, scalar1=mv.a[:, 0:1], scalar2=rs.a[:, 0:1], op0=ALU.subtract, op1=ALU.mult), [y, mv, rs], [y])
                    fw.op("act", lambda: A.activation(out=sg.a, in_=gs[j].a[:, n, :], func=AF.Silu), [gs[j]], [sg])
                    fw.op("dve", lambda: V.tensor_tensor(out=ro.a[:, n, :], in0=y.a, in1=sg.a, op=ALU.mult), [y, sg], [ro])
                for n in range(NC):
                    ld(Rt[n], Rt[n].a[:, c0:c0 + 128], ro.a[:, n, :], [ro])
            fw.barrier()
            chk()

        def gelu(dst, src, tmp, reads):
            fw.op("dve", lambda: V.tensor_tensor(out=tmp.a, in0=src.a, in1=src.a, op=ALU.mult), reads, [tmp])
            fw.op("dve", lambda: V.tensor_scalar(out=tmp.a, in0=tmp.a, scalar1=0.044715, scalar2=1.0, op0=ALU.mult, op1=ALU.add), [tmp], [tmp])
            fw.op("dve", lambda: V.tensor_tensor(out=tmp.a, in0=tmp.a, in1=src.a, op=ALU.mult), [tmp] + reads, [tmp])
            fw.op("act", lambda: A.activation(out=tmp.a, in_=tmp.a, func=AF.Sigmoid, scale=1.5957691216057308), [tmp], [tmp])
            fw.op("dve", lambda: V.tensor_tensor(out=dst.a, in0=tmp.a, in1=src.a, op=ALU.mult), [tmp] + reads, [dst])

        with ExitStack() as st:
            us = [sb(st, "us%d" % i, [128, NC, 128]) for i in range(2)]
            vs2 = [sb(st, "vs2%d" % i, [128, NC, 128]) for i in range(2)]
            gu = sb(st, "gu", [128, NC, 128]); gv = sb(st, "gv", [128, NC, 128]); tmpg = sb(st, "tmpg", [128, NC, 128])
            vn = sb(st, "vn", [128, NC, 128], BF16)
            wsf = sb(st, "wsf", [128, 8, 128]); wsb = sb(st, "wsb", [128, 8, 128], BF16)
            bsb = sb(st, "bsb", [128, 8])
            stt2 = sb(st, "bnst2", [128, 6]); mv2 = sb(st, "mv2", [128, 2]); rs2 = sb(st, "rs2", [128, 1])
            ro2 = [sb(st, "ro2%d" % i, [128, NC, 128]) for i in range(2)]
            pG = [ps(st, "pG%d" % i, [128, 128]) for i in range(2)]
            ld(wsf, wsf.a, wsT.a.rearrange("g q p -> q g p"), [wsT])
            ld(bsb, bsb.a, bsT.a, [bsT])
            fw.op("act", lambda: A.copy(out=wsb.a, in_=wsf.a), [wsf], [wsb])
            for g in range(8):
                j = g % 2
                c0 = g * 128
                ld(us[j], us[j].a, Zd[1:NT + 1, 4096 + c0:4096 + c0 + 128].rearrange("(n p) c -> p n c", p=128), Zt)
                ld(vs2[j], vs2[j].a, Zd[1:NT + 1, 5120 + c0:5120 + c0 + 128].rearrange("(n p) c -> p n c", p=128), Zt)
                gelu(gu, us[j], tmpg, [us[j]])
                gelu(gv, vs2[j], tmpg, [vs2[j]])
                for n in range(NC):
                    fw.op("dve", lambda: V.bn_stats(out=stt2.a, in_=gv.a[:, n, :]), [gv], [stt2])
                    fw.op("dve", lambda: V.bn_aggr(out=mv2.a, in_=stt2.a), [stt2], [mv2])
                    fw.op("dve", lambda: V.tensor_scalar(out=rs2.a, in0=mv2.a[:, 1:2], scalar1=EPS, scalar2=None, op0=ALU.add), [mv2], [rs2])
                    fw.op("act", lambda: A.activation(out=rs2.a, in_=rs2.a, func=AF.Sqrt), [rs2], [rs2])
                    fw.op("dve", lambda: V.reciprocal(out=rs2.a, in_=rs2.a), [rs2], [rs2])
                    fw.op("dve", lambda: V.tensor_scalar(out=vn.a[:, n, :], in0=gv.a[:, n, :], scalar1=mv2.a[:, 0:1], scalar2=rs2.a[:, 0:1], op0=ALU.subtract, op1=ALU.mult), [gv, mv2, rs2], [vn])
                for n in range(NC):
                    p = pG[n % 2]
                    fw.op("pe", lambda: T.matmul(p.a, lhsT=wsb.a[:, g, :], rhs=vn.a[:, n, :], start=True, stop=True), [wsb, vn], [p])
                    fw.op("dve", lambda: V.scalar_tensor_tensor(out=ro2[j].a[:, n, :], in0=p.a, scalar=bsb.a[:, g:g + 1], in1=gu.a[:, n, :], op0=ALU.add, op1=ALU.mult), [p, bsb, gu], [ro2[j]])
                for n in range(NC):
                    ld(Rt[n], Rt[n].a[:, 1024 + c0:1024 + c0 + 128], ro2[j].a[:, n, :], [ro2[j]])
            fw.barrier()
            chk()

        def outproj_and_moe(i, w_out, x_src_tiles, last):
            with ExitStack() as st:
                with ExitStack() as s2:
                    GT1 = sb(s2, "GT1", [128, D])
                    ld(GT1, GT1.a, modrow(i, 0, 2), [MOD])
                    rf = [sb(s2, "rf%d" % k, [128, D]) for k in range(2)]
                    rb = [sb(s2, "rb%d" % k, [128, D], BF16) for k in range(2)]
                    wb = [sb(s2, "wo%d" % k, [128, KC, 512], BF16) for k in range(2)]
                    xin = [sb(s2, "xin%d" % k, [128, 512]) for k in range(3)]
                    pT = [ps(s2, "pT%d" % k, [128, 8, 128], BF16) for k in range(2)]
                    pg = [ps(s2, "pg%d" % k, [128, 512]) for k in range(4)]
                    rT = sb(s2, "rT", [128, KC, NT], BF16)
                    for n in range(NC):
                        ld(rf[n % 2], rf[n % 2].a, Rt[n].a, [Rt[n]])
                        fw.op("act", lambda: A.copy(out=rb[n % 2].a, in_=rf[n % 2].a), [rf[n % 2]], [rb[n % 2]])
                        transpose_tile(rb[n % 2], rT, n * 128, pT)
                    ci = 0
                    for cb in range(4):
                        w = wb[cb % 2]
                        load_w(w, w_out.a[:, cb * 512:(cb + 1) * 512], w_out, KC)
                        for n in range(NC):
                            p = pg[ci % 4]; xi = xin[ci % 3]; ci += 1
                            xb_, xap = x_src_tiles[n]
                            ld(xi, xi.a, xap[:, cb * 512:(cb + 1) * 512], [xb_])
                            for k in range(KC):
                                fw.op("pe", lambda: T.matmul(p.a, lhsT=rT.a[:, k, n * 128:(n + 1) * 128], rhs=w.a[:, k, :], start=(k == 0), stop=(k == KC - 1)), [rT, w], [p])
                            fw.op("dve", lambda: V.tensor_tensor(out=p.a, in0=p.a, in1=GT1.a[:, cb * 512:(cb + 1) * 512], op=ALU.mult), [p, GT1], [p])
                            fw.op("dve", lambda: V.tensor_tensor(out=xi.a, in0=p.a, in1=xi.a, op=ALU.add), [p, xi], [xi])
                            ld(Xt[n], Xt[n].a[:, cb * 512:(cb + 1) * 512], xi.a, [xi])
                    fw.barrier()
                    chk()
                NS = 2 * NC + 16
                MA16 = sb(st, "MA16", [128, NC, 16]); MB16 = sb(st, "MB16", [128, NC, 16]); wAB = sb(st, "wAB", [128, NC, 2])
                slotAi = sb(st, "slotAi", [128, NC], I32); slotBi = sb(st, "slotBi", [128, NC], I32)
                WIi = sb(st, "WIi", [128, NS, 16], I32)
                with ExitStack() as s2:
                    Ftok = sb(s2, "Ftok", [128, NC, D], BF16)
                    Gf, SHf = make_GS(s2, i, 0, 3, 4, g_ffn.a[i:i + 1, :], g_ffn)
                    bufs = {"xt": [sb(s2, "xt%d" % k, [128, D]) for k in range(2)], "i": 0, "junk": sb(s2, "junk", [128, D]),
                            "ssq": sb(s2, "ssq", [128, 1]), "rstd": sb(s2, "rstd", [128, 1])}
                    ff = [sb(s2, "ff%d" % k, [128, D]) for k in range(2)]
                    fTf = sb(s2, "fTf", [128, KC, 128])
                    wrs = sb(s2, "wrs", [128, KC, 20]); brs = sb(s2, "brs", [128, 20])
                    L = sb(s2, "L", [128, 20]); m1 = sb(s2, "m1", [128, 4]); oh1 = sb(s2, "oh1", [128, 4]); e1 = sb(s2, "e1", [128, 4])
                    l2 = sb(s2, "l2", [128, 4]); l2b = sb(s2, "l2b", [128, 4]); ohA = sb(s2, "ohA", [128, 4]); ohB = sb(s2, "ohB", [128, 4])
                    pF = [ps(s2, "pF%d" % k, [128, 4, 128]) for k in range(2)]
                    pL = ps(s2, "pL", [128, 20])
                    ld(wrs, wrs.a, wr.a[i].rearrange("(k p) n -> p k n", p=128), [wr])
                    ld(brs, brs.a, br.a[i:i + 1, :].to_broadcast((128, 20)), [br])
                    for n in range(NC):
                        f = ff[n % 2]
                        norm_mod_tile(s2, bufs, Xt[n], Xt[n].a, Gf, SHf, out_f=f)
                        fw.op("act", lambda: A.copy(out=Ftok.a[:, n, :], in_=f.a), [f], [Ftok])
                        for k0 in range(0, KC, 4):
                            p = pF[(k0 // 4) % 2]
                            for k in range(k0, k0 + 4):
                                fw.op("pe", lambda: T.transpose(out=p.a[:, k - k0, :], in_=f.a[:, k * 128:(k + 1) * 128], identity=identF.a), [f, identF], [p])
                            fw.op("act", lambda: A.copy(out=fTf.a[:, k0:k0 + 4, :], in_=p.a), [p], [fTf])
                        for k in range(KC):
                            fw.op("pe", lambda: T.matmul(pL.a, lhsT=fTf.a[:, k, :], rhs=wrs.a[:, k, :], start=(k == 0), stop=(k == KC - 1)), [fTf, wrs], [pL])
                        fw.op("dve", lambda: V.tensor_tensor(out=L.a, in0=pL.a, in1=brs.a, op=ALU.add), [pL, brs], [L])
                        fw.op("dve", lambda: V.tensor_reduce(out=m1.a[:, 0:1], in_=L.a[:, 0:4], axis=mybir.AxisListType.X, op=ALU.max), [L], [m1])
                        fw.op("dve", lambda: V.tensor_scalar(out=oh1.a, in0=L.a[:, 0:4], scalar1=m1.a[:, 0:1], scalar2=None, op0=ALU.is_equal), [L, m1], [oh1])
                        fw.op("dve", lambda: V.tensor_scalar(out=e1.a, in0=L.a[:, 0:4], scalar1=m1.a[:, 0:1], scalar2=None, op0=ALU.subtract), [L, m1], [e1])
                        fw.op("act", lambda: A.activation(out=e1.a, in_=e1.a, func=AF.Exp), [e1], [e1])
                        fw.op("dve", lambda: V.tensor_reduce(out=m1.a[:, 1:2], in_=e1.a, axis=mybir.AxisListType.X, op=ALU.add), [e1], [m1])
                        fw.op("dve", lambda: V.reciprocal(out=m1.a[:, 1:2], in_=m1.a[:, 1:2]), [m1], [m1])
                        fw.op("dve", lambda: V.tensor_scalar(out=l2.a, in0=L.a[:, 4:8], scalar1=oh1.a[:, 0:1], scalar2=None, op0=ALU.mult), [L, oh1], [l2])
                        for g in range(1, 4):
                            fw.op("dve", lambda: V.scalar_tensor_tensor(out=l2.a, in0=L.a[:, 4 + 4 * g:8 + 4 * g], scalar=oh1.a[:, g:g + 1], in1=l2.a, op0=ALU.mult, op1=ALU.add), [L, oh1, l2], [l2])
                        fw.op("dve", lambda: V.tensor_reduce(out=m1.a[:, 2:3], in_=l2.a, axis=mybir.AxisListType.X, op=ALU.max), [l2], [m1])
                        fw.op("dve", lambda: V.tensor_scalar(out=ohA.a, in0=l2.a, scalar1=m1.a[:, 2:3], scalar2=None, op0=ALU.is_equal), [l2, m1], [ohA])
                        fw.op("dve", lambda: V.scalar_tensor_tensor(out=l2b.a, in0=ohA.a, scalar=-1e30, in1=l2.a, op0=ALU.mult, op1=ALU.add), [ohA, l2], [l2b])
                        fw.op("dve", lambda: V.tensor_reduce(out=m1.a[:, 3:4], in_=l2b.a, axis=mybir.AxisListType.X, op=ALU.max), [l2b], [m1])
                        fw.op("dve", lambda: V.tensor_scalar(out=ohB.a, in0=l2b.a, scalar1=m1.a[:, 3:4], scalar2=None, op0=ALU.is_equal), [l2b, m1], [ohB])
                        fw.op("dve", lambda: V.tensor_tensor(out=e1.a[:, 0:1], in0=m1.a[:, 3:4], in1=m1.a[:, 2:3], op=ALU.subtract), [m1], [e1])
                        fw.op("act", lambda: A.activation(out=e1.a[:, 0:1], in_=e1.a[:, 0:1], func=AF.Exp), [e1], [e1])
                        fw.op("dve", lambda: V.tensor_scalar(out=e1.a[:, 1:2], in0=e1.a[:, 0:1], scalar1=1.0, scalar2=None, op0=ALU.add), [e1], [e1])
                        fw.op("dve", lambda: V.reciprocal(out=e1.a[:, 1:2], in_=e1.a[:, 1:2]), [e1], [e1])
                        fw.op("dve", lambda: V.tensor_tensor(out=e1.a[:, 2:3], in0=e1.a[:, 0:1], in1=e1.a[:, 1:2], op=ALU.mult), [e1], [e1])
                        fw.op("dve", lambda: V.tensor_scalar(out=wAB.a[:, n, :], in0=e1.a[:, 1:3], scalar1=m1.a[:, 1:2], scalar2=None, op0=ALU.mult), [e1, m1], [wAB])
                        for g in range(4):
                            fw.op("dve", lambda: V.tensor_scalar(out=MA16.a[:, n, 4 * g:4 * g + 4], in0=ohA.a, scalar1=oh1.a[:, g:g + 1], scalar2=None, op0=ALU.mult), [ohA, oh1], [MA16])
                            fw.op("dve", lambda: V.tensor_scalar(out=MB16.a[:, n, 4 * g:4 * g + 4], in0=ohB.a, scalar1=oh1.a[:, g:g + 1], scalar2=None, op0=ALU.mult), [ohB, oh1], [MB16])
                    Mf = sb(s2, "Mf", [128, NC, 16]); Mb = sb(s2, "Mb", [128, NC * 16], BF16)
                    cntS = sb(s2, "cntS", [128, NC, 16]); ptS = sb(s2, "ptS", [128, NC, 16]); rk = sb(s2, "rk", [128, NC, 16]); rk2 = sb(s2, "rk2", [128, NC, 16])
                    ne = sb(s2, "ne", [128, 16]); tl = sb(s2, "tl", [128, 16]); se = sb(s2, "se", [128, 16]); st128 = sb(s2, "st128", [128, 16])
                    slf = sb(s2, "slf", [128, 2, NC]); ek = sb(s2, "ek", [128, NS]); chg = sb(s2, "chg", [128, NS]); off = sb(s2, "off", [128, NS])
                    WIf = sb(s2, "WIf", [128, NS, 16])
                    trib = sb(s2, "trib", [128, 2, 128], BF16)
                    pP = [ps(s2, "pP%d" % k, [128, NC * 16]) for k in range(2)]
                    fw.op("dve", lambda: V.tensor_copy(out=trib.a, in_=cs.a[:, TRI:TRI + 256]), [cs], [trib])
                    fw.op("dve", lambda: V.tensor_tensor(out=Mf.a, in0=MA16.a, in1=MB16.a, op=ALU.add), [MA16, MB16], [Mf])
                    fw.op("dve", lambda: V.tensor_copy(out=Mb.a, in_=Mf.a), [Mf], [Mb])
                    fw.op("pe", lambda: T.matmul(pP[0].a, lhsT=trib.a[:, 0, :], rhs=Mb.a, start=True, stop=True), [trib, Mb], [pP[0]])
                    fw.op("pe", lambda: T.matmul(pP[1].a, lhsT=trib.a[:, 1, :], rhs=Mb.a, start=True, stop=True), [trib, Mb], [pP[1]])
                    fw.op("act", lambda: A.copy(out=cntS.a, in_=pP[1].a), [pP[1]], [cntS])
                    fw.op("dve", lambda: V.memset(ptS.a[:, 0, :], 0.0), [], [ptS])
                    for n in range(1, NC):
                        fw.op("dve", lambda: V.tensor_tensor(out=ptS.a[:, n, :], in0=ptS.a[:, n - 1, :], in1=cntS.a[:, n - 1, :], op=ALU.add), [ptS, cntS], [ptS])
                    fw.op("dve", lambda: V.tensor_tensor(out=ne.a, in0=ptS.a[:, NC - 1, :], in1=cntS.a[:, NC - 1, :], op=ALU.add), [ptS, cntS], [ne])
                    fw.op("dve", lambda: V.memset(tl.a, 0.0), [], [tl])
                    for j in range(NC):
                        fw.op("dve", lambda: V.scalar_tensor_tensor(out=tl.a, in0=ne.a, scalar=128.0 * j, in1=tl.a, op0=ALU.is_gt, op1=ALU.add), [ne, tl], [tl])
                    fw.op("dve", lambda: V.memset(se.a[:, 0:1], 0.0), [], [se])
                    for e in range(1, 16):
                        fw.op("dve", lambda: V.tensor_tensor(out=se.a[:, e:e + 1], in0=se.a[:, e - 1:e], in1=tl.a[:, e - 1:e], op=ALU.add), [se, tl], [se])
                    fw.op("dve", lambda: V.tensor_scalar(out=st128.a, in0=se.a, scalar1=128.0, scalar2=None, op0=ALU.mult), [se], [st128])
                    fw.op("dve", lambda: V.tensor_tensor(out=rk.a, in0=pP[0].a, in1=ptS.a, op=ALU.add), [pP[0], ptS], [rk])
                    for n in range(NC):
                        fw.op("dve", lambda: V.tensor_tensor(out=rk.a[:, n, :], in0=rk.a[:, n, :], in1=st128.a, op=ALU.add), [rk, st128], [rk])
                    fw.op("dve", lambda: V.tensor_tensor(out=rk2.a, in0=rk.a, in1=MA16.a, op=ALU.mult), [rk, MA16], [rk2])
                    fw.op("dve", lambda: V.tensor_reduce(out=slf.a[:, 0, :], in_=rk2.a, axis=mybir.AxisListType.X, op=ALU.add), [rk2], [slf])
                    fw.op("dve", lambda: V.tensor_tensor(out=rk2.a, in0=rk.a, in1=MB16.a, op=ALU.mult), [rk, MB16], [rk2])
                    fw.op("dve", lambda: V.tensor_reduce(out=slf.a[:, 1, :], in_=rk2.a, axis=mybir.AxisListType.X, op=ALU.add), [rk2], [slf])
                    fw.op("dve", lambda: V.tensor_copy(out=slotAi.a, in_=slf.a[:, 0, :]), [slf], [slotAi])
                    fw.op("dve", lambda: V.tensor_copy(out=slotBi.a, in_=slf.a[:, 1, :]), [slf], [slotBi])
                    fw.op("dve", lambda: V.memset(ek.a, -1.0), [], [ek])
                    for e in range(16):
                        fw.op("dve", lambda: V.scalar_tensor_tensor(out=ek.a, in0=cs.a[:, KV:KV + NS], scalar=se.a[:, e:e + 1], in1=ek.a, op0=ALU.is_ge, op1=ALU.add), [cs, se, ek], [ek])
                    fw.op("dve", lambda: V.memset(chg.a[:, 0:1], 1.0), [], [chg])
                    fw.op("dve", lambda: V.tensor_tensor(out=chg.a[:, 1:NS], in0=ek.a[:, 1:NS], in1=ek.a[:, 0:NS - 1], op=ALU.not_equal), [ek], [chg])
                    fw.op("dve", lambda: V.tensor_scalar(out=off.a, in0=chg.a, scalar1=-1.0e6, scalar2=1.0e6, op0=ALU.mult, op1=ALU.add), [chg], [off])
                    fw.op("dve", lambda: V.scalar_tensor_tensor(out=off.a, in0=ek.a, scalar=1024.0, in1=off.a, op0=ALU.mult, op1=ALU.add), [ek, off], [off])
                    for k in range(NS):
                        fw.op("dve", lambda: V.tensor_scalar(out=WIf.a[:, k, :], in0=cs.a[:, BASE:BASE + 16], scalar1=off.a[:, k:k + 1], scalar2=None, op0=ALU.add), [cs, off], [WIf])
                    fw.op("dve", lambda: V.tensor_copy(out=WIi.a, in_=WIf.a), [WIf], [WIi])
                    for n in range(NC):
                        for sl_ in (slotAi, slotBi):
                            fw.dma("pool", lambda: G.indirect_dma_start(out=FS.a, out_offset=bass.IndirectOffsetOnAxis(ap=sl_.a[:, n:n + 1], axis=0), in_=Ftok.a[:, n, :], in_offset=None, bounds_check=bcS, oob_is_err=False), FS, [Ftok, sl_])
                    fw.barrier()
                    chk()
                with ExitStack() as s2:
                    w1c = [sb(s2, "w1c%d" % k, [128, D], BF16) for k in range(8)]
                    w3c = [sb(s2, "w3c%d" % k, [128, D], BF16) for k in range(8)]
                    w2c = [sb(s2, "w2c%d" % k, [128, D], BF16) for k in range(8)]
                    ftl = [sb(s2, "ftl%d" % k, [128, D], BF16) for k in range(2)]
                    fTk = [sb(s2, "fTk%d" % k, [128, KC, 128], BF16) for k in range(2)]
                    sl = [sb(s2, "sl%d" % k, [128, 512]) for k in range(2)]
                    ab = [sb(s2, "ab%d" % k, [128, DE], BF16) for k in range(2)]
                    aTt = [sb(s2, "aTt%d" % k, [128, 8, 128], BF16) for k in range(2)]
                    yo = [sb(s2, "yo%d" % k, [128, D]) for k in range(2)]
                    ph = [ps(s2, "ph%d" % k, [128, 512]) for k in range(4)]
                    pTf = [ps(s2, "pTf%d" % k, [128, 8, 128], BF16) for k in range(2)]
                    py = [ps(s2, "py%d" % k, [128, 512]) for k in range(2)]
                    yi = 0
                    for k in range(NS):
                        for j in range(8):
                            fw.dma("pool", lambda: G.indirect_dma_start(out=w1c[j].a, out_offset=None, in_=moe_w1[i].a, in_offset=bass.IndirectOffsetOnAxis(ap=WIi.a[:, k, j:j + 1], axis=0), bounds_check=bcW, oob_is_err=False), w1c[j], [moe_w1[i], WIi])
                        for j in range(8):
                            fw.dma("pool", lambda: G.indirect_dma_start(out=w3c[j].a, out_offset=None, in_=moe_w3[i].a, in_offset=bass.IndirectOffsetOnAxis(ap=WIi.a[:, k, j:j + 1], axis=0), bounds_check=bcW, oob_is_err=False), w3c[j], [moe_w3[i], WIi])
                        for j in range(8):
                            fw.dma("pool", lambda: G.indirect_dma_start(out=w2c[j].a, out_offset=None, in_=moe_w2[i].a, in_offset=bass.IndirectOffsetOnAxis(ap=WIi.a[:, k, 8 + j:9 + j], axis=0), bounds_check=bcW, oob_is_err=False), w2c[j], [moe_w2[i], WIi])
                        ft = ftl[k % 2]; fT_ = fTk[k % 2]; a_ = ab[k % 2]; at = aTt[k % 2]; y = yo[k % 2]
                        ld(ft, ft.a, FS.a[k * 128:(k + 1) * 128, :], [FS])
                        transpose_tile(ft, fT_, 0, pTf)
                        for hf in range(2):
                            p1 = ph[hf]
                            for kk in range(KC):
                                c0 = (kk % 2) * 1024 + hf * 512
                                fw.op("pe", lambda: T.matmul(p1.a, lhsT=fT_.a[:, kk, :], rhs=w1c[kk // 2].a[:, c0:c0 + 512], start=(kk == 0), stop=(kk == KC - 1)), [fT_, w1c[kk // 2]], [p1])
                            fw.op("act", lambda: A.activation(out=sl[hf].a, in_=p1.a, func=AF.Silu), [p1], [sl[hf]])
                        for hf in range(2):
                            p3 = ph[2 + hf]
                            for kk in range(KC):
                                c0 = (kk % 2) * 1024 + hf * 512
                                fw.op("pe", lambda: T.matmul(p3.a, lhsT=fT_.a[:, kk, :], rhs=w3c[kk // 2].a[:, c0:c0 + 512], start=(kk == 0), stop=(kk == KC - 1)), [fT_, w3c[kk // 2]], [p3])
                            fw.op("dve", lambda: V.tensor_tensor(out=a_.a[:, hf * 512:(hf + 1) * 512], in0=p3.a, in1=sl[hf].a, op=ALU.mult), [p3, sl[hf]], [a_])
                        transpose_tile(a_, at, 0, pTf[1:2], nk=8)
                        for cb in range(4):
                            p = py[yi % 2]; yi += 1
                            for k8 in range(8):
                                fw.op("pe", lambda: T.matmul(p.a, lhsT=at.a[:, k8, :], rhs=w2c[k8].a[:, cb * 512:(cb + 1) * 512], start=(k8 == 0), stop=(k8 == 7)), [at, w2c[k8]], [p])
                            if cb % 2:
                                fw.op("act", lambda: A.copy(out=y.a[:, cb * 512:(cb + 1) * 512], in_=p.a), [p], [y])
                            else:
                                fw.op("dve", lambda: V.tensor_copy(out=y.a[:, cb * 512:(cb + 1) * 512], in_=p.a), [p], [y])
                        ld(YS, YS.a[k * 128:(k + 1) * 128, :], y.a, [y])
                    fw.barrier()
                    chk()
                with ExitStack() as s2:
                    GT2 = sb(s2, "GT2", [128, D])
                    ld(GT2, GT2.a, modrow(i, 0, 5), [MOD])
                    YA = [sb(s2, "YA%d" % k, [128, D]) for k in range(2)]
                    YB = [sb(s2, "YB%d" % k, [128, D]) for k in range(2)]
                    xc = [sb(s2, "xc%d" % k, [128, D]) for k in range(2)]
                    for n in range(NC):
                        ya = YA[n % 2]; yb = YB[n % 2]; x_ = xc[n % 2]
                        fw.dma("pool", lambda: G.indirect_dma_start(out=ya.a, out_offset=None, in_=YS.a, in_offset=bass.IndirectOffsetOnAxis(ap=slotAi.a[:, n:n + 1], axis=0), bounds_check=bcS, oob_is_err=False), ya, [YS, slotAi])
                        fw.dma("pool", lambda: G.indirect_dma_start(out=yb.a, out_offset=None, in_=YS.a, in_offset=bass.IndirectOffsetOnAxis(ap=slotBi.a[:, n:n + 1], axis=0), bounds_check=bcS, oob_is_err=False), yb, [YS, slotBi])
                        ld(x_, x_.a, Xt[n].a, [Xt[n]])
                        fw.op("dve", lambda: V.tensor_scalar(out=ya.a, in0=ya.a, scalar1=wAB.a[:, n, 0:1], scalar2=None, op0=ALU.mult), [ya, wAB], [ya])
                        fw.op("dve", lambda: V.scalar_tensor_tensor(out=ya.a, in0=yb.a, scalar=wAB.a[:, n, 1:2], in1=ya.a, op0=ALU.mult, op1=ALU.add), [yb, wAB, ya], [ya])
                        fw.op("dve", lambda: V.scalar_tensor_tensor(out=ya.a, in0=ya.a, scalar=1.0, in1=GT2.a, op0=ALU.mult, op1=ALU.mult), [ya, GT2], [ya])
                        fw.op("dve", lambda: V.scalar_tensor_tensor(out=x_.a, in0=ya.a, scalar=1.0, in1=x_.a, op0=ALU.mult, op1=ALU.add), [ya, x_], [x_])
                        ld(Xt[n], Xt[n].a, x_.a, [x_])
                    fw.barrier()
                    chk()

        outproj_and_moe(0, ab_w_out, [(x_own, x_own.a[n * 128:(n + 1) * 128, :]) for n in range(NC)], False)

        with ExitStack() as st:
            Gl, SHl = make_GS(st, 1, 0, 0, 1, g_mix.a[1:2, :], g_mix)
            bufs = {"xt": [sb(st, "xt%d" % i, [128, D]) for i in range(2)], "i": 0, "junk": sb(st, "junk", [128, D]),
                    "ssq": sb(st, "ssq", [128, 1]), "rstd": sb(st, "rstd", [128, 1])}
            abf = [sb(st, "abf%d" % i, [128, D], BF16) for i in range(2)]
            aT = sb(st, "aT", [128, KC, NT], BF16)
            wb = [sb(st, "wb%d" % i, [128, KC, 512], BF16) for i in range(2)]
            osb = [sb(st, "osb%d" % i, [128, 512]) for i in range(3)]
            pT = [ps(st, "pT%d" % i, [128, 8, 128], BF16) for i in range(2)]
            pg = [ps(st, "pg%d" % i, [128, 512]) for i in range(4)]
            for n in range(NC):
                ab = abf[n % 2]
                norm_mod_tile(st, bufs, Xt[n], Xt[n].a, Gl, SHl, out_bf=ab)
                transpose_tile(ab, aT, n * 128, pT)
            ci = 0
            for cb in range(12):
                w = wb[cb % 2]
                load_w(w, cv_w_in.a[:, cb * 512:(cb + 1) * 512], cv_w_in, KC)
                for n in range(NC):
                    p = pg[ci % 4]; o = osb[ci % 3]; ci += 1
                    for k in range(KC):
                        fw.op("pe", lambda: T.matmul(p.a, lhsT=aT.a[:, k, n * 128:(n + 1) * 128], rhs=w.a[:, k, :], start=(k == 0), stop=(k == KC - 1)), [aT, w], [p])
                    if ci % 2:
                        fw.op("act", lambda: A.copy(out=o.a, in_=p.a), [p], [o])
                    else:
                        fw.op("dve", lambda: V.tensor_copy(out=o.a, in_=p.a), [p], [o])
                    ld(Zt[n], Zt[n].a[:, cb * 512:(cb + 1) * 512], o.a, [o])
            fw.barrier()
            chk()
        with ExitStack() as st:
            CW = sb(st, "CW", [128, 3, D]); CB = sb(st, "CB", [128, D])
            for j in range(3):
                ld(CW, CW.a[:, j, :], conv_w.a[j:j + 1, :].to_broadcast((128, D)), [conv_w])
            ld(CB, CB.a, conv_b.a.to_broadcast((128, D)), [conv_b])
            gc = [sb(st, "gc%d" % j, [128, D]) for j in range(3)]
            hv = [sb(st, "hv%d" % j, [128, D]) for j in range(3)]
            gbt = sb(st, "gbt", [128, D]); acc = sb(st, "acc", [128, D])
            zall = Zt + [Zpad, Zpad2]
            for n in range(NC):
                r0 = 1 + n * 128
                for j in range(3):
                    ld(gc[j], gc[j].a, Zd[r0 + j - 1:r0 + j - 1 + 128, 2048:4096], zall)
                    ld(hv[j], hv[j].a, Zd[r0 + j - 1:r0 + j - 1 + 128, 4096:6144], zall)
                ld(gbt, gbt.a, Zt[n].a[:, 0:2048], [Zt[n]])
                for j in range(3):
                    fw.op("dve", lambda: V.scalar_tensor_tensor(out=gc[j].a, in0=gc[j].a, scalar=1.0, in1=hv[j].a, op0=ALU.mult, op1=ALU.mult), [gc[j], hv[j]], [gc[j]])
                fw.op("dve", lambda: V.scalar_tensor_tensor(out=acc.a, in0=gc[0].a, scalar=cs.a[:, cc + 5:cc + 6], in1=CW.a[:, 0, :], op0=ALU.mult, op1=ALU.mult), [gc[0], cs, CW], [acc])
                fw.op("dve", lambda: V.scalar_tensor_tensor(out=acc.a, in0=acc.a, scalar=1.0, in1=CB.a, op0=ALU.mult, op1=ALU.add), [acc, CB], [acc])
                fw.op("dve", lambda: V.scalar_tensor_tensor(out=gc[1].a, in0=gc[1].a, scalar=1.0, in1=CW.a[:, 1, :], op0=ALU.mult, op1=ALU.mult), [gc[1], CW], [gc[1]])
                fw.op("dve", lambda: V.scalar_tensor_tensor(out=acc.a, in0=acc.a, scalar=1.0, in1=gc[1].a, op0=ALU.mult, op1=ALU.add), [acc, gc[1]], [acc])
                fw.op("dve", lambda: V.scalar_tensor_tensor(out=gc[2].a, in0=gc[2].a, scalar=cs.a[:, cc + 6:cc + 7], in1=CW.a[:, 2, :], op0=ALU.mult, op1=ALU.mult), [gc[2], cs, CW], [gc[2]])
                fw.op("dve", lambda: V.scalar_tensor_tensor(out=acc.a, in0=acc.a, scalar=1.0, in1=gc[2].a, op0=ALU.mult, op1=ALU.add), [acc, gc[2]], [acc])
                fw.op("dve", lambda: V.scalar_tensor_tensor(out=acc.a, in0=acc.a, scalar=1.0, in1=gbt.a, op0=ALU.mult, op1=ALU.mult), [acc, gbt], [acc])
                ld(Rt[n], Rt[n].a, acc.a, [acc])
            fw.barrier()
            chk()

        outproj_and_moe(1, cv_w_out, [(Xt[n], Xt[n].a) for n in range(NC)], True)

        with ExitStack() as st:
            Gfin = sb(st, "Gfin", [128, D])
            ld(Gfin, Gfin.a, g_final.a.to_broadcast((128, D)), [g_final])
            bufs = {"xt": [sb(st, "xt%d" % i, [128, D]) for i in range(2)], "i": 0, "junk": sb(st, "junk", [128, D]),
                    "ssq": sb(st, "ssq", [128, 1]), "rstd": sb(st, "rstd", [128, 1])}
            ot = [sb(st, "ot%d" % i, [128, D]) for i in range(2)]
            for n in range(NC):
                jk = norm_mod_tile(st, bufs, Xt[n], Xt[n].a, Gfin, None)
                fw.op("act", lambda: A.copy(out=ot[n % 2].a, in_=jk.a), [jk], [ot[n % 2]])
                ld(out, out.a[n * 128:(n + 1) * 128, :], ot[n % 2].a, [ot[n % 2]], k="sp")
            fw.barrier()
            chk()
    return nc


def rope_tables(pos):
    row = (pos // GRID_W).astype(np.float32)
    col = (pos % GRID_W).astype(np.float32)
    nf = HD // 4
    inv = (10000.0 ** (-np.arange(nf, dtype=np.float32) / nf)).astype(np.float32)
    ang = np.concatenate([row[:, None] * inv, col[:, None] * inv], axis=-1).astype(np.float32)
    return np.cos(ang).astype(np.float32), np.sin(ang).astype(np.float32)


def make_consts(NT):
    NC = NT // 128
    m = np.arange(128, dtype=np.float32)
    ident = np.eye(128, dtype=np.float32)
    R1 = np.maximum(m[None, :] - m[:, None], 0)
    R2 = np.maximum(m[:, None] - m[None, :], 0)
    cols = np.stack([127 - m, m, m + 1, 128 - m, np.full(128, 128.0, np.float32),
                     (np.arange(128) % GRID_W != 0).astype(np.float32),
                     (np.arange(128) % GRID_W != GRID_W - 1).astype(np.float32), np.zeros(128, np.float32)], axis=1)
    et = [128 * c + m for c in range(NC)] + [NT + 128 * c + m for c in range(2)] + [255 - 128 * c - m for c in range(2)]
    et = np.stack(et, axis=1)
    tri = (m[:, None] < m[None, :]).astype(np.float32)
    ones = np.ones((128, 128), np.float32)
    base1 = m[:, None] * 8 + np.arange(8, dtype=np.float32)[None, :]
    base2 = np.arange(8, dtype=np.float32)[None, :] * 128 + m[:, None]
    kv = np.broadcast_to(np.arange(2 * NC + 16, dtype=np.float32)[None, :], (128, 2 * NC + 16))
    return np.concatenate([ident, R1, R2, cols, et, tri, ones, base1, base2, kv], axis=1).astype(np.float32)


def prepare_inputs(inp, T):
    B = inp["x"].shape[0]
    NT = T // 2
    qs = np.float32(HD ** -0.5)
    cst = make_consts(NT)
    maps = []
    shared = {
        "w_mod": inp["w_mod"], "b_mod": inp["b_mod"], "g_mix": inp["g_mix"], "g_ffn": inp["g_ffn"],
        "g_final": inp["g_final"][None, :], "ab_w_in": inp["ab_w_in"][0], "ab_w_out": inp["ab_w_out"][0],
        "cv_w_in": inp["cv_w_in"][0], "cv_w_out": inp["cv_w_out"][0], "conv_b": inp["cv_conv_b"],
        "cst": cst,
        "wr": np.concatenate([inp["moe_w_r1"], inp["moe_w_r2"].transpose(0, 2, 1, 3).reshape(2, D, 16)], axis=2),
        "br": np.concatenate([inp["moe_b_r1"], inp["moe_b_r2"].reshape(2, 16)], axis=1),
    }
    for l in range(2):
        shared["moe_w1_%d" % l] = np.ascontiguousarray(inp["moe_w1"][l].reshape(16, 16, 128, DE).transpose(0, 2, 1, 3).reshape(16384, D))
        shared["moe_w3_%d" % l] = np.ascontiguousarray(inp["moe_w3"][l].reshape(16, 16, 128, DE).transpose(0, 2, 1, 3).reshape(16384, D))
        shared["moe_w2_%d" % l] = inp["moe_w2"][l].reshape(16384, D)
    shared = {k: np.ascontiguousarray(v, dtype=np.float32) for k, v in shared.items()}
    for b in range(B):
        for h in range(2):
            pos_all = np.arange(T)
            if h == 0:
                own = pos_all[:NT]; forg = pos_all[NT:]
                ctxl = inp["ctx"][b]
                dlv = inp["ret_decay_logit"][0].reshape(1, 16)
                ws = inp["sgu_w_s"][0]; bs = inp["sgu_b_s"][0]
                cw = inp["cv_conv_w"][0]
            else:
                own = pos_all[::-1][:NT]; forg = pos_all[:NT][::-1]
                ctxl = inp["ctx"][b][::-1]
                dlv = inp["ret_decay_logit"][0][::-1].reshape(1, 16)
                ws = inp["sgu_w_s"][0][:, ::-1, ::-1]; bs = inp["sgu_b_s"][0][:, ::-1]
                cw = inp["cv_conv_w"][0][::-1]
            co, so = rope_tables(own)
            cf, sf = rope_tables(forg)
            cT = np.stack([inp["c"][b].reshape(16, 128).T, inp["c_ctx"].reshape(16, 128).T], axis=2)
            m = dict(shared)
            m.update({
                "x_own": inp["x"][b][own], "x_for": inp["x"][b][forg], "ctx_l": ctxl, "cT": cT, "dl": dlv,
                "wsT": ws.transpose(0, 2, 1), "bsT": bs.T,
                "rope": np.stack([co * qs, so * qs, co, so, cf, sf]), "conv_w": cw,
            })
            maps.append({k: np.ascontiguousarray(v, dtype=np.float32) for k, v in m.items()})
    return maps


def kernel(**inputs):
    inp = {k: np.asarray(v) for k, v in inputs.items()}
    B, T, _ = inp["x"].shape
    NT = T // 2
    maps = prepare_inputs(inp, T)
    nc = build(NT)
    res = run_bass_kernel_spmd(nc, maps, core_ids=list(range(len(maps))))
    out = np.empty((B, T, D), np.float32)
    for b in range(B):
        out[b, :NT] = res.results[2 * b]["out"]
        out[b, NT:] = res.results[2 * b + 1]["out"][::-1]
    return out
```

```python
from contextlib import ExitStack
import numpy as np
import concourse.bass as bass
import concourse.mybir as mybir
from concourse.bass_utils import run_bass_kernel_spmd

F32 = mybir.dt.float32
BF16 = mybir.dt.bfloat16
I32 = mybir.dt.int32
AF = mybir.ActivationFunctionType
ALU = mybir.AluOpType

D = 2048
EPS = 1e-6
HD = 128
NH = 8
CTX = 256
GRID_W = 64
NEXP = 16
DE = 1024


class Buf:
    def __init__(self, ap, name):
        self.a = ap
        self.name = name
        self.last_write = None
        self.reads = []
        self.dsem = None
        self.dcount = 0


class FW:
    def __init__(self, nc):
        self.nc = nc
        self.engs = {"pe": nc.tensor, "act": nc.scalar, "dve": nc.vector, "pool": nc.gpsimd, "sp": nc.sync}
        self.sems = {}
        self.cnt = {}
        self.dcounts = {}
        self.waited = {k: {} for k in self.engs}
        for k in self.engs:
            self.sems[k] = nc.alloc_semaphore("s_" + k)
            self.cnt[k] = 0
        self.nbuf = 0
        self.free_dsems = []
        self.dbufs = []

    def _deps(self, reads, writes):
        deps = []
        for b in reads:
            if b.last_write is not None:
                deps.append(b.last_write)
        for b in writes:
            if b.last_write is not None:
                deps.append(b.last_write)
            deps.extend(b.reads)
        return deps

    def _emit_waits(self, ek, deps):
        eng = self.engs[ek]
        need = {}
        for (sk, v) in deps:
            if v > need.get(sk, 0):
                need[sk] = v
        for sk, v in need.items():
            if ek == "pe" and sk == "pe":
                continue
            if self.waited[ek].get(sk, 0) >= v:
                continue
            self.waited[ek][sk] = v
            eng.wait_ge(self.sems[sk], v)

    @staticmethod
    def _compact(reads):
        m = {}
        for sk, v in reads:
            if v > m.get(sk, 0):
                m[sk] = v
        return list(m.items())

    def op(self, ek, fn, reads=(), writes=()):
        deps = [b.last_write for b in reads if b.last_write is not None]
        for b in writes:
            for tok_ in ([b.last_write] if b.last_write is not None else []) + b.reads:
                if tok_[0] != ek:
                    deps.append(tok_)
        self._emit_waits(ek, deps)
        ins = fn()
        self.cnt[ek] += 1
        ins.then_inc(self.sems[ek], 1)
        tok = (ek, self.cnt[ek])
        for b in writes:
            b.last_write = tok
            b.reads = []
        for b in reads:
            if b not in writes:
                b.reads.append(tok)
                if len(b.reads) > 8:
                    b.reads = self._compact(b.reads)
        return ins

    def dma(self, qk, fn, dst, srcs=()):
        reads = list(srcs)
        writes = [dst]
        self._emit_waits(qk, self._deps(reads, writes))
        ins = fn()
        if dst.dsem is None:
            if self.free_dsems:
                key = self.free_dsems.pop()
                dst.dcount = self.dcounts[key]
            else:
                key = "d%d" % self.nbuf
                self.nbuf += 1
                self.sems[key] = self.nc.alloc_semaphore(key)
            dst.dsem = key
            self.dbufs.append(dst)
        key = dst.dsem
        dst.dcount += 16
        self.dcounts[key] = dst.dcount
        ins.then_inc(self.sems[key], 16)
        tok = (key, dst.dcount)
        dst.last_write = tok
        dst.reads = []
        for b in reads:
            b.reads.append(tok)
            if len(b.reads) > 8:
                b.reads = self._compact(b.reads)
        return ins

    def barrier(self):
        deps = [(k, self.cnt[k]) for k in self.engs if self.cnt[k] > 0]
        deps += list(self.dcounts.items())
        for ek in self.engs:
            self._emit_waits(ek, deps)
        for b in self.dbufs:
            self.free_dsems.append(b.dsem)
            b.dsem = None
        self.dbufs = []


class _Stop(Exception):
    pass


def build(NT, stop=99):
    try:
        return _build(NT, stop)
    except _Stop as e:
        return e.args[0]


def _build(NT, stop):
    NC = NT // 128
    stage = [0]

    import os
    substop = int(os.environ.get("KSUB", "0"))

    def sub(k):
        if stage[0] + 1 == stop and substop == k:
            fw.barrier()
            raise _Stop(nc)

    def chk():
        stage[0] += 1
        if stage[0] >= stop:
            raise _Stop(nc)
    KC = D // 128
    nc = bass.Bass("TRN2", target_bir_lowering=False)
    fw = FW(nc)
    V, A, G, T, S = nc.vector, nc.scalar, nc.gpsimd, nc.tensor, nc.sync

    def ein(name, shape):
        return Buf(nc.dram_tensor(name, shape, F32, kind="ExternalInput").ap(), name)

    def dint(name, shape, dt=F32):
        return Buf(nc.dram_tensor(name, shape, dt, kind="Internal").ap(), name)

    x_own = ein("x_own", [NT, D]); x_for = ein("x_for", [NT, D]); ctx_l = ein("ctx_l", [CTX, D])
    cT = ein("cT", [128, KC, 2])
    w_mod = ein("w_mod", [2, D, 6 * D]); b_mod = ein("b_mod", [2, 6 * D])
    g_mix = ein("g_mix", [2, D]); g_ffn = ein("g_ffn", [2, D]); g_final = ein("g_final", [1, D])
    ab_w_in = ein("ab_w_in", [D, 6144]); ab_w_out = ein("ab_w_out", [D, D])
    dl = ein("dl", [1, 16])
    wsT = ein("wsT", [8, 128, 128]); bsT = ein("bsT", [128, 8])
    rope = ein("rope", [6, NT, 64])
    cv_w_in = ein("cv_w_in", [D, 6144]); cv_w_out = ein("cv_w_out", [D, D])
    conv_w = ein("conv_w", [3, D]); conv_b = ein("conv_b", [1, D])
    wr = ein("wr", [2, D, 20]); br = ein("br", [2, 20])
    moe_w1 = [ein("moe_w1_%d" % l, [16384, D]) for l in range(2)]
    moe_w3 = [ein("moe_w3_%d" % l, [16384, D]) for l in range(2)]
    moe_w2 = [ein("moe_w2_%d" % l, [16384, D]) for l in range(2)]
    bcW = G.alloc_register("bcW"); G.reg_mov(bcW, 16383)
    bcS = G.alloc_register("bcS"); G.reg_mov(bcS, (2 * NC + 16) * 128 - 1)
    CW_ = 128 * 3 + 8 + NC + 4
    TRI = CW_; BASE = CW_ + 256; KV = BASE + 16
    CWT = KV + 2 * NC + 16
    cst = ein("cst", [128, CWT])
    out = Buf(nc.dram_tensor("out", [NT, D], F32, kind="ExternalOutput").ap(), "out")

    Xd = nc.dram_tensor("Xd", [NT, D], F32, kind="Internal").ap()
    Xt = [Buf(Xd[n * 128:(n + 1) * 128, :], "X%d" % n) for n in range(NC)]
    Zd = nc.dram_tensor("Zd", [NT + 2, 6144], F32, kind="Internal").ap()
    Zt = [Buf(Zd[1 + n * 128:1 + (n + 1) * 128, :], "Z%d" % n) for n in range(NC)]
    Zpad = Buf(Zd[0:1, :], "Zpad")
    Zfd = nc.dram_tensor("Zfd", [NT, 2048], F32, kind="Internal").ap()
    Zft = [Buf(Zfd[n * 128:(n + 1) * 128, :], "Zf%d" % n) for n in range(NC)]
    Zcd = nc.dram_tensor("Zcd", [CTX, 2048], F32, kind="Internal").ap()
    Zct = [Buf(Zcd[n * 128:(n + 1) * 128, :], "Zc%d" % n) for n in range(2)]
    Rd = nc.dram_tensor("Rd", [NT, D], F32, kind="Internal").ap()
    Rt = [Buf(Rd[n * 128:(n + 1) * 128, :], "R%d" % n) for n in range(NC)]
    NS_ = 2 * NC + 16
    FS = Buf(nc.dram_tensor("FSd", [NS_ * 128, D], BF16, kind="Internal").ap(), "FS")
    YS = Buf(nc.dram_tensor("YSd", [NS_ * 128, D], F32, kind="Internal").ap(), "YS")
    MODd = nc.dram_tensor("MODd", [2, 2, 6 * D], F32, kind="Internal").ap()
    MOD = Buf(MODd, "MOD")

    dq = ["sp", "act"]
    dqi = [0]

    def q():
        dqi[0] ^= 1
        return dq[dqi[0]]

    def qeng(k):
        return {"sp": S, "act": A, "pool": G}[k]

    def ld(dst, dst_ap, src_ap, srcs, k=None):
        k = k or q()
        fw.dma(k, lambda: qeng(k).dma_start(out=dst_ap, in_=src_ap), dst, srcs)

    with ExitStack() as glob:
        uid = [0]

        def sb(stack, name, shape, dt=F32):
            uid[0] += 1
            t = stack.enter_context(nc.sbuf_tensor("%s_%d" % (name, uid[0]), shape, dt))
            return Buf(t[:], name)

        def ps(stack, name, shape, dt=F32):
            uid[0] += 1
            t = stack.enter_context(nc.psum_tensor("%s_%d" % (name, uid[0]), shape, dt))
            return Buf(t[:], name)

        cs = sb(glob, "cs", [128, CWT])
        ld(cs, cs.a, cst.a, [cst])
        identf = cs.a[:, 0:128]
        R1 = cs.a[:, 128:256]
        R2 = cs.a[:, 256:384]
        cc = 384
        ET = 392
        identb = sb(glob, "identb", [128, 128], BF16)
        fw.op("dve", lambda: V.tensor_copy(out=identb.a, in_=identf), [cs], [identb])
        identF = sb(glob, "identF", [128, 128])
        fw.op("dve", lambda: V.tensor_copy(out=identF.a, in_=identf), [cs], [identF])
        Zpad2 = Buf(Zd[NT + 1:NT + 2, :], "Zpad2")
        with ExitStack() as st:
            zero = sb(st, "zero", [1, 6144])
            fw.op("dve", lambda: V.memset(zero.a, 0.0), [], [zero])
            ld(Zpad, Zd[0:1, :], zero.a, [zero])
            ld(Zpad2, Zd[NT + 1:NT + 2, :], zero.a, [zero])
            fw.barrier()
            chk()

        with ExitStack() as st:
            scT = sb(st, "scT", [128, KC, 2])
            ld(scT, scT.a, cT.a, [cT])
            fw.op("act", lambda: A.activation(out=scT.a, in_=scT.a, func=AF.Silu), [scT], [scT])
            wm = [sb(st, "wm%d" % i, [128, KC, 512]) for i in range(2)]
            bm = [sb(st, "bm%d" % i, [2, 512]) for i in range(2)]
            mo = [sb(st, "mo%d" % i, [2, 512]) for i in range(2)]
            pm = [ps(st, "pm%d" % i, [2, 512]) for i in range(2)]
            it = 0
            for i in range(2):
                for cb in range(24):
                    j = it % 2
                    it += 1
                    ld(wm[j], wm[j].a, w_mod.a[i, :, cb * 512:(cb + 1) * 512].rearrange("(k p) n -> p k n", p=128), [w_mod])
                    ld(bm[j], bm[j].a, b_mod.a[i:i + 1, cb * 512:(cb + 1) * 512].to_broadcast((2, 512)), [b_mod])
                    for k in range(KC):
                        fw.op("pe", lambda: T.matmul(pm[j].a, lhsT=scT.a[:, k, :], rhs=wm[j].a[:, k, :], start=(k == 0), stop=(k == KC - 1)), [scT, wm[j]], [pm[j]])
                    fw.op("dve", lambda: V.tensor_tensor(out=mo[j].a, in0=pm[j].a, in1=bm[j].a, op=ALU.add), [pm[j], bm[j]], [mo[j]])
                    ld(MOD, MODd[i, :, cb * 512:(cb + 1) * 512], mo[j].a, [mo[j]], k="sp")
            fw.barrier()
            chk()

        def modrow(i, row, j):
            return MODd[i, row:row + 1, j * D:(j + 1) * D].to_broadcast((128, D))

        def norm_mod_tile(st, bufs, src_buf, src_ap, Gt, SHt, out_bf=None, out_f=None):
            xt = bufs["xt"][bufs["i"] % 2]
            bufs["i"] += 1
            ld(xt, xt.a, src_ap, [src_buf])
            sub(8)
            junk, ssq, rstd = bufs["junk"], bufs["ssq"], bufs["rstd"]
            fw.op("act", lambda: A.activation(out=junk.a, in_=xt.a, func=AF.Square), [xt], [junk])
            fw.op("dve", lambda: V.tensor_reduce(out=ssq.a, in_=junk.a, axis=mybir.AxisListType.X, op=ALU.add), [junk], [ssq])
            fw.op("dve", lambda: V.tensor_scalar(out=ssq.a, in0=ssq.a, scalar1=1.0 / D, scalar2=EPS, op0=ALU.mult, op1=ALU.add), [ssq], [ssq])
            fw.op("act", lambda: A.activation(out=ssq.a, in_=ssq.a, func=AF.Sqrt), [ssq], [ssq])
            fw.op("dve", lambda: V.reciprocal(out=rstd.a, in_=ssq.a), [ssq], [rstd])
            fw.op("dve", lambda: V.scalar_tensor_tensor(out=junk.a, in0=xt.a, scalar=rstd.a[:, 0:1], in1=Gt.a, op0=ALU.mult, op1=ALU.mult), [xt, rstd, Gt], [junk])
            sub(9)
            if SHt is None:
                return junk
            if out_f is not None:
                fw.op("dve", lambda: V.scalar_tensor_tensor(out=out_f.a, in0=junk.a, scalar=1.0, in1=SHt.a, op0=ALU.mult, op1=ALU.add), [junk, SHt], [out_f])
                if out_bf is not None:
                    fw.op("act", lambda: A.copy(out=out_bf.a, in_=out_f.a), [out_f], [out_bf])
            else:
                fw.op("dve", lambda: V.tensor_tensor(out=out_bf.a, in0=junk.a, in1=SHt.a, op=ALU.add), [junk, SHt], [out_bf])
            return None

        gs_tmp = {}

        def make_GS(st, i, row, jsh, jsc, gvec_ap, gbuf):
            Gt = sb(st, "Gt%d%d%d" % (i, row, jsh), [128, D])
            SHt = sb(st, "SHt%d%d%d" % (i, row, jsh), [128, D])
            if "gtmp" not in gs_tmp or gs_tmp["st"] is not st:
                gs_tmp["gtmp"] = sb(st, "gtmp", [128, D]); gs_tmp["st"] = st
            tmp = gs_tmp["gtmp"]
            ld(Gt, Gt.a, modrow(i, row, jsc), [MOD])
            ld(tmp, tmp.a, gvec_ap.to_broadcast((128, D)), [gbuf])
            ld(SHt, SHt.a, modrow(i, row, jsh), [MOD])
            fw.op("dve", lambda: V.scalar_tensor_tensor(out=Gt.a, in0=Gt.a, scalar=1.0, in1=tmp.a, op0=ALU.add, op1=ALU.mult), [Gt, tmp], [Gt])
            return Gt, SHt

        def transpose_tile(src_bf, dstT, col0, pT, nk=KC):
            for k0 in range(0, nk, 8):
                p = pT[(k0 // 8) % len(pT)]
                for k in range(k0, min(nk, k0 + 8)):
                    fw.op("pe", lambda: T.transpose(out=p.a[:, k - k0, :], in_=src_bf.a[:, k * 128:(k + 1) * 128], identity=identb.a), [src_bf, identb], [p])
                n = min(nk, k0 + 8) - k0
                if (k0 // 8) % 2 == 0:
                    fw.op("act", lambda: A.copy(out=dstT.a[:, k0:k0 + n, col0:col0 + 128], in_=p.a[:, 0:n, :]), [p], [dstT])
                else:
                    fw.op("dve", lambda: V.tensor_copy(out=dstT.a[:, k0:k0 + n, col0:col0 + 128], in_=p.a[:, 0:n, :]), [p], [dstT])

        def load_w(wbuf, w_ap, wsrc, kc):
            fw.dma("pool", lambda: G.dma_start(out=wbuf.a, in_=w_ap.rearrange("(k p) n -> p k n", p=128)), wbuf, [wsrc])

        with ExitStack() as st:
            Gl, SHl = make_GS(st, 0, 0, 0, 1, g_mix.a[0:1, :], g_mix)
            Gc, SHc = make_GS(st, 0, 1, 0, 1, g_mix.a[0:1, :], g_mix)
            bufs = {"xt": [sb(st, "xt%d" % i, [128, D]) for i in range(1)] * 2, "i": 0, "junk": sb(st, "junk", [128, D]),
                    "ssq": sb(st, "ssq", [128, 1]), "rstd": sb(st, "rstd", [128, 1])}
            abf = [sb(st, "abf%d" % i, [128, D], BF16) for i in range(2)]
            aT = sb(st, "aT", [128, KC, NT], BF16)
            wb = [sb(st, "wb%d" % i, [128, KC, 512], BF16) for i in range(2)]
            osb = [sb(st, "osb%d" % i, [128, 512]) for i in range(3)]
            pT = [ps(st, "pT%d" % i, [128, 8, 128], BF16) for i in range(2)]
            pg = [ps(st, "pg%d" % i, [128, 512]) for i in range(4)]
            cnt = {"w": 0, "o": 0, "p": 0}

            def inproj(src_buf, src_ap_fn, ntiles, Gt, SHt, w_all, wsrc, cbs, zts, zcol0):
                for n in range(ntiles):
                    ab = abf[n % 2]
                    norm_mod_tile(st, bufs, src_buf, src_ap_fn(n), Gt, SHt, out_bf=ab)
                    transpose_tile(ab, aT, n * 128, pT)
                for cb in cbs:
                    w = wb[cnt["w"] % 2]
                    cnt["w"] += 1
                    load_w(w, w_all.a[:, cb * 512:(cb + 1) * 512], wsrc, KC)
                    for n in range(ntiles):
                        p = pg[cnt["p"] % 4]
                        cnt["p"] += 1
                        for k in range(KC):
                            fw.op("pe", lambda: T.matmul(p.a, lhsT=aT.a[:, k, n * 128:(n + 1) * 128], rhs=w.a[:, k, :], start=(k == 0), stop=(k == KC - 1)), [aT, w], [p])
                        o = osb[cnt["o"] % 3]
                        cnt["o"] += 1
                        if cnt["o"] % 2:
                            fw.op("act", lambda: A.copy(out=o.a, in_=p.a), [p], [o])
                        else:
                            fw.op("dve", lambda: V.tensor_copy(out=o.a, in_=p.a), [p], [o])
                        c0 = cb * 512 - zcol0
                        ld(zts[n], zts[n].a[:, c0:c0 + 512], o.a, [o])

            kvb = [2, 3, 4, 5]
            inproj(x_for, lambda n: x_for.a[n * 128:(n + 1) * 128, :], NC, Gl, SHl, ab_w_in, ab_w_in, kvb, Zft, 1024)
            inproj(ctx_l, lambda n: ctx_l.a[n * 128:(n + 1) * 128, :], 2, Gc, SHc, ab_w_in, ab_w_in, kvb, Zct, 1024)
            inproj(x_own, lambda n: x_own.a[n * 128:(n + 1) * 128, :], NC, Gl, SHl, ab_w_in, ab_w_in, list(range(12)), Zt, 0)
            fw.barrier()
            chk()

        with ExitStack() as st:
            lg = sb(st, "lg", [128, 16])
            t1 = sb(st, "t1", [128, 16]); t2 = sb(st, "t2", [128, 16]); t3 = sb(st, "t3", [128, 16]); t4 = sb(st, "t4", [128, 16])
            ld(lg, lg.a, dl.a.to_broadcast((128, 16)), [dl])
            fw.op("act", lambda: A.activation(out=t1.a, in_=lg.a, func=AF.Exp, scale=-1.0), [lg], [t1])
            fw.op("dve", lambda: V.tensor_scalar(out=t2.a, in0=t1.a, scalar1=-0.25, scalar2=1.0 / 3.0, op0=ALU.mult, op1=ALU.add), [t1], [t2])
            fw.op("dve", lambda: V.tensor_tensor(out=t2.a, in0=t2.a, in1=t1.a, op=ALU.mult), [t2, t1], [t2])
            fw.op("dve", lambda: V.tensor_scalar(out=t2.a, in0=t2.a, scalar1=-0.5, scalar2=None, op0=ALU.add), [t2], [t2])
            fw.op("dve", lambda: V.tensor_tensor(out=t2.a, in0=t2.a, in1=t1.a, op=ALU.mult), [t2, t1], [t2])
            fw.op("dve", lambda: V.tensor_scalar(out=t2.a, in0=t2.a, scalar1=1.0, scalar2=None, op0=ALU.add), [t2], [t2])
            fw.op("dve", lambda: V.tensor_tensor(out=t2.a, in0=t2.a, in1=t1.a, op=ALU.mult), [t2, t1], [t2])
            fw.op("dve", lambda: V.tensor_scalar(out=t3.a, in0=t1.a, scalar1=1.0, scalar2=None, op0=ALU.add), [t1], [t3])
            fw.op("act", lambda: A.activation(out=t3.a, in_=t3.a, func=AF.Ln), [t3], [t3])
            fw.op("dve", lambda: V.tensor_scalar(out=t4.a, in0=t1.a, scalar1=0.1, scalar2=None, op0=ALU.is_lt), [t1], [t4])
            fw.op("dve", lambda: V.tensor_tensor(out=t2.a, in0=t2.a, in1=t3.a, op=ALU.subtract), [t2, t3], [t2])
            fw.op("dve", lambda: V.tensor_tensor(out=t2.a, in0=t2.a, in1=t4.a, op=ALU.mult), [t2, t4], [t2])
            fw.op("dve", lambda: V.tensor_tensor(out=t2.a, in0=t2.a, in1=t3.a, op=ALU.add), [t2, t3], [t2])
            fw.op("dve", lambda: V.tensor_scalar(out=lg.a, in0=t2.a, scalar1=-1.0, scalar2=None, op0=ALU.mult), [t2], [lg])

            ropeT = sb(st, "ropeT", [128, 6, NC, 64])
            for r in range(6):
                ld(ropeT, ropeT.a[:, r, :, :], rope.a[r].rearrange("(n p) c -> p n c", p=128), [rope])
            NE = NC + 4
            ew = sb(st, "ew", [128, NE]); ewB = sb(st, "ewB", [128, NE])
            dcol = sb(st, "dcol", [128, 8])
            Dh = sb(st, "Dh", [128, 128]); dtmp = sb(st, "dtmp", [128, 128])
            qs = [sb(st, "qs%d" % i, [128, NC, 128]) for i in range(1)] * 2
            ks = [sb(st, "ks%d" % i, [128, NC, 128]) for i in range(1)] * 2
            vs = [sb(st, "vs%d" % i, [128, NC, 128]) for i in range(1)] * 2
            gs = [sb(st, "gs%d" % i, [128, NC, 128]) for i in range(1)] * 2
            kf = sb(st, "kf", [128, NC + 2, 128]); vf = sb(st, "vf", [128, NC + 2, 128])
            rq = sb(st, "rq", [128, NC, 128]); rk = sb(st, "rk", [128, NC, 128]); rtmp = sb(st, "rtmp", [128, NC, 64])
            qb = sb(st, "qb", [128, NC, 128], BF16); kb = sb(st, "kb", [128, NC, 128], BF16)
            vb = sb(st, "vb", [128, NC, 128], BF16); vA = sb(st, "vA", [128, NC, 128], BF16); vB = sb(st, "vB", [128, NC, 128], BF16)
            kfb = sb(st, "kfb", [128, NC + 2, 128], BF16); vfB = sb(st, "vfB", [128, NC + 2, 128], BF16); vcA = sb(st, "vcA", [128, 2, 128], BF16)
            SA = sb(st, "SA", [128, NC + 1, 128]); SB = sb(st, "SB", [128, NC + 1, 128])
            SAb = sb(st, "SAb", [128, NC + 1, 128], BF16); SBb = sb(st, "SBb", [128, NC + 1, 128], BF16)
            qT = [sb(st, "qT%d" % i, [128, 2, 128], BF16) for i in range(2)]
            ATb = [sb(st, "ATb%d" % i, [128, 128], BF16) for i in range(2)]
            ysb = [sb(st, "ysb%d" % i, [128, 128]) for i in range(2)]
            stt_ = sb(st, "bnst", [128, 6]); mv = sb(st, "mv", [128, 2]); rs = sb(st, "rs", [128, 1])
            sg = sb(st, "sg", [128, 128])
            rout = [sb(st, "rout%d" % i, [128, NC, 128]) for i in range(2)]
            yall = sb(st, "yall", [128, NC, 128]); stall = sb(st, "stall", [128, NC, 6]); mvall = sb(st, "mvall", [128, NC, 2]); rsall = sb(st, "rsall", [128, NC, 1])
            pS = [ps(st, "pS%d" % i, [128, 128]) for i in range(2)]
            pU = [ps(st, "pU%d" % i, [128, 2, 128]) for i in range(2)]
            pTq = ps(st, "pTq", [128, 2, 128], BF16)
            pSc = ps(st, "pSc", [128, 128])
            pY = ps(st, "pY", [128, 3, 128])

            def rope_apply(dst, src, ci, si, nchunk):
                x1 = src.a[:, 0:nchunk, 0:64]; x2 = src.a[:, 0:nchunk, 64:128]
                cth = ropeT.a[:, ci, 0:nchunk, :]; sth = ropeT.a[:, si, 0:nchunk, :]
                tm = rtmp.a[:, 0:nchunk, :]
                fw.op("dve", lambda: V.tensor_tensor(out=dst.a[:, 0:nchunk, 0:64], in0=x1, in1=cth, op=ALU.mult), [src, ropeT], [dst])
                fw.op("dve", lambda: V.tensor_tensor(out=tm, in0=x2, in1=sth, op=ALU.mult), [src, ropeT], [rtmp])
                fw.op("dve", lambda: V.tensor_tensor(out=dst.a[:, 0:nchunk, 0:64], in0=dst.a[:, 0:nchunk, 0:64], in1=tm, op=ALU.subtract), [dst, rtmp], [dst])
                fw.op("dve", lambda: V.tensor_tensor(out=dst.a[:, 0:nchunk, 64:128], in0=x1, in1=sth, op=ALU.mult), [src, ropeT], [dst])
                fw.op("dve", lambda: V.tensor_tensor(out=tm, in0=x2, in1=cth, op=ALU.mult), [src, ropeT], [rtmp])
                fw.op("dve", lambda: V.tensor_tensor(out=dst.a[:, 0:nchunk, 64:128], in0=dst.a[:, 0:nchunk, 64:128], in1=tm, op=ALU.add), [dst, rtmp], [dst])

            for h in range(NH):
                j = h % 2
                c0 = h * 128
                ld(qs[j], qs[j].a, Zd[1:NT + 1, c0:c0 + 128].rearrange("(n p) c -> p n c", p=128), Zt)
                ld(ks[j], ks[j].a, Zd[1:NT + 1, 1024 + c0:1024 + c0 + 128].rearrange("(n p) c -> p n c", p=128), Zt)
                ld(vs[j], vs[j].a, Zd[1:NT + 1, 2048 + c0:2048 + c0 + 128].rearrange("(n p) c -> p n c", p=128), Zt)
                ld(gs[j], gs[j].a, Zd[1:NT + 1, 3072 + c0:3072 + c0 + 128].rearrange("(n p) c -> p n c", p=128), Zt)
                ld(kf, kf.a[:, 0:NC, :], Zfd[:, c0:c0 + 128].rearrange("(n p) c -> p n c", p=128), Zft)
                ld(kf, kf.a[:, NC:NC + 2, :], Zcd[:, c0:c0 + 128].rearrange("(n p) c -> p n c", p=128), Zct)
                ld(vf, vf.a[:, 0:NC, :], Zfd[:, 1024 + c0:1024 + c0 + 128].rearrange("(n p) c -> p n c", p=128), Zft)
                ld(vf, vf.a[:, NC:NC + 2, :], Zcd[:, 1024 + c0:1024 + c0 + 128].rearrange("(n p) c -> p n c", p=128), Zct)
                lgA = lg.a[:, h:h + 1]; lgB = lg.a[:, 8 + h:9 + h]
                fw.op("dve", lambda: V.tensor_scalar(out=dcol.a[:, 0:1], in0=cs.a[:, cc + 0:cc + 1], scalar1=lgA, scalar2=None, op0=ALU.mult), [cs, lg], [dcol])
                fw.op("dve", lambda: V.tensor_scalar(out=dcol.a[:, 1:2], in0=cs.a[:, cc + 1:cc + 2], scalar1=lgB, scalar2=None, op0=ALU.mult), [cs, lg], [dcol])
                fw.op("dve", lambda: V.tensor_scalar(out=dcol.a[:, 2:3], in0=cs.a[:, cc + 2:cc + 3], scalar1=lgA, scalar2=None, op0=ALU.mult), [cs, lg], [dcol])
                fw.op("dve", lambda: V.tensor_scalar(out=dcol.a[:, 3:4], in0=cs.a[:, cc + 3:cc + 4], scalar1=lgB, scalar2=None, op0=ALU.mult), [cs, lg], [dcol])
                fw.op("dve", lambda: V.tensor_scalar(out=dcol.a[:, 4:5], in0=cs.a[:, cc + 4:cc + 5], scalar1=lgA, scalar2=None, op0=ALU.mult), [cs, lg], [dcol])
                fw.op("dve", lambda: V.tensor_scalar(out=dcol.a[:, 5:6], in0=cs.a[:, cc + 4:cc + 5], scalar1=lgB, scalar2=None, op0=ALU.mult), [cs, lg], [dcol])
                fw.op("act", lambda: A.activation(out=dcol.a[:, 0:6], in_=dcol.a[:, 0:6], func=AF.Exp), [dcol], [dcol])
                fw.op("dve", lambda: V.tensor_scalar(out=ewB.a[:, 0:NC + 2], in0=cs.a[:, ET:ET + NC + 2], scalar1=lgB, scalar2=None, op0=ALU.mult), [cs, lg], [ewB])
                fw.op("dve", lambda: V.tensor_scalar(out=ewB.a[:, NC + 2:NC + 4], in0=cs.a[:, ET + NC + 2:ET + NC + 4], scalar1=lgA, scalar2=None, op0=ALU.mult), [cs, lg], [ewB])
                fw.op("act", lambda: A.activation(out=ew.a, in_=ewB.a, func=AF.Exp), [ewB], [ew])
                fw.op("dve", lambda: V.tensor_scalar(out=dtmp.a, in0=R1, scalar1=lgA, scalar2=None, op0=ALU.mult), [cs, lg], [dtmp])
                fw.op("dve", lambda: V.scalar_tensor_tensor(out=dtmp.a, in0=R2, scalar=lgB, in1=dtmp.a, op0=ALU.mult, op1=ALU.add), [cs, lg, dtmp], [dtmp])
                fw.op("act", lambda: A.activation(out=Dh.a, in_=dtmp.a, func=AF.Exp), [dtmp], [Dh])
                rope_apply(rq, qs[j], 0, 1, NC)
                rope_apply(rk, ks[j], 2, 3, NC)
                fw.op("act", lambda: A.copy(out=qb.a, in_=rq.a), [rq], [qb])
                fw.op("act", lambda: A.copy(out=kb.a, in_=rk.a), [rk], [kb])
                fw.op("act", lambda: A.copy(out=vb.a, in_=vs[j].a), [vs[j]], [vb])
                fw.op("dve", lambda: V.tensor_scalar(out=vA.a, in0=vs[j].a, scalar1=dcol.a[:, 0:1], scalar2=None, op0=ALU.mult), [vs[j], dcol], [vA])
                fw.op("dve", lambda: V.tensor_scalar(out=vB.a, in0=vs[j].a, scalar1=dcol.a[:, 1:2], scalar2=None, op0=ALU.mult), [vs[j], dcol], [vB])
                rope_apply(rk, kf, 4, 5, NC)
                fw.op("act", lambda: A.copy(out=kfb.a[:, 0:NC, :], in_=rk.a), [rk], [kfb])
                fw.op("act", lambda: A.copy(out=kfb.a[:, NC:NC + 2, :], in_=kf.a[:, NC:NC + 2, :]), [kf], [kfb])
                for c in range(NC + 2):
                    fw.op("dve", lambda: V.tensor_scalar(out=vfB.a[:, c, :], in0=vf.a[:, c, :], scalar1=ew.a[:, c:c + 1], scalar2=None, op0=ALU.mult), [vf, ew], [vfB])
                for c in range(2):
                    fw.op("dve", lambda: V.tensor_scalar(out=vcA.a[:, c, :], in0=vf.a[:, NC + c, :], scalar1=ew.a[:, NC + 2 + c:NC + 3 + c], scalar2=None, op0=ALU.mult), [vf, ew], [vcA])
                for c in range(2):
                    fw.op("pe", lambda: T.matmul(pS[0].a, lhsT=kfb.a[:, NC + c, :], rhs=vcA.a[:, c, :], start=(c == 0), stop=(c == 1)), [kfb, vcA], [pS[0]])
                fw.op("act", lambda: A.copy(out=SA.a[:, 0, :], in_=pS[0].a), [pS[0]], [SA])
                for c in range(NC + 2):
                    fw.op("pe", lambda: T.matmul(pS[1].a, lhsT=kfb.a[:, c, :], rhs=vfB.a[:, c, :], start=(c == 0), stop=(c == NC + 1)), [kfb, vfB], [pS[1]])
                fw.op("act", lambda: A.copy(out=SB.a[:, NC, :], in_=pS[1].a), [pS[1]], [SB])
                for n in range(NC):
                    p = pU[n % 2]
                    fw.op("pe", lambda: T.matmul(p.a[:, 0, :], lhsT=kb.a[:, n, :], rhs=vA.a[:, n, :], start=True, stop=True), [kb, vA], [p])
                    fw.op("dve", lambda: V.scalar_tensor_tensor(out=SA.a[:, n + 1, :], in0=SA.a[:, n, :], scalar=dcol.a[:, 4:5], in1=p.a[:, 0, :], op0=ALU.mult, op1=ALU.add), [SA, dcol, p], [SA])
                for n in range(NC - 1, -1, -1):
                    p = pU[n % 2]
                    fw.op("pe", lambda: T.matmul(p.a[:, 1, :], lhsT=kb.a[:, n, :], rhs=vB.a[:, n, :], start=True, stop=True), [kb, vB], [p])
                    fw.op("dve", lambda: V.scalar_tensor_tensor(out=SB.a[:, n, :], in0=SB.a[:, n + 1, :], scalar=dcol.a[:, 5:6], in1=p.a[:, 1, :], op0=ALU.mult, op1=ALU.add), [SB, dcol, p], [SB])
                fw.op("act", lambda: A.copy(out=SAb.a, in_=SA.a), [SA], [SAb])
                fw.op("act", lambda: A.copy(out=SBb.a, in_=SB.a), [SB], [SBb])
                ro = rout[j]
                for n in range(NC):
                    qt = qT[n % 2]; at = ATb[n % 2]; y = ysb[n % 2]
                    fw.op("pe", lambda: T.transpose(out=pTq.a[:, 0, :], in_=qb.a[:, n, :], identity=identb.a), [qb, identb], [pTq])
                    fw.op("pe", lambda: T.transpose(out=pTq.a[:, 1, :], in_=kb.a[:, n, :], identity=identb.a), [kb, identb], [pTq])
                    fw.op("act", lambda: A.copy(out=qt.a, in_=pTq.a), [pTq], [qt])
                    fw.op("pe", lambda: T.matmul(pSc.a, lhsT=qt.a[:, 1, :], rhs=qt.a[:, 0, :], start=True, stop=True), [qt], [pSc])
                    fw.op("dve", lambda: V.tensor_tensor(out=at.a, in0=pSc.a, in1=Dh.a, op=ALU.mult), [pSc, Dh], [at])
                    fw.op("pe", lambda: T.matmul(pY.a[:, 0, :], lhsT=at.a, rhs=vb.a[:, n, :], start=True, stop=True), [at, vb], [pY])
                    fw.op("pe", lambda: T.matmul(pY.a[:, 1, :], lhsT=qt.a[:, 0, :], rhs=SAb.a[:, n, :], start=True, stop=True), [qt, SAb], [pY])
                    fw.op("pe", lambda: T.matmul(pY.a[:, 2, :], lhsT=qt.a[:, 0, :], rhs=SBb.a[:, n + 1, :], start=True, stop=True), [qt, SBb], [pY])
                    fw.op("act", lambda: A.copy(out=yall.a[:, n, :], in_=pY.a[:, 0, :]), [pY], [yall])
                    fw.op("dve", lambda: V.scalar_tensor_tensor(out=yall.a[:, n, :], in0=pY.a[:, 1, :], scalar=dcol.a[:, 2:3], in1=yall.a[:, n, :], op0=ALU.mult, op1=ALU.add), [pY, dcol, yall], [yall])
                    fw.op("dve", lambda: V.scalar_tensor_tensor(out=yall.a[:, n, :], in0=pY.a[:, 2, :], scalar=dcol.a[:, 3:4], in1=yall.a[:, n, :], op0=ALU.mult, op1=ALU.add), [pY, dcol, yall], [yall])
                for n in range(NC):
                    fw.op("dve", lambda: V.bn_stats(out=stall.a[:, n, :], in_=yall.a[:, n, :]), [yall], [stall])
                for n in range(NC):
                    fw.op("dve", lambda: V.bn_aggr(out=mvall.a[:, n, :], in_=stall.a[:, n, :]), [stall], [mvall])
                fw.op("dve", lambda: V.tensor_scalar(out=rsall.a, in0=mvall.a[:, :, 1:2], scalar1=EPS, scalar2=None, op0=ALU.add), [mvall], [rsall])
                fw.op("act", lambda: A.activation(out=rsall.a, in_=rsall.a, func=AF.Sqrt), [rsall], [rsall])
                fw.op("dve", lambda: V.reciprocal(out=rsall.a, in_=rsall.a), [rsall], [rsall])
                for n in range(NC):
                    fw.op("dve", lambda: V.tensor_scalar(out=yall.a[:, n, :], in0=yall.a[:, n, :], scalar1=mvall.a[:, n, 0:1], scalar2=rsall.a[:, n, :], op0=ALU.subtract, op1=ALU.mult), [yall, mvall, rsall], [yall])
                fw.op("act", lambda: A.activation(out=rq.a, in_=gs[j].a, func=AF.Silu), [gs[j]], [rq])
                fw.op("dve", lambda: V.tensor_tensor(out=ro.a, in0=yall.a, in1=rq.a, op=ALU.mult), [yall, rq], [ro])
                for n in range(NC):
                    ld(Rt[n], Rt[n].a[:, c0:c0 + 128], ro.a[:, n, :], [ro])
            fw.barrier()
            chk()

        def gelu(dst, src, tmp, reads):
            fw.op("dve", lambda: V.tensor_tensor(out=tmp.a, in0=src.a, in1=src.a, op=ALU.mult), reads, [tmp])
            fw.op("dve", lambda: V.tensor_scalar(out=tmp.a, in0=tmp.a, scalar1=0.044715, scalar2=1.0, op0=ALU.mult, op1=ALU.add), [tmp], [tmp])
            fw.op("dve", lambda: V.tensor_tensor(out=tmp.a, in0=tmp.a, in1=src.a, op=ALU.mult), [tmp] + reads, [tmp])
            fw.op("act", lambda: A.activation(out=tmp.a, in_=tmp.a, func=AF.Sigmoid, scale=1.5957691216057308), [tmp], [tmp])
            fw.op("dve", lambda: V.tensor_tensor(out=dst.a, in0=tmp.a, in1=src.a, op=ALU.mult), [tmp] + reads, [dst])

        with ExitStack() as st:
            us = [sb(st, "us%d" % i, [128, NC, 128]) for i in range(2)]
            vs2 = [sb(st, "vs2%d" % i, [128, NC, 128]) for i in range(2)]
            gu = sb(st, "gu", [128, NC, 128]); gv = sb(st, "gv", [128, NC, 128]); tmpg = sb(st, "tmpg", [128, NC, 128])
            vn = sb(st, "vn", [128, NC, 128], BF16)
            wsf = sb(st, "wsf", [128, 8, 128]); wsb = sb(st, "wsb", [128, 8, 128], BF16)
            bsb = sb(st, "bsb", [128, 8])
            stt2 = sb(st, "bnst2", [128, 6]); mv2 = sb(st, "mv2", [128, 2]); rs2 = sb(st, "rs2", [128, 1])
            ro2 = [sb(st, "ro2%d" % i, [128, NC, 128]) for i in range(2)]
            pG = [ps(st, "pG%d" % i, [128, 128]) for i in range(2)]
            ld(wsf, wsf.a, wsT.a.rearrange("g q p -> q g p"), [wsT])
            ld(bsb, bsb.a, bsT.a, [bsT])
            fw.op("act", lambda: A.copy(out=wsb.a, in_=wsf.a), [wsf], [wsb])
            for g in range(8):
                j = g % 2
                c0 = g * 128
                ld(us[j], us[j].a, Zd[1:NT + 1, 4096 + c0:4096 + c0 + 128].rearrange("(n p) c -> p n c", p=128), Zt)
                ld(vs2[j], vs2[j].a, Zd[1:NT + 1, 5120 + c0:5120 + c0 + 128].rearrange("(n p) c -> p n c", p=128), Zt)
                gelu(gu, us[j], tmpg, [us[j]])
                gelu(gv, vs2[j], tmpg, [vs2[j]])
                for n in range(NC):
                    fw.op("dve", lambda: V.bn_stats(out=stt2.a, in_=gv.a[:, n, :]), [gv], [stt2])
                    fw.op("dve", lambda: V.bn_aggr(out=mv2.a, in_=stt2.a), [stt2], [mv2])
                    fw.op("dve", lambda: V.tensor_scalar(out=rs2.a, in0=mv2.a[:, 1:2], scalar1=EPS, scalar2=None, op0=ALU.add), [mv2], [rs2])
                    fw.op("act", lambda: A.activation(out=rs2.a, in_=rs2.a, func=AF.Sqrt), [rs2], [rs2])
                    fw.op("dve", lambda: V.reciprocal(out=rs2.a, in_=rs2.a), [rs2], [rs2])
                    fw.op("dve", lambda: V.tensor_scalar(out=vn.a[:, n, :], in0=gv.a[:, n, :], scalar1=mv2.a[:, 0:1], scalar2=rs2.a[:, 0:1], op0=ALU.subtract, op1=ALU.mult), [gv, mv2, rs2], [vn])
                for n in range(NC):
                    p = pG[n % 2]
                    fw.op("pe", lambda: T.matmul(p.a, lhsT=wsb.a[:, g, :], rhs=vn.a[:, n, :], start=True, stop=True), [wsb, vn], [p])
                    fw.op("dve", lambda: V.scalar_tensor_tensor(out=ro2[j].a[:, n, :], in0=p.a, scalar=bsb.a[:, g:g + 1], in1=gu.a[:, n, :], op0=ALU.add, op1=ALU.mult), [p, bsb, gu], [ro2[j]])
                for n in range(NC):
                    ld(Rt[n], Rt[n].a[:, 1024 + c0:1024 + c0 + 128], ro2[j].a[:, n, :], [ro2[j]])
            fw.barrier()
            chk()

        def outproj_and_moe(i, w_out, x_src_tiles, last):
            with ExitStack() as st:
                with ExitStack() as s2:
                    GT1 = sb(s2, "GT1", [128, D])
                    ld(GT1, GT1.a, modrow(i, 0, 2), [MOD])
                    rf = [sb(s2, "rf%d" % k, [128, D]) for k in range(2)]
                    rb = [sb(s2, "rb%d" % k, [128, D], BF16) for k in range(2)]
                    wb = [sb(s2, "wo%d" % k, [128, KC, 512], BF16) for k in range(2)]
                    xin = [sb(s2, "xin%d" % k, [128, 512]) for k in range(3)]
                    pT = [ps(s2, "pT%d" % k, [128, 8, 128], BF16) for k in range(2)]
                    pg = [ps(s2, "pg%d" % k, [128, 512]) for k in range(4)]
                    rT = sb(s2, "rT", [128, KC, NT], BF16)
                    for n in range(NC):
                        ld(rf[n % 2], rf[n % 2].a, Rt[n].a, [Rt[n]])
                        fw.op("act", lambda: A.copy(out=rb[n % 2].a, in_=rf[n % 2].a), [rf[n % 2]], [rb[n % 2]])
                        transpose_tile(rb[n % 2], rT, n * 128, pT)
                    ci = 0
                    for cb in range(4):
                        w = wb[cb % 2]
                        load_w(w, w_out.a[:, cb * 512:(cb + 1) * 512], w_out, KC)
                        for n in range(NC):
                            p = pg[ci % 4]; xi = xin[ci % 3]; ci += 1
                            xb_, xap = x_src_tiles[n]
                            ld(xi, xi.a, xap[:, cb * 512:(cb + 1) * 512], [xb_])
                            for k in range(KC):
                                fw.op("pe", lambda: T.matmul(p.a, lhsT=rT.a[:, k, n * 128:(n + 1) * 128], rhs=w.a[:, k, :], start=(k == 0), stop=(k == KC - 1)), [rT, w], [p])
                            fw.op("dve", lambda: V.tensor_tensor(out=p.a, in0=p.a, in1=GT1.a[:, cb * 512:(cb + 1) * 512], op=ALU.mult), [p, GT1], [p])
                            fw.op("dve", lambda: V.tensor_tensor(out=xi.a, in0=p.a, in1=xi.a, op=ALU.add), [p, xi], [xi])
                            ld(Xt[n], Xt[n].a[:, cb * 512:(cb + 1) * 512], xi.a, [xi])
                    fw.barrier()
                    chk()
                NS = 2 * NC + 16
                MA16 = sb(st, "MA16", [128, NC, 16]); MB16 = sb(st, "MB16", [128, NC, 16]); wAB = sb(st, "wAB", [128, NC, 2])
                slotAi = sb(st, "slotAi", [128, NC], I32); slotBi = sb(st, "slotBi", [128, NC], I32)
                WIi = sb(st, "WIi", [128, NS, 16], I32)
                with ExitStack() as s2:
                    Ftok = sb(s2, "Ftok", [128, NC, D], BF16)
                    Gf, SHf = make_GS(s2, i, 0, 3, 4, g_ffn.a[i:i + 1, :], g_ffn)
                    bufs = {"xt": [sb(s2, "xt%d" % k, [128, D]) for k in range(2)], "i": 0, "junk": sb(s2, "junk", [128, D]),
                            "ssq": sb(s2, "ssq", [128, 1]), "rstd": sb(s2, "rstd", [128, 1])}
                    ff = [sb(s2, "ff%d" % k, [128, D]) for k in range(2)]
                    fTf = sb(s2, "fTf", [128, KC, 128])
                    wrs = sb(s2, "wrs", [128, KC, 20]); brs = sb(s2, "brs", [128, 20])
                    L = sb(s2, "L", [128, 20]); m1 = sb(s2, "m1", [128, 4]); oh1 = sb(s2, "oh1", [128, 4]); e1 = sb(s2, "e1", [128, 4])
                    l2 = sb(s2, "l2", [128, 4]); l2b = sb(s2, "l2b", [128, 4]); ohA = sb(s2, "ohA", [128, 4]); ohB = sb(s2, "ohB", [128, 4])
                    pF = [ps(s2, "pF%d" % k, [128, 4, 128]) for k in range(2)]
                    pL = ps(s2, "pL", [128, 20])
                    ld(wrs, wrs.a, wr.a[i].rearrange("(k p) n -> p k n", p=128), [wr])
                    ld(brs, brs.a, br.a[i:i + 1, :].to_broadcast((128, 20)), [br])
                    for n in range(NC):
                        f = ff[n % 2]
                        norm_mod_tile(s2, bufs, Xt[n], Xt[n].a, Gf, SHf, out_f=f)
                        fw.op("act", lambda: A.copy(out=Ftok.a[:, n, :], in_=f.a), [f], [Ftok])
                        for k0 in range(0, KC, 4):
                            p = pF[(k0 // 4) % 2]
                            for k in range(k0, k0 + 4):
                                fw.op("pe", lambda: T.transpose(out=p.a[:, k - k0, :], in_=f.a[:, k * 128:(k + 1) * 128], identity=identF.a), [f, identF], [p])
                            fw.op("act", lambda: A.copy(out=fTf.a[:, k0:k0 + 4, :], in_=p.a), [p], [fTf])
                        for k in range(KC):
                            fw.op("pe", lambda: T.matmul(pL.a, lhsT=fTf.a[:, k, :], rhs=wrs.a[:, k, :], start=(k == 0), stop=(k == KC - 1)), [fTf, wrs], [pL])
                        fw.op("dve", lambda: V.tensor_tensor(out=L.a, in0=pL.a, in1=brs.a, op=ALU.add), [pL, brs], [L])
                        fw.op("dve", lambda: V.tensor_reduce(out=m1.a[:, 0:1], in_=L.a[:, 0:4], axis=mybir.AxisListType.X, op=ALU.max), [L], [m1])
                        fw.op("dve", lambda: V.tensor_scalar(out=oh1.a, in0=L.a[:, 0:4], scalar1=m1.a[:, 0:1], scalar2=None, op0=ALU.is_equal), [L, m1], [oh1])
                        fw.op("dve", lambda: V.tensor_scalar(out=e1.a, in0=L.a[:, 0:4], scalar1=m1.a[:, 0:1], scalar2=None, op0=ALU.subtract), [L, m1], [e1])
                        fw.op("act", lambda: A.activation(out=e1.a, in_=e1.a, func=AF.Exp), [e1], [e1])
                        fw.op("dve", lambda: V.tensor_reduce(out=m1.a[:, 1:2], in_=e1.a, axis=mybir.AxisListType.X, op=ALU.add), [e1], [m1])
                        fw.op("dve", lambda: V.reciprocal(out=m1.a[:, 1:2], in_=m1.a[:, 1:2]), [m1], [m1])
                        fw.op("dve", lambda: V.tensor_scalar(out=l2.a, in0=L.a[:, 4:8], scalar1=oh1.a[:, 0:1], scalar2=None, op0=ALU.mult), [L, oh1], [l2])
                        for g in range(1, 4):
                            fw.op("dve", lambda: V.scalar_tensor_tensor(out=l2.a, in0=L.a[:, 4 + 4 * g:8 + 4 * g], scalar=oh1.a[:, g:g + 1], in1=l2.a, op0=ALU.mult, op1=ALU.add), [L, oh1, l2], [l2])
                        fw.op("dve", lambda: V.tensor_reduce(out=m1.a[:, 2:3], in_=l2.a, axis=mybir.AxisListType.X, op=ALU.max), [l2], [m1])
                        fw.op("dve", lambda: V.tensor_scalar(out=ohA.a, in0=l2.a, scalar1=m1.a[:, 2:3], scalar2=None, op0=ALU.is_equal), [l2, m1], [ohA])
                        fw.op("dve", lambda: V.scalar_tensor_tensor(out=l2b.a, in0=ohA.a, scalar=-1e30, in1=l2.a, op0=ALU.mult, op1=ALU.add), [ohA, l2], [l2b])
                        fw.op("dve", lambda: V.tensor_reduce(out=m1.a[:, 3:4], in_=l2b.a, axis=mybir.AxisListType.X, op=ALU.max), [l2b], [m1])
                        fw.op("dve", lambda: V.tensor_scalar(out=ohB.a, in0=l2b.a, scalar1=m1.a[:, 3:4], scalar2=None, op0=ALU.is_equal), [l2b, m1], [ohB])
                        fw.op("dve", lambda: V.tensor_tensor(out=e1.a[:, 0:1], in0=m1.a[:, 3:4], in1=m1.a[:, 2:3], op=ALU.subtract), [m1], [e1])
                        fw.op("act", lambda: A.activation(out=e1.a[:, 0:1], in_=e1.a[:, 0:1], func=AF.Exp), [e1], [e1])
                        fw.op("dve", lambda: V.tensor_scalar(out=e1.a[:, 1:2], in0=e1.a[:, 0:1], scalar1=1.0, scalar2=None, op0=ALU.add), [e1], [e1])
                        fw.op("dve", lambda: V.reciprocal(out=e1.a[:, 1:2], in_=e1.a[:, 1:2]), [e1], [e1])
                        fw.op("dve", lambda: V.tensor_tensor(out=e1.a[:, 2:3], in0=e1.a[:, 0:1], in1=e1.a[:, 1:2], op=ALU.mult), [e1], [e1])
                        fw.op("dve", lambda: V.tensor_scalar(out=wAB.a[:, n, :], in0=e1.a[:, 1:3], scalar1=m1.a[:, 1:2], scalar2=None, op0=ALU.mult), [e1, m1], [wAB])
                        for g in range(4):
                            fw.op("dve", lambda: V.tensor_scalar(out=MA16.a[:, n, 4 * g:4 * g + 4], in0=ohA.a, scalar1=oh1.a[:, g:g + 1], scalar2=None, op0=ALU.mult), [ohA, oh1], [MA16])
                            fw.op("dve", lambda: V.tensor_scalar(out=MB16.a[:, n, 4 * g:4 * g + 4], in0=ohB.a, scalar1=oh1.a[:, g:g + 1], scalar2=None, op0=ALU.mult), [ohB, oh1], [MB16])
                    Mf = sb(s2, "Mf", [128, NC, 16]); Mb = sb(s2, "Mb", [128, NC * 16], BF16)
                    cntS = sb(s2, "cntS", [128, NC, 16]); ptS = sb(s2, "ptS", [128, NC, 16]); rk = sb(s2, "rk", [128, NC, 16]); rk2 = sb(s2, "rk2", [128, NC, 16])
                    ne = sb(s2, "ne", [128, 16]); tl = sb(s2, "tl", [128, 16]); se = sb(s2, "se", [128, 16]); st128 = sb(s2, "st128", [128, 16])
                    slf = sb(s2, "slf", [128, 2, NC]); ek = sb(s2, "ek", [128, NS]); chg = sb(s2, "chg", [128, NS]); off = sb(s2, "off", [128, NS])
                    WIf = sb(s2, "WIf", [128, NS, 16])
                    trib = sb(s2, "trib", [128, 2, 128], BF16)
                    pP = [ps(s2, "pP%d" % k, [128, NC * 16]) for k in range(2)]
                    fw.op("dve", lambda: V.tensor_copy(out=trib.a, in_=cs.a[:, TRI:TRI + 256]), [cs], [trib])
                    fw.op("dve", lambda: V.tensor_tensor(out=Mf.a, in0=MA16.a, in1=MB16.a, op=ALU.add), [MA16, MB16], [Mf])
                    fw.op("dve", lambda: V.tensor_copy(out=Mb.a, in_=Mf.a), [Mf], [Mb])
                    fw.op("pe", lambda: T.matmul(pP[0].a, lhsT=trib.a[:, 0, :], rhs=Mb.a, start=True, stop=True), [trib, Mb], [pP[0]])
                    fw.op("pe", lambda: T.matmul(pP[1].a, lhsT=trib.a[:, 1, :], rhs=Mb.a, start=True, stop=True), [trib, Mb], [pP[1]])
                    fw.op("act", lambda: A.copy(out=cntS.a, in_=pP[1].a), [pP[1]], [cntS])
                    fw.op("dve", lambda: V.memset(ptS.a[:, 0, :], 0.0), [], [ptS])
                    for n in range(1, NC):
                        fw.op("dve", lambda: V.tensor_tensor(out=ptS.a[:, n, :], in0=ptS.a[:, n - 1, :], in1=cntS.a[:, n - 1, :], op=ALU.add), [ptS, cntS], [ptS])
                    fw.op("dve", lambda: V.tensor_tensor(out=ne.a, in0=ptS.a[:, NC - 1, :], in1=cntS.a[:, NC - 1, :], op=ALU.add), [ptS, cntS], [ne])
                    fw.op("dve", lambda: V.memset(tl.a, 0.0), [], [tl])
                    for j in range(NC):
                        fw.op("dve", lambda: V.scalar_tensor_tensor(out=tl.a, in0=ne.a, scalar=128.0 * j, in1=tl.a, op0=ALU.is_gt, op1=ALU.add), [ne, tl], [tl])
                    fw.op("dve", lambda: V.memset(se.a[:, 0:1], 0.0), [], [se])
                    for e in range(1, 16):
                        fw.op("dve", lambda: V.tensor_tensor(out=se.a[:, e:e + 1], in0=se.a[:, e - 1:e], in1=tl.a[:, e - 1:e], op=ALU.add), [se, tl], [se])
                    fw.op("dve", lambda: V.tensor_scalar(out=st128.a, in0=se.a, scalar1=128.0, scalar2=None, op0=ALU.mult), [se], [st128])
                    fw.op("dve", lambda: V.tensor_tensor(out=rk.a, in0=pP[0].a, in1=ptS.a, op=ALU.add), [pP[0], ptS], [rk])
                    for n in range(NC):
                        fw.op("dve", lambda: V.tensor_tensor(out=rk.a[:, n, :], in0=rk.a[:, n, :], in1=st128.a, op=ALU.add), [rk, st128], [rk])
                    fw.op("dve", lambda: V.tensor_tensor(out=rk2.a, in0=rk.a, in1=MA16.a, op=ALU.mult), [rk, MA16], [rk2])
                    fw.op("dve", lambda: V.tensor_reduce(out=slf.a[:, 0, :], in_=rk2.a, axis=mybir.AxisListType.X, op=ALU.add), [rk2], [slf])
                    fw.op("dve", lambda: V.tensor_tensor(out=rk2.a, in0=rk.a, in1=MB16.a, op=ALU.mult), [rk, MB16], [rk2])
                    fw.op("dve", lambda: V.tensor_reduce(out=slf.a[:, 1, :], in_=rk2.a, axis=mybir.AxisListType.X, op=ALU.add), [rk2], [slf])
                    fw.op("dve", lambda: V.tensor_copy(out=slotAi.a, in_=slf.a[:, 0, :]), [slf], [slotAi])
                    fw.op("dve", lambda: V.tensor_copy(out=slotBi.a, in_=slf.a[:, 1, :]), [slf], [slotBi])
                    fw.op("dve", lambda: V.memset(ek.a, -1.0), [], [ek])
                    for e in range(16):
                        fw.op("dve", lambda: V.scalar_tensor_tensor(out=ek.a, in0=cs.a[:, KV:KV + NS], scalar=se.a[:, e:e + 1], in1=ek.a, op0=ALU.is_ge, op1=ALU.add), [cs, se, ek], [ek])
                    fw.op("dve", lambda: V.memset(chg.a[:, 0:1], 1.0), [], [chg])
                    fw.op("dve", lambda: V.tensor_tensor(out=chg.a[:, 1:NS], in0=ek.a[:, 1:NS], in1=ek.a[:, 0:NS - 1], op=ALU.not_equal), [ek], [chg])
                    fw.op("dve", lambda: V.tensor_scalar(out=off.a, in0=chg.a, scalar1=-1.0e6, scalar2=1.0e6, op0=ALU.mult, op1=ALU.add), [chg], [off])
                    fw.op("dve", lambda: V.scalar_tensor_tensor(out=off.a, in0=ek.a, scalar=1024.0, in1=off.a, op0=ALU.mult, op1=ALU.add), [ek, off], [off])
                    for k in range(NS):
                        fw.op("dve", lambda: V.tensor_scalar(out=WIf.a[:, k, :], in0=cs.a[:, BASE:BASE + 16], scalar1=off.a[:, k:k + 1], scalar2=None, op0=ALU.add), [cs, off], [WIf])
                    fw.op("dve", lambda: V.tensor_copy(out=WIi.a, in_=WIf.a), [WIf], [WIi])
                    for n in range(NC):
                        for sl_ in (slotAi, slotBi):
                            fw.dma("pool", lambda: G.indirect_dma_start(out=FS.a, out_offset=bass.IndirectOffsetOnAxis(ap=sl_.a[:, n:n + 1], axis=0), in_=Ftok.a[:, n, :], in_offset=None, bounds_check=bcS, oob_is_err=False), FS, [Ftok, sl_])
                    fw.barrier()
                    chk()
                with ExitStack() as s2:
                    w1c = [sb(s2, "w1c%d" % k, [128, D], BF16) for k in range(8)]
                    w3c = [sb(s2, "w3c%d" % k, [128, D], BF16) for k in range(8)]
                    w2c = [sb(s2, "w2c%d" % k, [128, D], BF16) for k in range(8)]
                    ftl = [sb(s2, "ftl%d" % k, [128, D], BF16) for k in range(2)]
                    fTk = [sb(s2, "fTk%d" % k, [128, KC, 128], BF16) for k in range(2)]
                    sl = [sb(s2, "sl%d" % k, [128, 512]) for k in range(2)]
                    ab = [sb(s2, "ab%d" % k, [128, DE], BF16) for k in range(2)]
                    aTt = [sb(s2, "aTt%d" % k, [128, 8, 128], BF16) for k in range(2)]
                    yo = [sb(s2, "yo%d" % k, [128, D]) for k in range(2)]
                    ph = [ps(s2, "ph%d" % k, [128, 512]) for k in range(4)]
                    pTf = [ps(s2, "pTf%d" % k, [128, 8, 128], BF16) for k in range(2)]
                    py = [ps(s2, "py%d" % k, [128, 512]) for k in range(2)]
                    yi = 0
                    for k in range(NS):
                        for j in range(8):
                            fw.dma("pool", lambda: G.indirect_dma_start(out=w1c[j].a, out_offset=None, in_=moe_w1[i].a, in_offset=bass.IndirectOffsetOnAxis(ap=WIi.a[:, k, j:j + 1], axis=0), bounds_check=bcW, oob_is_err=False), w1c[j], [moe_w1[i], WIi])
                        for j in range(8):
                            fw.dma("pool", lambda: G.indirect_dma_start(out=w3c[j].a, out_offset=None, in_=moe_w3[i].a, in_offset=bass.IndirectOffsetOnAxis(ap=WIi.a[:, k, j:j + 1], axis=0), bounds_check=bcW, oob_is_err=False), w3c[j], [moe_w3[i], WIi])
                        for j in range(8):
                            fw.dma("pool", lambda: G.indirect_dma_start(out=w2c[j].a, out_offset=None, in_=moe_w2[i].a, in_offset=bass.IndirectOffsetOnAxis(ap=WIi.a[:, k, 8 + j:9 + j], axis=0), bounds_check=bcW, oob_is_err=False), w2c[j], [moe_w2[i], WIi])
                        ft = ftl[k % 2]; fT_ = fTk[k % 2]; a_ = ab[k % 2]; at = aTt[k % 2]; y = yo[k % 2]
                        ld(ft, ft.a, FS.a[k * 128:(k + 1) * 128, :], [FS])
                        transpose_tile(ft, fT_, 0, pTf)
                        for hf in range(2):
                            p1 = ph[hf]
                            for kk in range(KC):
                                c0 = (kk % 2) * 1024 + hf * 512
                                fw.op("pe", lambda: T.matmul(p1.a, lhsT=fT_.a[:, kk, :], rhs=w1c[kk // 2].a[:, c0:c0 + 512], start=(kk == 0), stop=(kk == KC - 1)), [fT_, w1c[kk // 2]], [p1])
                            fw.op("act", lambda: A.activation(out=sl[hf].a, in_=p1.a, func=AF.Silu), [p1], [sl[hf]])
                        for hf in range(2):
                            p3 = ph[2 + hf]
                            for kk in range(KC):
                                c0 = (kk % 2) * 1024 + hf * 512
                                fw.op("pe", lambda: T.matmul(p3.a, lhsT=fT_.a[:, kk, :], rhs=w3c[kk // 2].a[:, c0:c0 + 512], start=(kk == 0), stop=(kk == KC - 1)), [fT_, w3c[kk // 2]], [p3])
                            fw.op("dve", lambda: V.tensor_tensor(out=a_.a[:, hf * 512:(hf + 1) * 512], in0=p3.a, in1=sl[hf].a, op=ALU.mult), [p3, sl[hf]], [a_])
                        transpose_tile(a_, at, 0, pTf[1:2], nk=8)
                        for cb in range(4):
                            p = py[yi % 2]; yi += 1
                            for k8 in range(8):
                                fw.op("pe", lambda: T.matmul(p.a, lhsT=at.a[:, k8, :], rhs=w2c[k8].a[:, cb * 512:(cb + 1) * 512], start=(k8 == 0), stop=(k8 == 7)), [at, w2c[k8]], [p])
                            if cb % 2:
                                fw.op("act", lambda: A.copy(out=y.a[:, cb * 512:(cb + 1) * 512], in_=p.a), [p], [y])
                            else:
                                fw.op("dve", lambda: V.tensor_copy(out=y.a[:, cb * 512:(cb + 1) * 512], in_=p.a), [p], [y])
                        ld(YS, YS.a[k * 128:(k + 1) * 128, :], y.a, [y])
                    fw.barrier()
                    chk()
                with ExitStack() as s2:
                    GT2 = sb(s2, "GT2", [128, D])
                    ld(GT2, GT2.a, modrow(i, 0, 5), [MOD])
                    YA = [sb(s2, "YA%d" % k, [128, D]) for k in range(2)]
                    YB = [sb(s2, "YB%d" % k, [128, D]) for k in range(2)]
                    xc = [sb(s2, "xc%d" % k, [128, D]) for k in range(2)]
                    for n in range(NC):
                        ya = YA[n % 2]; yb = YB[n % 2]; x_ = xc[n % 2]
                        fw.dma("pool", lambda: G.indirect_dma_start(out=ya.a, out_offset=None, in_=YS.a, in_offset=bass.IndirectOffsetOnAxis(ap=slotAi.a[:, n:n + 1], axis=0), bounds_check=bcS, oob_is_err=False), ya, [YS, slotAi])
                        fw.dma("pool", lambda: G.indirect_dma_start(out=yb.a, out_offset=None, in_=YS.a, in_offset=bass.IndirectOffsetOnAxis(ap=slotBi.a[:, n:n + 1], axis=0), bounds_check=bcS, oob_is_err=False), yb, [YS, slotBi])
                        ld(x_, x_.a, Xt[n].a, [Xt[n]])
                        fw.op("dve", lambda: V.tensor_scalar(out=ya.a, in0=ya.a, scalar1=wAB.a[:, n, 0:1], scalar2=None, op0=ALU.mult), [ya, wAB], [ya])
                        fw.op("dve", lambda: V.scalar_tensor_tensor(out=ya.a, in0=yb.a, scalar=wAB.a[:, n, 1:2], in1=ya.a, op0=ALU.mult, op1=ALU.add), [yb, wAB, ya], [ya])
                        fw.op("dve", lambda: V.scalar_tensor_tensor(out=ya.a, in0=ya.a, scalar=1.0, in1=GT2.a, op0=ALU.mult, op1=ALU.mult), [ya, GT2], [ya])
                        fw.op("dve", lambda: V.scalar_tensor_tensor(out=x_.a, in0=ya.a, scalar=1.0, in1=x_.a, op0=ALU.mult, op1=ALU.add), [ya, x_], [x_])
                        ld(Xt[n], Xt[n].a, x_.a, [x_])
                    fw.barrier()
                    chk()

        outproj_and_moe(0, ab_w_out, [(x_own, x_own.a[n * 128:(n + 1) * 128, :]) for n in range(NC)], False)

        with ExitStack() as st:
            Gl, SHl = make_GS(st, 1, 0, 0, 1, g_mix.a[1:2, :], g_mix)
            bufs = {"xt": [sb(st, "xt%d" % i, [128, D]) for i in range(2)], "i": 0, "junk": sb(st, "junk", [128, D]),
                    "ssq": sb(st, "ssq", [128, 1]), "rstd": sb(st, "rstd", [128, 1])}
            abf = [sb(st, "abf%d" % i, [128, D], BF16) for i in range(2)]
            aT = sb(st, "aT", [128, KC, NT], BF16)
            wb = [sb(st, "wb%d" % i, [128, KC, 512], BF16) for i in range(2)]
            osb = [sb(st, "osb%d" % i, [128, 512]) for i in range(3)]
            pT = [ps(st, "pT%d" % i, [128, 8, 128], BF16) for i in range(2)]
            pg = [ps(st, "pg%d" % i, [128, 512]) for i in range(4)]
            for n in range(NC):
                ab = abf[n % 2]
                norm_mod_tile(st, bufs, Xt[n], Xt[n].a, Gl, SHl, out_bf=ab)
                transpose_tile(ab, aT, n * 128, pT)
            ci = 0
            for cb in range(12):
                w = wb[cb % 2]
                load_w(w, cv_w_in.a[:, cb * 512:(cb + 1) * 512], cv_w_in, KC)
                for n in range(NC):
                    p = pg[ci % 4]; o = osb[ci % 3]; ci += 1
                    for k in range(KC):
                        fw.op("pe", lambda: T.matmul(p.a, lhsT=aT.a[:, k, n * 128:(n + 1) * 128], rhs=w.a[:, k, :], start=(k == 0), stop=(k == KC - 1)), [aT, w], [p])
                    if ci % 2:
                        fw.op("act", lambda: A.copy(out=o.a, in_=p.a), [p], [o])
                    else:
                        fw.op("dve", lambda: V.tensor_copy(out=o.a, in_=p.a), [p], [o])
                    ld(Zt[n], Zt[n].a[:, cb * 512:(cb + 1) * 512], o.a, [o])
            fw.barrier()
            chk()
        with ExitStack() as st:
            CW = sb(st, "CW", [128, 3, D]); CB = sb(st, "CB", [128, D])
            for j in range(3):
                ld(CW, CW.a[:, j, :], conv_w.a[j:j + 1, :].to_broadcast((128, D)), [conv_w])
            ld(CB, CB.a, conv_b.a.to_broadcast((128, D)), [conv_b])
            gc = [sb(st, "gc%d" % j, [128, D]) for j in range(3)]
            hv = [sb(st, "hv%d" % j, [128, D]) for j in range(3)]
            gbt = sb(st, "gbt", [128, D]); acc = sb(st, "acc", [128, D])
            zall = Zt + [Zpad, Zpad2]
            for n in range(NC):
                r0 = 1 + n * 128
                for j in range(3):
                    ld(gc[j], gc[j].a, Zd[r0 + j - 1:r0 + j - 1 + 128, 2048:4096], zall)
                    ld(hv[j], hv[j].a, Zd[r0 + j - 1:r0 + j - 1 + 128, 4096:6144], zall)
                ld(gbt, gbt.a, Zt[n].a[:, 0:2048], [Zt[n]])
                for j in range(3):
                    fw.op("dve", lambda: V.scalar_tensor_tensor(out=gc[j].a, in0=gc[j].a, scalar=1.0, in1=hv[j].a, op0=ALU.mult, op1=ALU.mult), [gc[j], hv[j]], [gc[j]])
                fw.op("dve", lambda: V.scalar_tensor_tensor(out=acc.a, in0=gc[0].a, scalar=cs.a[:, cc + 5:cc + 6], in1=CW.a[:, 0, :], op0=ALU.mult, op1=ALU.mult), [gc[0], cs, CW], [acc])
                fw.op("dve", lambda: V.scalar_tensor_tensor(out=acc.a, in0=acc.a, scalar=1.0, in1=CB.a, op0=ALU.mult, op1=ALU.add), [acc, CB], [acc])
                fw.op("dve", lambda: V.scalar_tensor_tensor(out=gc[1].a, in0=gc[1].a, scalar=1.0, in1=CW.a[:, 1, :], op0=ALU.mult, op1=ALU.mult), [gc[1], CW], [gc[1]])
                fw.op("dve", lambda: V.scalar_tensor_tensor(out=acc.a, in0=acc.a, scalar=1.0, in1=gc[1].a, op0=ALU.mult, op1=ALU.add), [acc, gc[1]], [acc])
                fw.op("dve", lambda: V.scalar_tensor_tensor(out=gc[2].a, in0=gc[2].a, scalar=cs.a[:, cc + 6:cc + 7], in1=CW.a[:, 2, :], op0=ALU.mult, op1=ALU.mult), [gc[2], cs, CW], [gc[2]])
                fw.op("dve", lambda: V.scalar_tensor_tensor(out=acc.a, in0=acc.a, scalar=1.0, in1=gc[2].a, op0=ALU.mult, op1=ALU.add), [acc, gc[2]], [acc])
                fw.op("dve", lambda: V.scalar_tensor_tensor(out=acc.a, in0=acc.a, scalar=1.0, in1=gbt.a, op0=ALU.mult, op1=ALU.mult), [acc, gbt], [acc])
                ld(Rt[n], Rt[n].a, acc.a, [acc])
            fw.barrier()
            chk()

        outproj_and_moe(1, cv_w_out, [(Xt[n], Xt[n].a) for n in range(NC)], True)

        with ExitStack() as st:
            Gfin = sb(st, "Gfin", [128, D])
            ld(Gfin, Gfin.a, g_final.a.to_broadcast((128, D)), [g_final])
            bufs = {"xt": [sb(st, "xt%d" % i, [128, D]) for i in range(2)], "i": 0, "junk": sb(st, "junk", [128, D]),
                    "ssq": sb(st, "ssq", [128, 1]), "rstd": sb(st, "rstd", [128, 1])}
            ot = [sb(st, "ot%d" % i, [128, D]) for i in range(2)]
            for n in range(NC):
                jk = norm_mod_tile(st, bufs, Xt[n], Xt[n].a, Gfin, None)
                fw.op("act", lambda: A.copy(out=ot[n % 2].a, in_=jk.a), [jk], [ot[n % 2]])
                ld(out, out.a[n * 128:(n + 1) * 128, :], ot[n % 2].a, [ot[n % 2]], k="sp")
            fw.barrier()
            chk()
    return nc


def rope_tables(pos):
    row = (pos // GRID_W).astype(np.float32)
    col = (pos % GRID_W).astype(np.float32)
    nf = HD // 4
    inv = (10000.0 ** (-np.arange(nf, dtype=np.float32) / nf)).astype(np.float32)
    ang = np.concatenate([row[:, None] * inv, col[:, None] * inv], axis=-1).astype(np.float32)
    return np.cos(ang).astype(np.float32), np.sin(ang).astype(np.float32)


def make_consts(NT):
    NC = NT // 128
    m = np.arange(128, dtype=np.float32)
    ident = np.eye(128, dtype=np.float32)
    R1 = np.maximum(m[None, :] - m[:, None], 0)
    R2 = np.maximum(m[:, None] - m[None, :], 0)
    cols = np.stack([127 - m, m, m + 1, 128 - m, np.full(128, 128.0, np.float32),
                     (np.arange(128) % GRID_W != 0).astype(np.float32),
                     (np.arange(128) % GRID_W != GRID_W - 1).astype(np.float32), np.zeros(128, np.float32)], axis=1)
    et = [128 * c + m for c in range(NC)] + [NT + 128 * c + m for c in range(2)] + [255 - 128 * c - m for c in range(2)]
    et = np.stack(et, axis=1)
    tri = (m[:, None] < m[None, :]).astype(np.float32)
    ones = np.ones((128, 128), np.float32)
    base1 = m[:, None] * 8 + np.arange(8, dtype=np.float32)[None, :]
    base2 = np.arange(8, dtype=np.float32)[None, :] * 128 + m[:, None]
    kv = np.broadcast_to(np.arange(2 * NC + 16, dtype=np.float32)[None, :], (128, 2 * NC + 16))
    return np.concatenate([ident, R1, R2, cols, et, tri, ones, base1, base2, kv], axis=1).astype(np.float32)


def prepare_inputs(inp, T):
    B = inp["x"].shape[0]
    NT = T // 2
    qs = np.float32(HD ** -0.5)
    cst = make_consts(NT)
    maps = []
    shared = {
        "w_mod": inp["w_mod"], "b_mod": inp["b_mod"], "g_mix": inp["g_mix"], "g_ffn": inp["g_ffn"],
        "g_final": inp["g_final"][None, :], "ab_w_in": inp["ab_w_in"][0], "ab_w_out": inp["ab_w_out"][0],
        "cv_w_in": inp["cv_w_in"][0], "cv_w_out": inp["cv_w_out"][0], "conv_b": inp["cv_conv_b"],
        "cst": cst,
        "wr": np.concatenate([inp["moe_w_r1"], inp["moe_w_r2"].transpose(0, 2, 1, 3).reshape(2, D, 16)], axis=2),
        "br": np.concatenate([inp["moe_b_r1"], inp["moe_b_r2"].reshape(2, 16)], axis=1),
    }
    for l in range(2):
        shared["moe_w1_%d" % l] = np.ascontiguousarray(inp["moe_w1"][l].reshape(16, 16, 128, DE).transpose(0, 2, 1, 3).reshape(16384, D))
        shared["moe_w3_%d" % l] = np.ascontiguousarray(inp["moe_w3"][l].reshape(16, 16, 128, DE).transpose(0, 2, 1, 3).reshape(16384, D))
        shared["moe_w2_%d" % l] = inp["moe_w2"][l].reshape(16384, D)
    shared = {k: np.ascontiguousarray(v, dtype=np.float32) for k, v in shared.items()}
    for b in range(B):
        for h in range(2):
            pos_all = np.arange(T)
            if h == 0:
                own = pos_all[:NT]; forg = pos_all[NT:]
                ctxl = inp["ctx"][b]
                dlv = inp["ret_decay_logit"][0].reshape(1, 16)
                ws = inp["sgu_w_s"][0]; bs = inp["sgu_b_s"][0]
                cw = inp["cv_conv_w"][0]
            else:
                own = pos_all[::-1][:NT]; forg = pos_all[:NT][::-1]
                ctxl = inp["ctx"][b][::-1]
                dlv = inp["ret_decay_logit"][0][::-1].reshape(1, 16)
                ws = inp["sgu_w_s"][0][:, ::-1, ::-1]; bs = inp["sgu_b_s"][0][:, ::-1]
                cw = inp["cv_conv_w"][0][::-1]
            co, so = rope_tables(own)
            cf, sf = rope_tables(forg)
            cT = np.stack([inp["c"][b].reshape(16, 128).T, inp["c_ctx"].reshape(16, 128).T], axis=2)
            m = dict(shared)
            m.update({
                "x_own": inp["x"][b][own], "x_for": inp["x"][b][forg], "ctx_l": ctxl, "cT": cT, "dl": dlv,
                "wsT": ws.transpose(0, 2, 1), "bsT": bs.T,
                "rope": np.stack([co * qs, so * qs, co, so, cf, sf]), "conv_w": cw,
            })
            maps.append({k: np.ascontiguousarray(v, dtype=np.float32) for k, v in m.items()})
    return maps


def kernel(**inputs):
    inp = {k: np.asarray(v) for k, v in inputs.items()}
    B, T, _ = inp["x"].shape
    NT = T // 2
    maps = prepare_inputs(inp, T)
    nc = build(NT)
    res = run_bass_kernel_spmd(nc, maps, core_ids=list(range(len(maps))))
    out = np.empty((B, T, D), np.float32)
    for b in range(B):
        out[b, :NT] = res.results[2 * b]["out"]
        out[b, NT:] = res.results[2 * b + 1]["out"][::-1]
    return out
```

```python
from contextlib import ExitStack
import numpy as np
import concourse.bass as bass
import concourse.mybir as mybir
from concourse.bass_utils import run_bass_kernel_spmd

F32 = mybir.dt.float32
BF16 = mybir.dt.bfloat16
I32 = mybir.dt.int32
AF = mybir.ActivationFunctionType
ALU = mybir.AluOpType

D = 2048
EPS = 1e-6
HD = 128
NH = 8
CTX = 256
GRID_W = 64
NEXP = 16
DE = 1024


class Buf:
    def __init__(self, ap, name):
        self.a = ap
        self.name = name
        self.last_write = None
        self.reads = []
        self.dsem = None
        self.dcount = 0


class FW:
    def __init__(self, nc):
        self.nc = nc
        self.engs = {"pe": nc.tensor, "act": nc.scalar, "dve": nc.vector, "pool": nc.gpsimd, "sp": nc.sync}
        self.sems = {}
        self.cnt = {}
        self.dcounts = {}
        self.waited = {k: {} for k in self.engs}
        for k in self.engs:
            self.sems[k] = nc.alloc_semaphore("s_" + k)
            self.cnt[k] = 0
        self.nbuf = 0
        self.free_dsems = []
        self.dbufs = []

    def _deps(self, reads, writes):
        deps = []
        for b in reads:
            if b.last_write is not None:
                deps.append(b.last_write)
        for b in writes:
            if b.last_write is not None:
                deps.append(b.last_write)
            deps.extend(b.reads)
        return deps

    def _emit_waits(self, ek, deps):
        eng = self.engs[ek]
        need = {}
        for (sk, v) in deps:
            if v > need.get(sk, 0):
                need[sk] = v
        for sk, v in need.items():
            if ek == "pe" and sk == "pe":
                continue
            if self.waited[ek].get(sk, 0) >= v:
                continue
            self.waited[ek][sk] = v
            eng.wait_ge(self.sems[sk], v)

    @staticmethod
    def _compact(reads):
        m = {}
        for sk, v in reads:
            if v > m.get(sk, 0):
                m[sk] = v
        return list(m.items())

    def op(self, ek, fn, reads=(), writes=()):
        deps = [b.last_write for b in reads if b.last_write is not None]
        for b in writes:
            for tok_ in ([b.last_write] if b.last_write is not None else []) + b.reads:
                if tok_[0] != ek:
                    deps.append(tok_)
        self._emit_waits(ek, deps)
        ins = fn()
        self.cnt[ek] += 1
        ins.then_inc(self.sems[ek], 1)
        tok = (ek, self.cnt[ek])
        for b in writes:
            b.last_write = tok
            b.reads = []
        for b in reads:
            if b not in writes:
                b.reads.append(tok)
                if len(b.reads) > 8:
                    b.reads = self._compact(b.reads)
        return ins

    def dma(self, qk, fn, dst, srcs=()):
        reads = list(srcs)
        writes = [dst]
        self._emit_waits(qk, self._deps(reads, writes))
        ins = fn()
        if dst.dsem is None:
            if self.free_dsems:
                key = self.free_dsems.pop()
                dst.dcount = self.dcounts[key]
            else:
                key = "d%d" % self.nbuf
                self.nbuf += 1
                self.sems[key] = self.nc.alloc_semaphore(key)
            dst.dsem = key
            self.dbufs.append(dst)
        key = dst.dsem
        dst.dcount += 16
        self.dcounts[key] = dst.dcount
        ins.then_inc(self.sems[key], 16)
        tok = (key, dst.dcount)
        dst.last_write = tok
        dst.reads = []
        for b in reads:
            b.reads.append(tok)
            if len(b.reads) > 8:
                b.reads = self._compact(b.reads)
        return ins

    def barrier(self):
        deps = [(k, self.cnt[k]) for k in self.engs if self.cnt[k] > 0]
        deps += list(self.dcounts.items())
        for ek in self.engs:
            self._emit_waits(ek, deps)
        for b in self.dbufs:
            self.free_dsems.append(b.dsem)
            b.dsem = None
        self.dbufs = []


class _Stop(Exception):
    pass


def build(NT, stop=99):
    try:
        return _build(NT, stop)
    except _Stop as e:
        return e.args[0]


def _build(NT, stop):
    NC = NT // 128
    stage = [0]

    import os
    substop = int(os.environ.get("KSUB", "0"))

    def sub(k):
        if stage[0] + 1 == stop and substop == k:
            fw.barrier()
            raise _Stop(nc)

    def chk():
        stage[0] += 1
        if stage[0] >= stop:
            raise _Stop(nc)
    KC = D // 128
    nc = bass.Bass("TRN2", target_bir_lowering=False)
    fw = FW(nc)
    V, A, G, T, S = nc.vector, nc.scalar, nc.gpsimd, nc.tensor, nc.sync

    def ein(name, shape):
        return Buf(nc.dram_tensor(name, shape, F32, kind="ExternalInput").ap(), name)

    def dint(name, shape, dt=F32):
        return Buf(nc.dram_tensor(name, shape, dt, kind="Internal").ap(), name)

    x_own = ein("x_own", [NT, D]); x_for = ein("x_for", [NT, D]); ctx_l = ein("ctx_l", [CTX, D])
    cT = ein("cT", [128, KC, 2])
    w_mod = ein("w_mod", [2, D, 6 * D]); b_mod = ein("b_mod", [2, 6 * D])
    g_mix = ein("g_mix", [2, D]); g_ffn = ein("g_ffn", [2, D]); g_final = ein("g_final", [1, D])
    ab_w_in = ein("ab_w_in", [D, 6144]); ab_w_out = ein("ab_w_out", [D, D])
    dl = ein("dl", [1, 16])
    wsT = ein("wsT", [8, 128, 128]); bsT = ein("bsT", [128, 8])
    rope = ein("rope", [6, NT, 64])
    cv_w_in = ein("cv_w_in", [D, 6144]); cv_w_out = ein("cv_w_out", [D, D])
    conv_w = ein("conv_w", [3, D]); conv_b = ein("conv_b", [1, D])
    wr = ein("wr", [2, D, 20]); br = ein("br", [2, 20])
    moe_w1 = [ein("moe_w1_%d" % l, [16384, D]) for l in range(2)]
    moe_w3 = [ein("moe_w3_%d" % l, [16384, D]) for l in range(2)]
    moe_w2 = [ein("moe_w2_%d" % l, [16384, D]) for l in range(2)]
    bcW = G.alloc_register("bcW"); G.reg_mov(bcW, 16383)
    bcS = G.alloc_register("bcS"); G.reg_mov(bcS, (2 * NC + 16) * 128 - 1)
    CW_ = 128 * 3 + 8 + NC + 4
    TRI = CW_; BASE = CW_ + 256; KV = BASE + 16
    CWT = KV + 2 * NC + 16
    cst = ein("cst", [128, CWT])
    out = Buf(nc.dram_tensor("out", [NT, D], F32, kind="ExternalOutput").ap(), "out")

    Xd = nc.dram_tensor("Xd", [NT, D], F32, kind="Internal").ap()
    Xt = [Buf(Xd[n * 128:(n + 1) * 128, :], "X%d" % n) for n in range(NC)]
    Zd = nc.dram_tensor("Zd", [NT + 2, 6144], F32, kind="Internal").ap()
    Zt = [Buf(Zd[1 + n * 128:1 + (n + 1) * 128, :], "Z%d" % n) for n in range(NC)]
    Zpad = Buf(Zd[0:1, :], "Zpad")
    Zfd = nc.dram_tensor("Zfd", [NT, 2048], F32, kind="Internal").ap()
    Zft = [Buf(Zfd[n * 128:(n + 1) * 128, :], "Zf%d" % n) for n in range(NC)]
    Zcd = nc.dram_tensor("Zcd", [CTX, 2048], F32, kind="Internal").ap()
    Zct = [Buf(Zcd[n * 128:(n + 1) * 128, :], "Zc%d" % n) for n in range(2)]
    Rd = nc.dram_tensor("Rd", [NT, D], F32, kind="Internal").ap()
    Rt = [Buf(Rd[n * 128:(n + 1) * 128, :], "R%d" % n) for n in range(NC)]
    NS_ = 2 * NC + 16
    FS = Buf(nc.dram_tensor("FSd", [NS_ * 128, D], BF16, kind="Internal").ap(), "FS")
    YS = Buf(nc.dram_tensor("YSd", [NS_ * 128, D], F32, kind="Internal").ap(), "YS")
    MODd = nc.dram_tensor("MODd", [2, 2, 6 * D], F32, kind="Internal").ap()
    MOD = Buf(MODd, "MOD")

    dq = ["sp", "act"]
    dqi = [0]

    def q():
        dqi[0] ^= 1
        return dq[dqi[0]]

    def qeng(k):
        return {"sp": S, "act": A, "pool": G}[k]

    def ld(dst, dst_ap, src_ap, srcs, k=None):
        k = k or q()
        fw.dma(k, lambda: qeng(k).dma_start(out=dst_ap, in_=src_ap), dst, srcs)

    with ExitStack() as glob:
        uid = [0]

        def sb(stack, name, shape, dt=F32):
            uid[0] += 1
            t = stack.enter_context(nc.sbuf_tensor("%s_%d" % (name, uid[0]), shape, dt))
            return Buf(t[:], name)

        def ps(stack, name, shape, dt=F32):
            uid[0] += 1
            t = stack.enter_context(nc.psum_tensor("%s_%d" % (name, uid[0]), shape, dt))
            return Buf(t[:], name)

        cs = sb(glob, "cs", [128, CWT])
        ld(cs, cs.a, cst.a, [cst])
        identf = cs.a[:, 0:128]
        R1 = cs.a[:, 128:256]
        R2 = cs.a[:, 256:384]
        cc = 384
        ET = 392
        identb = sb(glob, "identb", [128, 128], BF16)
        fw.op("dve", lambda: V.tensor_copy(out=identb.a, in_=identf), [cs], [identb])
        identF = sb(glob, "identF", [128, 128])
        fw.op("dve", lambda: V.tensor_copy(out=identF.a, in_=identf), [cs], [identF])
        Zpad2 = Buf(Zd[NT + 1:NT + 2, :], "Zpad2")
        with ExitStack() as st:
            zero = sb(st, "zero", [1, 6144])
            fw.op("dve", lambda: V.memset(zero.a, 0.0), [], [zero])
            ld(Zpad, Zd[0:1, :], zero.a, [zero])
            ld(Zpad2, Zd[NT + 1:NT + 2, :], zero.a, [zero])
            fw.barrier()
            chk()

        with ExitStack() as st:
            scT = sb(st, "scT", [128, KC, 2])
            ld(scT, scT.a, cT.a, [cT])
            fw.op("act", lambda: A.activation(out=scT.a, in_=scT.a, func=AF.Silu), [scT], [scT])
            wm = [sb(st, "wm%d" % i, [128, KC, 512]) for i in range(2)]
            bm = [sb(st, "bm%d" % i, [2, 512]) for i in range(2)]
            mo = [sb(st, "mo%d" % i, [2, 512]) for i in range(2)]
            pm = [ps(st, "pm%d" % i, [2, 512]) for i in range(2)]
            it = 0
            for i in range(2):
                for cb in range(24):
                    j = it % 2
                    it += 1
                    ld(wm[j], wm[j].a, w_mod.a[i, :, cb * 512:(cb + 1) * 512].rearrange("(k p) n -> p k n", p=128), [w_mod])
                    ld(bm[j], bm[j].a, b_mod.a[i:i + 1, cb * 512:(cb + 1) * 512].to_broadcast((2, 512)), [b_mod])
                    for k in range(KC):
                        fw.op("pe", lambda: T.matmul(pm[j].a, lhsT=scT.a[:, k, :], rhs=wm[j].a[:, k, :], start=(k == 0), stop=(k == KC - 1)), [scT, wm[j]], [pm[j]])
                    fw.op("dve", lambda: V.tensor_tensor(out=mo[j].a, in0=pm[j].a, in1=bm[j].a, op=ALU.add), [pm[j], bm[j]], [mo[j]])
                    ld(MOD, MODd[i, :, cb * 512:(cb + 1) * 512], mo[j].a, [mo[j]], k="sp")
            fw.barrier()
            chk()

        def modrow(i, row, j):
            return MODd[i, row:row + 1, j * D:(j + 1) * D].to_broadcast((128, D))

        def norm_mod_tile(st, bufs, src_buf, src_ap, Gt, SHt, out_bf=None, out_f=None):
            xt = bufs["xt"][bufs["i"] % 2]
            bufs["i"] += 1
            ld(xt, xt.a, src_ap, [src_buf])
            sub(8)
            junk, ssq, rstd = bufs["junk"], bufs["ssq"], bufs["rstd"]
            fw.op("act", lambda: A.activation(out=junk.a, in_=xt.a, func=AF.Square), [xt], [junk])
            fw.op("dve", lambda: V.tensor_reduce(out=ssq.a, in_=junk.a, axis=mybir.AxisListType.X, op=ALU.add), [junk], [ssq])
            fw.op("dve", lambda: V.tensor_scalar(out=ssq.a, in0=ssq.a, scalar1=1.0 / D, scalar2=EPS, op0=ALU.mult, op1=ALU.add), [ssq], [ssq])
            fw.op("act", lambda: A.activation(out=ssq.a, in_=ssq.a, func=AF.Sqrt), [ssq], [ssq])
            fw.op("dve", lambda: V.reciprocal(out=rstd.a, in_=ssq.a), [ssq], [rstd])
            fw.op("dve", lambda: V.scalar_tensor_tensor(out=junk.a, in0=xt.a, scalar=rstd.a[:, 0:1], in1=Gt.a, op0=ALU.mult, op1=ALU.mult), [xt, rstd, Gt], [junk])
            sub(9)
            if SHt is None:
                return junk
            if out_f is not None:
                fw.op("dve", lambda: V.scalar_tensor_tensor(out=out_f.a, in0=junk.a, scalar=1.0, in1=SHt.a, op0=ALU.mult, op1=ALU.add), [junk, SHt], [out_f])
                if out_bf is not None:
                    fw.op("act", lambda: A.copy(out=out_bf.a, in_=out_f.a), [out_f], [out_bf])
            else:
                fw.op("dve", lambda: V.tensor_tensor(out=out_bf.a, in0=junk.a, in1=SHt.a, op=ALU.add), [junk, SHt], [out_bf])
            return None

        gs_tmp = {}

        def make_GS(st, i, row, jsh, jsc, gvec_ap, gbuf):
            Gt = sb(st, "Gt%d%d%d" % (i, row, jsh), [128, D])
            SHt = sb(st, "SHt%d%d%d" % (i, row, jsh), [128, D])
            if "gtmp" not in gs_tmp or gs_tmp["st"] is not st:
                gs_tmp["gtmp"] = sb(st, "gtmp", [128, D]); gs_tmp["st"] = st
            tmp = gs_tmp["gtmp"]
            ld(Gt, Gt.a, modrow(i, row, jsc), [MOD])
            ld(tmp, tmp.a, gvec_ap.to_broadcast((128, D)), [gbuf])
            ld(SHt, SHt.a, modrow(i, row, jsh), [MOD])
            fw.op("dve", lambda: V.scalar_tensor_tensor(out=Gt.a, in0=Gt.a, scalar=1.0, in1=tmp.a, op0=ALU.add, op1=ALU.mult), [Gt, tmp], [Gt])
            return Gt, SHt

        def transpose_tile(src_bf, dstT, col0, pT, nk=KC):
            for k0 in range(0, nk, 8):
                p = pT[(k0 // 8) % len(pT)]
                for k in range(k0, min(nk, k0 + 8)):
                    fw.op("pe", lambda: T.transpose(out=p.a[:, k - k0, :], in_=src_bf.a[:, k * 128:(k + 1) * 128], identity=identb.a), [src_bf, identb], [p])
                n = min(nk, k0 + 8) - k0
                if (k0 // 8) % 2 == 0:
                    fw.op("act", lambda: A.copy(out=dstT.a[:, k0:k0 + n, col0:col0 + 128], in_=p.a[:, 0:n, :]), [p], [dstT])
                else:
                    fw.op("dve", lambda: V.tensor_copy(out=dstT.a[:, k0:k0 + n, col0:col0 + 128], in_=p.a[:, 0:n, :]), [p], [dstT])

        def load_w(wbuf, w_ap, wsrc, kc):
            fw.dma("pool", lambda: G.dma_start(out=wbuf.a, in_=w_ap.rearrange("(k p) n -> p k n", p=128)), wbuf, [wsrc])

        with ExitStack() as st:
            Gl, SHl = make_GS(st, 0, 0, 0, 1, g_mix.a[0:1, :], g_mix)
            Gc, SHc = make_GS(st, 0, 1, 0, 1, g_mix.a[0:1, :], g_mix)
            bufs = {"xt": [sb(st, "xt%d" % i, [128, D]) for i in range(1)] * 2, "i": 0, "junk": sb(st, "junk", [128, D]),
                    "ssq": sb(st, "ssq", [128, 1]), "rstd": sb(st, "rstd", [128, 1])}
            abf = [sb(st, "abf%d" % i, [128, D], BF16) for i in range(2)]
            aT = sb(st, "aT", [128, KC, NT], BF16)
            wb = [sb(st, "wb%d" % i, [128, KC, 512], BF16) for i in range(2)]
            osb = [sb(st, "osb%d" % i, [128, 512]) for i in range(3)]
            pT = [ps(st, "pT%d" % i, [128, 8, 128], BF16) for i in range(2)]
            pg = [ps(st, "pg%d" % i, [128, 512]) for i in range(4)]
            cnt = {"w": 0, "o": 0, "p": 0}

            def inproj(src_buf, src_ap_fn, ntiles, Gt, SHt, w_all, wsrc, cbs, zts, zcol0):
                for n in range(ntiles):
                    ab = abf[n % 2]
                    norm_mod_tile(st, bufs, src_buf, src_ap_fn(n), Gt, SHt, out_bf=ab)
                    transpose_tile(ab, aT, n * 128, pT)
                for cb in cbs:
                    w = wb[cnt["w"] % 2]
                    cnt["w"] += 1
                    load_w(w, w_all.a[:, cb * 512:(cb + 1) * 512], wsrc, KC)
                    for n in range(ntiles):
                        p = pg[cnt["p"] % 4]
                        cnt["p"] += 1
                        for k in range(KC):
                            fw.op("pe", lambda: T.matmul(p.a, lhsT=aT.a[:, k, n * 128:(n + 1) * 128], rhs=w.a[:, k, :], start=(k == 0), stop=(k == KC - 1)), [aT, w], [p])
                        o = osb[cnt["o"] % 3]
                        cnt["o"] += 1
                        if cnt["o"] % 2:
                            fw.op("act", lambda: A.copy(out=o.a, in_=p.a), [p], [o])
                        else:
                            fw.op("dve", lambda: V.tensor_copy(out=o.a, in_=p.a), [p], [o])
                        c0 = cb * 512 - zcol0
                        ld(zts[n], zts[n].a[:, c0:c0 + 512], o.a, [o])

            kvb = [2, 3, 4, 5]
            inproj(x_for, lambda n: x_for.a[n * 128:(n + 1) * 128, :], NC, Gl, SHl, ab_w_in, ab_w_in, kvb, Zft, 1024)
            inproj(ctx_l, lambda n: ctx_l.a[n * 128:(n + 1) * 128, :], 2, Gc, SHc, ab_w_in, ab_w_in, kvb, Zct, 1024)
            inproj(x_own, lambda n: x_own.a[n * 128:(n + 1) * 128, :], NC, Gl, SHl, ab_w_in, ab_w_in, list(range(12)), Zt, 0)
            fw.barrier()
            chk()

        with ExitStack() as st:
            lg = sb(st, "lg", [128, 16])
            t1 = sb(st, "t1", [128, 16]); t2 = sb(st, "t2", [128, 16]); t3 = sb(st, "t3", [128, 16]); t4 = sb(st, "t4", [128, 16])
            ld(lg, lg.a, dl.a.to_broadcast((128, 16)), [dl])
            fw.op("act", lambda: A.activation(out=t1.a, in_=lg.a, func=AF.Exp, scale=-1.0), [lg], [t1])
            fw.op("dve", lambda: V.tensor_scalar(out=t2.a, in0=t1.a, scalar1=-0.25, scalar2=1.0 / 3.0, op0=ALU.mult, op1=ALU.add), [t1], [t2])
            fw.op("dve", lambda: V.tensor_tensor(out=t2.a, in0=t2.a, in1=t1.a, op=ALU.mult), [t2, t1], [t2])
            fw.op("dve", lambda: V.tensor_scalar(out=t2.a, in0=t2.a, scalar1=-0.5, scalar2=None, op0=ALU.add), [t2], [t2])
            fw.op("dve", lambda: V.tensor_tensor(out=t2.a, in0=t2.a, in1=t1.a, op=ALU.mult), [t2, t1], [t2])
            fw.op("dve", lambda: V.tensor_scalar(out=t2.a, in0=t2.a, scalar1=1.0, scalar2=None, op0=ALU.add), [t2], [t2])
            fw.op("dve", lambda: V.tensor_tensor(out=t2.a, in0=t2.a, in1=t1.a, op=ALU.mult), [t2, t1], [t2])
            fw.op("dve", lambda: V.tensor_scalar(out=t3.a, in0=t1.a, scalar1=1.0, scalar2=None, op0=ALU.add), [t1], [t3])
            fw.op("act", lambda: A.activation(out=t3.a, in_=t3.a, func=AF.Ln), [t3], [t3])
            fw.op("dve", lambda: V.tensor_scalar(out=t4.a, in0=t1.a, scalar1=0.1, scalar2=None, op0=ALU.is_lt), [t1], [t4])
            fw.op("dve", lambda: V.tensor_tensor(out=t2.a, in0=t2.a, in1=t3.a, op=ALU.subtract), [t2, t3], [t2])
            fw.op("dve", lambda: V.tensor_tensor(out=t2.a, in0=t2.a, in1=t4.a, op=ALU.mult), [t2, t4], [t2])
            fw.op("dve", lambda: V.tensor_tensor(out=t2.a, in0=t2.a, in1=t3.a, op=ALU.add), [t2, t3], [t2])
            fw.op("dve", lambda: V.tensor_scalar(out=lg.a, in0=t2.a, scalar1=-1.0, scalar2=None, op0=ALU.mult), [t2], [lg])

            ropeT = sb(st, "ropeT", [128, 6, NC, 64])
            for r in range(6):
                ld(ropeT, ropeT.a[:, r, :, :], rope.a[r].rearrange("(n p) c -> p n c", p=128), [rope])
            NE = NC + 4
            ew = sb(st, "ew", [128, NE]); ewB = sb(st, "ewB", [128, NE])
            dcol = sb(st, "dcol", [128, 8])
            Dh = sb(st, "Dh", [128, 128]); dtmp = sb(st, "dtmp", [128, 128])
            qs = [sb(st, "qs%d" % i, [128, NC, 128]) for i in range(1)] * 2
            ks = [sb(st, "ks%d" % i, [128, NC, 128]) for i in range(1)] * 2
            vs = [sb(st, "vs%d" % i, [128, NC, 128]) for i in range(1)] * 2
            gs = [sb(st, "gs%d" % i, [128, NC, 128]) for i in range(1)] * 2
            kf = sb(st, "kf", [128, NC + 2, 128]); vf = sb(st, "vf", [128, NC + 2, 128])
            rq = sb(st, "rq", [128, NC, 128]); rk = sb(st, "rk", [128, NC, 128]); rtmp = sb(st, "rtmp", [128, NC, 64])
            qb = sb(st, "qb", [128, NC, 128], BF16); kb = sb(st, "kb", [128, NC, 128], BF16)
            vb = sb(st, "vb", [128, NC, 128], BF16); vA = sb(st, "vA", [128, NC, 128], BF16); vB = sb(st, "vB", [128, NC, 128], BF16)
            kfb = sb(st, "kfb", [128, NC + 2, 128], BF16); vfB = sb(st, "vfB", [128, NC + 2, 128], BF16); vcA = sb(st, "vcA", [128, 2, 128], BF16)
            SA = sb(st, "SA", [128, NC + 1, 128]); SB = sb(st, "SB", [128, NC + 1, 128])
            SAb = sb(st, "SAb", [128, NC + 1, 128], BF16); SBb = sb(st, "SBb", [128, NC + 1, 128], BF16)
            qT = [sb(st, "qT%d" % i, [128, 2, 128], BF16) for i in range(2)]
            ATb = [sb(st, "ATb%d" % i, [128, 128], BF16) for i in range(2)]
            ysb = [sb(st, "ysb%d" % i, [128, 128]) for i in range(2)]
            stt_ = sb(st, "bnst", [128, 6]); mv = sb(st, "mv", [128, 2]); rs = sb(st, "rs", [128, 1])
            sg = sb(st, "sg", [128, 128])
            rout = [sb(st, "rout%d" % i, [128, NC, 128]) for i in range(2)]
            yall = sb(st, "yall", [128, NC, 128]); stall = sb(st, "stall", [128, NC, 6]); mvall = sb(st, "mvall", [128, NC, 2]); rsall = sb(st, "rsall", [128, NC, 1])
            pS = [ps(st, "pS%d" % i, [128, 128]) for i in range(2)]
            pU = [ps(st, "pU%d" % i, [128, 2, 128]) for i in range(2)]
            pTq = ps(st, "pTq", [128, 2, 128], BF16)
            pSc = ps(st, "pSc", [128, 128])
            pY = ps(st, "pY", [128, 3, 128])

            def rope_apply(dst, src, ci, si, nchunk):
                x1 = src.a[:, 0:nchunk, 0:64]; x2 = src.a[:, 0:nchunk, 64:128]
                cth = ropeT.a[:, ci, 0:nchunk, :]; sth = ropeT.a[:, si, 0:nchunk, :]
                tm = rtmp.a[:, 0:nchunk, :]
                fw.op("dve", lambda: V.tensor_tensor(out=dst.a[:, 0:nchunk, 0:64], in0=x1, in1=cth, op=ALU.mult), [src, ropeT], [dst])
                fw.op("dve", lambda: V.tensor_tensor(out=tm, in0=x2, in1=sth, op=ALU.mult), [src, ropeT], [rtmp])
                fw.op("dve", lambda: V.tensor_tensor(out=dst.a[:, 0:nchunk, 0:64], in0=dst.a[:, 0:nchunk, 0:64], in1=tm, op=ALU.subtract), [dst, rtmp], [dst])
                fw.op("dve", lambda: V.tensor_tensor(out=dst.a[:, 0:nchunk, 64:128], in0=x1, in1=sth, op=ALU.mult), [src, ropeT], [dst])
                fw.op("dve", lambda: V.tensor_tensor(out=tm, in0=x2, in1=cth, op=ALU.mult), [src, ropeT], [rtmp])
                fw.op("dve", lambda: V.tensor_tensor(out=dst.a[:, 0:nchunk, 64:128], in0=dst.a[:, 0:nchunk, 64:128], in1=tm, op=ALU.add), [dst, rtmp], [dst])

            for h in range(NH):
                j = h % 2
                c0 = h * 128
                ld(qs[j], qs[j].a, Zd[1:NT + 1, c0:c0 + 128].rearrange("(n p) c -> p n c", p=128), Zt)
                ld(ks[j], ks[j].a, Zd[1:NT + 1, 1024 + c0:1024 + c0 + 128].rearrange("(n p) c -> p n c", p=128), Zt)
                ld(vs[j], vs[j].a, Zd[1:NT + 1, 2048 + c0:2048 + c0 + 128].rearrange("(n p) c -> p n c", p=128), Zt)
                ld(gs[j], gs[j].a, Zd[1:NT + 1, 3072 + c0:3072 + c0 + 128].rearrange("(n p) c -> p n c", p=128), Zt)
                ld(kf, kf.a[:, 0:NC, :], Zfd[:, c0:c0 + 128].rearrange("(n p) c -> p n c", p=128), Zft)
                ld(kf, kf.a[:, NC:NC + 2, :], Zcd[:, c0:c0 + 128].rearrange("(n p) c -> p n c", p=128), Zct)
                ld(vf, vf.a[:, 0:NC, :], Zfd[:, 1024 + c0:1024 + c0 + 128].rearrange("(n p) c -> p n c", p=128), Zft)
                ld(vf, vf.a[:, NC:NC + 2, :], Zcd[:, 1024 + c0:1024 + c0 + 128].rearrange("(n p) c -> p n c", p=128), Zct)
                lgA = lg.a[:, h:h + 1]; lgB = lg.a[:, 8 + h:9 + h]
                fw.op("dve", lambda: V.tensor_scalar(out=dcol.a[:, 0:1], in0=cs.a[:, cc + 0:cc + 1], scalar1=lgA, scalar2=None, op0=ALU.mult), [cs, lg], [dcol])
                fw.op("dve", lambda: V.tensor_scalar(out=dcol.a[:, 1:2], in0=cs.a[:, cc + 1:cc + 2], scalar1=lgB, scalar2=None, op0=ALU.mult), [cs, lg], [dcol])
                fw.op("dve", lambda: V.tensor_scalar(out=dcol.a[:, 2:3], in0=cs.a[:, cc + 2:cc + 3], scalar1=lgA, scalar2=None, op0=ALU.mult), [cs, lg], [dcol])
                fw.op("dve", lambda: V.tensor_scalar(out=dcol.a[:, 3:4], in0=cs.a[:, cc + 3:cc + 4], scalar1=lgB, scalar2=None, op0=ALU.mult), [cs, lg], [dcol])
                fw.op("dve", lambda: V.tensor_scalar(out=dcol.a[:, 4:5], in0=cs.a[:, cc + 4:cc + 5], scalar1=lgA, scalar2=None, op0=ALU.mult), [cs, lg], [dcol])
                fw.op("dve", lambda: V.tensor_scalar(out=dcol.a[:, 5:6], in0=cs.a[:, cc + 4:cc + 5], scalar1=lgB, scalar2=None, op0=ALU.mult), [cs, lg], [dcol])
                fw.op("act", lambda: A.activation(out=dcol.a[:, 0:6], in_=dcol.a[:, 0:6], func=AF.Exp), [dcol], [dcol])
                fw.op("dve", lambda: V.tensor_scalar(out=ewB.a[:, 0:NC + 2], in0=cs.a[:, ET:ET + NC + 2], scalar1=lgB, scalar2=None, op0=ALU.mult), [cs, lg], [ewB])
                fw.op("dve", lambda: V.tensor_scalar(out=ewB.a[:, NC + 2:NC + 4], in0=cs.a[:, ET + NC + 2:ET + NC + 4], scalar1=lgA, scalar2=None, op0=ALU.mult), [cs, lg], [ewB])
                fw.op("act", lambda: A.activation(out=ew.a, in_=ewB.a, func=AF.Exp), [ewB], [ew])
                fw.op("dve", lambda: V.tensor_scalar(out=dtmp.a, in0=R1, scalar1=lgA, scalar2=None, op0=ALU.mult), [cs, lg], [dtmp])
                fw.op("dve", lambda: V.scalar_tensor_tensor(out=dtmp.a, in0=R2, scalar=lgB, in1=dtmp.a, op0=ALU.mult, op1=ALU.add), [cs, lg, dtmp], [dtmp])
                fw.op("act", lambda: A.activation(out=Dh.a, in_=dtmp.a, func=AF.Exp), [dtmp], [Dh])
                rope_apply(rq, qs[j], 0, 1, NC)
                rope_apply(rk, ks[j], 2, 3, NC)
                fw.op("act", lambda: A.copy(out=qb.a, in_=rq.a), [rq], [qb])
                fw.op("act", lambda: A.copy(out=kb.a, in_=rk.a), [rk], [kb])
                fw.op("act", lambda: A.copy(out=vb.a, in_=vs[j].a), [vs[j]], [vb])
                fw.op("dve", lambda: V.tensor_scalar(out=vA.a, in0=vs[j].a, scalar1=dcol.a[:, 0:1], scalar2=None, op0=ALU.mult), [vs[j], dcol], [vA])
                fw.op("dve", lambda: V.tensor_scalar(out=vB.a, in0=vs[j].a, scalar1=dcol.a[:, 1:2], scalar2=None, op0=ALU.mult), [vs[j], dcol], [vB])
                rope_apply(rk, kf, 4, 5, NC)
                fw.op("act", lambda: A.copy(out=kfb.a[:, 0:NC, :], in_=rk.a), [rk], [kfb])
                fw.op("act", lambda: A.copy(out=kfb.a[:, NC:NC + 2, :], in_=kf.a[:, NC:NC + 2, :]), [kf], [kfb])
                for c in range(NC + 2):
                    fw.op("dve", lambda: V.tensor_scalar(out=vfB.a[:, c, :], in0=vf.a[:, c, :], scalar1=ew.a[:, c:c + 1], scalar2=None, op0=ALU.mult), [vf, ew], [vfB])
                for c in range(2):
                    fw.op("dve", lambda: V.tensor_scalar(out=vcA.a[:, c, :], in0=vf.a[:, NC + c, :], scalar1=ew.a[:, NC + 2 + c:NC + 3 + c], scalar2=None, op0=ALU.mult), [vf, ew], [vcA])
                for c in range(2):
                    fw.op("pe", lambda: T.matmul(pS[0].a, lhsT=kfb.a[:, NC + c, :], rhs=vcA.a[:, c, :], start=(c == 0), stop=(c == 1)), [kfb, vcA], [pS[0]])
                fw.op("act", lambda: A.copy(out=SA.a[:, 0, :], in_=pS[0].a), [pS[0]], [SA])
                for c in range(NC + 2):
                    fw.op("pe", lambda: T.matmul(pS[1].a, lhsT=kfb.a[:, c, :], rhs=vfB.a[:, c, :], start=(c == 0), stop=(c == NC + 1)), [kfb, vfB], [pS[1]])
                fw.op("act", lambda: A.copy(out=SB.a[:, NC, :], in_=pS[1].a), [pS[1]], [SB])
                for n in range(NC):
                    p = pU[n % 2]
                    fw.op("pe", lambda: T.matmul(p.a[:, 0, :], lhsT=kb.a[:, n, :], rhs=vA.a[:, n, :], start=True, stop=True), [kb, vA], [p])
                    fw.op("dve", lambda: V.scalar_tensor_tensor(out=SA.a[:, n + 1, :], in0=SA.a[:, n, :], scalar=dcol.a[:, 4:5], in1=p.a[:, 0, :], op0=ALU.mult, op1=ALU.add), [SA, dcol, p], [SA])
                for n in range(NC - 1, -1, -1):
                    p = pU[n % 2]
                    fw.op("pe", lambda: T.matmul(p.a[:, 1, :], lhsT=kb.a[:, n, :], rhs=vB.a[:, n, :], start=True, stop=True), [kb, vB], [p])
                    fw.op("dve", lambda: V.scalar_tensor_tensor(out=SB.a[:, n, :], in0=SB.a[:, n + 1, :], scalar=dcol.a[:, 5:6], in1=p.a[:, 1, :], op0=ALU.mult, op1=ALU.add), [SB, dcol, p], [SB])
                fw.op("act", lambda: A.copy(out=SAb.a, in_=SA.a), [SA], [SAb])
                fw.op("act", lambda: A.copy(out=SBb.a, in_=SB.a), [SB], [SBb])
                ro = rout[j]
                for n in range(NC):
                    qt = qT[n % 2]; at = ATb[n % 2]; y = ysb[n % 2]
                    fw.op("pe", lambda: T.transpose(out=pTq.a[:, 0, :], in_=qb.a[:, n, :], identity=identb.a), [qb, identb], [pTq])
                    fw.op("pe", lambda: T.transpose(out=pTq.a[:, 1, :], in_=kb.a[:, n, :], identity=identb.a), [kb, identb], [pTq])
                    fw.op("act", lambda: A.copy(out=qt.a, in_=pTq.a), [pTq], [qt])
                    fw.op("pe", lambda: T.matmul(pSc.a, lhsT=qt.a[:, 1, :], rhs=qt.a[:, 0, :], start=True, stop=True), [qt], [pSc])
                    fw.op("dve", lambda: V.tensor_tensor(out=at.a, in0=pSc.a, in1=Dh.a, op=ALU.mult), [pSc, Dh], [at])
                    fw.op("pe", lambda: T.matmul(pY.a[:, 0, :], lhsT=at.a, rhs=vb.a[:, n, :], start=True, stop=True), [at, vb], [pY])
                    fw.op("pe", lambda: T.matmul(pY.a[:, 1, :], lhsT=qt.a[:, 0, :], rhs=SAb.a[:, n, :], start=True, stop=True), [qt, SAb], [pY])
                    fw.op("pe", lambda: T.matmul(pY.a[:, 2, :], lhsT=qt.a[:, 0, :], rhs=SBb.a[:, n + 1, :], start=True, stop=True), [qt, SBb], [pY])
                    fw.op("act", lambda: A.copy(out=yall.a[:, n, :], in_=pY.a[:, 0, :]), [pY], [yall])
                    fw.op("dve", lambda: V.scalar_tensor_tensor(out=yall.a[:, n, :], in0=pY.a[:, 1, :], scalar=dcol.a[:, 2:3], in1=yall.a[:, n, :], op0=ALU.mult, op1=ALU.add), [pY, dcol, yall], [yall])
                    fw.op("dve", lambda: V.scalar_tensor_tensor(out=yall.a[:, n, :], in0=pY.a[:, 2, :], scalar=dcol.a[:, 3:4], in1=yall.a[:, n, :], op0=ALU.mult, op1=ALU.add), [pY, dcol, yall], [yall])
                for n in range(NC):
                    fw.op("dve", lambda: V.bn_stats(out=stall.a[:, n, :], in_=yall.a[:, n, :]), [yall], [stall])
                for n in range(NC):
                    fw.op("dve", lambda: V.bn_aggr(out=mvall.a[:, n, :], in_=stall.a[:, n, :]), [stall], [mvall])
                fw.op("dve", lambda: V.tensor_scalar(out=rsall.a, in0=mvall.a[:, :, 1:2], scalar1=EPS, scalar2=None, op0=ALU.add), [mvall], [rsall])
                fw.op("act", lambda: A.activation(out=rsall.a, in_=rsall.a, func=AF.Sqrt), [rsall], [rsall])
                fw.op("dve", lambda: V.reciprocal(out=rsall.a, in_=rsall.a), [rsall], [rsall])
                for n in range(NC):
                    fw.op("dve", lambda: V.tensor_scalar(out=yall.a[:, n, :], in0=yall.a[:, n, :], scalar1=mvall.a[:, n, 0:1], scalar2=rsall.a[:, n, :], op0=ALU.subtract, op1=ALU.mult), [yall, mvall, rsall], [yall])
                fw.op("act", lambda: A.activation(out=rq.a, in_=gs[j].a, func=AF.Silu), [gs[j]], [rq])
                fw.op("dve", lambda: V.tensor_tensor(out=ro.a, in0=yall.a, in1=rq.a, op=ALU.mult), [yall, rq], [ro])
                for n in range(NC):
                    ld(Rt[n], Rt[n].a[:, c0:c0 + 128], ro.a[:, n, :], [ro])
            fw.barrier()
            chk()

        def gelu(dst, src, tmp, reads):
            fw.op("dve", lambda: V.tensor_tensor(out=tmp.a, in0=src.a, in1=src.a, op=ALU.mult), reads, [tmp])
            fw.op("dve", lambda: V.tensor_scalar(out=tmp.a, in0=tmp.a, scalar1=0.044715, scalar2=1.0, op0=ALU.mult, op1=ALU.add), [tmp], [tmp])
            fw.op("dve", lambda: V.tensor_tensor(out=tmp.a, in0=tmp.a, in1=src.a, op=ALU.mult), [tmp] + reads, [tmp])
            fw.op("act", lambda: A.activation(out=tmp.a, in_=tmp.a, func=AF.Sigmoid, scale=1.5957691216057308), [tmp], [tmp])
            fw.op("dve", lambda: V.tensor_tensor(out=dst.a, in0=tmp.a, in1=src.a, op=ALU.mult), [tmp] + reads, [dst])

        with ExitStack() as st:
            us = [sb(st, "us%d" % i, [128, NC, 128]) for i in range(2)]
            vs2 = [sb(st, "vs2%d" % i, [128, NC, 128]) for i in range(2)]
            gu = sb(st, "gu", [128, NC, 128]); gv = sb(st, "gv", [128, NC, 128]); tmpg = sb(st, "tmpg", [128, NC, 128])
            vn = sb(st, "vn", [128, NC, 128], BF16)
            wsf = sb(st, "wsf", [128, 8, 128]); wsb = sb(st, "wsb", [128, 8, 128], BF16)
            bsb = sb(st, "bsb", [128, 8])
            stall2 = sb(st, "stall2", [128, NC, 6]); mvall2 = sb(st, "mvall2", [128, NC, 2]); rsall2 = sb(st, "rsall2", [128, NC, 1])
            ro2 = [sb(st, "ro2%d" % i, [128, NC, 128]) for i in range(2)]
            pG = [ps(st, "pG%d" % i, [128, 128]) for i in range(2)]
            ld(wsf, wsf.a, wsT.a.rearrange("g q p -> q g p"), [wsT])
            ld(bsb, bsb.a, bsT.a, [bsT])
            fw.op("act", lambda: A.copy(out=wsb.a, in_=wsf.a), [wsf], [wsb])
            for g in range(8):
                j = g % 2
                c0 = g * 128
                ld(us[j], us[j].a, Zd[1:NT + 1, 4096 + c0:4096 + c0 + 128].rearrange("(n p) c -> p n c", p=128), Zt)
                ld(vs2[j], vs2[j].a, Zd[1:NT + 1, 5120 + c0:5120 + c0 + 128].rearrange("(n p) c -> p n c", p=128), Zt)
                gelu(gu, us[j], tmpg, [us[j]])
                gelu(gv, vs2[j], tmpg, [vs2[j]])
                for n in range(NC):
                    fw.op("dve", lambda: V.bn_stats(out=stall2.a[:, n, :], in_=gv.a[:, n, :]), [gv], [stall2])
                for n in range(NC):
                    fw.op("dve", lambda: V.bn_aggr(out=mvall2.a[:, n, :], in_=stall2.a[:, n, :]), [stall2], [mvall2])
                fw.op("dve", lambda: V.tensor_scalar(out=rsall2.a, in0=mvall2.a[:, :, 1:2], scalar1=EPS, scalar2=None, op0=ALU.add), [mvall2], [rsall2])
                fw.op("act", lambda: A.activation(out=rsall2.a, in_=rsall2.a, func=AF.Sqrt), [rsall2], [rsall2])
                fw.op("dve", lambda: V.reciprocal(out=rsall2.a, in_=rsall2.a), [rsall2], [rsall2])
                for n in range(NC):
                    fw.op("dve", lambda: V.tensor_scalar(out=vn.a[:, n, :], in0=gv.a[:, n, :], scalar1=mvall2.a[:, n, 0:1], scalar2=rsall2.a[:, n, :], op0=ALU.subtract, op1=ALU.mult), [gv, mvall2, rsall2], [vn])
                for n in range(NC):
                    p = pG[n % 2]
                    fw.op("pe", lambda: T.matmul(p.a, lhsT=wsb.a[:, g, :], rhs=vn.a[:, n, :], start=True, stop=True), [wsb, vn], [p])
                    fw.op("dve", lambda: V.scalar_tensor_tensor(out=ro2[j].a[:, n, :], in0=p.a, scalar=bsb.a[:, g:g + 1], in1=gu.a[:, n, :], op0=ALU.add, op1=ALU.mult), [p, bsb, gu], [ro2[j]])
                for n in range(NC):
                    ld(Rt[n], Rt[n].a[:, 1024 + c0:1024 + c0 + 128], ro2[j].a[:, n, :], [ro2[j]])
            fw.barrier()
            chk()

        def outproj_and_moe(i, w_out, x_src_tiles, last):
            with ExitStack() as st:
                with ExitStack() as s2:
                    GT1 = sb(s2, "GT1", [128, D])
                    ld(GT1, GT1.a, modrow(i, 0, 2), [MOD])
                    rf = [sb(s2, "rf%d" % k, [128, D]) for k in range(2)]
                    rb = [sb(s2, "rb%d" % k, [128, D], BF16) for k in range(2)]
                    wb = [sb(s2, "wo%d" % k, [128, KC, 512], BF16) for k in range(2)]
                    xin = [sb(s2, "xin%d" % k, [128, 512]) for k in range(3)]
                    pT = [ps(s2, "pT%d" % k, [128, 8, 128], BF16) for k in range(2)]
                    pg = [ps(s2, "pg%d" % k, [128, 512]) for k in range(4)]
                    rT = sb(s2, "rT", [128, KC, NT], BF16)
                    for n in range(NC):
                        ld(rf[n % 2], rf[n % 2].a, Rt[n].a, [Rt[n]])
                        fw.op("act", lambda: A.copy(out=rb[n % 2].a, in_=rf[n % 2].a), [rf[n % 2]], [rb[n % 2]])
                        transpose_tile(rb[n % 2], rT, n * 128, pT)
                    ci = 0
                    for cb in range(4):
                        w = wb[cb % 2]
                        load_w(w, w_out.a[:, cb * 512:(cb + 1) * 512], w_out, KC)
                        for n in range(NC):
                            p = pg[ci % 4]; xi = xin[ci % 3]; ci += 1
                            xb_, xap = x_src_tiles[n]
                            ld(xi, xi.a, xap[:, cb * 512:(cb + 1) * 512], [xb_])
                            for k in range(KC):
                                fw.op("pe", lambda: T.matmul(p.a, lhsT=rT.a[:, k, n * 128:(n + 1) * 128], rhs=w.a[:, k, :], start=(k == 0), stop=(k == KC - 1)), [rT, w], [p])
                            fw.op("dve", lambda: V.tensor_tensor(out=p.a, in0=p.a, in1=GT1.a[:, cb * 512:(cb + 1) * 512], op=ALU.mult), [p, GT1], [p])
                            fw.op("dve", lambda: V.tensor_tensor(out=xi.a, in0=p.a, in1=xi.a, op=ALU.add), [p, xi], [xi])
                            ld(Xt[n], Xt[n].a[:, cb * 512:(cb + 1) * 512], xi.a, [xi])
                    fw.barrier()
                    chk()
                NS = 2 * NC + 16
                MA16 = sb(st, "MA16", [128, NC, 16]); MB16 = sb(st, "MB16", [128, NC, 16]); wAB = sb(st, "wAB", [128, NC, 2])
                slotAi = sb(st, "slotAi", [128, NC], I32); slotBi = sb(st, "slotBi", [128, NC], I32)
                WIi = sb(st, "WIi", [128, NS, 16], I32)
                with ExitStack() as s2:
                    Ftok = sb(s2, "Ftok", [128, NC, D], BF16)
                    Gf, SHf = make_GS(s2, i, 0, 3, 4, g_ffn.a[i:i + 1, :], g_ffn)
                    bufs = {"xt": [sb(s2, "xt%d" % k, [128, D]) for k in range(2)], "i": 0, "junk": sb(s2, "junk", [128, D]),
                            "ssq": sb(s2, "ssq", [128, 1]), "rstd": sb(s2, "rstd", [128, 1])}
                    ff = [sb(s2, "ff%d" % k, [128, D]) for k in range(2)]
                    fTf = sb(s2, "fTf", [128, KC, 128])
                    wrs = sb(s2, "wrs", [128, KC, 20]); brs = sb(s2, "brs", [128, 20])
                    L = sb(s2, "L", [128, 20]); m1 = sb(s2, "m1", [128, 4]); oh1 = sb(s2, "oh1", [128, 4]); e1 = sb(s2, "e1", [128, 4])
                    l2 = sb(s2, "l2", [128, 4]); l2b = sb(s2, "l2b", [128, 4]); ohA = sb(s2, "ohA", [128, 4]); ohB = sb(s2, "ohB", [128, 4])
                    pF = [ps(s2, "pF%d" % k, [128, 4, 128]) for k in range(2)]
                    pL = ps(s2, "pL", [128, 20])
                    ld(wrs, wrs.a, wr.a[i].rearrange("(k p) n -> p k n", p=128), [wr])
                    ld(brs, brs.a, br.a[i:i + 1, :].to_broadcast((128, 20)), [br])
                    for n in range(NC):
                        f = ff[n % 2]
                        norm_mod_tile(s2, bufs, Xt[n], Xt[n].a, Gf, SHf, out_f=f)
                        fw.op("act", lambda: A.copy(out=Ftok.a[:, n, :], in_=f.a), [f], [Ftok])
                        for k0 in range(0, KC, 4):
                            p = pF[(k0 // 4) % 2]
                            for k in range(k0, k0 + 4):
                                fw.op("pe", lambda: T.transpose(out=p.a[:, k - k0, :], in_=f.a[:, k * 128:(k + 1) * 128], identity=identF.a), [f, identF], [p])
                            fw.op("act", lambda: A.copy(out=fTf.a[:, k0:k0 + 4, :], in_=p.a), [p], [fTf])
                        for k in range(KC):
                            fw.op("pe", lambda: T.matmul(pL.a, lhsT=fTf.a[:, k, :], rhs=wrs.a[:, k, :], start=(k == 0), stop=(k == KC - 1)), [fTf, wrs], [pL])
                        fw.op("dve", lambda: V.tensor_tensor(out=L.a, in0=pL.a, in1=brs.a, op=ALU.add), [pL, brs], [L])
                        fw.op("dve", lambda: V.tensor_reduce(out=m1.a[:, 0:1], in_=L.a[:, 0:4], axis=mybir.AxisListType.X, op=ALU.max), [L], [m1])
                        fw.op("dve", lambda: V.tensor_scalar(out=oh1.a, in0=L.a[:, 0:4], scalar1=m1.a[:, 0:1], scalar2=None, op0=ALU.is_equal), [L, m1], [oh1])
                        fw.op("dve", lambda: V.tensor_scalar(out=e1.a, in0=L.a[:, 0:4], scalar1=m1.a[:, 0:1], scalar2=None, op0=ALU.subtract), [L, m1], [e1])
                        fw.op("act", lambda: A.activation(out=e1.a, in_=e1.a, func=AF.Exp), [e1], [e1])
                        fw.op("dve", lambda: V.tensor_reduce(out=m1.a[:, 1:2], in_=e1.a, axis=mybir.AxisListType.X, op=ALU.add), [e1], [m1])
                        fw.op("dve", lambda: V.reciprocal(out=m1.a[:, 1:2], in_=m1.a[:, 1:2]), [m1], [m1])
                        fw.op("dve", lambda: V.tensor_scalar(out=l2.a, in0=L.a[:, 4:8], scalar1=oh1.a[:, 0:1], scalar2=None, op0=ALU.mult), [L, oh1], [l2])
                        for g in range(1, 4):
                            fw.op("dve", lambda: V.scalar_tensor_tensor(out=l2.a, in0=L.a[:, 4 + 4 * g:8 + 4 * g], scalar=oh1.a[:, g:g + 1], in1=l2.a, op0=ALU.mult, op1=ALU.add), [L, oh1, l2], [l2])
                        fw.op("dve", lambda: V.tensor_reduce(out=m1.a[:, 2:3], in_=l2.a, axis=mybir.AxisListType.X, op=ALU.max), [l2], [m1])
                        fw.op("dve", lambda: V.tensor_scalar(out=ohA.a, in0=l2.a, scalar1=m1.a[:, 2:3], scalar2=None, op0=ALU.is_equal), [l2, m1], [ohA])
                        fw.op("dve", lambda: V.scalar_tensor_tensor(out=l2b.a, in0=ohA.a, scalar=-1e30, in1=l2.a, op0=ALU.mult, op1=ALU.add), [ohA, l2], [l2b])
                        fw.op("dve", lambda: V.tensor_reduce(out=m1.a[:, 3:4], in_=l2b.a, axis=mybir.AxisListType.X, op=ALU.max), [l2b], [m1])
                        fw.op("dve", lambda: V.tensor_scalar(out=ohB.a, in0=l2b.a, scalar1=m1.a[:, 3:4], scalar2=None, op0=ALU.is_equal), [l2b, m1], [ohB])
                        fw.op("dve", lambda: V.tensor_tensor(out=e1.a[:, 0:1], in0=m1.a[:, 3:4], in1=m1.a[:, 2:3], op=ALU.subtract), [m1], [e1])
                        fw.op("act", lambda: A.activation(out=e1.a[:, 0:1], in_=e1.a[:, 0:1], func=AF.Exp), [e1], [e1])
                        fw.op("dve", lambda: V.tensor_scalar(out=e1.a[:, 1:2], in0=e1.a[:, 0:1], scalar1=1.0, scalar2=None, op0=ALU.add), [e1], [e1])
                        fw.op("dve", lambda: V.reciprocal(out=e1.a[:, 1:2], in_=e1.a[:, 1:2]), [e1], [e1])
                        fw.op("dve", lambda: V.tensor_tensor(out=e1.a[:, 2:3], in0=e1.a[:, 0:1], in1=e1.a[:, 1:2], op=ALU.mult), [e1], [e1])
                        fw.op("dve", lambda: V.tensor_scalar(out=wAB.a[:, n, :], in0=e1.a[:, 1:3], scalar1=m1.a[:, 1:2], scalar2=None, op0=ALU.mult), [e1, m1], [wAB])
                        for g in range(4):
                            fw.op("dve", lambda: V.tensor_scalar(out=MA16.a[:, n, 4 * g:4 * g + 4], in0=ohA.a, scalar1=oh1.a[:, g:g + 1], scalar2=None, op0=ALU.mult), [ohA, oh1], [MA16])
                            fw.op("dve", lambda: V.tensor_scalar(out=MB16.a[:, n, 4 * g:4 * g + 4], in0=ohB.a, scalar1=oh1.a[:, g:g + 1], scalar2=None, op0=ALU.mult), [ohB, oh1], [MB16])
                    Mf = sb(s2, "Mf", [128, NC, 16]); Mb = sb(s2, "Mb", [128, NC * 16], BF16)
                    cntS = sb(s2, "cntS", [128, NC, 16]); ptS = sb(s2, "ptS", [128, NC, 16]); rk = sb(s2, "rk", [128, NC, 16]); rk2 = sb(s2, "rk2", [128, NC, 16])
                    ne = sb(s2, "ne", [128, 16]); tl = sb(s2, "tl", [128, 16]); se = sb(s2, "se", [128, 16]); st128 = sb(s2, "st128", [128, 16])
                    slf = sb(s2, "slf", [128, 2, NC]); ek = sb(s2, "ek", [128, NS]); chg = sb(s2, "chg", [128, NS]); off = sb(s2, "off", [128, NS])
                    WIf = sb(s2, "WIf", [128, NS, 16])
                    trib = sb(s2, "trib", [128, 2, 128], BF16)
                    pP = [ps(s2, "pP%d" % k, [128, NC * 16]) for k in range(2)]
                    fw.op("dve", lambda: V.tensor_copy(out=trib.a, in_=cs.a[:, TRI:TRI + 256]), [cs], [trib])
                    fw.op("dve", lambda: V.tensor_tensor(out=Mf.a, in0=MA16.a, in1=MB16.a, op=ALU.add), [MA16, MB16], [Mf])
                    fw.op("dve", lambda: V.tensor_copy(out=Mb.a, in_=Mf.a), [Mf], [Mb])
                    fw.op("pe", lambda: T.matmul(pP[0].a, lhsT=trib.a[:, 0, :], rhs=Mb.a, start=True, stop=True), [trib, Mb], [pP[0]])
                    fw.op("pe", lambda: T.matmul(pP[1].a, lhsT=trib.a[:, 1, :], rhs=Mb.a, start=True, stop=True), [trib, Mb], [pP[1]])
                    fw.op("act", lambda: A.copy(out=cntS.a, in_=pP[1].a), [pP[1]], [cntS])
                    fw.op("dve", lambda: V.memset(ptS.a[:, 0, :], 0.0), [], [ptS])
                    for n in range(1, NC):
                        fw.op("dve", lambda: V.tensor_tensor(out=ptS.a[:, n, :], in0=ptS.a[:, n - 1, :], in1=cntS.a[:, n - 1, :], op=ALU.add), [ptS, cntS], [ptS])
                    fw.op("dve", lambda: V.tensor_tensor(out=ne.a, in0=ptS.a[:, NC - 1, :], in1=cntS.a[:, NC - 1, :], op=ALU.add), [ptS, cntS], [ne])
                    fw.op("dve", lambda: V.memset(tl.a, 0.0), [], [tl])
                    for j in range(NC):
                        fw.op("dve", lambda: V.scalar_tensor_tensor(out=tl.a, in0=ne.a, scalar=128.0 * j, in1=tl.a, op0=ALU.is_gt, op1=ALU.add), [ne, tl], [tl])
                    fw.op("dve", lambda: V.memset(se.a[:, 0:1], 0.0), [], [se])
                    for e in range(1, 16):
                        fw.op("dve", lambda: V.tensor_tensor(out=se.a[:, e:e + 1], in0=se.a[:, e - 1:e], in1=tl.a[:, e - 1:e], op=ALU.add), [se, tl], [se])
                    fw.op("dve", lambda: V.tensor_scalar(out=st128.a, in0=se.a, scalar1=128.0, scalar2=None, op0=ALU.mult), [se], [st128])
                    fw.op("dve", lambda: V.tensor_tensor(out=rk.a, in0=pP[0].a, in1=ptS.a, op=ALU.add), [pP[0], ptS], [rk])
                    for n in range(NC):
                        fw.op("dve", lambda: V.tensor_tensor(out=rk.a[:, n, :], in0=rk.a[:, n, :], in1=st128.a, op=ALU.add), [rk, st128], [rk])
                    fw.op("dve", lambda: V.tensor_tensor(out=rk2.a, in0=rk.a, in1=MA16.a, op=ALU.mult), [rk, MA16], [rk2])
                    fw.op("dve", lambda: V.tensor_reduce(out=slf.a[:, 0, :], in_=rk2.a, axis=mybir.AxisListType.X, op=ALU.add), [rk2], [slf])
                    fw.op("dve", lambda: V.tensor_tensor(out=rk2.a, in0=rk.a, in1=MB16.a, op=ALU.mult), [rk, MB16], [rk2])
                    fw.op("dve", lambda: V.tensor_reduce(out=slf.a[:, 1, :], in_=rk2.a, axis=mybir.AxisListType.X, op=ALU.add), [rk2], [slf])
                    fw.op("dve", lambda: V.tensor_copy(out=slotAi.a, in_=slf.a[:, 0, :]), [slf], [slotAi])
                    fw.op("dve", lambda: V.tensor_copy(out=slotBi.a, in_=slf.a[:, 1, :]), [slf], [slotBi])
                    fw.op("dve", lambda: V.memset(ek.a, -1.0), [], [ek])
                    for e in range(16):
                        fw.op("dve", lambda: V.scalar_tensor_tensor(out=ek.a, in0=cs.a[:, KV:KV + NS], scalar=se.a[:, e:e + 1], in1=ek.a, op0=ALU.is_ge, op1=ALU.add), [cs, se, ek], [ek])
                    fw.op("dve", lambda: V.memset(chg.a[:, 0:1], 1.0), [], [chg])
                    fw.op("dve", lambda: V.tensor_tensor(out=chg.a[:, 1:NS], in0=ek.a[:, 1:NS], in1=ek.a[:, 0:NS - 1], op=ALU.not_equal), [ek], [chg])
                    fw.op("dve", lambda: V.tensor_scalar(out=off.a, in0=chg.a, scalar1=-1.0e6, scalar2=1.0e6, op0=ALU.mult, op1=ALU.add), [chg], [off])
                    fw.op("dve", lambda: V.scalar_tensor_tensor(out=off.a, in0=ek.a, scalar=1024.0, in1=off.a, op0=ALU.mult, op1=ALU.add), [ek, off], [off])
                    for k in range(NS):
                        fw.op("dve", lambda: V.tensor_scalar(out=WIf.a[:, k, :], in0=cs.a[:, BASE:BASE + 16], scalar1=off.a[:, k:k + 1], scalar2=None, op0=ALU.add), [cs, off], [WIf])
                    fw.op("dve", lambda: V.tensor_copy(out=WIi.a, in_=WIf.a), [WIf], [WIi])
                    for n in range(NC):
                        for sl_ in (slotAi, slotBi):
                            fw.dma("pool", lambda: G.indirect_dma_start(out=FS.a, out_offset=bass.IndirectOffsetOnAxis(ap=sl_.a[:, n:n + 1], axis=0), in_=Ftok.a[:, n, :], in_offset=None, bounds_check=bcS, oob_is_err=False), FS, [Ftok, sl_])
                    fw.barrier()
                    chk()
                with ExitStack() as s2:
                    w1c = [sb(s2, "w1c%d" % k, [128, D], BF16) for k in range(8)]
                    w3c = [sb(s2, "w3c%d" % k, [128, D], BF16) for k in range(8)]
                    w2c = [sb(s2, "w2c%d" % k, [128, D], BF16) for k in range(8)]
                    ftl = [sb(s2, "ftl%d" % k, [128, D], BF16) for k in range(2)]
                    fTk = [sb(s2, "fTk%d" % k, [128, KC, 128], BF16) for k in range(2)]
                    sl = [sb(s2, "sl%d" % k, [128, 512]) for k in range(2)]
                    ab = [sb(s2, "ab%d" % k, [128, DE], BF16) for k in range(2)]
                    aTt = [sb(s2, "aTt%d" % k, [128, 8, 128], BF16) for k in range(2)]
                    yo = [sb(s2, "yo%d" % k, [128, D]) for k in range(2)]
                    ph = [ps(s2, "ph%d" % k, [128, 512]) for k in range(4)]
                    pTf = [ps(s2, "pTf%d" % k, [128, 8, 128], BF16) for k in range(2)]
                    py = [ps(s2, "py%d" % k, [128, 512]) for k in range(2)]
                    yi = 0
                    for k in range(NS):
                        for j in range(8):
                            fw.dma("pool", lambda: G.indirect_dma_start(out=w1c[j].a, out_offset=None, in_=moe_w1[i].a, in_offset=bass.IndirectOffsetOnAxis(ap=WIi.a[:, k, j:j + 1], axis=0), bounds_check=bcW, oob_is_err=False), w1c[j], [moe_w1[i], WIi])
                        for j in range(8):
                            fw.dma("pool", lambda: G.indirect_dma_start(out=w3c[j].a, out_offset=None, in_=moe_w3[i].a, in_offset=bass.IndirectOffsetOnAxis(ap=WIi.a[:, k, j:j + 1], axis=0), bounds_check=bcW, oob_is_err=False), w3c[j], [moe_w3[i], WIi])
                        for j in range(8):
                            fw.dma("pool", lambda: G.indirect_dma_start(out=w2c[j].a, out_offset=None, in_=moe_w2[i].a, in_offset=bass.IndirectOffsetOnAxis(ap=WIi.a[:, k, 8 + j:9 + j], axis=0), bounds_check=bcW, oob_is_err=False), w2c[j], [moe_w2[i], WIi])
                        ft = ftl[k % 2]; fT_ = fTk[k % 2]; a_ = ab[k % 2]; at = aTt[k % 2]; y = yo[k % 2]
                        ld(ft, ft.a, FS.a[k * 128:(k + 1) * 128, :], [FS])
                        transpose_tile(ft, fT_, 0, pTf)
                        for hf in range(2):
                            p1 = ph[hf]
                            for kk in range(KC):
                                c0 = (kk % 2) * 1024 + hf * 512
                                fw.op("pe", lambda: T.matmul(p1.a, lhsT=fT_.a[:, kk, :], rhs=w1c[kk // 2].a[:, c0:c0 + 512], start=(kk == 0), stop=(kk == KC - 1)), [fT_, w1c[kk // 2]], [p1])
                            fw.op("act", lambda: A.activation(out=sl[hf].a, in_=p1.a, func=AF.Silu), [p1], [sl[hf]])
                        for hf in range(2):
                            p3 = ph[2 + hf]
                            for kk in range(KC):
                                c0 = (kk % 2) * 1024 + hf * 512
                                fw.op("pe", lambda: T.matmul(p3.a, lhsT=fT_.a[:, kk, :], rhs=w3c[kk // 2].a[:, c0:c0 + 512], start=(kk == 0), stop=(kk == KC - 1)), [fT_, w3c[kk // 2]], [p3])
                            fw.op("dve", lambda: V.tensor_tensor(out=a_.a[:, hf * 512:(hf + 1) * 512], in0=p3.a, in1=sl[hf].a, op=ALU.mult), [p3, sl[hf]], [a_])
                        transpose_tile(a_, at, 0, pTf[1:2], nk=8)
                        for cb in range(4):
                            p = py[yi % 2]; yi += 1
                            for k8 in range(8):
                                fw.op("pe", lambda: T.matmul(p.a, lhsT=at.a[:, k8, :], rhs=w2c[k8].a[:, cb * 512:(cb + 1) * 512], start=(k8 == 0), stop=(k8 == 7)), [at, w2c[k8]], [p])
                            if cb % 2:
                                fw.op("act", lambda: A.copy(out=y.a[:, cb * 512:(cb + 1) * 512], in_=p.a), [p], [y])
                            else:
                                fw.op("dve", lambda: V.tensor_copy(out=y.a[:, cb * 512:(cb + 1) * 512], in_=p.a), [p], [y])
                        ld(YS, YS.a[k * 128:(k + 1) * 128, :], y.a, [y])
                    fw.barrier()
                    chk()
                with ExitStack() as s2:
                    GT2 = sb(s2, "GT2", [128, D])
                    ld(GT2, GT2.a, modrow(i, 0, 5), [MOD])
                    YA = [sb(s2, "YA%d" % k, [128, D]) for k in range(2)]
                    YB = [sb(s2, "YB%d" % k, [128, D]) for k in range(2)]
                    xc = [sb(s2, "xc%d" % k, [128, D]) for k in range(2)]
                    for n in range(NC):
                        ya = YA[n % 2]; yb = YB[n % 2]; x_ = xc[n % 2]
                        fw.dma("pool", lambda: G.indirect_dma_start(out=ya.a, out_offset=None, in_=YS.a, in_offset=bass.IndirectOffsetOnAxis(ap=slotAi.a[:, n:n + 1], axis=0), bounds_check=bcS, oob_is_err=False), ya, [YS, slotAi])
                        fw.dma("pool", lambda: G.indirect_dma_start(out=yb.a, out_offset=None, in_=YS.a, in_offset=bass.IndirectOffsetOnAxis(ap=slotBi.a[:, n:n + 1], axis=0), bounds_check=bcS, oob_is_err=False), yb, [YS, slotBi])
                        ld(x_, x_.a, Xt[n].a, [Xt[n]])
                        fw.op("dve", lambda: V.tensor_scalar(out=ya.a, in0=ya.a, scalar1=wAB.a[:, n, 0:1], scalar2=None, op0=ALU.mult), [ya, wAB], [ya])
                        fw.op("dve", lambda: V.scalar_tensor_tensor(out=ya.a, in0=yb.a, scalar=wAB.a[:, n, 1:2], in1=ya.a, op0=ALU.mult, op1=ALU.add), [yb, wAB, ya], [ya])
                        fw.op("dve", lambda: V.scalar_tensor_tensor(out=ya.a, in0=ya.a, scalar=1.0, in1=GT2.a, op0=ALU.mult, op1=ALU.mult), [ya, GT2], [ya])
                        fw.op("dve", lambda: V.scalar_tensor_tensor(out=x_.a, in0=ya.a, scalar=1.0, in1=x_.a, op0=ALU.mult, op1=ALU.add), [ya, x_], [x_])
                        ld(Xt[n], Xt[n].a, x_.a, [x_])
                    fw.barrier()
                    chk()

        outproj_and_moe(0, ab_w_out, [(x_own, x_own.a[n * 128:(n + 1) * 128, :]) for n in range(NC)], False)

        with ExitStack() as st:
            Gl, SHl = make_GS(st, 1, 0, 0, 1, g_mix.a[1:2, :], g_mix)
            bufs = {"xt": [sb(st, "xt%d" % i, [128, D]) for i in range(2)], "i": 0, "junk": sb(st, "junk", [128, D]),
                    "ssq": sb(st, "ssq", [128, 1]), "rstd": sb(st, "rstd", [128, 1])}
            abf = [sb(st, "abf%d" % i, [128, D], BF16) for i in range(2)]
            aT = sb(st, "aT", [128, KC, NT], BF16)
            wb = [sb(st, "wb%d" % i, [128, KC, 512], BF16) for i in range(2)]
            osb = [sb(st, "osb%d" % i, [128, 512]) for i in range(3)]
            pT = [ps(st, "pT%d" % i, [128, 8, 128], BF16) for i in range(2)]
            pg = [ps(st, "pg%d" % i, [128, 512]) for i in range(4)]
            for n in range(NC):
                ab = abf[n % 2]
                norm_mod_tile(st, bufs, Xt[n], Xt[n].a, Gl, SHl, out_bf=ab)
                transpose_tile(ab, aT, n * 128, pT)
            ci = 0
            for cb in range(12):
                w = wb[cb % 2]
                load_w(w, cv_w_in.a[:, cb * 512:(cb + 1) * 512], cv_w_in, KC)
                for n in range(NC):
                    p = pg[ci % 4]; o = osb[ci % 3]; ci += 1
                    for k in range(KC):
                        fw.op("pe", lambda: T.matmul(p.a, lhsT=aT.a[:, k, n * 128:(n + 1) * 128], rhs=w.a[:, k, :], start=(k == 0), stop=(k == KC - 1)), [aT, w], [p])
                    if ci % 2:
                        fw.op("act", lambda: A.copy(out=o.a, in_=p.a), [p], [o])
                    else:
                        fw.op("dve", lambda: V.tensor_copy(out=o.a, in_=p.a), [p], [o])
                    ld(Zt[n], Zt[n].a[:, cb * 512:(cb + 1) * 512], o.a, [o])
            fw.barrier()
            chk()
        with ExitStack() as st:
            CW = sb(st, "CW", [128, 3, D]); CB = sb(st, "CB", [128, D])
            for j in range(3):
                ld(CW, CW.a[:, j, :], conv_w.a[j:j + 1, :].to_broadcast((128, D)), [conv_w])
            ld(CB, CB.a, conv_b.a.to_broadcast((128, D)), [conv_b])
            gc = [sb(st, "gc%d" % j, [128, D]) for j in range(3)]
            hv = [sb(st, "hv%d" % j, [128, D]) for j in range(3)]
            gbt = sb(st, "gbt", [128, D]); acc = sb(st, "acc", [128, D])
            zall = Zt + [Zpad, Zpad2]
            for n in range(NC):
                r0 = 1 + n * 128
                for j in range(3):
                    ld(gc[j], gc[j].a, Zd[r0 + j - 1:r0 + j - 1 + 128, 2048:4096], zall)
                    ld(hv[j], hv[j].a, Zd[r0 + j - 1:r0 + j - 1 + 128, 4096:6144], zall)
                ld(gbt, gbt.a, Zt[n].a[:, 0:2048], [Zt[n]])
                for j in range(3):
                    fw.op("dve", lambda: V.scalar_tensor_tensor(out=gc[j].a, in0=gc[j].a, scalar=1.0, in1=hv[j].a, op0=ALU.mult, op1=ALU.mult), [gc[j], hv[j]], [gc[j]])
                fw.op("dve", lambda: V.scalar_tensor_tensor(out=acc.a, in0=gc[0].a, scalar=cs.a[:, cc + 5:cc + 6], in1=CW.a[:, 0, :], op0=ALU.mult, op1=ALU.mult), [gc[0], cs, CW], [acc])
                fw.op("dve", lambda: V.scalar_tensor_tensor(out=acc.a, in0=acc.a, scalar=1.0, in1=CB.a, op0=ALU.mult, op1=ALU.add), [acc, CB], [acc])
                fw.op("dve", lambda: V.scalar_tensor_tensor(out=gc[1].a, in0=gc[1].a, scalar=1.0, in1=CW.a[:, 1, :], op0=ALU.mult, op1=ALU.mult), [gc[1], CW], [gc[1]])
                fw.op("dve", lambda: V.scalar_tensor_tensor(out=acc.a, in0=acc.a, scalar=1.0, in1=gc[1].a, op0=ALU.mult, op1=ALU.add), [acc, gc[1]], [acc])
                fw.op("dve", lambda: V.scalar_tensor_tensor(out=gc[2].a, in0=gc[2].a, scalar=cs.a[:, cc + 6:cc + 7], in1=CW.a[:, 2, :], op0=ALU.mult, op1=ALU.mult), [gc[2], cs, CW], [gc[2]])
                fw.op("dve", lambda: V.scalar_tensor_tensor(out=acc.a, in0=acc.a, scalar=1.0, in1=gc[2].a, op0=ALU.mult, op1=ALU.add), [acc, gc[2]], [acc])
                fw.op("dve", lambda: V.scalar_tensor_tensor(out=acc.a, in0=acc.a, scalar=1.0, in1=gbt.a, op0=ALU.mult, op1=ALU.mult), [acc, gbt], [acc])
                ld(Rt[n], Rt[n].a, acc.a, [acc])
            fw.barrier()
            chk()

        outproj_and_moe(1, cv_w_out, [(Xt[n], Xt[n].a) for n in range(NC)], True)

        with ExitStack() as st:
            Gfin = sb(st, "Gfin", [128, D])
            ld(Gfin, Gfin.a, g_final.a.to_broadcast((128, D)), [g_final])
            bufs = {"xt": [sb(st, "xt%d" % i, [128, D]) for i in range(2)], "i": 0, "junk": sb(st, "junk", [128, D]),
                    "ssq": sb(st, "ssq", [128, 1]), "rstd": sb(st, "rstd", [128, 1])}
            ot = [sb(st, "ot%d" % i, [128, D]) for i in range(2)]
            for n in range(NC):
                jk = norm_mod_tile(st, bufs, Xt[n], Xt[n].a, Gfin, None)
                fw.op("act", lambda: A.copy(out=ot[n % 2].a, in_=jk.a), [jk], [ot[n % 2]])
                ld(out, out.a[n * 128:(n + 1) * 128, :], ot[n % 2].a, [ot[n % 2]], k="sp")
            fw.barrier()
            chk()
    return nc


def rope_tables(pos):
    row = (pos // GRID_W).astype(np.float32)
    col = (pos % GRID_W).astype(np.float32)
    nf = HD // 4
    inv = (10000.0 ** (-np.arange(nf, dtype=np.float32) / nf)).astype(np.float32)
    ang = np.concatenate([row[:, None] * inv, col[:, None] * inv], axis=-1).astype(np.float32)
    return np.cos(ang).astype(np.float32), np.sin(ang).astype(np.float32)


def make_consts(NT):
    NC = NT // 128
    m = np.arange(128, dtype=np.float32)
    ident = np.eye(128, dtype=np.float32)
    R1 = np.maximum(m[None, :] - m[:, None], 0)
    R2 = np.maximum(m[:, None] - m[None, :], 0)
    cols = np.stack([127 - m, m, m + 1, 128 - m, np.full(128, 128.0, np.float32),
                     (np.arange(128) % GRID_W != 0).astype(np.float32),
                     (np.arange(128) % GRID_W != GRID_W - 1).astype(np.float32), np.zeros(128, np.float32)], axis=1)
    et = [128 * c + m for c in range(NC)] + [NT + 128 * c + m for c in range(2)] + [255 - 128 * c - m for c in range(2)]
    et = np.stack(et, axis=1)
    tri = (m[:, None] < m[None, :]).astype(np.float32)
    ones = np.ones((128, 128), np.float32)
    base1 = m[:, None] * 8 + np.arange(8, dtype=np.float32)[None, :]
    base2 = np.arange(8, dtype=np.float32)[None, :] * 128 + m[:, None]
    kv = np.broadcast_to(np.arange(2 * NC + 16, dtype=np.float32)[None, :], (128, 2 * NC + 16))
    return np.concatenate([ident, R1, R2, cols, et, tri, ones, base1, base2, kv], axis=1).astype(np.float32)


def prepare_inputs(inp, T):
    B = inp["x"].shape[0]
    NT = T // 2
    qs = np.float32(HD ** -0.5)
    cst = make_consts(NT)
    maps = []
    shared = {
        "w_mod": inp["w_mod"], "b_mod": inp["b_mod"], "g_mix": inp["g_mix"], "g_ffn": inp["g_ffn"],
        "g_final": inp["g_final"][None, :], "ab_w_in": inp["ab_w_in"][0], "ab_w_out": inp["ab_w_out"][0],
        "cv_w_in": inp["cv_w_in"][0], "cv_w_out": inp["cv_w_out"][0], "conv_b": inp["cv_conv_b"],
        "cst": cst,
        "wr": np.concatenate([inp["moe_w_r1"], inp["moe_w_r2"].transpose(0, 2, 1, 3).reshape(2, D, 16)], axis=2),
        "br": np.concatenate([inp["moe_b_r1"], inp["moe_b_r2"].reshape(2, 16)], axis=1),
    }
    for l in range(2):
        shared["moe_w1_%d" % l] = np.ascontiguousarray(inp["moe_w1"][l].reshape(16, 16, 128, DE).transpose(0, 2, 1, 3).reshape(16384, D))
        shared["moe_w3_%d" % l] = np.ascontiguousarray(inp["moe_w3"][l].reshape(16, 16, 128, DE).transpose(0, 2, 1, 3).reshape(16384, D))
        shared["moe_w2_%d" % l] = inp["moe_w2"][l].reshape(16384, D)
    shared = {k: np.ascontiguousarray(v, dtype=np.float32) for k, v in shared.items()}
    for b in range(B):
        for h in range(2):
            pos_all = np.arange(T)
            if h == 0:
                own = pos_all[:NT]; forg = pos_all[NT:]
                ctxl = inp["ctx"][b]
                dlv = inp["ret_decay_logit"][0].reshape(1, 16)
                ws = inp["sgu_w_s"][0]; bs = inp["sgu_b_s"][0]
                cw = inp["cv_conv_w"][0]
            else:
                own = pos_all[::-1][:NT]; forg = pos_all[:NT][::-1]
                ctxl = inp["ctx"][b][::-1]
                dlv = inp["ret_decay_logit"][0][::-1].reshape(1, 16)
                ws = inp["sgu_w_s"][0][:, ::-1, ::-1]; bs = inp["sgu_b_s"][0][:, ::-1]
                cw = inp["cv_conv_w"][0][::-1]
            co, so = rope_tables(own)
            cf, sf = rope_tables(forg)
            cT = np.stack([inp["c"][b].reshape(16, 128).T, inp["c_ctx"].reshape(16, 128).T], axis=2)
            m = dict(shared)
            m.update({
                "x_own": inp["x"][b][own], "x_for": inp["x"][b][forg], "ctx_l": ctxl, "cT": cT, "dl": dlv,
                "wsT": ws.transpose(0, 2, 1), "bsT": bs.T,
                "rope": np.stack([co * qs, so * qs, co, so, cf, sf]), "conv_w": cw,
            })
            maps.append({k: np.ascontiguousarray(v, dtype=np.float32) for k, v in m.items()})
    return maps


def kernel(**inputs):
    inp = {k: np.asarray(v) for k, v in inputs.items()}
    B, T, _ = inp["x"].shape
    NT = T // 2
    maps = prepare_inputs(inp, T)
    nc = build(NT)
    res = run_bass_kernel_spmd(nc, maps, core_ids=list(range(len(maps))))
    out = np.empty((B, T, D), np.float32)
    for b in range(B):
        out[b, :NT] = res.results[2 * b]["out"]
        out[b, NT:] = res.results[2 * b + 1]["out"][::-1]
    return out
```
